# Optimizing a Trainium2 kernel written in Bass

```python
import math
import jax, jax.numpy as jnp
from jax import lax
import numpy as np

D_MODEL = 2048
BATCH = 4
SEQ = 2048
DEPTH = 2
DEC_BATCH = 128
DEC_SEQ = 1
PAST_LEN = 16384
PAGE_SIZE = 128

ML_HEADS = 4
ML_DV = D_MODEL // 8
ML_DK = ML_DV
ML_WIDTH = ML_HEADS * ML_DV
S5_CH = 16
S5_WIDTH = D_MODEL // 4
S5_GROUPS = S5_WIDTH // S5_CH
S5_STATE = 64
SSD_HEAD_DIM = 64
SSD_WIDTH = D_MODEL // 4
SSD_HEADS = SSD_WIDTH // SSD_HEAD_DIM
SSD_GROUPS = 2
SSD_STATE = 128
SSD_CONV = 4
SSD_CONV_CH = SSD_WIDTH + 2 * SSD_GROUPS * SSD_STATE
MIX_WIDTH = ML_WIDTH + S5_WIDTH + SSD_WIDTH
IN_SIZES = (ML_HEADS * ML_DK, ML_HEADS * ML_DK, ML_WIDTH, ML_WIDTH, ML_HEADS, ML_HEADS,
            S5_WIDTH, SSD_WIDTH, SSD_CONV_CH, SSD_HEADS)
CHUNK = 64
D_FF = 5632
N_EXPERTS = 8
TOP_K = 2
D_FF_EXPERT = 7168
PLE_DIM = 256
RMS_EPS = 1e-6

kernel_name = 'hybrid_mlstm_s5_ssd_decoder_step'


def rmsnorm(x, g):
    xf = x.astype(jnp.float32)
    y = xf * lax.rsqrt(jnp.mean(xf * xf, axis=-1, keepdims=True) + RMS_EPS)
    return (y * g.astype(jnp.float32)).astype(x.dtype)


def swiglu(c, wg, wu, wd):
    return (jax.nn.silu(c @ wg) * (c @ wu)) @ wd


def moe_swiglu(c, w_router, b_router, wg, wu, wd):
    f32 = jnp.float32
    logits = c.astype(f32) @ w_router.astype(f32) + b_router.astype(f32)
    top_v, top_i = lax.top_k(logits, TOP_K)
    gates = jax.nn.softmax(top_v, axis=-1)
    combine = jnp.einsum('blk,blke->ble', gates, jax.nn.one_hot(top_i, N_EXPERTS, dtype=f32))
    out = jnp.zeros_like(c)
    for e in range(N_EXPERTS):
        out = out + combine[..., e:e + 1].astype(c.dtype) * swiglu(c, wg[e], wu[e], wd[e])
    return out


def _to_chunks(t, cl):
    b, l = t.shape[:2]
    t = t.reshape((b, l // cl, cl) + t.shape[2:])
    return jnp.swapaxes(jnp.moveaxis(t, 1, 0), 2, 3)


def _from_chunks(t):
    t = jnp.moveaxis(jnp.swapaxes(t, 2, 3), 0, 1)
    return t.reshape((t.shape[0], t.shape[1] * t.shape[2]) + t.shape[3:])


def mlstm_scan(q, k, v, i_pre, f_pre, C0, n0, m0):
    L = q.shape[1]
    cl = math.gcd(L, CHUNK)
    causal = jnp.tril(jnp.ones((cl, cl), dtype=bool))
    xs = tuple(_to_chunks(t, cl) for t in (q, k, v, i_pre, f_pre))

    def step(carry, inp):
        C, n, m = carry
        qc, kc, vc, ic, fc = inp
        b = jnp.cumsum(jax.nn.log_sigmoid(fc), axis=-1)
        d = jnp.where(causal, b[..., :, None] - b[..., None, :] + ic[..., None, :], -jnp.inf)
        inter = b + m[..., None]
        m_t = jnp.maximum(inter, jnp.max(d, axis=-1))
        w = jnp.exp(d - m_t[..., None])
        g = jnp.exp(inter - m_t)
        s = jnp.einsum('bhtd,bhsd->bhts', qc, kc) * w
        num = jnp.einsum('bhts,bhsv->bhtv', s, vc) + g[..., None] * jnp.einsum('bhtd,bhdv->bhtv', qc, C)
        den = jnp.sum(s, axis=-1) + g * jnp.einsum('bhtd,bhd->bht', qc, n)
        h = num / jnp.maximum(jnp.abs(den), jnp.exp(-m_t))[..., None]
        dl = b[..., -1:] - b + ic
        m_new = jnp.maximum(b[..., -1] + m, jnp.max(dl, axis=-1))
        ws = jnp.exp(dl - m_new[..., None])
        gl = jnp.exp(b[..., -1] + m - m_new)
        kw = kc * ws[..., None]
        C_new = gl[..., None, None] * C + jnp.einsum('bhsd,bhsv->bhdv', kw, vc)
        n_new = gl[..., None] * n + jnp.sum(kw, axis=-2)
        return (C_new, n_new, m_new), h

    (C, n, m), h = lax.scan(step, (C0, n0, m0), xs)
    return _from_chunks(h), C, n, m


def s5_scan(u, lam_re, lam_im, log_dt, b_re, b_im, c_re, c_im, d_skip, sre0, sim0):
    f32 = jnp.float32
    lam = lax.complex(lam_re.astype(f32), lam_im.astype(f32))
    dt = jnp.exp(log_dt.astype(f32))[:, None]
    lam_bar = jnp.exp(lam * dt)
    b_bar = ((lam_bar - 1.0) / lam)[..., None] * lax.complex(b_re.astype(f32), b_im.astype(f32))
    bu = jnp.einsum('gpc,blgc->blgp', b_bar, u.astype(jnp.complex64))
    s0 = lax.complex(sre0.astype(f32), sim0.astype(f32))
    bu = bu.at[:, 0].add(lam_bar * s0)
    a_el = jnp.broadcast_to(lam_bar, bu.shape)

    def comb(e1, e2):
        a1, x1 = e1
        a2, x2 = e2
        return a1 * a2, a2 * x1 + x2

    _, xs = lax.associative_scan(comb, (a_el, bu), axis=1)
    c_c = lax.complex(c_re.astype(f32), c_im.astype(f32))
    y = jnp.real(jnp.einsum('gcp,blgp->blgc', c_c, xs)) + d_skip.astype(f32) * u
    last = xs[:, -1]
    return y, jnp.real(last), jnp.imag(last)


def ssd_scan(x, dt, A, bm, cm, S0):
    L = x.shape[1]
    cl = math.gcd(L, CHUNK)
    causal = jnp.tril(jnp.ones((cl, cl), dtype=bool))
    xs = tuple(_to_chunks(t, cl) for t in (x * dt[..., None], dt * A, bm, cm))

    def step(S, inp):
        xc, ac, bc, cc = inp
        cum = jnp.cumsum(ac, axis=-1)
        seg = jnp.exp(jnp.where(causal, cum[..., :, None] - cum[..., None, :], -jnp.inf))
        scores = jnp.einsum('bhtn,bhsn->bhts', cc, bc) * seg
        y = jnp.einsum('bhts,bhsp->bhtp', scores, xc) + jnp.exp(cum)[..., None] * jnp.einsum('bhtn,bhpn->bhtp', cc, S)
        w_end = jnp.exp(cum[..., -1:] - cum)
        S_new = jnp.exp(cum[..., -1])[..., None, None] * S + jnp.einsum('bhsp,bhsn->bhpn', xc * w_end[..., None], bc)
        return S_new, y

    S, y = lax.scan(step, S0, xs)
    return _from_chunks(y), S


def _hybrid_mixer(a, C0, n0, m0, sre0, sim0, ssd0, conv0, w_in, b_ig, b_fg, g_ml,
                  lam_re, lam_im, log_dt, s5_b_re, s5_b_im, s5_c_re, s5_c_im, s5_d, w_glu, b_glu, g_s5,
                  conv_w, conv_b, dt_bias, a_log, ssd_d, g_ssd, w_out):
    f32 = jnp.float32
    bsz, L, _ = a.shape
    zin = (a @ w_in).astype(f32)
    offs = np.cumsum(IN_SIZES)[:-1].tolist()
    q, k, v, o, ig, fg, u, z, xbc, dtr = jnp.split(zin, offs, axis=-1)

    q = q.reshape(bsz, L, ML_HEADS, ML_DK)
    k = k.reshape(bsz, L, ML_HEADS, ML_DK) * (ML_DK ** -0.5)
    v = v.reshape(bsz, L, ML_HEADS, ML_DV)
    h_ml, C1, n1, m1 = mlstm_scan(q, k, v, ig + b_ig.astype(f32), fg + b_fg.astype(f32),
                                  C0.astype(f32), n0.astype(f32), m0.astype(f32))
    h_ml = jax.nn.sigmoid(o) * rmsnorm(h_ml, g_ml).reshape(bsz, L, ML_WIDTH)

    y5, sre1, sim1 = s5_scan(u.reshape(bsz, L, S5_GROUPS, S5_CH), lam_re, lam_im, log_dt,
                             s5_b_re, s5_b_im, s5_c_re, s5_c_im, s5_d, sre0, sim0)
    y5 = jax.nn.gelu(y5.reshape(bsz, L, S5_WIDTH))
    y5 = rmsnorm(y5 * jax.nn.sigmoid(y5 @ w_glu.astype(f32) + b_glu.astype(f32)), g_s5)

    xp = jnp.concatenate([conv0.astype(f32), xbc], axis=1)
    cw = conv_w.astype(f32)
    xc = conv_b.astype(f32) + sum(cw[j] * xp[:, j:j + L] for j in range(SSD_CONV))
    conv1 = xp[:, L:]
    xc = jax.nn.silu(xc)
    xs_, bm, cm = jnp.split(xc, [SSD_WIDTH, SSD_WIDTH + SSD_GROUPS * SSD_STATE], axis=-1)
    xs_ = xs_.reshape(bsz, L, SSD_HEADS, SSD_HEAD_DIM)
    rep = SSD_HEADS // SSD_GROUPS
    bm = jnp.repeat(bm.reshape(bsz, L, SSD_GROUPS, SSD_STATE), rep, axis=2)
    cm = jnp.repeat(cm.reshape(bsz, L, SSD_GROUPS, SSD_STATE), rep, axis=2)
    dt = jax.nn.softplus(dtr + dt_bias.astype(f32))
    A = -jnp.exp(a_log.astype(f32))
    y_ssd, ssd1 = ssd_scan(xs_, dt, A, bm, cm, ssd0.astype(f32))
    y_ssd = y_ssd + ssd_d.astype(f32)[:, None] * xs_
    y_ssd = rmsnorm(y_ssd.reshape(bsz, L, SSD_WIDTH) * jax.nn.silu(z), g_ssd)

    out = jnp.concatenate([h_ml, y5, y_ssd], axis=-1).astype(a.dtype) @ w_out
    return out, (C1, n1, m1, sre1, sim1, ssd1, conv1)


def _trunk(x, p, C0, n0, m0, sre0, sim0, ssd0, conv0, wts):
    (g_mix, w_in, b_igate, b_fgate, g_ml, s5_lam_re, s5_lam_im, s5_log_dt, s5_b_re, s5_b_im,
     s5_c_re, s5_c_im, s5_d, s5_w_glu, s5_b_glu, g_s5, ssd_conv_w, ssd_conv_b, ssd_dt_bias,
     ssd_a_log, ssd_d, g_ssd, w_out, g_ffn, ffn_w_gate, ffn_w_up, ffn_w_down, w_router, b_router,
     moe_w_gate, moe_w_up, moe_w_down, g_ple, w_ple, w_ple_gate, g_final) = wts
    h = x
    new_states = [[] for _ in range(7)]
    for i in range(DEPTH):
        mix, st = _hybrid_mixer(rmsnorm(h, g_mix[i]), C0[i], n0[i], m0[i], sre0[i], sim0[i], ssd0[i], conv0[i],
                                w_in[i], b_igate[i], b_fgate[i], g_ml[i], s5_lam_re[i], s5_lam_im[i], s5_log_dt[i],
                                s5_b_re[i], s5_b_im[i], s5_c_re[i], s5_c_im[i], s5_d[i], s5_w_glu[i], s5_b_glu[i],
                                g_s5[i], ssd_conv_w[i], ssd_conv_b[i], ssd_dt_bias[i], ssd_a_log[i], ssd_d[i],
                                g_ssd[i], w_out[i])
        for lst, s in zip(new_states, st):
            lst.append(s)
        h = h + mix
        c = rmsnorm(h, g_ffn[i])
        j = i // 2
        if i % 2 == 0:
            h = h + swiglu(c, ffn_w_gate[j], ffn_w_up[j], ffn_w_down[j])
        else:
            h = h + moe_swiglu(c, w_router[j], b_router[j], moe_w_gate[j], moe_w_up[j], moe_w_down[j])
        e = rmsnorm(h, g_ple[i])
        h = h + (p[i] @ w_ple[i]) * jax.nn.sigmoid(e @ w_ple_gate[i])
    return rmsnorm(h, g_final), tuple(jnp.stack(lst) for lst in new_states)


def setup_inputs(seed: int = 0) -> dict:
    key = jax.random.key(seed)
    keys = iter(jax.random.split(key, 64))
    f32 = jnp.float32

    def nrm(shape, scale):
        return jax.random.normal(next(keys), shape, f32) * scale

    def unif(shape, lo, hi):
        return jax.random.uniform(next(keys), shape, f32, lo, hi)

    n_dense = (DEPTH + 1) // 2
    n_moe = DEPTH // 2
    n_in = sum(IN_SIZES)
    dt_ssd = jnp.exp(unif((DEPTH, SSD_HEADS), math.log(1e-3), math.log(1e-1)))
    return {
        'x_prompt': nrm((BATCH, SEQ, D_MODEL), 1.0),
        'x_sample': nrm((DEC_BATCH, DEC_SEQ, D_MODEL), 1.0),
        'state_mlstm_C': nrm((DEPTH, DEC_BATCH, ML_HEADS, ML_DK, ML_DV), 0.1),
        'state_mlstm_n': nrm((DEPTH, DEC_BATCH, ML_HEADS, ML_DK), 0.3),
        'state_mlstm_m': nrm((DEPTH, DEC_BATCH, ML_HEADS), 1.0),
        'state_s5_re': nrm((DEPTH, DEC_BATCH, S5_GROUPS, S5_STATE), 0.1),
        'state_s5_im': nrm((DEPTH, DEC_BATCH, S5_GROUPS, S5_STATE), 0.1),
        'state_ssd': nrm((DEPTH, DEC_BATCH, SSD_HEADS, SSD_HEAD_DIM, SSD_STATE), 0.3),
        'cache_conv': nrm((DEPTH, DEC_BATCH, SSD_CONV - 1, SSD_CONV_CH), 1.0),
        'p_prompt': nrm((DEPTH, BATCH, SEQ, PLE_DIM), 1.0),
        'p_sample': nrm((DEPTH, DEC_BATCH, DEC_SEQ, PLE_DIM), 1.0),
        'g_mix': 1.0 + nrm((DEPTH, D_MODEL), 0.02),
        'w_in': nrm((DEPTH, D_MODEL, n_in), D_MODEL ** -0.5),
        'b_igate': nrm((DEPTH, ML_HEADS), 0.1),
        'b_fgate': jnp.linspace(3.0, 6.0, ML_HEADS, dtype=f32) + nrm((DEPTH, ML_HEADS), 0.1),
        'g_ml': 1.0 + nrm((DEPTH, ML_HEADS, ML_DV), 0.02),
        's5_lam_re': -0.5 + nrm((DEPTH, S5_GROUPS, S5_STATE), 0.01),
        's5_lam_im': jnp.pi * jnp.arange(S5_STATE, dtype=f32) + nrm((DEPTH, S5_GROUPS, S5_STATE), 0.01),
        's5_log_dt': unif((DEPTH, S5_GROUPS), math.log(1e-3), math.log(1e-1)),
        's5_b_re': nrm((DEPTH, S5_GROUPS, S5_STATE, S5_CH), (2 * S5_CH) ** -0.5),
        's5_b_im': nrm((DEPTH, S5_GROUPS, S5_STATE, S5_CH), (2 * S5_CH) ** -0.5),
        's5_c_re': nrm((DEPTH, S5_GROUPS, S5_CH, S5_STATE), (2 * S5_STATE) ** -0.5),
        's5_c_im': nrm((DEPTH, S5_GROUPS, S5_CH, S5_STATE), (2 * S5_STATE) ** -0.5),
        's5_d': nrm((DEPTH, S5_GROUPS, S5_CH), 1.0),
        's5_w_glu': nrm((DEPTH, S5_WIDTH, S5_WIDTH), S5_WIDTH ** -0.5),
        's5_b_glu': nrm((DEPTH, S5_WIDTH), 0.02),
        'g_s5': 1.0 + nrm((DEPTH, S5_WIDTH), 0.02),
        'ssd_conv_w': nrm((DEPTH, SSD_CONV, SSD_CONV_CH), SSD_CONV ** -0.5),
        'ssd_conv_b': nrm((DEPTH, SSD_CONV_CH), 0.02),
        'ssd_dt_bias': dt_ssd + jnp.log(-jnp.expm1(-dt_ssd)),
        'ssd_a_log': jnp.log(unif((DEPTH, SSD_HEADS), 1.0, 16.0)),
        'ssd_d': 1.0 + nrm((DEPTH, SSD_HEADS), 0.1),
        'g_ssd': 1.0 + nrm((DEPTH, SSD_WIDTH), 0.02),
        'w_out': nrm((DEPTH, MIX_WIDTH, D_MODEL), MIX_WIDTH ** -0.5),
        'g_ffn': 1.0 + nrm((DEPTH, D_MODEL), 0.02),
        'ffn_w_gate': nrm((n_dense, D_MODEL, D_FF), D_MODEL ** -0.5),
        'ffn_w_up': nrm((n_dense, D_MODEL, D_FF), D_MODEL ** -0.5),
        'ffn_w_down': nrm((n_dense, D_FF, D_MODEL), D_FF ** -0.5),
        'w_router': nrm((n_moe, D_MODEL, N_EXPERTS), D_MODEL ** -0.5),
        'b_router': nrm((n_moe, N_EXPERTS), 0.01),
        'moe_w_gate': nrm((n_moe, N_EXPERTS, D_MODEL, D_FF_EXPERT), D_MODEL ** -0.5),
        'moe_w_up': nrm((n_moe, N_EXPERTS, D_MODEL, D_FF_EXPERT), D_MODEL ** -0.5),
        'moe_w_down': nrm((n_moe, N_EXPERTS, D_FF_EXPERT, D_MODEL), D_FF_EXPERT ** -0.5),
        'g_ple': 1.0 + nrm((DEPTH, D_MODEL), 0.02),
        'w_ple': nrm((DEPTH, PLE_DIM, D_MODEL), PLE_DIM ** -0.5),
        'w_ple_gate': nrm((DEPTH, D_MODEL, D_MODEL), D_MODEL ** -0.5),
        'g_final': 1.0 + nrm((D_MODEL,), 0.02),
    }


def reference(x_prompt, x_sample, state_mlstm_C, state_mlstm_n, state_mlstm_m, state_s5_re, state_s5_im,
              state_ssd, cache_conv, p_prompt, p_sample, g_mix, w_in, b_igate, b_fgate, g_ml,
              s5_lam_re, s5_lam_im, s5_log_dt, s5_b_re, s5_b_im, s5_c_re, s5_c_im, s5_d, s5_w_glu, s5_b_glu,
              g_s5, ssd_conv_w, ssd_conv_b, ssd_dt_bias, ssd_a_log, ssd_d, g_ssd, w_out, g_ffn,
              ffn_w_gate, ffn_w_up, ffn_w_down, w_router, b_router, moe_w_gate, moe_w_up, moe_w_down,
              g_ple, w_ple, w_ple_gate, g_final):
    wts = (g_mix, w_in, b_igate, b_fgate, g_ml, s5_lam_re, s5_lam_im, s5_log_dt, s5_b_re, s5_b_im,
           s5_c_re, s5_c_im, s5_d, s5_w_glu, s5_b_glu, g_s5, ssd_conv_w, ssd_conv_b, ssd_dt_bias,
           ssd_a_log, ssd_d, g_ssd, w_out, g_ffn, ffn_w_gate, ffn_w_up, ffn_w_down, w_router, b_router,
           moe_w_gate, moe_w_up, moe_w_down, g_ple, w_ple, w_ple_gate, g_final)
    bp = x_prompt.shape[0]

    def zeros(*shape):
        return jnp.zeros((DEPTH, bp) + shape, jnp.float32)

    y_prompt, (C_p, n_p, m_p, s5re_p, s5im_p, ssd_p, conv_p) = _trunk(
        x_prompt, p_prompt, zeros(ML_HEADS, ML_DK, ML_DV), zeros(ML_HEADS, ML_DK), zeros(ML_HEADS),
        zeros(S5_GROUPS, S5_STATE), zeros(S5_GROUPS, S5_STATE),
        zeros(SSD_HEADS, SSD_HEAD_DIM, SSD_STATE), zeros(SSD_CONV - 1, SSD_CONV_CH), wts)
    y_sample, (C_s, n_s, m_s, s5re_s, s5im_s, ssd_s, conv_s) = _trunk(
        x_sample, p_sample, state_mlstm_C, state_mlstm_n, state_mlstm_m, state_s5_re, state_s5_im,
        state_ssd, cache_conv, wts)
    return (y_prompt, y_sample, C_p, n_p, m_p, s5re_p, s5im_p, ssd_p, conv_p,
            C_s, n_s, m_s, s5re_s, s5im_s, ssd_s, conv_s)
```

```python
import math
import numpy as np
import concourse.bass as bass
import concourse.mybir as mybir
from concourse.bass_utils import run_bass_kernel_spmd
from contextlib import ExitStack

F32 = mybir.dt.float32
BF16 = mybir.dt.bfloat16
AF = mybir.ActivationFunctionType
ALU = mybir.AluOpType
AX = mybir.AxisListType

L = 2048
NS = 16
NT = L + NS
XW = 2080
D = 2048
KC = 16
DEPTH = 2
N_IN = 6160
D_FF = 5632
D_FFE = 7168
NE = 8
EPS = 1e-6
TB = [(0, 512), (512, 512), (1024, 512), (1536, 512), (2048, 16)]
CHUNKS = [(c * 64, 64, c) for c in range(32)] + [(L + j, 1, 32 + j) for j in range(NS)]
NCI = 48
NO = 1040
TBO = [(0, 512), (512, 512), (1024, 16)]
NEG = -30000.0


class Tk:
    __slots__ = ("name", "lw", "rd", "mw", "dsem", "dcnt")

    def __init__(self, name):
        self.name = name
        self.lw = None
        self.rd = []
        self.mw = []
        self.dsem = None
        self.dcnt = 0


class T:
    __slots__ = ("a", "k")

    def __init__(self, a, name):
        self.a = a
        self.k = Tk(name)


class Ins:
    __slots__ = ("eng", "fn", "waits", "needed", "val", "dsem", "dval")

    def __init__(self, eng, fn):
        self.eng = eng
        self.fn = fn
        self.waits = []
        self.needed = False
        self.val = None
        self.dsem = None
        self.dval = None


class Sched:
    ENGS = ("pe", "act", "dve", "pool", "sp")

    def __init__(self, nc, stack):
        self.nc = nc
        self.stack = stack
        self.prog = {e: [] for e in self.ENGS}
        self.esem = {e: stack.enter_context(nc.semaphore("es_" + e)) for e in self.ENGS}
        self.ecnt = {e: 0 for e in self.ENGS}
        self.known = {e: {} for e in self.ENGS}
        self.nsem = 0
        self.out_events = []
        self.toks = []
        self.pend_dma = []
        self.last = {e: None for e in self.ENGS}
        self.semfree = []
        self.semcnt = {}
        self.phase_mark = 0

    def phase_begin(self):
        self.phase_mark = len(self.toks)

    def phase_end(self):
        for k in self.toks[self.phase_mark:]:
            if k.dsem is not None:
                self.semfree.append(k.dsem)
                k.dsem = None
        del self.toks[self.phase_mark:]

    def tok(self, name):
        k = Tk(name)
        self.toks.append(k)
        return k

    def _deps(self, ins, reads, writes, mwrites):
        evs = []
        for r in reads:
            if r.lw is not None:
                evs.append(r.lw)
            evs.extend(r.mw)
        for w in writes:
            if w.lw is not None:
                evs.append(w.lw)
            evs.extend(w.mw)
            evs.extend(w.rd)
        for w in mwrites:
            if w.lw is not None:
                evs.append(w.lw)
            evs.extend(w.rd)
        seen = set()
        for ev in evs:
            if id(ev) in seen or ev is ins:
                continue
            seen.add(id(ev))
            if ev.dsem is None and ev.eng == "pe" and ins.eng == "pe" and ins.dsem is None:
                continue
            ins.waits.append(ev)
            ev.needed = True
        for r in reads:
            r.rd.append(ins)
        for w in writes:
            w.lw = ins
            w.rd = []
            w.mw = []
        for w in mwrites:
            if w.rd:
                w.rd = []
                w.mw = []
                w.lw = None
            w.mw.append(ins)

    def op(self, eng, fn, reads=(), writes=()):
        ins = Ins(eng, fn)
        self._deps(ins, [r.k if isinstance(r, T) else r for r in reads],
                   [w.k if isinstance(w, T) else w for w in writes], [])
        self.prog[eng].append(ins)
        self.last[eng] = ins
        return ins

    def dma(self, q, out_ap, in_ap, tok, reads=(), writes=(), mwrites=(), is_output=False, **kw):
        if isinstance(tok, T):
            tok = tok.k
        if tok.dsem is None:
            if self.semfree:
                tok.dsem = self.semfree.pop()
            else:
                tok.dsem = self.stack.enter_context(self.nc.semaphore("ds_%d" % self.nsem))
                self.nsem += 1
        ins = Ins(q, lambda e: e.dma_start(out=out_ap, in_=in_ap, **kw))
        c = self.semcnt.get(id(tok.dsem), 0) + 16
        self.semcnt[id(tok.dsem)] = c
        ins.dsem = tok.dsem
        ins.dval = c
        self._deps(ins, [r.k if isinstance(r, T) else r for r in reads],
                   [w.k if isinstance(w, T) else w for w in writes],
                   [w.k if isinstance(w, T) else w for w in mwrites])
        self.prog[q].append(ins)
        self.pend_dma.append(ins)
        if is_output:
            self.out_events.append(ins)
        return ins

    def dma_fn(self, q, fn, tok, reads=(), writes=(), mwrites=()):
        if isinstance(tok, T):
            tok = tok.k
        if tok.dsem is None:
            if self.semfree:
                tok.dsem = self.semfree.pop()
            else:
                tok.dsem = self.stack.enter_context(self.nc.semaphore("ds_%d" % self.nsem))
                self.nsem += 1
        ins = Ins(q, fn)
        c = self.semcnt.get(id(tok.dsem), 0) + 16
        self.semcnt[id(tok.dsem)] = c
        ins.dsem = tok.dsem
        ins.dval = c
        self._deps(ins, [r.k if isinstance(r, T) else r for r in reads],
                   [w.k if isinstance(w, T) else w for w in writes],
                   [w.k if isinstance(w, T) else w for w in mwrites])
        self.prog[q].append(ins)
        self.pend_dma.append(ins)
        return ins

    def _emit_engine(self, e, engh):
        known = self.known[e]
        for ins in self.prog[e]:
            need = {}
            for ev in ins.waits:
                if ev.dsem is not None:
                    key, sem, val = ("d", id(ev.dsem)), ev.dsem, ev.dval
                else:
                    key, sem, val = ("e", ev.eng), self.esem[ev.eng], ev.val
                if known.get(key, 0) >= val:
                    continue
                if key not in need or need[key][1] < val:
                    need[key] = (sem, val)
            for key, (sem, val) in need.items():
                engh.wait_ge(sem, val)
                known[key] = val
            if ins.fn is None:
                continue
            bi = ins.fn(engh)
            if ins.dsem is not None:
                bi.then_inc(ins.dsem, 16)
            elif ins.needed:
                bi.then_inc(self.esem[e], 1)

    def flush(self, final=False):
        nc = self.nc
        bar = Ins("sp", lambda e: e.nop())
        seen = set()
        for ev in self.pend_dma:
            if ev.eng != "sp" or True:
                key = (id(ev.dsem))
                bar.waits.append(ev)
        for e in self.ENGS:
            if e != "sp" and self.last[e] is not None:
                self.last[e].needed = True
                bar.waits.append(self.last[e])
        bar.needed = True
        self.prog["sp"].append(bar)
        for e in self.ENGS:
            if e != "sp":
                w = Ins(e, None)
                w.waits.append(bar)
                self.prog[e].append(w)
        for e in self.ENGS:
            c = self.ecnt[e]
            for ins in self.prog[e]:
                if ins.dsem is None and ins.needed and ins.fn is not None:
                    c += 1
                    ins.val = c
            self.ecnt[e] = c
        with nc.Block() as block:
            @block.tensor
            def _(eng):
                self._emit_engine("pe", eng)

            @block.scalar
            def _(eng):
                self._emit_engine("act", eng)

            @block.vector
            def _(eng):
                self._emit_engine("dve", eng)

            @block.gpsimd
            def _(eng):
                self._emit_engine("pool", eng)

            @block.sync
            def _(eng):
                self._emit_engine("sp", eng)
        self.prog = {e: [] for e in self.ENGS}
        self.pend_dma = []
        self.last = {e: None for e in self.ENGS}
        for k in self.toks:
            k.lw = None
            k.rd = []
            k.mw = []


_UID = [0]


class Pool:
    def __init__(self, K, name, shape, dt, n, space="sb"):
        _UID[0] += 1
        name = "%s_%d_" % (name, _UID[0])
        self.bufs = []
        for i in range(n):
            if space == "sb":
                a = K.st.enter_context(K.nc.sbuf_tensor("%s%d" % (name, i), list(shape), dt))
            else:
                a = K.st.enter_context(K.nc.psum_tensor("%s%d" % (name, i), list(shape), dt))
            t = T(a, "%s%d" % (name, i))
            K.S.toks.append(t.k)
            self.bufs.append(t)
        self.i = 0

    def next(self):
        b = self.bufs[self.i % len(self.bufs)]
        self.i += 1
        return b


class _Phase:
    def __init__(self, K):
        self.K = K

    def __enter__(self):
        self.pst = ExitStack()
        self.pst.__enter__()
        self.K.st = self.pst
        self.K.S.phase_begin()
        return self

    def __exit__(self, *a):
        if a[0] is None:
            self.K.S.flush()
            self.K.S.phase_end()
        self.K.st = self.K.gst
        return self.pst.__exit__(*a)


class Builder:
    def __init__(self, nc, st, dbg=None):
        self.nc = nc
        self.gst = st
        self.st = st
        self.S = Sched(nc, st)
        self.dbg = dbg or set()
        self.inputs = {}
        self.outputs = {}

    def phase(self):
        return _Phase(self)

    def din(self, name, shape, dt=F32):
        a = self.nc.dram_tensor(name, list(shape), dt, kind="ExternalInput").ap()
        t = T(a, name)
        self.inputs[name] = t
        return t

    def dout(self, name, shape, dt=F32):
        a = self.nc.dram_tensor(name, list(shape), dt, kind="ExternalOutput").ap()
        t = T(a, name)
        self.S.toks.append(t.k)
        self.outputs[name] = t
        return t

    def dscr(self, name, shape, dt=F32):
        kind = "ExternalOutput" if name in self.dbg else "Internal"
        a = self.nc.dram_tensor(name, list(shape), dt, kind=kind).ap()
        t = T(a, name)
        self.S.toks.append(t.k)
        return t

    def sb(self, name, shape, dt=F32):
        _UID[0] += 1
        name = "%s_%d" % (name, _UID[0])
        a = self.st.enter_context(self.nc.sbuf_tensor(name, list(shape), dt))
        t = T(a, name)
        self.S.toks.append(t.k)
        return t

    def view(self, ap, name):
        t = T(ap, name)
        self.S.toks.append(t.k)
        return t

    def pe(self, out, lhsT, rhs, start, stop, reads, writes):
        self.S.op("pe", lambda e: e.matmul(out, lhsT, rhs, start=start, stop=stop), reads=reads, writes=writes)

    def tr(self, out, in_, ident, reads, writes):
        self.S.op("pe", lambda e: e.transpose(out, in_, ident), reads=reads, writes=writes)

    def act(self, out, in_, func, reads, writes, scale=1.0, bias=None, accum=None):
        def f(e):
            kw = {}
            if bias is not None:
                kw["bias"] = bias
            if accum is not None:
                kw["accum_out"] = accum
            return e.activation(out=out, in_=in_, func=func, scale=scale, **kw)
        self.S.op("act", f, reads=reads, writes=writes)

    def tt(self, eng, out, in0, in1, op, reads, writes):
        self.S.op(eng, lambda e: e.tensor_tensor(out, in0, in1, op), reads=reads, writes=writes)

    def ts(self, eng, out, in0, s1, op0, reads, writes, s2=None, op1=None):
        if op1 is None:
            self.S.op(eng, lambda e: e.tensor_scalar(out, in0, s1, None, op0), reads=reads, writes=writes)
        else:
            self.S.op(eng, lambda e: e.tensor_scalar(out, in0, s1, s2, op0, op1), reads=reads, writes=writes)

    def stt(self, out, in0, scalar, in1, op0, op1, reads, writes):
        self.S.op("dve", lambda e: e.scalar_tensor_tensor(out, in0, scalar, in1, op0, op1), reads=reads, writes=writes)

    def cp(self, eng, out, in_, reads, writes):
        if eng == "act":
            self.S.op("act", lambda e: e.copy(out, in_), reads=reads, writes=writes)
        else:
            self.S.op(eng, lambda e: e.tensor_copy(out, in_), reads=reads, writes=writes)

    def memset(self, eng, ap, val, writes):
        self.S.op(eng, lambda e: e.memset(ap, val), writes=writes)

    def recip(self, out, in_, reads, writes):
        self.S.op("dve", lambda e: e.reciprocal(out, in_), reads=reads, writes=writes)

    def scan(self, out, d0, d1, init, op0, op1, reads, writes):
        self.S.op("dve", lambda e: e.tensor_tensor_scan(out, d0, d1, init, op0, op1), reads=reads, writes=writes)

    def ld(self, dst_ap, src_ap, tok, reads=(), writes=(), q="sp", **kw):
        self.S.dma(q, dst_ap, src_ap, tok, reads=reads, writes=writes, **kw)

    def stq(self, dst_ap, src_ap, tok, reads=(), mwrites=(), writes=(), q="sp", is_output=False, **kw):
        self.S.dma(q, dst_ap, src_ap, tok, reads=reads, mwrites=mwrites, writes=writes, is_output=is_output, **kw)


def build_program(dbg=None, stages=99):
    nc = bass.Bass("TRN2", target_bir_lowering=False)
    with ExitStack() as gst:
        K = Builder(nc, gst, dbg)
        S = K.S
        I = {}
        def din(name, shape):
            I[name] = K.din(name, shape)
            return I[name]
        din("xT", [D, NT])
        din("pT", [DEPTH, 256, NT])
        din("sC", [DEPTH, NS, 4, 256, 256]); din("sn", [DEPTH, NS, 4, 256]); din("smT", [DEPTH, 4, NS])
        din("s5reT", [DEPTH, 128, 16, NS]); din("s5imT", [DEPTH, 128, 16, NS])
        din("ssdT", [DEPTH, NS, 8, 128, 64]); din("convT", [DEPTH, 128, 8, 3, NS])
        for nm in ("g_mix", "g_ffn", "g_ple"):
            din(nm, [DEPTH, 128, 16])
        din("g_final", [128, 16])
        din("w_in", [DEPTH, D, N_IN]); din("w_out", [DEPTH, D, D])
        din("b_ig", [DEPTH, 4, 1]); din("b_fg", [DEPTH, 4, 1]); din("gml_rep", [DEPTH, 64, 1024])
        for nm in ("lam_re", "lam_im", "logdt"):
            din(nm, [DEPTH, 128, 16])
        din("Bre", [DEPTH, 16, 128, 128]); din("Bim", [DEPTH, 16, 128, 128])
        din("Cre", [DEPTH, 16, 128, 128]); din("Cim", [DEPTH, 16, 128, 128])
        din("s5d", [DEPTH, 128, 4]); din("w_glu", [DEPTH, 512, 512]); din("b_glu", [DEPTH, 128, 4]); din("g_s5", [DEPTH, 128, 4])
        din("conv_w", [DEPTH, 128, 8, 4]); din("conv_b", [DEPTH, 128, 8])
        din("dt_bias", [DEPTH, 8, 1]); din("a_log", [DEPTH, 8, 1]); din("ssdd_rep", [DEPTH, 64, 8]); din("gssd_rep", [DEPTH, 64, 512])
        din("ffn_wg", [D_FF // 256, 128, 4096]); din("ffn_wu", [D_FF // 256, 128, 4096]); din("ffn_wd", [D_FF // 256, 128, 4096])
        din("w_router", [D, NE]); din("b_router_rep", [128, NE])
        din("moe_wg", [NE, D_FFE // 256, 128, 4096]); din("moe_wu", [NE, D_FFE // 256, 128, 4096]); din("moe_wd", [NE, D_FFE // 256, 128, 4096])
        din("w_ple", [DEPTH, 256, D]); din("w_pleg", [DEPTH, D, D])
        din("ident", [128, 128]); din("nident", [128, 128]); din("maskT", [64, 64])
        din("half", [128, 2])
        din("sel4", [4, 4, 128]); din("sel8", [8, 8, 128]); din("sel8e", [8, 8, 128])
        O = {}
        def dout(name, shape):
            O[name] = K.dout(name, shape)
            return O[name]
        dout("yT", [D, NO])
        dout("C_p", [DEPTH, 4, 256, 256]); dout("n_p", [DEPTH, 4, 256]); dout("m_pT", [DEPTH, 4, 1])
        dout("s5re_pT", [DEPTH, 128, 16]); dout("s5im_pT", [DEPTH, 128, 16])
        dout("ssd_pT", [DEPTH, 8, 128, 64]); dout("conv_pT", [DEPTH, 128, 8, 3])
        dout("C_s", [DEPTH, NS, 4, 256, 256]); dout("n_s", [DEPTH, NS, 4, 256]); dout("m_sT", [DEPTH, 4, NS])
        dout("s5re_sT", [DEPTH, 128, 16, NS]); dout("s5im_sT", [DEPTH, 128, 16, NS])
        dout("ssd_sT", [DEPTH, NS, 8, 128, 64]); dout("conv_sT", [DEPTH, 128, 8, 3, NS])
        hT = K.dscr("hT", [D, NT])
        qT_d = K.dscr("qT_d", [1024, NT]); kT_d = K.dscr("kT_d", [1024, NT])
        k_d = K.dscr("k_d", [NT, 1024]); v_d = K.dscr("v_d", [NT, 1024]); go_d = K.dscr("go_d", [NT, 1024])
        uT_d = K.dscr("uT_d", [512, NT]); zs_d = K.dscr("zs_d", [NT, 512]); xbcT_d = K.dscr("xbcT_d", [1024, NT])
        xcT_d = K.dscr("xcT_d", [1024, NT])
        mix_d = K.dscr("mix_d", [D, NT])
        hO = K.dscr("hO", [D, NO])
        mixsw = K.dscr("mixsw", [128, KC * XW], BF16)
        halfsb = K.sb("halfsb", [128, 2])
        K.ld(halfsb.a[:], I["half"].a, halfsb, writes=[halfsb])

        def blend(dstA, srcB, toks_r, tok_w):
            K.ts("dve", dstA, dstA, halfsb.a[:, 0:1], ALU.mult, list(toks_r) + [halfsb], [tok_w])
            K.stt(dstA, srcB, halfsb.a[:, 1:2], dstA, ALU.mult, ALU.add, list(toks_r) + [halfsb, tok_w], [tok_w])

        XTraw = K.sb("XT", [128, KC * XW // 2], F32)
        XTb = XTraw.a[:].bitcast(BF16).rearrange("p (k t) -> p k t", k=KC)
        XTf = XTraw.a[:].rearrange("p (k t) -> p k t", k=KC)
        tXT = XTraw
        ident = K.sb("ident_sb", [128, 128]); nident = K.sb("nident_sb", [128, 128]); maskT = K.sb("maskT_sb", [64, 64])
        K.ld(ident.a[:], I["ident"].a, ident, writes=[ident]); K.ld(nident.a[:], I["nident"].a, nident, writes=[nident])
        K.ld(maskT.a[:], I["maskT"].a, maskT, writes=[maskT])
        ones_bf = K.sb("ones_bf", [128, 128], BF16); ones_f = K.sb("ones_f", [128, 128])
        K.memset("dve", ones_bf.a[:], 1.0, [ones_bf]); K.memset("dve", ones_f.a[:], 1.0, [ones_f])
        epsb = K.sb("epsb", [128, 1]); K.memset("dve", epsb.a[:], EPS, [epsb])
        halfpi = K.sb("halfpi", [128, 1]); K.memset("dve", halfpi.a[:], math.pi / 2, [halfpi])
        gn = {}
        for nm in ("g_mix", "g_ffn", "g_ple"):
            gn[nm] = K.sb(nm + "_sb", [128, DEPTH, 16])
            for l in range(DEPTH):
                K.ld(gn[nm].a[:, l, :], I[nm].a[l], gn[nm], writes=[gn[nm]])
        gfin = K.sb("gfin_sb", [128, 16]); K.ld(gfin.a[:], I["g_final"].a, gfin, writes=[gfin])
        PS = Pool(K, "ps", [128, 512], F32, 8, space="ps")

        class ListPool:
            def __init__(self, bufs):
                self.bufs = bufs
                self.i = 0

            def next(self):
                b = self.bufs[self.i % len(self.bufs)]
                self.i += 1
                return b
        PSs = ListPool([K.view(PS.bufs[b].a[:, j * 64:(j + 1) * 64], "pss%d_%d" % (b, j)) for b in range(4) for j in range(8)])
        PSb = ListPool([K.view(PS.bufs[b].a, "psb%d" % b) for b in range(4, 8)])
        K.PSs, K.PSb = PSs, PSb
        S.flush()

        def rstd_from_ps(ps, n, inv_d, rs, parts=128):
            K.act(rs.a[:parts, :n], ps.a[:parts, :n], AF.Sqrt, [ps, epsb], [rs], scale=inv_d, bias=epsb.a[:parts, :])
            K.recip(rs.a[:parts, :n], rs.a[:parts, :n], [rs], [rs])

        def norm_stage(src, gsb_ap, gtile, t0, n, dst_ap, dst_tile, P):
            hb = P["hblk"].next()
            K.ld(hb.a[:, :, :n], src.a.rearrange("(k p) t -> p k t", p=128)[:, :, t0:t0 + n], hb, reads=[src], writes=[hb])
            sq = P["sq"].next()
            K.act(sq.a[:, :, :n], hb.a[:, :, :n], AF.Square, [hb], [sq])
            ps = PS.next()
            for k in range(KC):
                K.pe(ps.a[:, :n], ones_bf.a[:], sq.a[:, k, :n], k == 0, k == KC - 1, [sq, ones_bf], [ps])
            rs = P["rs"].next()
            rstd_from_ps(ps, n, 1.0 / D, rs)
            for k in range(KC):
                K.stt(dst_ap[:, k, :], hb.a[:, k, :n], gsb_ap[:, k:k + 1], rs.a[:, :n], ALU.mult, ALU.mult,
                      [hb, rs, gtile], [dst_tile])
            return hb

        def wload(P, W_ap, kc, pw, name="w"):
            wb = P[name].next()
            K.ld(wb.a[:, :kc, :pw], W_ap.rearrange("(k p) c -> p k c", p=128), wb, writes=[wb], q="pool")
            return wb

        def proj_fm(P, xap, xt, kc, W_ap, c0, ncols, evac, tblocks):
            for p0 in range(0, ncols, 512):
                pw = min(512, ncols - p0)
                wb = wload(P, W_ap[:, c0 + p0:c0 + p0 + pw], kc, pw)
                for m0 in range(0, pw, 128):
                    mw = min(128, pw - m0)
                    for (t0, n) in tblocks:
                        ps = PS.next()
                        for k in range(kc):
                            K.pe(ps.a[:mw, :n], wb.a[:, k, m0:m0 + mw], xap[:, k, t0:t0 + n], k == 0, k == kc - 1, [wb, xt], [ps])
                        evac(ps, p0 + m0, mw, t0, n)

        def proj_tm(P, xap, xt, kc, W_ap, c0, ncols, evac):
            for p0 in range(0, ncols, 512):
                pw = min(512, ncols - p0)
                wb = wload(P, W_ap[:, c0 + p0:c0 + p0 + pw], kc, pw)
                for j in range(17):
                    t0 = j * 128
                    n = 128 if j < 16 else NS
                    ps = PS.next()
                    for k in range(kc):
                        K.pe(ps.a[:n, :pw], xap[:, k, t0:t0 + n], wb.a[:, k, :pw], k == 0, k == kc - 1, [wb, xt], [ps])
                    evac(ps, p0, pw, t0, n)

        evi = [0]
        def evac_copy_to_dram(P, ps, pr, fr, dst_ap, dst, scale=None, func=None, mul=None, multile=None):
            stg = P["stg"].next()
            if func is not None:
                K.act(stg.a[:pr, :fr], ps.a[:pr, :fr], func, [ps], [stg])
                if mul is not None:
                    K.tt("dve", stg.a[:pr, :fr], stg.a[:pr, :fr], mul, ALU.mult, [stg, multile], [stg])
            elif scale is not None:
                K.act(stg.a[:pr, :fr], ps.a[:pr, :fr], AF.Copy, [ps], [stg], scale=scale)
            else:
                evi[0] += 1
                K.cp("act" if evi[0] % 2 else "dve", stg.a[:pr, :fr], ps.a[:pr, :fr], [ps], [stg])
            K.stq(dst_ap, stg.a[:pr, :fr], stg, reads=[stg], mwrites=[dst])

        S.dma("sp", hT.a, I["xT"].a, hT, writes=[hT])
        S.flush()

        for l in range(DEPTH):
            if stages < 1:
                break
            with K.phase():
                P = {"hblk": Pool(K, "hblk", [128, KC, 512], F32, 1), "sq": Pool(K, "sq", [128, KC, 512], BF16, 1),
                     "rs": Pool(K, "rs", [128, 512], F32, 2), "w": Pool(K, "w", [128, KC, 512], BF16, 3),
                     "stg": Pool(K, "stg", [128, 512], F32, 4)}
                gi = K.sb("gi", [4, NT]); gf = K.sb("gf", [4, NT]); gdt = K.sb("gdt", [8, NT])
                gml = K.sb("gml", [128, 1024])
                for r in range(2):
                    K.ld(gml.a[r * 64:(r + 1) * 64, :], I["gml_rep"].a[l], gml, writes=[gml])
                for (t0, n) in TB:
                    norm_stage(hT, gn["g_mix"].a[:, l, :], gn["g_mix"], t0, n, XTb[:, :, t0:t0 + n], tXT, P)
                W = I["w_in"].a[l]
                proj_fm(P, XTb, tXT, KC, W, 0, 1024, lambda ps, co, mw, t0, n: evac_copy_to_dram(P, ps, mw, n, qT_d.a[co:co + mw, t0:t0 + n], qT_d), TB)
                proj_fm(P, XTb, tXT, KC, W, 1024, 1024, lambda ps, co, mw, t0, n: evac_copy_to_dram(P, ps, mw, n, kT_d.a[co:co + mw, t0:t0 + n], kT_d, scale=1.0 / 16.0), TB)
                proj_tm(P, XTb, tXT, KC, W, 1024, 1024, lambda ps, co, pw, t0, n: evac_copy_to_dram(P, ps, n, pw, k_d.a[t0:t0 + n, co:co + pw], k_d, scale=1.0 / 16.0))
                proj_tm(P, XTb, tXT, KC, W, 2048, 1024, lambda ps, co, pw, t0, n: evac_copy_to_dram(P, ps, n, pw, v_d.a[t0:t0 + n, co:co + pw], v_d))
                proj_tm(P, XTb, tXT, KC, W, 3072, 1024, lambda ps, co, pw, t0, n: evac_copy_to_dram(P, ps, n, pw, go_d.a[t0:t0 + n, co:co + pw], go_d, func=AF.Sigmoid, mul=gml.a[:n, co:co + pw], multile=gml))
                gsc = {}
                for nm, c0, nr in (("gi_d", 4096, 4), ("gf_d", 4100, 4), ("gdt_d", 6152, 8)):
                    gsc[nm] = K.dscr(nm + str(l), [nr, NT])
                    proj_fm(P, XTb, tXT, KC, W, c0, nr, lambda ps, co, mw, t0, n, nm=nm: evac_copy_to_dram(P, ps, mw, n, gsc[nm].a[co:co + mw, t0:t0 + n], gsc[nm]), TB)
                proj_fm(P, XTb, tXT, KC, W, 4104, 512, lambda ps, co, mw, t0, n: evac_copy_to_dram(P, ps, mw, n, uT_d.a[co:co + mw, t0:t0 + n], uT_d), TB)
                proj_tm(P, XTb, tXT, KC, W, 4616, 512, lambda ps, co, pw, t0, n: evac_copy_to_dram(P, ps, n, pw, zs_d.a[t0:t0 + n, co:co + pw], zs_d, func=AF.Silu))
                proj_fm(P, XTb, tXT, KC, W, 5128, 1024, lambda ps, co, mw, t0, n: evac_copy_to_dram(P, ps, mw, n, xbcT_d.a[co:co + mw, t0:t0 + n], xbcT_d), TB)
            if stages < 2:
                break
            mixers(K, S, PS, I, O, l, dict(hT=hT, qT_d=qT_d, kT_d=kT_d, k_d=k_d, v_d=v_d, go_d=go_d, uT_d=uT_d, zs_d=zs_d,
                                           xbcT_d=xbcT_d, xcT_d=xcT_d, gi_d=gsc["gi_d"], gf_d=gsc["gf_d"], gdt_d=gsc["gdt_d"]),
                   XTb, tXT, ident, nident, maskT, ones_f, epsb, halfpi, gst, stages)
            if "mix_d" in K.dbg:
                with K.phase():
                    stg = Pool(K, "mstg", [128, 512], F32, 2)
                    for k in range(KC):
                        for (t0, n) in TB:
                            s_ = stg.next()
                            K.cp("dve", s_.a[:, :n], XTb[:, k, t0:t0 + n], [tXT], [s_])
                            K.stq(mix_d.a[k * 128:(k + 1) * 128, t0:t0 + n], s_.a[:, :n], s_, reads=[s_], mwrites=[mix_d])
            if stages < 3:
                break
            own = (l == DEPTH - 1)
            hcur = hO if own else hT
            tbl = TBO if own else TB
            if own:
                with K.phase():
                    for k in range(KC):
                        blend(XTb[:, k, 0:1024], XTb[:, k, 1024:2048], [tXT], tXT)
                    for k in range(KC):
                        K.cp("dve", XTb[:, k, 1024:NO], XTb[:, k, L:NT], [tXT], [tXT])
                    hsw = Pool(K, "hsw", [128, 2, 1024], F32, 2)
                    for k in range(KC):
                        t_ = hsw.next()
                        K.ld(t_.a[:], hT.a[k * 128:(k + 1) * 128, 0:L].rearrange("p (h t) -> p h t", h=2), t_, reads=[hT], writes=[t_])
                        blend(t_.a[:, 0, :], t_.a[:, 1, :], [t_], t_)
                        K.stq(hO.a[k * 128:(k + 1) * 128, 0:1024], t_.a[:, 0, :], t_, reads=[t_], mwrites=[hO])
                    S.dma("sp", hO.a[:, 1024:NO], hT.a[:, L:NT], hO, reads=[hT], mwrites=[hO])
            with K.phase():
                P = {"w": Pool(K, "w", [128, KC, 512], BF16, 3), "stg": Pool(K, "stg", [128, 512], F32, 4),
                     "hb": Pool(K, "hb", [128, 512], F32, 3)}
                def ev_res(ps, co, mw, t0, n):
                    hb = P["hb"].next()
                    K.ld(hb.a[:mw, :n], hcur.a[co:co + mw, t0:t0 + n], hb, reads=[hcur], writes=[hb])
                    stg = P["stg"].next()
                    K.tt("dve", stg.a[:mw, :n], ps.a[:mw, :n], hb.a[:mw, :n], ALU.add, [ps, hb], [stg])
                    K.stq(hcur.a[co:co + mw, t0:t0 + n], stg.a[:mw, :n], stg, reads=[stg], mwrites=[hcur])
                proj_fm(P, XTb, tXT, KC, I["w_out"].a[l], 0, D, ev_res, tbl)
            if stages < 4:
                break
            ffn_phase(K, S, PS, I, l, hcur, XTf, tXT, gn, ones_bf, epsb, ident, gst, norm_stage, stages, own)
            if stages < 5:
                break
            with K.phase():
                P = {"hblk": Pool(K, "hblk", [128, KC, 512], F32, 1), "sq": Pool(K, "sq", [128, KC, 512], BF16, 1),
                     "rs": Pool(K, "rs", [128, 512], F32, 2), "w": Pool(K, "w", [128, KC, 512], BF16, 2),
                     "stg": Pool(K, "stg", [128, 512], F32, 3), "hb": Pool(K, "hb", [128, 512], F32, 2)}
                pTf = K.sb("pTf", [128, 2, NT]); pTb = K.sb("pTb", [128, 2, NT], BF16)
                pTv = I["pT"].a[l].rearrange("(k p) t -> p k t", p=128)
                K.ld(pTf.a[:], pTv, pTf, writes=[pTf])
                if own:
                    for k in range(2):
                        blend(pTf.a[:, k, 0:1024], pTf.a[:, k, 1024:2048], [pTf], pTf)
                    for k in range(2):
                        K.cp("dve", pTf.a[:, k, 1024:NO], pTf.a[:, k, L:NT], [pTf], [pTf])
                K.cp("act", pTb.a[:], pTf.a[:], [pTf], [pTb])
                wple = K.sb("wple", [128, 2, D], BF16)
                K.ld(wple.a[:], I["w_ple"].a[l].rearrange("(k p) c -> p k c", p=128), wple, writes=[wple], q="pool")
                for (t0, n) in tbl:
                    norm_stage(hcur, gn["g_ple"].a[:, l, :], gn["g_ple"], t0, n, XTb[:, :, t0:t0 + n], tXT, P)
                def ev_ple(ps, co, mw, t0, n):
                    sg = P["stg"].next()
                    K.act(sg.a[:mw, :n], ps.a[:mw, :n], AF.Sigmoid, [ps], [sg])
                    ps2 = PS.next()
                    for k in range(2):
                        K.pe(ps2.a[:mw, :n], wple.a[:, k, co:co + mw], pTb.a[:, k, t0:t0 + n], k == 0, k == 1, [wple, pTb], [ps2])
                    K.tt("dve", sg.a[:mw, :n], ps2.a[:mw, :n], sg.a[:mw, :n], ALU.mult, [ps2, sg], [sg])
                    hb = P["hb"].next()
                    K.ld(hb.a[:mw, :n], hcur.a[co:co + mw, t0:t0 + n], hb, reads=[hcur], writes=[hb])
                    K.tt("dve", sg.a[:mw, :n], sg.a[:mw, :n], hb.a[:mw, :n], ALU.add, [sg, hb], [sg])
                    K.stq(hcur.a[co:co + mw, t0:t0 + n], sg.a[:mw, :n], sg, reads=[sg], mwrites=[hcur])
                proj_fm(P, XTb, tXT, KC, I["w_pleg"].a[l], 0, D, ev_ple, tbl)
        with K.phase():
            P = {"hblk": Pool(K, "hblk", [128, KC, 512], F32, 1), "sq": Pool(K, "sq", [128, KC, 512], BF16, 1),
                 "rs": Pool(K, "rs", [128, 512], F32, 2)}
            yb = K.sb("yb", [128, KC, 512])
            for (t0, n) in TBO:
                norm_stage(hO, gfin.a, gfin, t0, n, yb.a[:, :, :n], yb, P)
                K.stq(O["yT"].a.rearrange("(k p) t -> p k t", p=128)[:, :, t0:t0 + n], yb.a[:, :, :n], yb, reads=[yb], mwrites=[O["yT"]], is_output=True)
    return nc, K


def mixers(K, S, PS, I, O, l, Dm, XTb, tXT, ident, nident, maskT, ones_f, epsb, halfpi, gst, stages):
    with K.phase():
        rowt = [K.sb("row%d" % i, [8, NT]) for i in range(7)]
        A, Bt, Ct, Dt, Et, Fa, Gm = rowt
        onesr = K.sb("onesr", [8, 1]); K.memset("dve", onesr.a[:], 1.0, [onesr])
        TM = K.sb("TM", [64, NCI, 20])
        TMB = K.sb("TMB", [64, NCI, 32])
        GLb = K.sb("GLb", [128, 4 * NCI]); DECb = K.sb("DECb", [128, 8 * NCI])
        sel4 = K.sb("sel4", [4, 4, 128]); K.ld(sel4.a[:], I["sel4"].a, sel4, writes=[sel4])
        sel8 = K.sb("sel8", [8, 8, 128]); K.ld(sel8.a[:], I["sel8"].a, sel8, writes=[sel8])
        small = Pool(K, "small", [8, NCI], F32, 4)

        def to_tm(src, nr, dst, col0):
            for (t0, cl, ci) in CHUNKS:
                ps = PS.next()
                K.tr(ps.a[:cl, :nr], src.a[:nr, t0:t0 + cl], ident.a[:nr, :nr], [src, ident], [ps])
                K.cp("act" if ci % 2 else "dve", dst.a[:cl, ci, col0:col0 + nr], ps.a[:cl, :nr], [ps], [dst])

        def bcast_rows(rows, nr, sel, dst, ncol):
            for h in range(nr):
                ps = PS.next()
                K.pe(ps.a[:, :ncol], sel.a[:nr, h, :], rows.a[:nr, :ncol], True, True, [sel, rows], [ps])
                K.cp("act", dst.a[:, h * ncol:(h + 1) * ncol], ps.a[:, :ncol], [ps], [dst])

        def chunkview(t, nr):
            return t.a[:nr, :L].rearrange("p (c t) -> p c t", t=64)

        def prevlast(Mt, nr, init_s, prev, last):
            K.memset("dve", prev.a[:nr, 0:1], 0.0, [prev])
            K.cp("dve", prev.a[:nr, 1:32], Mt.a[:nr, 63:L - 64:64], [Mt], [prev])
            if init_s is None:
                K.memset("dve", prev.a[:nr, 32:NCI], 0.0, [prev])
            else:
                K.cp("dve", prev.a[:nr, 32:NCI], init_s, [Mt], [prev])
            K.cp("dve", last.a[:nr, 0:32], Mt.a[:nr, 63:L:64], [Mt], [last])
            K.cp("dve", last.a[:nr, 32:NCI], Mt.a[:nr, L:NT], [Mt], [last])

        def sub_chunk(out, x, cvals, nr, sign):
            cb = cvals.a[:nr, 0:32].unsqueeze(2).to_broadcast([nr, 32, 64])
            if sign > 0:
                K.tt("dve", chunkview(out, nr), chunkview(x, nr), cb, ALU.subtract, [x, cvals], [out])
                K.tt("dve", out.a[:nr, L:NT], x.a[:nr, L:NT], cvals.a[:nr, 32:NCI], ALU.subtract, [x, cvals], [out])
            else:
                K.tt("dve", chunkview(out, nr), cb, chunkview(x, nr), ALU.subtract, [x, cvals], [out])
                K.tt("dve", out.a[:nr, L:NT], cvals.a[:nr, 32:NCI], x.a[:nr, L:NT], ALU.subtract, [x, cvals], [out])

        def softplus_neg(x, t1, t2, nr, neg_in):
            K.act(t1.a[:nr, :], x.a[:nr, :], AF.Abs, [x], [t1])
            K.act(t1.a[:nr, :], t1.a[:nr, :], AF.Exp, [t1], [t1], scale=-1.0)
            K.act(t1.a[:nr, :], t1.a[:nr, :], AF.Ln, [t1], [t1], bias=1.0)
            K.ts("dve", t2.a[:nr, :], x.a[:nr, :], 0.0, ALU.min if neg_in else ALU.max, [x], [t2])

        bi = K.sb("bi", [4, 1]); bf = K.sb("bf", [4, 1]); m0s = K.sb("m0s", [4, NS])
        K.ld(bi.a[:], I["b_ig"].a[l], bi, writes=[bi]); K.ld(bf.a[:], I["b_fg"].a[l], bf, writes=[bf])
        K.ld(m0s.a[:], I["smT"].a[l], m0s, writes=[m0s])
        K.ld(A.a[:4, :], Dm["gi_d"].a, A, reads=[Dm["gi_d"]], writes=[A])
        K.ld(Bt.a[:4, :], Dm["gf_d"].a, Bt, reads=[Dm["gf_d"]], writes=[Bt])
        K.ts("dve", A.a[:4, :], A.a[:4, :], bi.a[:, 0:1], ALU.add, [A, bi], [A])
        K.ts("dve", Bt.a[:4, :], Bt.a[:4, :], bf.a[:, 0:1], ALU.add, [Bt, bf], [Bt])
        softplus_neg(Bt, Ct, Dt, 4, True)
        K.tt("dve", Bt.a[:4, :], Dt.a[:4, :], Ct.a[:4, :], ALU.subtract, [Dt, Ct], [Bt])
        K.scan(Et.a[:4, :L], onesr.a[:4, 0:1].to_broadcast([4, L]), Bt.a[:4, :L], 0.0, ALU.mult, ALU.add, [onesr, Bt], [Et])
        K.cp("dve", Et.a[:4, L:NT], Bt.a[:4, L:NT], [Bt], [Et])
        K.tt("dve", Fa.a[:4, :], A.a[:4, :], Et.a[:4, :], ALU.subtract, [A, Et], [Fa])
        K.scan(Gm.a[:4, :L], Fa.a[:4, :L], Fa.a[:4, :L], 0.0, ALU.max, ALU.max, [Fa], [Gm])
        K.tt("dve", Gm.a[:4, L:NT], Fa.a[:4, L:NT], m0s.a[:], ALU.max, [Fa, m0s], [Gm])
        Mprev = small.next(); Mlast = small.next()
        prevlast(Gm, 4, m0s.a[:], Mprev, Mlast)
        glr = small.next()
        K.tt("dve", glr.a[:4, :], Mprev.a[:4, :], Mlast.a[:4, :], ALU.subtract, [Mprev, Mlast], [glr])
        K.act(glr.a[:4, :], glr.a[:4, :], AF.Exp, [glr], [glr])
        bcast_rows(glr, 4, sel4, GLb, NCI)
        sub_chunk(A, Gm, Mprev, 4, -1)
        K.act(A.a[:4, :], A.a[:4, :], AF.Exp, [A], [A])
        sub_chunk(Dt, Fa, Mlast, 4, +1)
        K.act(Dt.a[:4, :], Dt.a[:4, :], AF.Exp, [Dt], [Dt])
        K.tt("dve", Ct.a[:4, :], Et.a[:4, :], Gm.a[:4, :], ALU.add, [Et, Gm], [Ct])
        K.stq(O["m_pT"].a[l], Ct.a[:4, L - 1:L], Ct, reads=[Ct], mwrites=[O["m_pT"]], is_output=True)
        K.stq(O["m_sT"].a[l], Ct.a[:4, L:NT], Ct, reads=[Ct], mwrites=[O["m_sT"]], is_output=True)
        K.act(Ct.a[:4, :], Ct.a[:4, :], AF.Exp, [Ct], [Ct], scale=-1.0)
        for src, c0 in ((Fa, 0), (Gm, 4), (A, 8), (Ct, 12), (Dt, 16)):
            to_tm(src, 4, TM, c0)

        cpool = {"q": Pool(K, "mq", [128, 8, 64], F32, 2), "k": Pool(K, "mk", [128, 8, 64], F32, 2),
                 "kt": Pool(K, "mkt", [64, 1024], F32, 1), "v": Pool(K, "mv", [64, 4, 257], F32, 2),
                 "go": Pool(K, "mgo", [64, 1024], F32, 1), "hm": Pool(K, "mhm", [64, 1024], F32, 1),
                 "w": Pool(K, "mw", [64, 64], F32, 8), "sw": Pool(K, "msw", [64, 64], F32, 8),
                 "nsb": Pool(K, "mnsb", [64, 257], F32, 4), "res": Pool(K, "mres", [64, 257], F32, 4),
                 "kw": Pool(K, "mkw", [64, 256], F32, 4), "sc": Pool(K, "msc", [64, 4, 4], F32, 3),
                 "junk": Pool(K, "mjunk", [64, 256], F32, 2)}
        for b_ in cpool["v"].bufs:
            K.memset("dve", b_.a[:, :, 256:257], 1.0, [b_])
        Cst = [K.sb("Caug%d" % h, [128, 2, 257]) for h in range(4)]

        PSs, PSb = PS, PS

        def mlstm_chunk(t0, cl, ci, Caug):
            nk = dict(allow_slow_non_contiguous=True) if cl == 1 else {}
            q = cpool["q"].next(); kT = cpool["k"].next(); kt = cpool["kt"].next(); v = cpool["v"].next(); go = cpool["go"].next()
            K.ld(q.a[:, :, :cl], Dm["qT_d"].a.rearrange("(j p) t -> p j t", p=128)[:, :, t0:t0 + cl], q, reads=[Dm["qT_d"]], writes=[q], **nk)
            K.ld(kT.a[:, :, :cl], Dm["kT_d"].a.rearrange("(j p) t -> p j t", p=128)[:, :, t0:t0 + cl], kT, reads=[Dm["kT_d"]], writes=[kT], **nk)
            K.ld(kt.a[:cl, :], Dm["k_d"].a[t0:t0 + cl, :], kt, reads=[Dm["k_d"]], writes=[kt])
            K.ld(v.a[:cl, :, 0:256], Dm["v_d"].a[t0:t0 + cl, :].rearrange("t (h d) -> t h d", h=4), v, reads=[Dm["v_d"]], writes=[v])
            K.ld(go.a[:cl, :], Dm["go_d"].a[t0:t0 + cl, :], go, reads=[Dm["go_d"]], writes=[go])
            hm = cpool["hm"].next()
            H = range(4)
            ps_s = {}; ps_d = {}; w = {}; sw = {}; ps_n = {}; ps_i = {}; nsb = {}; res = {}; sc = {}; kw = {}
            for h in H:
                ps_s[h] = PSs.next()
                for kc in range(2):
                    K.pe(ps_s[h].a[:cl, :cl], kT.a[:, h * 2 + kc, :cl], q.a[:, h * 2 + kc, :cl], kc == 0, kc == 1, [kT, q], [ps_s[h]])
            for h in H:
                ps_d[h] = PSs.next()
                K.pe(ps_d[h].a[:cl, :cl], TM.a[:cl, ci, 4 + h:5 + h].to_broadcast([cl, cl]), nident.a[:cl, :cl], True, False, [TM, nident], [ps_d[h]])
                K.pe(ps_d[h].a[:cl, :cl], ident.a[:cl, :cl], maskT.a[:cl, :cl], False, False, [ident, maskT], [ps_d[h]])
                K.pe(ps_d[h].a[:cl, :cl], ident.a[:cl, :cl], TM.a[:cl, ci, h:h + 1].to_broadcast([cl, cl]), False, True, [ident, TM], [ps_d[h]])
            for h in H:
                w[h] = cpool["w"].next()
                K.act(w[h].a[:cl, :cl], ps_d[h].a[:cl, :cl], AF.Exp, [ps_d[h]], [w[h]])
            for h in H:
                kw[h] = cpool["kw"].next()
                K.act(kw[h].a[:cl, :], kt.a[:cl, h * 256:(h + 1) * 256], AF.Copy, [kt, TM], [kw[h]], scale=TM.a[:cl, ci, 16 + h:17 + h])
            for h in H:
                sw[h] = cpool["sw"].next()
                K.tt("dve", sw[h].a[:cl, :cl], ps_s[h].a[:cl, :cl], w[h].a[:cl, :cl], ALU.mult, [ps_s[h], w[h]], [sw[h]])
            for h in H:
                ps_n[h] = PSb.next()
                K.pe(ps_n[h].a[:cl, :257], sw[h].a[:cl, :cl], v.a[:cl, h, :], True, True, [sw[h], v], [ps_n[h]])
            for h in H:
                nsb[h] = cpool["nsb"].next()
                K.cp("act", nsb[h].a[:cl, :], ps_n[h].a[:cl, :257], [ps_n[h]], [nsb[h]])
            for h in H:
                ps_i[h] = PSb.next()
                for kc in range(2):
                    K.pe(ps_i[h].a[:cl, :257], q.a[:, h * 2 + kc, :cl], Caug[h].a[:, kc, :], kc == 0, kc == 1, [q, Caug[h]], [ps_i[h]])
            for h in H:
                res[h] = cpool["res"].next()
                K.stt(res[h].a[:cl, :], ps_i[h].a[:cl, :257], TM.a[:cl, ci, 8 + h:9 + h], nsb[h].a[:cl, :], ALU.mult, ALU.add, [ps_i[h], TM, nsb[h]], [res[h]])
            for hp in range(2):
                pcs = {}
                for h in (2 * hp, 2 * hp + 1):
                    for kc in range(2):
                        pcs[(h, kc)] = PSb.next()
                        K.pe(pcs[(h, kc)].a[:, :257], kw[h].a[:cl, kc * 128:(kc + 1) * 128], v.a[:cl, h, :], True, True, [kw[h], v], [pcs[(h, kc)]])
                for h in (2 * hp, 2 * hp + 1):
                    for kc in range(2):
                        K.stt(Caug[h].a[:, kc, :], Caug[h].a[:, kc, :], GLb.a[:, h * NCI + ci:h * NCI + ci + 1], pcs[(h, kc)].a[:, :257], ALU.mult, ALU.add, [Caug[h], GLb, pcs[(h, kc)]], [Caug[h]])
            sca = cpool["sc"].next()
            for h in H:
                K.act(sca.a[:cl, h, 0:1], res[h].a[:cl, 256:257], AF.Abs, [res[h]], [sca])
            K.tt("dve", sca.a[:cl, :, 0], sca.a[:cl, :, 0], TM.a[:cl, ci, 12:16], ALU.max, [sca, TM], [sca])
            K.recip(sca.a[:cl, :, 0], sca.a[:cl, :, 0], [sca], [sca])
            for h in H:
                junk = cpool["junk"].next()
                K.act(junk.a[:cl, :], res[h].a[:cl, 0:256], AF.Square, [res[h], sca], [junk, sca], scale=sca.a[:cl, h, 0:1], accum=sca.a[:cl, h, 1:2])
            K.act(sca.a[:cl, :, 2], sca.a[:cl, :, 1], AF.Sqrt, [sca, epsb], [sca], scale=1.0 / 256.0, bias=epsb.a[:cl, :])
            K.recip(sca.a[:cl, :, 2], sca.a[:cl, :, 2], [sca], [sca])
            K.tt("dve", sca.a[:cl, :, 3], sca.a[:cl, :, 2], sca.a[:cl, :, 0], ALU.mult, [sca], [sca])
            for h in H:
                K.stt(hm.a[:cl, h * 256:(h + 1) * 256], res[h].a[:cl, 0:256], sca.a[:cl, h, 3:4], go.a[:cl, h * 256:(h + 1) * 256], ALU.mult, ALU.mult, [res[h], sca, go], [hm])
            for j in range(8):
                ps = PSs.next()
                K.tr(ps.a[:, :cl], hm.a[:cl, j * 128:(j + 1) * 128], ident.a[:cl, :cl], [hm, ident], [ps])
                K.cp("act" if j % 2 else "dve", XTb[:, j, t0:t0 + cl], ps.a[:, :cl], [ps], [tXT])

        for h in range(4):
            K.memset("dve", Cst[h].a[:], 0.0, [Cst[h]])
        for (t0, cl, ci) in CHUNKS[:32]:
            mlstm_chunk(t0, cl, ci, Cst)
        for h in range(4):
            K.stq(O["C_p"].a[l, h].rearrange("(kc p) d -> p kc d", p=128), Cst[h].a[:, :, 0:256], Cst[h], reads=[Cst[h]], mwrites=[O["C_p"]], is_output=True)
            K.stq(O["n_p"].a[l, h].rearrange("(kc p) -> p kc", p=128), Cst[h].a[:, :, 256], Cst[h], reads=[Cst[h]], mwrites=[O["n_p"]], is_output=True, allow_slow_non_contiguous=True)
        for (t0, cl, ci) in CHUNKS[32:]:
            j = ci - 32
            Cs = [Cst[h] for h in range(4)]
            for h in range(4):
                K.ld(Cs[h].a[:, :, 0:256], I["sC"].a[l, j, h].rearrange("(kc p) d -> p kc d", p=128), Cs[h], writes=[Cs[h]])
                K.ld(Cs[h].a[:, :, 256], I["sn"].a[l, j, h].rearrange("(kc p) -> p kc", p=128), Cs[h], writes=[Cs[h]], allow_slow_non_contiguous=True)
            mlstm_chunk(t0, cl, ci, Cs)
            for h in range(4):
                K.stq(O["C_s"].a[l, j, h].rearrange("(kc p) d -> p kc d", p=128), Cs[h].a[:, :, 0:256], Cs[h], reads=[Cs[h]], mwrites=[O["C_s"]], is_output=True)
                K.stq(O["n_s"].a[l, j, h].rearrange("(kc p) -> p kc", p=128), Cs[h].a[:, :, 256], Cs[h], reads=[Cs[h]], mwrites=[O["n_s"]], is_output=True, allow_slow_non_contiguous=True)
    if stages >= 2.3:
        ssd_mixer(K, S, PS, I, O, l, Dm, XTb, tXT, ident, nident, maskT, epsb)
    if stages >= 2.6:
        s5_mixer(K, S, PS, I, O, l, Dm, XTb, tXT, ident, ones_f, epsb, halfpi)


def _mk_helpers(K, PS, ident):
    def to_tm(src, nr, dst, col0):
        for (t0, cl, ci) in CHUNKS:
            ps = PS.next()
            K.tr(ps.a[:cl, :nr], src.a[:nr, t0:t0 + cl], ident.a[:nr, :nr], [src, ident], [ps])
            K.cp("act" if ci % 2 else "dve", dst.a[:cl, ci, col0:col0 + nr], ps.a[:cl, :nr], [ps], [dst])

    def bcast_rows(rows, nr, sel, dst, ncol):
        for h in range(nr):
            ps = PS.next()
            K.pe(ps.a[:, :ncol], sel.a[:nr, h, :], rows.a[:nr, :ncol], True, True, [sel, rows], [ps])
            K.cp("act", dst.a[:, h * ncol:(h + 1) * ncol], ps.a[:, :ncol], [ps], [dst])

    def chunkview(t, nr):
        return t.a[:nr, :L].rearrange("p (c t) -> p c t", t=64)

    def prevlast(Mt, nr, init_s, prev, last):
        K.memset("dve", prev.a[:nr, 0:1], 0.0, [prev])
        K.cp("dve", prev.a[:nr, 1:32], Mt.a[:nr, 63:L - 64:64], [Mt], [prev])
        if init_s is None:
            K.memset("dve", prev.a[:nr, 32:NCI], 0.0, [prev])
        else:
            K.cp("dve", prev.a[:nr, 32:NCI], init_s, [Mt], [prev])
        K.cp("dve", last.a[:nr, 0:32], Mt.a[:nr, 63:L:64], [Mt], [last])
        K.cp("dve", last.a[:nr, 32:NCI], Mt.a[:nr, L:NT], [Mt], [last])

    def sub_chunk(out, x, cvals, nr, sign):
        cb = cvals.a[:nr, 0:32].unsqueeze(2).to_broadcast([nr, 32, 64])
        if sign > 0:
            K.tt("dve", chunkview(out, nr), chunkview(x, nr), cb, ALU.subtract, [x, cvals], [out])
            K.tt("dve", out.a[:nr, L:NT], x.a[:nr, L:NT], cvals.a[:nr, 32:NCI], ALU.subtract, [x, cvals], [out])
        else:
            K.tt("dve", chunkview(out, nr), cb, chunkview(x, nr), ALU.subtract, [x, cvals], [out])
            K.tt("dve", out.a[:nr, L:NT], cvals.a[:nr, 32:NCI], x.a[:nr, L:NT], ALU.subtract, [x, cvals], [out])
    return to_tm, bcast_rows, prevlast, sub_chunk


def ssd_mixer(K, S, PS, I, O, l, Dm, XTb, tXT, ident, nident, maskT, epsb):
    xbcT_d, xcT_d = Dm["xbcT_d"], Dm["xcT_d"]
    with K.phase():
        to_tm, bcast_rows, prevlast, sub_chunk = _mk_helpers(K, PS, ident)
        cw = K.sb("cw", [128, 8, 4]); cb = K.sb("cb", [128, 8]); cin = K.sb("cin", [128, 8, 3, NS])
        K.ld(cw.a[:], I["conv_w"].a[l], cw, writes=[cw]); K.ld(cb.a[:], I["conv_b"].a[l], cb, writes=[cb])
        K.ld(cin.a[:], I["convT"].a[l], cin, writes=[cin])
        xp = Pool(K, "xp", [128, 3 + 512], F32, 2); xo = Pool(K, "xo", [128, 512], F32, 2)
        for j in range(8):
            rows = slice(j * 128, (j + 1) * 128)
            for (t0, n) in TB[:4]:
                x = xp.next()
                K.ld(x.a[:, 3:3 + n], xbcT_d.a[rows, t0:t0 + n], x, reads=[xbcT_d], writes=[x])
                if t0 == 0:
                    K.memset("dve", x.a[:, 0:3], 0.0, [x])
                else:
                    K.ld(x.a[:, 0:3], xbcT_d.a[rows, t0 - 3:t0], x, reads=[xbcT_d], writes=[x])
                o = xo.next()
                K.ts("dve", o.a[:, :n], x.a[:, 3:3 + n], cw.a[:, j, 3:4], ALU.mult, [x, cw], [o])
                for tap in (2, 1, 0):
                    K.stt(o.a[:, :n], x.a[:, tap:tap + n], cw.a[:, j, tap:tap + 1], o.a[:, :n], ALU.mult, ALU.add, [x, cw, o], [o])
                K.act(o.a[:, :n], o.a[:, :n], AF.Silu, [o, cb], [o], bias=cb.a[:, j:j + 1])
                K.stq(xcT_d.a[rows, t0:t0 + n], o.a[:, :n], o, reads=[o], mwrites=[xcT_d])
                if t0 == 1536:
                    K.stq(O["conv_pT"].a[l, :, j, :], x.a[:, 512:515], x, reads=[x], mwrites=[O["conv_pT"]], is_output=True)
            x = xp.next()
            K.ld(x.a[:, 0:NS], xbcT_d.a[rows, L:NT], x, reads=[xbcT_d], writes=[x])
            o = xo.next()
            K.ts("dve", o.a[:, :NS], x.a[:, 0:NS], cw.a[:, j, 3:4], ALU.mult, [x, cw], [o])
            for tap in (2, 1, 0):
                K.stt(o.a[:, :NS], cin.a[:, j, tap, :], cw.a[:, j, tap:tap + 1], o.a[:, :NS], ALU.mult, ALU.add, [cin, cw, o], [o])
            K.act(o.a[:, :NS], o.a[:, :NS], AF.Silu, [o, cb], [o], bias=cb.a[:, j:j + 1])
            K.stq(xcT_d.a[rows, L:NT], o.a[:, :NS], o, reads=[o], mwrites=[xcT_d])
            K.stq(O["conv_sT"].a[l, :, j, 0:2, :], cin.a[:, j, 1:3, :], cin, reads=[cin], mwrites=[O["conv_sT"]], is_output=True)
            K.stq(O["conv_sT"].a[l, :, j, 2, :], x.a[:, 0:NS], x, reads=[x], mwrites=[O["conv_sT"]], is_output=True)
        A, Bt, Ct, Dt, Et, Fa, Gm = [K.sb("srow%d" % i, [8, NT]) for i in range(7)]
        onesr = K.sb("onesr", [8, 1]); K.memset("dve", onesr.a[:], 1.0, [onesr])
        TMB = K.sb("TMB", [64, NCI, 32]); DECb = K.sb("DECb", [128, 8 * NCI])
        sel8 = K.sb("sel8", [8, 8, 128]); K.ld(sel8.a[:], I["sel8"].a, sel8, writes=[sel8])
        small = Pool(K, "ssmall", [8, NCI], F32, 4)
        dtb = K.sb("dtb", [8, 1]); alog = K.sb("alog", [8, 1])
        K.ld(dtb.a[:], I["dt_bias"].a[l], dtb, writes=[dtb]); K.ld(alog.a[:], I["a_log"].a[l], alog, writes=[alog])
        K.act(alog.a[:], alog.a[:], AF.Exp, [alog], [alog])
        K.ts("dve", alog.a[:], alog.a[:], -1.0, ALU.mult, [alog], [alog])
        K.ld(A.a[:, :], Dm["gdt_d"].a, A, reads=[Dm["gdt_d"]], writes=[A])
        K.ts("dve", A.a[:, :], A.a[:, :], dtb.a[:, 0:1], ALU.add, [A, dtb], [A])
        K.act(Ct.a[:, :], A.a[:, :], AF.Abs, [A], [Ct])
        K.act(Ct.a[:, :], Ct.a[:, :], AF.Exp, [Ct], [Ct], scale=-1.0)
        K.act(Ct.a[:, :], Ct.a[:, :], AF.Ln, [Ct], [Ct], bias=1.0)
        K.ts("dve", Dt.a[:, :], A.a[:, :], 0.0, ALU.max, [A], [Dt])
        K.tt("dve", A.a[:, :], Dt.a[:, :], Ct.a[:, :], ALU.add, [Dt, Ct], [A])
        K.ts("dve", Bt.a[:, :], A.a[:, :], alog.a[:, 0:1], ALU.mult, [A, alog], [Bt])
        K.scan(Et.a[:, :L], onesr.a[:, 0:1].to_broadcast([8, L]), Bt.a[:, :L], 0.0, ALU.mult, ALU.add, [onesr, Bt], [Et])
        K.cp("dve", Et.a[:, L:NT], Bt.a[:, L:NT], [Bt], [Et])
        Gprev = small.next(); Glast = small.next(); decr = small.next()
        prevlast(Et, 8, None, Gprev, Glast)
        K.tt("dve", decr.a[:, :], Glast.a[:, :], Gprev.a[:, :], ALU.subtract, [Glast, Gprev], [decr])
        K.act(decr.a[:, :], decr.a[:, :], AF.Exp, [decr], [decr])
        bcast_rows(decr, 8, sel8, DECb, NCI)
        sub_chunk(Fa, Et, Gprev, 8, +1)
        K.act(Fa.a[:, :], Fa.a[:, :], AF.Exp, [Fa], [Fa])
        sub_chunk(Gm, Et, Glast, 8, -1)
        K.act(Gm.a[:, :], Gm.a[:, :], AF.Exp, [Gm], [Gm])
        for src, c0 in ((Et, 0), (Fa, 8), (Gm, 16), (A, 24)):
            to_tm(src, 8, TMB, c0)
        dd = K.sb("ssdd", [64, 8]); gs = K.sb("gssd", [64, 512])
        K.ld(dd.a[:], I["ssdd_rep"].a[l], dd, writes=[dd]); K.ld(gs.a[:], I["gssd_rep"].a[l], gs, writes=[gs])
        cp = {"xc": Pool(K, "sxc", [128, 8, 64], F32, 2), "zs": Pool(K, "szs", [64, 512], F32, 2),
              "xtok": Pool(K, "sxt", [64, 512], F32, 2), "btok": Pool(K, "sbt", [64, 256], F32, 2),
              "sc": Pool(K, "ssc", [64, 2, 64], F32, 2), "seg": Pool(K, "sseg", [64, 64], F32, 8),
              "xdt": Pool(K, "sxdt", [64, 64], F32, 8), "xw": Pool(K, "sxw", [64, 64], F32, 8),
              "y1": Pool(K, "sy1", [64, 64], F32, 8), "yss": Pool(K, "syss", [64, 512], F32, 2),
              "s": Pool(K, "ss", [64, 4], F32, 2), "junk": Pool(K, "sjunk", [64, 512], F32, 1)}
        ST = [K.sb("ST%d" % h, [128, 64]) for h in range(8)]

        def chunk(t0, cl, ci):
            nk = dict(allow_slow_non_contiguous=True) if cl == 1 else {}
            xc = cp["xc"].next(); zs = cp["zs"].next()
            K.ld(xc.a[:, :, :cl], xcT_d.a.rearrange("(j p) t -> p j t", p=128)[:, :, t0:t0 + cl], xc, reads=[xcT_d], writes=[xc], **nk)
            K.ld(zs.a[:cl, :], Dm["zs_d"].a[t0:t0 + cl, :], zs, reads=[Dm["zs_d"]], writes=[zs])
            xtok = cp["xtok"].next(); btok = cp["btok"].next()
            for j in range(6):
                ps = PS.next()
                K.tr(ps.a[:cl, :128], xc.a[:, j, :cl], ident.a[:, :], [xc, ident], [ps])
                if j < 4:
                    K.cp("act" if j % 2 else "dve", xtok.a[:cl, j * 128:(j + 1) * 128], ps.a[:cl, :128], [ps], [xtok])
                else:
                    K.cp("act" if j % 2 else "dve", btok.a[:cl, (j - 4) * 128:(j - 3) * 128], ps.a[:cl, :128], [ps], [btok])
            sc = cp["sc"].next()
            for g in range(2):
                ps = PS.next()
                K.pe(ps.a[:cl, :cl], xc.a[:, 4 + g, :cl], xc.a[:, 6 + g, :cl], True, True, [xc], [ps])
                K.cp("act", sc.a[:cl, g, :cl], ps.a[:cl, :cl], [ps], [sc])
            yss = cp["yss"].next()
            for g in range(2):
                HH = range(4 * g, 4 * g + 4)
                ps_d = {}; seg = {}; xdt = {}; ps1 = {}; ps2 = {}; y1 = {}; xw = {}; ps3 = {}
                for h in HH:
                    ps_d[h] = PS.next()
                    K.pe(ps_d[h].a[:cl, :cl], TMB.a[:cl, ci, h:h + 1].to_broadcast([cl, cl]), ident.a[:cl, :cl], True, False, [TMB, ident], [ps_d[h]])
                    K.pe(ps_d[h].a[:cl, :cl], ident.a[:cl, :cl], maskT.a[:cl, :cl], False, False, [ident, maskT], [ps_d[h]])
                    K.pe(ps_d[h].a[:cl, :cl], nident.a[:cl, :cl], TMB.a[:cl, ci, h:h + 1].to_broadcast([cl, cl]), False, True, [nident, TMB], [ps_d[h]])
                for h in HH:
                    seg[h] = cp["seg"].next()
                    K.act(seg[h].a[:cl, :cl], ps_d[h].a[:cl, :cl], AF.Exp, [ps_d[h]], [seg[h]])
                for h in HH:
                    xdt[h] = cp["xdt"].next()
                    K.act(xdt[h].a[:cl, :], xtok.a[:cl, h * 64:(h + 1) * 64], AF.Copy, [xtok, TMB], [xdt[h]], scale=TMB.a[:cl, ci, 24 + h:25 + h])
                for h in HH:
                    K.tt("dve", seg[h].a[:cl, :cl], seg[h].a[:cl, :cl], sc.a[:cl, g, :cl], ALU.mult, [seg[h], sc], [seg[h]])
                for h in HH:
                    xw[h] = cp["xw"].next()
                    K.act(xw[h].a[:cl, :], xdt[h].a[:cl, :], AF.Copy, [xdt[h], TMB], [xw[h]], scale=TMB.a[:cl, ci, 16 + h:17 + h])
                for h in HH:
                    ps1[h] = PS.next()
                    K.pe(ps1[h].a[:cl, :64], seg[h].a[:cl, :cl], xdt[h].a[:cl, :], True, True, [seg[h], xdt[h]], [ps1[h]])
                for h in HH:
                    ps2[h] = PS.next()
                    K.pe(ps2[h].a[:cl, :64], xc.a[:, 6 + g, :cl], ST[h].a[:, :], True, True, [xc, ST[h]], [ps2[h]])
                for h in HH:
                    y1[h] = cp["y1"].next()
                    K.cp("act", y1[h].a[:cl, :], ps1[h].a[:cl, :64], [ps1[h]], [y1[h]])
                for h in HH:
                    K.stt(y1[h].a[:cl, :], ps2[h].a[:cl, :64], TMB.a[:cl, ci, 8 + h:9 + h], y1[h].a[:cl, :], ALU.mult, ALU.add, [ps2[h], TMB, y1[h]], [y1[h]])
                for h in HH:
                    ps3[h] = PS.next()
                    K.pe(ps3[h].a[:, :64], btok.a[:cl, g * 128:(g + 1) * 128], xw[h].a[:cl, :], True, True, [btok, xw[h]], [ps3[h]])
                for h in HH:
                    K.stt(ST[h].a[:, :], ST[h].a[:, :], DECb.a[:, h * NCI + ci:h * NCI + ci + 1], ps3[h].a[:, :64], ALU.mult, ALU.add, [ST[h], DECb, ps3[h]], [ST[h]])
                for h in HH:
                    K.stt(yss.a[:cl, h * 64:(h + 1) * 64], xtok.a[:cl, h * 64:(h + 1) * 64], dd.a[:cl, h:h + 1], y1[h].a[:cl, :], ALU.mult, ALU.add, [xtok, dd, y1[h]], [yss])
            K.tt("dve", yss.a[:cl, :], yss.a[:cl, :], zs.a[:cl, :], ALU.mult, [yss, zs], [yss])
            s_ = cp["s"].next(); junk = cp["junk"].next()
            K.act(junk.a[:cl, :], yss.a[:cl, :], AF.Square, [yss], [junk, s_], accum=s_.a[:cl, 0:1])
            K.act(s_.a[:cl, 1:2], s_.a[:cl, 0:1], AF.Sqrt, [s_, epsb], [s_], scale=1.0 / 512.0, bias=epsb.a[:cl, :])
            K.recip(s_.a[:cl, 1:2], s_.a[:cl, 1:2], [s_], [s_])
            K.stt(yss.a[:cl, :], yss.a[:cl, :], s_.a[:cl, 1:2], gs.a[:cl, :], ALU.mult, ALU.mult, [yss, s_, gs], [yss])
            for j in range(4):
                ps = PS.next()
                K.tr(ps.a[:, :cl], yss.a[:cl, j * 128:(j + 1) * 128], ident.a[:cl, :cl], [yss, ident], [ps])
                K.cp("act" if j % 2 else "dve", XTb[:, 12 + j, t0:t0 + cl], ps.a[:, :cl], [ps], [tXT])

        for h in range(8):
            K.memset("dve", ST[h].a[:], 0.0, [ST[h]])
        for (t0, cl, ci) in CHUNKS[:32]:
            chunk(t0, cl, ci)
        for h in range(8):
            K.stq(O["ssd_pT"].a[l, h], ST[h].a[:, :], ST[h], reads=[ST[h]], mwrites=[O["ssd_pT"]], is_output=True)
        for (t0, cl, ci) in CHUNKS[32:]:
            j = ci - 32
            for h in range(8):
                K.ld(ST[h].a[:, :], I["ssdT"].a[l, j, h], ST[h], writes=[ST[h]])
            chunk(t0, cl, ci)
            for h in range(8):
                K.stq(O["ssd_sT"].a[l, j, h], ST[h].a[:, :], ST[h], reads=[ST[h]], mwrites=[O["ssd_sT"]], is_output=True)


def s5_mixer(K, S, PS, I, O, l, Dm, XTb, tXT, ident, ones_f, epsb, halfpi):
    uT_d = Dm["uT_d"]
    with K.phase():
        def P16(name):
            return K.sb(name, [128, 16])
        lre, lim, dtt, th, r, c, s_, t1, t2, t3, lbr, lbi, nlbi, kr, ki, den, ka, cn, sn = [P16("s5p%d" % i) for i in range(19)]
        K.ld(lre.a[:], I["lam_re"].a[l], lre, writes=[lre]); K.ld(lim.a[:], I["lam_im"].a[l], lim, writes=[lim])
        K.ld(dtt.a[:], I["logdt"].a[l], dtt, writes=[dtt])
        K.act(dtt.a[:], dtt.a[:], AF.Exp, [dtt], [dtt])
        K.tt("dve", th.a[:], lim.a[:], dtt.a[:], ALU.mult, [lim, dtt], [th])
        K.tt("dve", r.a[:], lre.a[:], dtt.a[:], ALU.mult, [lre, dtt], [r])
        K.act(r.a[:], r.a[:], AF.Exp, [r], [r])
        K.act(s_.a[:], th.a[:], AF.Sin, [th], [s_], scale=1.0 / 32.0)
        K.act(c.a[:], th.a[:], AF.Sin, [th, halfpi], [c], scale=1.0 / 32.0, bias=halfpi.a[:, :])

        def cdouble(cc, ss):
            K.tt("dve", t1.a[:], ss.a[:], cc.a[:], ALU.mult, [ss, cc], [t1])
            K.tt("dve", t2.a[:], cc.a[:], cc.a[:], ALU.mult, [cc], [t2])
            K.tt("dve", t3.a[:], ss.a[:], ss.a[:], ALU.mult, [ss], [t3])
            K.ts("dve", ss.a[:], t1.a[:], 2.0, ALU.mult, [t1], [ss])
            K.tt("dve", cc.a[:], t2.a[:], t3.a[:], ALU.subtract, [t2, t3], [cc])
        for _ in range(5):
            cdouble(c, s_)
        K.tt("dve", lbr.a[:], r.a[:], c.a[:], ALU.mult, [r, c], [lbr])
        K.tt("dve", lbi.a[:], r.a[:], s_.a[:], ALU.mult, [r, s_], [lbi])
        K.ts("dve", nlbi.a[:], lbi.a[:], -1.0, ALU.mult, [lbi], [nlbi])
        K.ts("dve", ka.a[:], lbr.a[:], -1.0, ALU.add, [lbr], [ka])
        K.tt("dve", t1.a[:], lre.a[:], lre.a[:], ALU.mult, [lre], [t1])
        K.tt("dve", t2.a[:], lim.a[:], lim.a[:], ALU.mult, [lim], [t2])
        K.tt("dve", den.a[:], t1.a[:], t2.a[:], ALU.add, [t1, t2], [den])
        K.recip(den.a[:], den.a[:], [den], [den])
        K.tt("dve", t1.a[:], ka.a[:], lre.a[:], ALU.mult, [ka, lre], [t1])
        K.tt("dve", t2.a[:], lbi.a[:], lim.a[:], ALU.mult, [lbi, lim], [t2])
        K.tt("dve", kr.a[:], t1.a[:], t2.a[:], ALU.add, [t1, t2], [kr])
        K.tt("dve", kr.a[:], kr.a[:], den.a[:], ALU.mult, [kr, den], [kr])
        K.tt("dve", t1.a[:], lbi.a[:], lre.a[:], ALU.mult, [lbi, lre], [t1])
        K.tt("dve", t2.a[:], ka.a[:], lim.a[:], ALU.mult, [ka, lim], [t2])
        K.tt("dve", ki.a[:], t1.a[:], t2.a[:], ALU.subtract, [t1, t2], [ki])
        K.tt("dve", ki.a[:], ki.a[:], den.a[:], ALU.mult, [ki, den], [ki])
        tabC = K.sb("tabC", [128, 16, 128]); tabS = K.sb("tabS", [128, 16, 128])
        tmpA = K.sb("tmpA", [128, 16, 64]); tmpB = K.sb("tmpB", [128, 16, 64])
        K.cp("dve", tabC.a[:, :, 0], c.a[:], [c], [tabC]); K.cp("dve", tabS.a[:, :, 0], s_.a[:], [s_], [tabS])
        K.cp("dve", cn.a[:], c.a[:], [c], [cn]); K.cp("dve", sn.a[:], s_.a[:], [s_], [sn])
        nn = 1
        while nn < 128:
            cb_ = cn.a[:, :].unsqueeze(2).to_broadcast([128, 16, nn]); sb_ = sn.a[:, :].unsqueeze(2).to_broadcast([128, 16, nn])
            K.tt("dve", tmpA.a[:, :, :nn], tabC.a[:, :, 0:nn], cb_, ALU.mult, [tabC, cn], [tmpA])
            K.tt("dve", tmpB.a[:, :, :nn], tabS.a[:, :, 0:nn], sb_, ALU.mult, [tabS, sn], [tmpB])
            K.tt("dve", tmpA.a[:, :, :nn], tmpA.a[:, :, :nn], tmpB.a[:, :, :nn], ALU.subtract, [tmpA, tmpB], [tmpA])
            K.tt("dve", tmpB.a[:, :, :nn], tabS.a[:, :, 0:nn], cb_, ALU.mult, [tabS, cn], [tmpB])
            K.cp("dve", tabC.a[:, :, nn:2 * nn], tmpA.a[:, :, :nn], [tmpA], [tabC])
            K.tt("dve", tmpA.a[:, :, :nn], tabC.a[:, :, 0:nn], sb_, ALU.mult, [tabC, sn], [tmpA])
            K.tt("dve", tabS.a[:, :, nn:2 * nn], tmpB.a[:, :, :nn], tmpA.a[:, :, :nn], ALU.add, [tmpA, tmpB], [tabS])
            cdouble(cn, sn)
            nn *= 2
        Bm = {}
        for nm in ("Bre", "Bim", "Cre", "Cim"):
            Bm[nm] = K.sb(nm + "_sb", [128, 16, 128])
            K.ld(Bm[nm].a[:], I[nm].a[l].rearrange("s k m -> k s m"), Bm[nm], writes=[Bm[nm]])
        s5d = K.sb("s5d", [128, 4]); bglu = K.sb("bglu", [128, 4]); gs5 = K.sb("gs5", [128, 4])
        K.ld(s5d.a[:], I["s5d"].a[l], s5d, writes=[s5d]); K.ld(bglu.a[:], I["b_glu"].a[l], bglu, writes=[bglu]); K.ld(gs5.a[:], I["g_s5"].a[l], gs5, writes=[gs5])
        Xr = K.sb("Xr", [128, 16]); Xi = K.sb("Xi", [128, 16])
        K.memset("dve", Xr.a[:], 0.0, [Xr]); K.memset("dve", Xi.a[:], 0.0, [Xi])
        y5g_d = K.dscr("y5g_d%d" % l, [512, NT])
        W8 = lambda nm: K.sb(nm, [128, 8, 128])
        BUr, BUi, Wr, Wi, Zr, Zi, T1, T2, Xr_, Xi_, nXi_ = [W8("s5w%d" % i) for i in range(11)]
        ubp = Pool(K, "ub", [128, 4, 128], F32, 2)
        gp = Pool(K, "s5g", [128, 128], F32, 3)

        def bu_calc(ub, sc, n, our, oui, tA, tB):
            j = sc // 4
            ps = PS.next()
            K.pe(ps.a[:, 0:n], Bm["Bre"].a[:, sc, :], ub.a[:, j, :n], True, True, [Bm["Bre"], ub], [ps])
            K.pe(ps.a[:, 128:128 + n], Bm["Bim"].a[:, sc, :], ub.a[:, j, :n], True, True, [Bm["Bim"], ub], [ps])
            return ps

        def y_out(ub, j, n, t0, xr_of, nxi_of, rd):
            ps_y = PS.next()
            for q in range(4):
                sc = 4 * j + q
                K.pe(ps_y.a[:, :n], Bm["Cre"].a[:, sc, :], xr_of(sc), q == 0, False, [Bm["Cre"]] + rd, [ps_y])
                K.pe(ps_y.a[:, :n], Bm["Cim"].a[:, sc, :], nxi_of(sc), False, q == 3, [Bm["Cim"]] + rd, [ps_y])
            yv = gp.next(); tg = gp.next()
            K.stt(yv.a[:, :n], ub.a[:, j, :n], s5d.a[:, j:j + 1], ps_y.a[:, :n], ALU.mult, ALU.add, [ub, s5d, ps_y], [yv])
            K.tt("dve", tg.a[:, :n], yv.a[:, :n], yv.a[:, :n], ALU.mult, [yv], [tg])
            K.ts("dve", tg.a[:, :n], tg.a[:, :n], 0.044715, ALU.mult, [tg], [tg], s2=1.0, op1=ALU.add)
            K.tt("dve", tg.a[:, :n], tg.a[:, :n], yv.a[:, :n], ALU.mult, [tg, yv], [tg])
            K.act(tg.a[:, :n], tg.a[:, :n], AF.Tanh, [tg], [tg], scale=0.7978845608028654)
            K.ts("dve", tg.a[:, :n], tg.a[:, :n], 1.0, ALU.add, [tg], [tg], s2=0.5, op1=ALU.mult)
            K.tt("dve", tg.a[:, :n], tg.a[:, :n], yv.a[:, :n], ALU.mult, [tg, yv], [tg])
            K.stq(y5g_d.a[j * 128:(j + 1) * 128, t0:t0 + n], tg.a[:, :n], tg, reads=[tg], mwrites=[y5g_d])

        for tc in range(16):
            t0 = tc * 128
            n = 128
            ub = ubp.next()
            K.ld(ub.a[:, :, :n], uT_d.a.rearrange("(j p) t -> p j t", p=128)[:, :, t0:t0 + n], ub, reads=[uT_d], writes=[ub])
            for h2 in range(2):
                for i in range(8):
                    sc = 8 * h2 + i
                    ps = bu_calc(ub, sc, n, None, None, None, None)
                    K.ts("dve", T1.a[:, i, :n], ps.a[:, 128:128 + n], ki.a[:, sc:sc + 1], ALU.mult, [ps, ki], [T1])
                    K.stt(BUr.a[:, i, :n], ps.a[:, 0:n], kr.a[:, sc:sc + 1], T1.a[:, i, :n], ALU.mult, ALU.subtract, [ps, kr, T1], [BUr])
                    K.ts("dve", T2.a[:, i, :n], ps.a[:, 0:n], ki.a[:, sc:sc + 1], ALU.mult, [ps, ki], [T2])
                    K.stt(BUi.a[:, i, :n], ps.a[:, 128:128 + n], kr.a[:, sc:sc + 1], T2.a[:, i, :n], ALU.mult, ALU.add, [ps, kr, T2], [BUi])
                Cs = tabC.a[:, 8 * h2:8 * h2 + 8, :]; Ss = tabS.a[:, 8 * h2:8 * h2 + 8, :]
                K.tt("dve", T1.a[:], BUi.a[:], Ss, ALU.mult, [BUi, tabS], [T1])
                K.tt("pool", Wr.a[:], BUr.a[:], Cs, ALU.mult, [BUr, tabC], [Wr])
                K.tt("dve", Wr.a[:], Wr.a[:], T1.a[:], ALU.add, [Wr, T1], [Wr])
                K.tt("pool", T2.a[:], BUr.a[:], Ss, ALU.mult, [BUr, tabS], [T2])
                K.tt("dve", Wi.a[:], BUi.a[:], Cs, ALU.mult, [BUi, tabC], [Wi])
                K.tt("dve", Wi.a[:], Wi.a[:], T2.a[:], ALU.subtract, [Wi, T2], [Wi])
                for i in range(8):
                    sc = 8 * h2 + i
                    rb = r.a[:, sc:sc + 1].to_broadcast([128, n])
                    K.scan(Zr.a[:, i, :], rb, Wr.a[:, i, :], Xr.a[:, sc:sc + 1], ALU.mult, ALU.add, [r, Wr, Xr], [Zr])
                    K.scan(Zi.a[:, i, :], rb, Wi.a[:, i, :], Xi.a[:, sc:sc + 1], ALU.mult, ALU.add, [r, Wi, Xi], [Zi])
                K.tt("dve", T1.a[:], Zi.a[:], Ss, ALU.mult, [Zi, tabS], [T1])
                K.tt("pool", Xr_.a[:], Zr.a[:], Cs, ALU.mult, [Zr, tabC], [Xr_])
                K.tt("dve", Xr_.a[:], Xr_.a[:], T1.a[:], ALU.subtract, [Xr_, T1], [Xr_])
                K.tt("pool", T2.a[:], Zr.a[:], Ss, ALU.mult, [Zr, tabS], [T2])
                K.tt("dve", T1.a[:], Zi.a[:], Cs, ALU.mult, [Zi, tabC], [T1])
                K.tt("dve", Xi_.a[:], T2.a[:], T1.a[:], ALU.add, [T2, T1], [Xi_])
                K.ts("dve", nXi_.a[:], Xi_.a[:], -1.0, ALU.mult, [Xi_], [nXi_])
                K.cp("dve", Xr.a[:, 8 * h2:8 * h2 + 8], Xr_.a[:, :, n - 1], [Xr_], [Xr])
                K.cp("dve", Xi.a[:, 8 * h2:8 * h2 + 8], Xi_.a[:, :, n - 1], [Xi_], [Xi])
                for jj in range(2):
                    j = 2 * h2 + jj
                    y_out(ub, j, n, t0, lambda sc: Xr_.a[:, sc - 8 * h2, :n], lambda sc: nXi_.a[:, sc - 8 * h2, :n], [Xr_, nXi_])
        K.stq(O["s5re_pT"].a[l], Xr.a[:], Xr, reads=[Xr], mwrites=[O["s5re_pT"]], is_output=True)
        K.stq(O["s5im_pT"].a[l], Xi.a[:], Xi, reads=[Xi], mwrites=[O["s5im_pT"]], is_output=True)
        x0r = K.sb("x0r", [128, 16, NS]); x0i = K.sb("x0i", [128, 16, NS])
        K.ld(x0r.a[:], I["s5reT"].a[l], x0r, writes=[x0r]); K.ld(x0i.a[:], I["s5imT"].a[l], x0i, writes=[x0i])
        SXr = K.sb("SXr", [128, 16, NS]); SXi = K.sb("SXi", [128, 16, NS]); SnXi = K.sb("SnXi", [128, 16, NS])
        SBr = K.sb("SBr", [128, 16, NS]); SBi = K.sb("SBi", [128, 16, NS])
        ub = ubp.next()
        K.ld(ub.a[:, :, :NS], uT_d.a.rearrange("(j p) t -> p j t", p=128)[:, :, L:NT], ub, reads=[uT_d], writes=[ub])
        for sc in range(16):
            ps = bu_calc(ub, sc, NS, None, None, None, None)
            K.ts("dve", SBr.a[:, sc, :], ps.a[:, 128:128 + NS], ki.a[:, sc:sc + 1], ALU.mult, [ps, ki], [SBr])
            K.stt(SBr.a[:, sc, :], ps.a[:, 0:NS], kr.a[:, sc:sc + 1], SBr.a[:, sc, :], ALU.mult, ALU.subtract, [ps, kr, SBr], [SBr])
            K.ts("dve", SBi.a[:, sc, :], ps.a[:, 0:NS], ki.a[:, sc:sc + 1], ALU.mult, [ps, ki], [SBi])
            K.stt(SBi.a[:, sc, :], ps.a[:, 128:128 + NS], kr.a[:, sc:sc + 1], SBi.a[:, sc, :], ALU.mult, ALU.add, [ps, kr, SBi], [SBi])
            K.stt(SXr.a[:, sc, :], x0r.a[:, sc, :], lbr.a[:, sc:sc + 1], SBr.a[:, sc, :], ALU.mult, ALU.add, [x0r, lbr, SBr], [SXr])
            K.stt(SXr.a[:, sc, :], x0i.a[:, sc, :], nlbi.a[:, sc:sc + 1], SXr.a[:, sc, :], ALU.mult, ALU.add, [x0i, nlbi, SXr], [SXr])
            K.stt(SXi.a[:, sc, :], x0r.a[:, sc, :], lbi.a[:, sc:sc + 1], SBi.a[:, sc, :], ALU.mult, ALU.add, [x0r, lbi, SBi], [SXi])
            K.stt(SXi.a[:, sc, :], x0i.a[:, sc, :], lbr.a[:, sc:sc + 1], SXi.a[:, sc, :], ALU.mult, ALU.add, [x0i, lbr, SXi], [SXi])
        K.ts("dve", SnXi.a[:], SXi.a[:], -1.0, ALU.mult, [SXi], [SnXi])
        K.stq(O["s5re_sT"].a[l], SXr.a[:], SXr, reads=[SXr], mwrites=[O["s5re_sT"]], is_output=True)
        K.stq(O["s5im_sT"].a[l], SXi.a[:], SXi, reads=[SXi], mwrites=[O["s5im_sT"]], is_output=True)
        for j in range(4):
            y_out(ub, j, NS, L, lambda sc: SXr.a[:, sc, :], lambda sc: SnXi.a[:, sc, :], [SXr, SnXi])
    with K.phase():
        s5d = K.sb("s5d", [128, 4]); bglu = K.sb("bglu", [128, 4]); gs5 = K.sb("gs5", [128, 4])
        K.ld(bglu.a[:], I["b_glu"].a[l], bglu, writes=[bglu]); K.ld(gs5.a[:], I["g_s5"].a[l], gs5, writes=[gs5])
        ybp = Pool(K, "y5b", [128, 4, 512], F32, 2)
        wglu = K.sb("wglu", [128, 4, 512])
        K.ld(wglu.a[:], I["w_glu"].a[l].rearrange("(k p) c -> p k c", p=128), wglu, writes=[wglu])
        yg = Pool(K, "ygl", [128, 4, 512], F32, 2); sqp = Pool(K, "s5sq", [128, 4, 512], F32, 1); rsp = Pool(K, "s5rs", [128, 512], F32, 2)
        for (t0, n) in TB:
            ygl = yg.next()
            yb = ybp.next()
            K.ld(yb.a[:, :, :n], y5g_d.a.rearrange("(j p) t -> p j t", p=128)[:, :, t0:t0 + n], yb, reads=[y5g_d], writes=[yb])
            for m in range(4):
                ps = PS.next()
                for j in range(4):
                    K.pe(ps.a[:, :n], wglu.a[:, j, m * 128:(m + 1) * 128], yb.a[:, j, :n], j == 0, j == 3, [wglu, yb], [ps])
                K.act(ygl.a[:, m, :n], ps.a[:, :n], AF.Sigmoid, [ps, bglu], [ygl], bias=bglu.a[:, m:m + 1])
                K.tt("dve", ygl.a[:, m, :n], ygl.a[:, m, :n], yb.a[:, m, :n], ALU.mult, [ygl, yb], [ygl])
            sq = sqp.next()
            K.act(sq.a[:, :, :n], ygl.a[:, :, :n], AF.Square, [ygl], [sq])
            ps = PS.next()
            for m in range(4):
                K.pe(ps.a[:, :n], ones_f.a[:], sq.a[:, m, :n], m == 0, m == 3, [ones_f, sq], [ps])
            rs = rsp.next()
            K.act(rs.a[:, :n], ps.a[:, :n], AF.Sqrt, [ps, epsb], [rs], scale=1.0 / 512.0, bias=epsb.a[:, :])
            K.recip(rs.a[:, :n], rs.a[:, :n], [rs], [rs])
            for m in range(4):
                K.stt(XTb[:, 8 + m, t0:t0 + n], ygl.a[:, m, :n], gs5.a[:, m:m + 1], rs.a[:, :n], ALU.mult, ALU.mult, [ygl, gs5, rs], [tXT])


def ffn_phase(K, S, PS, I, l, hT, XTf, tXT, gn, ones_bf, epsb, ident, gst, norm_stage, stages, own):
    moe = (l % 2 == 1)
    if moe:
        experts = [(I["moe_wg"].a[e], I["moe_wu"].a[e], I["moe_wd"].a[e]) for e in range(NE)]
        dff = D_FFE
    else:
        experts = [(I["ffn_wg"].a, I["ffn_wu"].a, I["ffn_wd"].a)]
        dff = D_FF
    B3 = [(0, 347), (347, 347), (694, 346)]
    SBS = [(0, [(0, 512), (512, 512)]), (1024, B3)]
    if own:
        SBS = [(0, B3)]
    for (c0, blocks) in SBS:
        nsb = sum(n for _, n in blocks)
        with K.phase():
            P = {"hblk": Pool(K, "fhblk", [128, KC, 128], F32, 1), "sq": Pool(K, "fsq", [128, KC, 128], BF16, 1),
                 "rs": Pool(K, "frs", [128, 512], F32, 2)}
            cT = K.sb("cT", [128, KC, 1040], BF16)
            for q0 in range(0, nsb, 128):
                n = min(128, nsb - q0)
                norm_stage(hT, gn["g_ffn"].a[:, l, :], gn["g_ffn"], c0 + q0, n, cT.a[:, :, q0:q0 + n], cT, P)
            wgp = Pool(K, "fwg", [128, KC, 256], BF16, 2); wup = Pool(K, "fwu", [128, KC, 256], BF16, 2)
            wdp = Pool(K, "fwd", [128, 2, D], BF16, 3); h1p = Pool(K, "fh1", [128, 2, 1040], BF16, 2)
            sgp = Pool(K, "fsg", [128, 512], F32, 3); hbp = Pool(K, "fhb", [128, 512], F32, 2)
            ntile = (nsb + 127) // 128
            if moe:
                wr = K.sb("wr", [128, KC, NE], BF16)
                K.ld(wr.a[:], I["w_router"].a.rearrange("(k p) c -> p k c", p=128), wr, writes=[wr], q="pool")
                brep = K.sb("brep", [128, NE]); K.ld(brep.a[:], I["b_router_rep"].a, brep, writes=[brep])
                comb = K.sb("comb", [128, 9, NE]); combB = Pool(K, "combB", [128, 1040], F32, 2)
                rt = Pool(K, "rt", [128, 4, NE], F32, 2)
                for i in range(ntile):
                    q0 = i * 128
                    n = min(128, nsb - q0)
                    ps = PS.next()
                    for k in range(KC):
                        K.pe(ps.a[:n, :NE], cT.a[:, k, q0:q0 + n], wr.a[:, k, :], k == 0, k == KC - 1, [cT, wr], [ps])
                    t = rt.next()
                    lg = t.a[:n, 0, :]; mx = t.a[:n, 1, :]; ex = t.a[:n, 2, :]; sc = t.a[:n, 3, :]
                    K.tt("dve", lg, ps.a[:n, :NE], brep.a[:n, :], ALU.add, [ps, brep], [t])
                    K.S.op("dve", lambda e, mx=mx, lg=lg: e.max(out=mx, in_=lg), reads=[t.k], writes=[t.k])
                    K.ts("dve", sc[:, 0:1], mx[:, 0:1], -1.0, ALU.mult, [t], [t])
                    K.act(ex, lg, AF.Exp, [t], [t], bias=sc[:, 0:1])
                    K.act(sc[:, 1:2], mx[:, 1:2], AF.Exp, [t], [t], bias=sc[:, 0:1])
                    K.ts("dve", sc[:, 1:2], sc[:, 1:2], 1.0, ALU.add, [t], [t])
                    K.recip(sc[:, 1:2], sc[:, 1:2], [t], [t])
                    K.ts("dve", lg, lg, mx[:, 1:2], ALU.is_ge, [t], [t])
                    K.stt(comb.a[:n, i, :], ex, sc[:, 1:2], lg, ALU.mult, ALU.mult, [t], [comb])
            panels = [(ei, f0) for ei in range(len(experts)) for f0 in range(0, dff, 256)]
            npan = len(panels)
            cBs = {}
            loaded = {}

            def loads(p):
                ei, f0 = panels[p]
                Wg, Wu, Wd = experts[ei]
                wg = wgp.next(); wu = wup.next(); wd = wdp.next()
                pi = f0 // 256
                for hh in range(2):
                    K.ld(wg.a[:].rearrange("p k c -> p (k c)")[:, hh * 2048:(hh + 1) * 2048], Wg[pi][:, hh * 2048:(hh + 1) * 2048], wg, writes=[wg], q="pool")
                for hh in range(2):
                    K.ld(wu.a[:].rearrange("p k c -> p (k c)")[:, hh * 2048:(hh + 1) * 2048], Wu[pi][:, hh * 2048:(hh + 1) * 2048], wu, writes=[wu], q="pool")
                for hh in range(2):
                    K.ld(wd.a[:].rearrange("p k c -> p (k c)")[:, hh * 2048:(hh + 1) * 2048], Wd[pi][:, hh * 2048:(hh + 1) * 2048], wd, writes=[wd], q="pool")
                loaded[p] = (wg, wu, wd)

            def gate_up(p):
                ei, f0 = panels[p]
                wg, wu, wd = loaded[p]
                if moe and ei not in cBs:
                    cB = combB.next()
                    for i in range(ntile):
                        q0 = i * 128
                        n = min(128, nsb - q0)
                        ps = PS.next()
                        K.pe(ps.a[:, :n], comb.a[:n, i, ei:ei + 1].to_broadcast([n, 128]), ident.a[:n, :n], True, True, [comb, ident], [ps])
                        K.cp("act", cB.a[:, q0:q0 + n], ps.a[:, :n], [ps], [cB])
                    cBs.clear()
                    cBs[ei] = cB
                h1 = h1p.next()
                for m in range(2):
                    for (b0, n) in blocks:
                        pg = PS.next(); pu = PS.next()
                        for k in range(KC):
                            K.pe(pg.a[:, :n], wg.a[:, k, m * 128:(m + 1) * 128], cT.a[:, k, b0:b0 + n], k == 0, k == KC - 1, [wg, cT], [pg])
                        for k in range(KC):
                            K.pe(pu.a[:, :n], wu.a[:, k, m * 128:(m + 1) * 128], cT.a[:, k, b0:b0 + n], k == 0, k == KC - 1, [wu, cT], [pu])
                        sg = sgp.next()
                        K.act(sg.a[:, :n], pg.a[:, :n], AF.Silu, [pg], [sg])
                        if moe:
                            K.tt("pool", sg.a[:, :n], sg.a[:, :n], cBs[ei].a[:, b0:b0 + n], ALU.mult, [sg, cBs[ei]], [sg])
                        K.tt("dve", h1.a[:, m, b0:b0 + n], sg.a[:, :n], pu.a[:, :n], ALU.mult, [sg, pu], [h1])
                return h1

            def down(p, h1):
                wg, wu, wd = loaded.pop(p)
                for mo in range(KC):
                    for (b0, n) in blocks:
                        ps = PS.next()
                        for k in range(2):
                            K.pe(ps.a[:, :n], wd.a[:, k, mo * 128:(mo + 1) * 128], h1.a[:, k, b0:b0 + n], k == 0, k == 1, [wd, h1], [ps])
                        if p == 0:
                            K.cp("act", XTf[:, mo, b0:b0 + n], ps.a[:, :n], [ps], [tXT])
                        else:
                            K.tt("dve", XTf[:, mo, b0:b0 + n], ps.a[:, :n], XTf[:, mo, b0:b0 + n], ALU.add, [ps, tXT], [tXT])

            loads(0)
            prev_h1 = None
            for p in range(npan + 1):
                if p + 1 < npan:
                    loads(p + 1)
                cur_h1 = gate_up(p) if p < npan else None
                if p >= 1:
                    down(p - 1, prev_h1)
                prev_h1 = cur_h1
            for mo in range(KC):
                for (b0, n) in blocks:
                    hb = hbp.next()
                    K.ld(hb.a[:, :n], hT.a[mo * 128:(mo + 1) * 128, c0 + b0:c0 + b0 + n], hb, reads=[hT], writes=[hb])
                    K.tt("dve", hb.a[:, :n], hb.a[:, :n], XTf[:, mo, b0:b0 + n], ALU.add, [hb, tXT], [hb])
                    K.stq(hT.a[mo * 128:(mo + 1) * 128, c0 + b0:c0 + b0 + n], hb.a[:, :n], hb, reads=[hb], mwrites=[hT])


def _consts():
    ident = np.eye(128, dtype=np.float32)
    maskT = np.where(np.arange(64)[:, None] <= np.arange(64)[None, :], 0.0, NEG).astype(np.float32)
    sel4 = np.zeros((4, 4, 128), np.float32)
    for h in range(4):
        sel4[h, h, :] = 1.0
    sel8 = np.zeros((8, 8, 128), np.float32)
    for h in range(8):
        sel8[h, h, :] = 1.0
    return dict(ident=ident, nident=-ident, maskT=maskT, sel4=sel4, sel8=sel8, sel8e=sel8.copy())


def _fm(v):
    v = np.asarray(v, np.float32)
    return np.ascontiguousarray(v.reshape(v.shape[:-1] + (v.shape[-1] // 128, 128)).swapaxes(-1, -2))


def prep_shared(inp):
    f = lambda a: np.ascontiguousarray(np.asarray(a, np.float32))
    sh = {}
    for nm in ("g_mix", "g_ffn", "g_ple"):
        sh[nm] = _fm(inp[nm])
    sh["g_final"] = _fm(inp["g_final"])
    sh["w_in"] = f(inp["w_in"]); sh["w_out"] = f(inp["w_out"])
    sh["b_ig"] = f(inp["b_igate"]).reshape(DEPTH, 4, 1); sh["b_fg"] = f(inp["b_fgate"]).reshape(DEPTH, 4, 1)
    sh["gml_rep"] = np.ascontiguousarray(np.broadcast_to(f(inp["g_ml"]).reshape(DEPTH, 1, 1024), (DEPTH, 64, 1024)))
    sh["lam_re"] = _fm(f(inp["s5_lam_re"]).reshape(DEPTH, 2048)); sh["lam_im"] = _fm(f(inp["s5_lam_im"]).reshape(DEPTH, 2048))
    sh["logdt"] = _fm(np.repeat(f(inp["s5_log_dt"]), 64, axis=1))
    bre = f(inp["s5_b_re"]); bim = f(inp["s5_b_im"]); cre = f(inp["s5_c_re"]); cim = f(inp["s5_c_im"])
    Bre = np.zeros((DEPTH, 16, 128, 128), np.float32); Bim = np.zeros_like(Bre); Cre = np.zeros_like(Bre); Cim = np.zeros_like(Bre)
    for g in range(32):
        sc, g2, gl = g // 2, g % 2, g % 8
        Bre[:, sc, gl * 16:(gl + 1) * 16, g2 * 64:(g2 + 1) * 64] = bre[:, g].transpose(0, 2, 1)
        Bim[:, sc, gl * 16:(gl + 1) * 16, g2 * 64:(g2 + 1) * 64] = bim[:, g].transpose(0, 2, 1)
        Cre[:, sc, g2 * 64:(g2 + 1) * 64, gl * 16:(gl + 1) * 16] = cre[:, g].transpose(0, 2, 1)
        Cim[:, sc, g2 * 64:(g2 + 1) * 64, gl * 16:(gl + 1) * 16] = cim[:, g].transpose(0, 2, 1)
    sh["Bre"], sh["Bim"], sh["Cre"], sh["Cim"] = Bre, Bim, Cre, Cim
    fm4 = lambda v: np.ascontiguousarray(f(v).reshape(DEPTH, 4, 128).swapaxes(1, 2))
    sh["s5d"] = fm4(f(inp["s5_d"]).reshape(DEPTH, 512)); sh["b_glu"] = fm4(inp["s5_b_glu"]); sh["g_s5"] = fm4(inp["g_s5"])
    sh["w_glu"] = f(inp["s5_w_glu"])
    sh["conv_w"] = np.ascontiguousarray(f(inp["ssd_conv_w"]).reshape(DEPTH, 4, 8, 128).transpose(0, 3, 2, 1))
    sh["conv_b"] = np.ascontiguousarray(f(inp["ssd_conv_b"]).reshape(DEPTH, 8, 128).swapaxes(1, 2))
    sh["dt_bias"] = f(inp["ssd_dt_bias"]).reshape(DEPTH, 8, 1); sh["a_log"] = f(inp["ssd_a_log"]).reshape(DEPTH, 8, 1)
    sh["ssdd_rep"] = np.ascontiguousarray(np.broadcast_to(f(inp["ssd_d"]).reshape(DEPTH, 1, 8), (DEPTH, 64, 8)))
    sh["gssd_rep"] = np.ascontiguousarray(np.broadcast_to(f(inp["g_ssd"]).reshape(DEPTH, 1, 512), (DEPTH, 64, 512)))
    def pan_in(W):
        npan = W.shape[1] // 256
        return np.ascontiguousarray(W.reshape(16, 128, npan, 256).transpose(2, 1, 0, 3)).reshape(npan, 128, 4096)

    def pan_dn(W):
        npan = W.shape[0] // 256
        return np.ascontiguousarray(W.reshape(npan, 2, 128, 2048).transpose(0, 2, 1, 3)).reshape(npan, 128, 4096)
    sh["ffn_wg"] = pan_in(f(inp["ffn_w_gate"])[0]); sh["ffn_wu"] = pan_in(f(inp["ffn_w_up"])[0]); sh["ffn_wd"] = pan_dn(f(inp["ffn_w_down"])[0])
    sh["w_router"] = f(inp["w_router"])[0]
    sh["b_router_rep"] = np.ascontiguousarray(np.broadcast_to(f(inp["b_router"])[0].reshape(1, NE), (128, NE)))
    sh["moe_wg"] = np.stack([pan_in(np.asarray(inp["moe_w_gate"][0][e], np.float32)) for e in range(NE)])
    sh["moe_wu"] = np.stack([pan_in(np.asarray(inp["moe_w_up"][0][e], np.float32)) for e in range(NE)])
    sh["moe_wd"] = np.stack([pan_dn(np.asarray(inp["moe_w_down"][0][e], np.float32)) for e in range(NE)])
    sh["w_ple"] = f(inp["w_ple"]); sh["w_pleg"] = f(inp["w_ple_gate"])
    sh.update(_consts())
    return sh


def prep_core(inp, c):
    f = lambda a: np.ascontiguousarray(np.asarray(a, np.float32))
    b = c % 4
    ss = slice(c * NS, (c + 1) * NS)
    m = {}
    m["xT"] = np.ascontiguousarray(np.concatenate([f(inp["x_prompt"])[b], f(inp["x_sample"])[ss, 0]], axis=0).T)
    m["pT"] = np.ascontiguousarray(np.concatenate([f(inp["p_prompt"])[:, b], f(inp["p_sample"])[:, ss, 0]], axis=1).transpose(0, 2, 1))
    m["sC"] = f(inp["state_mlstm_C"])[:, ss]; m["sn"] = f(inp["state_mlstm_n"])[:, ss]
    m["smT"] = np.ascontiguousarray(f(inp["state_mlstm_m"])[:, ss].transpose(0, 2, 1))
    s5 = lambda a: np.ascontiguousarray(f(a)[:, ss].reshape(DEPTH, NS, 16, 128).transpose(0, 3, 2, 1))
    m["s5reT"] = s5(inp["state_s5_re"]); m["s5imT"] = s5(inp["state_s5_im"])
    m["ssdT"] = np.ascontiguousarray(f(inp["state_ssd"])[:, ss].transpose(0, 1, 2, 4, 3))
    m["convT"] = np.ascontiguousarray(f(inp["cache_conv"])[:, ss].reshape(DEPTH, NS, 3, 8, 128).transpose(0, 4, 3, 2, 1))
    m["half"] = np.ascontiguousarray(np.broadcast_to(np.array([[1.0, 0.0]] if c < 4 else [[0.0, 1.0]], np.float32), (128, 2)))
    return m


_NC_CACHE = {}


def run_device(inputs, dbg=None, stages=99, cores=8, trace=False):
    key = (tuple(sorted(dbg or ())), stages)
    if key not in _NC_CACHE:
        _NC_CACHE[key] = build_program(dbg, stages)
    nc, K = _NC_CACHE[key]
    sh = prep_shared(inputs)
    in_maps = []
    for c in range(cores):
        m = dict(sh)
        m.update(prep_core(inputs, c))
        in_maps.append(m)
    if trace:
        res = run_bass_kernel_spmd(nc, in_maps, core_ids=list(range(cores)), trace=True)
        print("EXEC_NS", res.exec_time_ns)
    else:
        res = run_bass_kernel_spmd(nc, in_maps, core_ids=list(range(cores)))
    return res.results


def kernel(**inputs):
    R = run_device(inputs)
    B = 4
    y_p = np.stack([np.concatenate([R[b]["yT"][:, :1024].T, R[b + 4]["yT"][:, :1024].T], axis=0) for b in range(B)])
    y_s = np.concatenate([R[c]["yT"][:, 1024:NO].T for c in range(8)], axis=0)[:, None, :]
    st = lambda nm, idx: np.stack([R[b][nm] for b in range(B)], axis=1)
    C_p = np.stack([R[b]["C_p"] for b in range(B)], axis=1)
    n_p = np.stack([R[b]["n_p"] for b in range(B)], axis=1)
    m_p = np.stack([R[b]["m_pT"][:, :, 0] for b in range(B)], axis=1)
    unfm = lambda a: a.swapaxes(-1, -2).reshape(a.shape[:-2] + (32, 64))
    s5re_p = np.stack([unfm(R[b]["s5re_pT"]) for b in range(B)], axis=1)
    s5im_p = np.stack([unfm(R[b]["s5im_pT"]) for b in range(B)], axis=1)
    ssd_p = np.stack([R[b]["ssd_pT"].transpose(0, 1, 3, 2) for b in range(B)], axis=1)
    conv_p = np.stack([R[b]["conv_pT"].transpose(0, 3, 2, 1).reshape(DEPTH, 3, 1024) for b in range(B)], axis=1)
    C_s = np.concatenate([R[c]["C_s"] for c in range(8)], axis=1)
    n_s = np.concatenate([R[c]["n_s"] for c in range(8)], axis=1)
    m_s = np.concatenate([R[c]["m_sT"].transpose(0, 2, 1) for c in range(8)], axis=1)
    uns = lambda a: a.transpose(0, 3, 2, 1).reshape(DEPTH, NS, 32, 64)
    s5re_s = np.concatenate([uns(R[c]["s5re_sT"]) for c in range(8)], axis=1)
    s5im_s = np.concatenate([uns(R[c]["s5im_sT"]) for c in range(8)], axis=1)
    ssd_s = np.concatenate([R[c]["ssd_sT"].transpose(0, 1, 2, 4, 3) for c in range(8)], axis=1)
    conv_s = np.concatenate([R[c]["conv_sT"].transpose(0, 4, 3, 2, 1).reshape(DEPTH, NS, 3, 1024) for c in range(8)], axis=1)
    outs = (y_p, y_s, C_p, n_p, m_p, s5re_p, s5im_p, ssd_p, conv_p, C_s, n_s, m_s, s5re_s, s5im_s, ssd_s, conv_s)
    return tuple(np.ascontiguousarray(o, dtype=np.float32) for o in outs)
```

```python
import math
import numpy as np
import concourse.bass as bass
import concourse.mybir as mybir
from concourse.bass_utils import run_bass_kernel_spmd
from contextlib import ExitStack

F32 = mybir.dt.float32
BF16 = mybir.dt.bfloat16
AF = mybir.ActivationFunctionType
ALU = mybir.AluOpType
AX = mybir.AxisListType

L = 2048
NS = 16
NT = L + NS
XW = 2080
D = 2048
KC = 16
DEPTH = 2
N_IN = 6160
D_FF = 5632
D_FFE = 7168
NE = 8
EPS = 1e-6
TB = [(0, 512), (512, 512), (1024, 512), (1536, 512), (2048, 16)]
CHUNKS = [(c * 64, 64, c) for c in range(32)] + [(L + j, 1, 32 + j) for j in range(NS)]
NCI = 48
NO = 1040
TBO = [(0, 512), (512, 512), (1024, 16)]
NEG = -30000.0


class Tk:
    __slots__ = ("name", "lw", "rd", "mw", "dsem", "dcnt")

    def __init__(self, name):
        self.name = name
        self.lw = None
        self.rd = []
        self.mw = []
        self.dsem = None
        self.dcnt = 0


class T:
    __slots__ = ("a", "k")

    def __init__(self, a, name):
        self.a = a
        self.k = Tk(name)


class Ins:
    __slots__ = ("eng", "fn", "waits", "needed", "val", "dsem", "dval")

    def __init__(self, eng, fn):
        self.eng = eng
        self.fn = fn
        self.waits = []
        self.needed = False
        self.val = None
        self.dsem = None
        self.dval = None


class Sched:
    ENGS = ("pe", "act", "dve", "pool", "sp")

    def __init__(self, nc, stack):
        self.nc = nc
        self.stack = stack
        self.prog = {e: [] for e in self.ENGS}
        self.esem = {e: stack.enter_context(nc.semaphore("es_" + e)) for e in self.ENGS}
        self.ecnt = {e: 0 for e in self.ENGS}
        self.known = {e: {} for e in self.ENGS}
        self.nsem = 0
        self.out_events = []
        self.toks = []
        self.pend_dma = []
        self.last = {e: None for e in self.ENGS}
        self.semfree = []
        self.semcnt = {}
        self.phase_mark = 0

    def phase_begin(self):
        self.phase_mark = len(self.toks)

    def phase_end(self):
        for k in self.toks[self.phase_mark:]:
            if k.dsem is not None:
                self.semfree.append(k.dsem)
                k.dsem = None
        del self.toks[self.phase_mark:]

    def tok(self, name):
        k = Tk(name)
        self.toks.append(k)
        return k

    def _deps(self, ins, reads, writes, mwrites):
        evs = []
        for r in reads:
            if r.lw is not None:
                evs.append(r.lw)
            evs.extend(r.mw)
        for w in writes:
            if w.lw is not None:
                evs.append(w.lw)
            evs.extend(w.mw)
            evs.extend(w.rd)
        for w in mwrites:
            if w.lw is not None:
                evs.append(w.lw)
            evs.extend(w.rd)
        seen = set()
        for ev in evs:
            if id(ev) in seen or ev is ins:
                continue
            seen.add(id(ev))
            if ev.dsem is None and ev.eng == "pe" and ins.eng == "pe" and ins.dsem is None:
                continue
            ins.waits.append(ev)
            ev.needed = True
        for r in reads:
            r.rd.append(ins)
        for w in writes:
            w.lw = ins
            w.rd = []
            w.mw = []
        for w in mwrites:
            if w.rd:
                w.rd = []
                w.mw = []
                w.lw = None
            w.mw.append(ins)

    def op(self, eng, fn, reads=(), writes=()):
        ins = Ins(eng, fn)
        self._deps(ins, [r.k if isinstance(r, T) else r for r in reads],
                   [w.k if isinstance(w, T) else w for w in writes], [])
        self.prog[eng].append(ins)
        self.last[eng] = ins
        return ins

    def dma(self, q, out_ap, in_ap, tok, reads=(), writes=(), mwrites=(), is_output=False, **kw):
        if isinstance(tok, T):
            tok = tok.k
        if tok.dsem is None:
            if self.semfree:
                tok.dsem = self.semfree.pop()
            else:
                tok.dsem = self.stack.enter_context(self.nc.semaphore("ds_%d" % self.nsem))
                self.nsem += 1
        ins = Ins(q, lambda e: e.dma_start(out=out_ap, in_=in_ap, **kw))
        c = self.semcnt.get(id(tok.dsem), 0) + 16
        self.semcnt[id(tok.dsem)] = c
        ins.dsem = tok.dsem
        ins.dval = c
        self._deps(ins, [r.k if isinstance(r, T) else r for r in reads],
                   [w.k if isinstance(w, T) else w for w in writes],
                   [w.k if isinstance(w, T) else w for w in mwrites])
        self.prog[q].append(ins)
        self.pend_dma.append(ins)
        if is_output:
            self.out_events.append(ins)
        return ins

    def dma_fn(self, q, fn, tok, reads=(), writes=(), mwrites=()):
        if isinstance(tok, T):
            tok = tok.k
        if tok.dsem is None:
            if self.semfree:
                tok.dsem = self.semfree.pop()
            else:
                tok.dsem = self.stack.enter_context(self.nc.semaphore("ds_%d" % self.nsem))
                self.nsem += 1
        ins = Ins(q, fn)
        c = self.semcnt.get(id(tok.dsem), 0) + 16
        self.semcnt[id(tok.dsem)] = c
        ins.dsem = tok.dsem
        ins.dval = c
        self._deps(ins, [r.k if isinstance(r, T) else r for r in reads],
                   [w.k if isinstance(w, T) else w for w in writes],
                   [w.k if isinstance(w, T) else w for w in mwrites])
        self.prog[q].append(ins)
        self.pend_dma.append(ins)
        return ins

    def _emit_engine(self, e, engh):
        known = self.known[e]
        for ins in self.prog[e]:
            need = {}
            for ev in ins.waits:
                if ev.dsem is not None:
                    key, sem, val = ("d", id(ev.dsem)), ev.dsem, ev.dval
                else:
                    key, sem, val = ("e", ev.eng), self.esem[ev.eng], ev.val
                if known.get(key, 0) >= val:
                    continue
                if key not in need or need[key][1] < val:
                    need[key] = (sem, val)
            for key, (sem, val) in need.items():
                engh.wait_ge(sem, val)
                known[key] = val
            if ins.fn is None:
                continue
            bi = ins.fn(engh)
            if ins.dsem is not None:
                bi.then_inc(ins.dsem, 16)
            elif ins.needed:
                bi.then_inc(self.esem[e], 1)

    def flush(self, final=False):
        nc = self.nc
        bar = Ins("sp", lambda e: e.nop())
        seen = set()
        for ev in self.pend_dma:
            if ev.eng != "sp" or True:
                key = (id(ev.dsem))
                bar.waits.append(ev)
        for e in self.ENGS:
            if e != "sp" and self.last[e] is not None:
                self.last[e].needed = True
                bar.waits.append(self.last[e])
        bar.needed = True
        self.prog["sp"].append(bar)
        for e in self.ENGS:
            if e != "sp":
                w = Ins(e, None)
                w.waits.append(bar)
                self.prog[e].append(w)
        for e in self.ENGS:
            c = self.ecnt[e]
            for ins in self.prog[e]:
                if ins.dsem is None and ins.needed and ins.fn is not None:
                    c += 1
                    ins.val = c
            self.ecnt[e] = c
        with nc.Block() as block:
            @block.tensor
            def _(eng):
                self._emit_engine("pe", eng)

            @block.scalar
            def _(eng):
                self._emit_engine("act", eng)

            @block.vector
            def _(eng):
                self._emit_engine("dve", eng)

            @block.gpsimd
            def _(eng):
                self._emit_engine("pool", eng)

            @block.sync
            def _(eng):
                self._emit_engine("sp", eng)
        self.prog = {e: [] for e in self.ENGS}
        self.pend_dma = []
        self.last = {e: None for e in self.ENGS}
        for k in self.toks:
            k.lw = None
            k.rd = []
            k.mw = []


_UID = [0]


class Pool:
    def __init__(self, K, name, shape, dt, n, space="sb"):
        _UID[0] += 1
        name = "%s_%d_" % (name, _UID[0])
        self.bufs = []
        for i in range(n):
            if space == "sb":
                a = K.st.enter_context(K.nc.sbuf_tensor("%s%d" % (name, i), list(shape), dt))
            else:
                a = K.st.enter_context(K.nc.psum_tensor("%s%d" % (name, i), list(shape), dt))
            t = T(a, "%s%d" % (name, i))
            K.S.toks.append(t.k)
            self.bufs.append(t)
        self.i = 0

    def next(self):
        b = self.bufs[self.i % len(self.bufs)]
        self.i += 1
        return b


class _Phase:
    def __init__(self, K):
        self.K = K

    def __enter__(self):
        self.pst = ExitStack()
        self.pst.__enter__()
        self.K.st = self.pst
        self.K.S.phase_begin()
        return self

    def __exit__(self, *a):
        if a[0] is None:
            self.K.S.flush()
            self.K.S.phase_end()
        self.K.st = self.K.gst
        return self.pst.__exit__(*a)


class Builder:
    def __init__(self, nc, st, dbg=None):
        self.nc = nc
        self.gst = st
        self.st = st
        self.S = Sched(nc, st)
        self.dbg = dbg or set()
        self.inputs = {}
        self.outputs = {}

    def phase(self):
        return _Phase(self)

    def din(self, name, shape, dt=F32):
        a = self.nc.dram_tensor(name, list(shape), dt, kind="ExternalInput").ap()
        t = T(a, name)
        self.inputs[name] = t
        return t

    def dout(self, name, shape, dt=F32):
        a = self.nc.dram_tensor(name, list(shape), dt, kind="ExternalOutput").ap()
        t = T(a, name)
        self.S.toks.append(t.k)
        self.outputs[name] = t
        return t

    def dscr(self, name, shape, dt=F32):
        kind = "ExternalOutput" if name in self.dbg else "Internal"
        a = self.nc.dram_tensor(name, list(shape), dt, kind=kind).ap()
        t = T(a, name)
        self.S.toks.append(t.k)
        return t

    def sb(self, name, shape, dt=F32):
        _UID[0] += 1
        name = "%s_%d" % (name, _UID[0])
        a = self.st.enter_context(self.nc.sbuf_tensor(name, list(shape), dt))
        t = T(a, name)
        self.S.toks.append(t.k)
        return t

    def view(self, ap, name):
        t = T(ap, name)
        self.S.toks.append(t.k)
        return t

    def pe(self, out, lhsT, rhs, start, stop, reads, writes):
        self.S.op("pe", lambda e: e.matmul(out, lhsT, rhs, start=start, stop=stop), reads=reads, writes=writes)

    def tr(self, out, in_, ident, reads, writes):
        self.S.op("pe", lambda e: e.transpose(out, in_, ident), reads=reads, writes=writes)

    def act(self, out, in_, func, reads, writes, scale=1.0, bias=None, accum=None):
        def f(e):
            kw = {}
            if bias is not None:
                kw["bias"] = bias
            if accum is not None:
                kw["accum_out"] = accum
            return e.activation(out=out, in_=in_, func=func, scale=scale, **kw)
        self.S.op("act", f, reads=reads, writes=writes)

    def tt(self, eng, out, in0, in1, op, reads, writes):
        self.S.op(eng, lambda e: e.tensor_tensor(out, in0, in1, op), reads=reads, writes=writes)

    def ts(self, eng, out, in0, s1, op0, reads, writes, s2=None, op1=None):
        if op1 is None:
            self.S.op(eng, lambda e: e.tensor_scalar(out, in0, s1, None, op0), reads=reads, writes=writes)
        else:
            self.S.op(eng, lambda e: e.tensor_scalar(out, in0, s1, s2, op0, op1), reads=reads, writes=writes)

    def stt(self, out, in0, scalar, in1, op0, op1, reads, writes):
        self.S.op("dve", lambda e: e.scalar_tensor_tensor(out, in0, scalar, in1, op0, op1), reads=reads, writes=writes)

    def cp(self, eng, out, in_, reads, writes):
        if eng == "act":
            self.S.op("act", lambda e: e.copy(out, in_), reads=reads, writes=writes)
        else:
            self.S.op(eng, lambda e: e.tensor_copy(out, in_), reads=reads, writes=writes)

    def memset(self, eng, ap, val, writes):
        self.S.op(eng, lambda e: e.memset(ap, val), writes=writes)

    def recip(self, out, in_, reads, writes):
        self.S.op("dve", lambda e: e.reciprocal(out, in_), reads=reads, writes=writes)

    def scan(self, out, d0, d1, init, op0, op1, reads, writes):
        self.S.op("dve", lambda e: e.tensor_tensor_scan(out, d0, d1, init, op0, op1), reads=reads, writes=writes)

    def ld(self, dst_ap, src_ap, tok, reads=(), writes=(), q="sp", **kw):
        self.S.dma(q, dst_ap, src_ap, tok, reads=reads, writes=writes, **kw)

    def stq(self, dst_ap, src_ap, tok, reads=(), mwrites=(), writes=(), q="sp", is_output=False, **kw):
        self.S.dma(q, dst_ap, src_ap, tok, reads=reads, mwrites=mwrites, writes=writes, is_output=is_output, **kw)


def build_program(dbg=None, stages=99):
    nc = bass.Bass("TRN2", target_bir_lowering=False)
    with ExitStack() as gst:
        K = Builder(nc, gst, dbg)
        S = K.S
        I = {}
        def din(name, shape):
            I[name] = K.din(name, shape)
            return I[name]
        din("xT", [D, NT])
        din("pT", [DEPTH, 256, NT])
        din("sC", [DEPTH, NS, 4, 256, 256]); din("sn", [DEPTH, NS, 4, 256]); din("smT", [DEPTH, 4, NS])
        din("s5reT", [DEPTH, 128, 16, NS]); din("s5imT", [DEPTH, 128, 16, NS])
        din("ssdT", [DEPTH, NS, 8, 128, 64]); din("convT", [DEPTH, 128, 8, 3, NS])
        for nm in ("g_mix", "g_ffn", "g_ple"):
            din(nm, [DEPTH, 128, 16])
        din("g_final", [128, 16])
        din("w_in", [DEPTH, D, N_IN]); din("w_out", [DEPTH, D, D])
        din("b_ig", [DEPTH, 4, 1]); din("b_fg", [DEPTH, 4, 1]); din("gml_rep", [DEPTH, 64, 1024])
        for nm in ("lam_re", "lam_im", "logdt"):
            din(nm, [DEPTH, 128, 16])
        din("Bre", [DEPTH, 16, 128, 128]); din("Bim", [DEPTH, 16, 128, 128])
        din("Cre", [DEPTH, 16, 128, 128]); din("Cim", [DEPTH, 16, 128, 128])
        din("s5d", [DEPTH, 128, 4]); din("w_glu", [DEPTH, 512, 512]); din("b_glu", [DEPTH, 128, 4]); din("g_s5", [DEPTH, 128, 4])
        din("conv_w", [DEPTH, 128, 8, 4]); din("conv_b", [DEPTH, 128, 8])
        din("dt_bias", [DEPTH, 8, 1]); din("a_log", [DEPTH, 8, 1]); din("ssdd_rep", [DEPTH, 64, 8]); din("gssd_rep", [DEPTH, 64, 512])
        din("ffn_wg", [D_FF // 256, 128, 4096]); din("ffn_wu", [D_FF // 256, 128, 4096]); din("ffn_wd", [D_FF // 256, 128, 4096])
        din("w_router", [D, NE]); din("b_router_rep", [128, NE])
        din("moe_wg", [NE, D_FFE // 256, 128, 4096]); din("moe_wu", [NE, D_FFE // 256, 128, 4096]); din("moe_wd", [NE, D_FFE // 256, 128, 4096])
        din("w_ple", [DEPTH, 256, D]); din("w_pleg", [DEPTH, D, D])
        din("ident", [128, 128]); din("nident", [128, 128]); din("maskT", [64, 64])
        din("half", [128, 2])
        din("sel4", [4, 4, 128]); din("sel8", [8, 8, 128]); din("sel8e", [8, 8, 128])
        O = {}
        def dout(name, shape):
            O[name] = K.dout(name, shape)
            return O[name]
        dout("yT", [D, NO])
        dout("C_p", [DEPTH, 4, 256, 256]); dout("n_p", [DEPTH, 4, 256]); dout("m_pT", [DEPTH, 4, 1])
        dout("s5re_pT", [DEPTH, 128, 16]); dout("s5im_pT", [DEPTH, 128, 16])
        dout("ssd_pT", [DEPTH, 8, 128, 64]); dout("conv_pT", [DEPTH, 128, 8, 3])
        dout("C_s", [DEPTH, NS, 4, 256, 256]); dout("n_s", [DEPTH, NS, 4, 256]); dout("m_sT", [DEPTH, 4, NS])
        dout("s5re_sT", [DEPTH, 128, 16, NS]); dout("s5im_sT", [DEPTH, 128, 16, NS])
        dout("ssd_sT", [DEPTH, NS, 8, 128, 64]); dout("conv_sT", [DEPTH, 128, 8, 3, NS])
        hT = K.dscr("hT", [D, NT])
        qT_d = K.dscr("qT_d", [1024, NT]); kT_d = K.dscr("kT_d", [1024, NT])
        k_d = K.dscr("k_d", [NT, 1024]); v_d = K.dscr("v_d", [NT, 1024]); go_d = K.dscr("go_d", [NT, 1024])
        uT_d = K.dscr("uT_d", [512, NT]); zs_d = K.dscr("zs_d", [NT, 512]); xbcT_d = K.dscr("xbcT_d", [1024, NT])
        xcT_d = K.dscr("xcT_d", [1024, NT])
        mix_d = K.dscr("mix_d", [D, NT])
        hO = K.dscr("hO", [D, NO])
        mixsw = K.dscr("mixsw", [128, KC * XW], BF16)
        halfsb = K.sb("halfsb", [128, 2])
        K.ld(halfsb.a[:], I["half"].a, halfsb, writes=[halfsb])

        def blend(dstA, srcB, toks_r, tok_w):
            K.ts("dve", dstA, dstA, halfsb.a[:, 0:1], ALU.mult, list(toks_r) + [halfsb], [tok_w])
            K.stt(dstA, srcB, halfsb.a[:, 1:2], dstA, ALU.mult, ALU.add, list(toks_r) + [halfsb, tok_w], [tok_w])

        XTraw = K.sb("XT", [128, KC * XW // 2], F32)
        XTb = XTraw.a[:].bitcast(BF16).rearrange("p (k t) -> p k t", k=KC)
        XTf = XTraw.a[:].rearrange("p (k t) -> p k t", k=KC)
        tXT = XTraw
        ident = K.sb("ident_sb", [128, 128]); nident = K.sb("nident_sb", [128, 128]); maskT = K.sb("maskT_sb", [64, 64])
        K.ld(ident.a[:], I["ident"].a, ident, writes=[ident]); K.ld(nident.a[:], I["nident"].a, nident, writes=[nident])
        K.ld(maskT.a[:], I["maskT"].a, maskT, writes=[maskT])
        ones_bf = K.sb("ones_bf", [128, 128], BF16); ones_f = K.sb("ones_f", [128, 128])
        K.memset("dve", ones_bf.a[:], 1.0, [ones_bf]); K.memset("dve", ones_f.a[:], 1.0, [ones_f])
        epsb = K.sb("epsb", [128, 1]); K.memset("dve", epsb.a[:], EPS, [epsb])
        halfpi = K.sb("halfpi", [128, 1]); K.memset("dve", halfpi.a[:], math.pi / 2, [halfpi])
        gn = {}
        for nm in ("g_mix", "g_ffn", "g_ple"):
            gn[nm] = K.sb(nm + "_sb", [128, DEPTH, 16])
            for l in range(DEPTH):
                K.ld(gn[nm].a[:, l, :], I[nm].a[l], gn[nm], writes=[gn[nm]])
        gfin = K.sb("gfin_sb", [128, 16]); K.ld(gfin.a[:], I["g_final"].a, gfin, writes=[gfin])
        PS = Pool(K, "ps", [128, 512], F32, 8, space="ps")

        class ListPool:
            def __init__(self, bufs):
                self.bufs = bufs
                self.i = 0

            def next(self):
                b = self.bufs[self.i % len(self.bufs)]
                self.i += 1
                return b
        PSs = ListPool([K.view(PS.bufs[b].a[:, j * 64:(j + 1) * 64], "pss%d_%d" % (b, j)) for b in range(4) for j in range(8)])
        PSb = ListPool([K.view(PS.bufs[b].a, "psb%d" % b) for b in range(4, 8)])
        K.PSs, K.PSb = PSs, PSb
        S.flush()

        def rstd_from_ps(ps, n, inv_d, rs, parts=128):
            K.act(rs.a[:parts, :n], ps.a[:parts, :n], AF.Sqrt, [ps, epsb], [rs], scale=inv_d, bias=epsb.a[:parts, :])
            K.recip(rs.a[:parts, :n], rs.a[:parts, :n], [rs], [rs])

        def norm_stage(src, gsb_ap, gtile, t0, n, dst_ap, dst_tile, P):
            hb = P["hblk"].next()
            K.ld(hb.a[:, :, :n], src.a.rearrange("(k p) t -> p k t", p=128)[:, :, t0:t0 + n], hb, reads=[src], writes=[hb])
            sq = P["sq"].next()
            K.act(sq.a[:, :, :n], hb.a[:, :, :n], AF.Square, [hb], [sq])
            ps = PS.next()
            for k in range(KC):
                K.pe(ps.a[:, :n], ones_bf.a[:], sq.a[:, k, :n], k == 0, k == KC - 1, [sq, ones_bf], [ps])
            rs = P["rs"].next()
            rstd_from_ps(ps, n, 1.0 / D, rs)
            for k in range(KC):
                K.stt(dst_ap[:, k, :], hb.a[:, k, :n], gsb_ap[:, k:k + 1], rs.a[:, :n], ALU.mult, ALU.mult,
                      [hb, rs, gtile], [dst_tile])
            return hb

        def wload(P, W_ap, kc, pw, name="w"):
            wb = P[name].next()
            K.ld(wb.a[:, :kc, :pw], W_ap.rearrange("(k p) c -> p k c", p=128), wb, writes=[wb], q="pool")
            return wb

        def proj_fm(P, xap, xt, kc, W_ap, c0, ncols, evac, tblocks):
            for p0 in range(0, ncols, 512):
                pw = min(512, ncols - p0)
                wb = wload(P, W_ap[:, c0 + p0:c0 + p0 + pw], kc, pw)
                for m0 in range(0, pw, 128):
                    mw = min(128, pw - m0)
                    for (t0, n) in tblocks:
                        ps = PS.next()
                        for k in range(kc):
                            K.pe(ps.a[:mw, :n], wb.a[:, k, m0:m0 + mw], xap[:, k, t0:t0 + n], k == 0, k == kc - 1, [wb, xt], [ps])
                        evac(ps, p0 + m0, mw, t0, n)

        def proj_tm(P, xap, xt, kc, W_ap, c0, ncols, evac):
            for p0 in range(0, ncols, 512):
                pw = min(512, ncols - p0)
                wb = wload(P, W_ap[:, c0 + p0:c0 + p0 + pw], kc, pw)
                for j in range(17):
                    t0 = j * 128
                    n = 128 if j < 16 else NS
                    ps = PS.next()
                    for k in range(kc):
                        K.pe(ps.a[:n, :pw], xap[:, k, t0:t0 + n], wb.a[:, k, :pw], k == 0, k == kc - 1, [wb, xt], [ps])
                    evac(ps, p0, pw, t0, n)

        evi = [0]
        def evac_copy_to_dram(P, ps, pr, fr, dst_ap, dst, scale=None, func=None, mul=None, multile=None):
            stg = P["stg"].next()
            if func is not None:
                K.act(stg.a[:pr, :fr], ps.a[:pr, :fr], func, [ps], [stg])
                if mul is not None:
                    K.tt("dve", stg.a[:pr, :fr], stg.a[:pr, :fr], mul, ALU.mult, [stg, multile], [stg])
            elif scale is not None:
                K.act(stg.a[:pr, :fr], ps.a[:pr, :fr], AF.Copy, [ps], [stg], scale=scale)
            else:
                evi[0] += 1
                K.cp("act" if evi[0] % 2 else "dve", stg.a[:pr, :fr], ps.a[:pr, :fr], [ps], [stg])
            K.stq(dst_ap, stg.a[:pr, :fr], stg, reads=[stg], mwrites=[dst])

        S.dma("sp", hT.a, I["xT"].a, hT, writes=[hT])
        S.flush()

        for l in range(DEPTH):
            if stages < 1:
                break
            with K.phase():
                P = {"hblk": Pool(K, "hblk", [128, KC, 512], F32, 1), "sq": Pool(K, "sq", [128, KC, 512], BF16, 1),
                     "rs": Pool(K, "rs", [128, 512], F32, 2), "w": Pool(K, "w", [128, KC, 512], BF16, 3),
                     "stg": Pool(K, "stg", [128, 512], F32, 4)}
                gi = K.sb("gi", [4, NT]); gf = K.sb("gf", [4, NT]); gdt = K.sb("gdt", [8, NT])
                gml = K.sb("gml", [128, 1024])
                for r in range(2):
                    K.ld(gml.a[r * 64:(r + 1) * 64, :], I["gml_rep"].a[l], gml, writes=[gml])
                for (t0, n) in TB:
                    norm_stage(hT, gn["g_mix"].a[:, l, :], gn["g_mix"], t0, n, XTb[:, :, t0:t0 + n], tXT, P)
                W = I["w_in"].a[l]
                proj_fm(P, XTb, tXT, KC, W, 0, 1024, lambda ps, co, mw, t0, n: evac_copy_to_dram(P, ps, mw, n, qT_d.a[co:co + mw, t0:t0 + n], qT_d), TB)
                proj_fm(P, XTb, tXT, KC, W, 1024, 1024, lambda ps, co, mw, t0, n: evac_copy_to_dram(P, ps, mw, n, kT_d.a[co:co + mw, t0:t0 + n], kT_d, scale=1.0 / 16.0), TB)
                proj_tm(P, XTb, tXT, KC, W, 1024, 1024, lambda ps, co, pw, t0, n: evac_copy_to_dram(P, ps, n, pw, k_d.a[t0:t0 + n, co:co + pw], k_d, scale=1.0 / 16.0))
                proj_tm(P, XTb, tXT, KC, W, 2048, 1024, lambda ps, co, pw, t0, n: evac_copy_to_dram(P, ps, n, pw, v_d.a[t0:t0 + n, co:co + pw], v_d))
                proj_tm(P, XTb, tXT, KC, W, 3072, 1024, lambda ps, co, pw, t0, n: evac_copy_to_dram(P, ps, n, pw, go_d.a[t0:t0 + n, co:co + pw], go_d, func=AF.Sigmoid, mul=gml.a[:n, co:co + pw], multile=gml))
                gsc = {}
                for nm, c0, nr in (("gi_d", 4096, 4), ("gf_d", 4100, 4), ("gdt_d", 6152, 8)):
                    gsc[nm] = K.dscr(nm + str(l), [nr, NT])
                    proj_fm(P, XTb, tXT, KC, W, c0, nr, lambda ps, co, mw, t0, n, nm=nm: evac_copy_to_dram(P, ps, mw, n, gsc[nm].a[co:co + mw, t0:t0 + n], gsc[nm]), TB)
                proj_fm(P, XTb, tXT, KC, W, 4104, 512, lambda ps, co, mw, t0, n: evac_copy_to_dram(P, ps, mw, n, uT_d.a[co:co + mw, t0:t0 + n], uT_d), TB)
                proj_tm(P, XTb, tXT, KC, W, 4616, 512, lambda ps, co, pw, t0, n: evac_copy_to_dram(P, ps, n, pw, zs_d.a[t0:t0 + n, co:co + pw], zs_d, func=AF.Silu))
                proj_fm(P, XTb, tXT, KC, W, 5128, 1024, lambda ps, co, mw, t0, n: evac_copy_to_dram(P, ps, mw, n, xbcT_d.a[co:co + mw, t0:t0 + n], xbcT_d), TB)
            if stages < 2:
                break
            mixers(K, S, PS, I, O, l, dict(hT=hT, qT_d=qT_d, kT_d=kT_d, k_d=k_d, v_d=v_d, go_d=go_d, uT_d=uT_d, zs_d=zs_d,
                                           xbcT_d=xbcT_d, xcT_d=xcT_d, gi_d=gsc["gi_d"], gf_d=gsc["gf_d"], gdt_d=gsc["gdt_d"]),
                   XTb, tXT, ident, nident, maskT, ones_f, epsb, halfpi, gst, stages)
            if "mix_d" in K.dbg:
                with K.phase():
                    stg = Pool(K, "mstg", [128, 512], F32, 2)
                    for k in range(KC):
                        for (t0, n) in TB:
                            s_ = stg.next()
                            K.cp("dve", s_.a[:, :n], XTb[:, k, t0:t0 + n], [tXT], [s_])
                            K.stq(mix_d.a[k * 128:(k + 1) * 128, t0:t0 + n], s_.a[:, :n], s_, reads=[s_], mwrites=[mix_d])
            if stages < 3:
                break
            own = (l == DEPTH - 1)
            hcur = hO if own else hT
            tbl = TBO if own else TB
            if own:
                with K.phase():
                    for k in range(KC):
                        blend(XTb[:, k, 0:1024], XTb[:, k, 1024:2048], [tXT], tXT)
                    for k in range(KC):
                        K.cp("dve", XTb[:, k, 1024:NO], XTb[:, k, L:NT], [tXT], [tXT])
                    hsw = Pool(K, "hsw", [128, 2, 1024], F32, 2)
                    for k in range(KC):
                        t_ = hsw.next()
                        K.ld(t_.a[:], hT.a[k * 128:(k + 1) * 128, 0:L].rearrange("p (h t) -> p h t", h=2), t_, reads=[hT], writes=[t_])
                        blend(t_.a[:, 0, :], t_.a[:, 1, :], [t_], t_)
                        K.stq(hO.a[k * 128:(k + 1) * 128, 0:1024], t_.a[:, 0, :], t_, reads=[t_], mwrites=[hO])
                    S.dma("sp", hO.a[:, 1024:NO], hT.a[:, L:NT], hO, reads=[hT], mwrites=[hO])
            with K.phase():
                P = {"w": Pool(K, "w", [128, KC, 512], BF16, 3), "stg": Pool(K, "stg", [128, 512], F32, 4),
                     "hb": Pool(K, "hb", [128, 512], F32, 3)}
                def ev_res(ps, co, mw, t0, n):
                    hb = P["hb"].next()
                    K.ld(hb.a[:mw, :n], hcur.a[co:co + mw, t0:t0 + n], hb, reads=[hcur], writes=[hb])
                    stg = P["stg"].next()
                    K.tt("dve", stg.a[:mw, :n], ps.a[:mw, :n], hb.a[:mw, :n], ALU.add, [ps, hb], [stg])
                    K.stq(hcur.a[co:co + mw, t0:t0 + n], stg.a[:mw, :n], stg, reads=[stg], mwrites=[hcur])
                proj_fm(P, XTb, tXT, KC, I["w_out"].a[l], 0, D, ev_res, tbl)
            if stages < 4:
                break
            ffn_phase(K, S, PS, I, l, hcur, XTf, tXT, gn, ones_bf, epsb, ident, gst, norm_stage, stages, own)
            if stages < 5:
                break
            with K.phase():
                P = {"hblk": Pool(K, "hblk", [128, KC, 512], F32, 1), "sq": Pool(K, "sq", [128, KC, 512], BF16, 1),
                     "rs": Pool(K, "rs", [128, 512], F32, 2), "w": Pool(K, "w", [128, KC, 512], BF16, 2),
                     "stg": Pool(K, "stg", [128, 512], F32, 3), "hb": Pool(K, "hb", [128, 512], F32, 2)}
                pTf = K.sb("pTf", [128, 2, NT]); pTb = K.sb("pTb", [128, 2, NT], BF16)
                pTv = I["pT"].a[l].rearrange("(k p) t -> p k t", p=128)
                K.ld(pTf.a[:], pTv, pTf, writes=[pTf])
                if own:
                    for k in range(2):
                        blend(pTf.a[:, k, 0:1024], pTf.a[:, k, 1024:2048], [pTf], pTf)
                    for k in range(2):
                        K.cp("dve", pTf.a[:, k, 1024:NO], pTf.a[:, k, L:NT], [pTf], [pTf])
                K.cp("act", pTb.a[:], pTf.a[:], [pTf], [pTb])
                wple = K.sb("wple", [128, 2, D], BF16)
                K.ld(wple.a[:], I["w_ple"].a[l].rearrange("(k p) c -> p k c", p=128), wple, writes=[wple], q="pool")
                for (t0, n) in tbl:
                    norm_stage(hcur, gn["g_ple"].a[:, l, :], gn["g_ple"], t0, n, XTb[:, :, t0:t0 + n], tXT, P)
                def ev_ple(ps, co, mw, t0, n):
                    sg = P["stg"].next()
                    K.act(sg.a[:mw, :n], ps.a[:mw, :n], AF.Sigmoid, [ps], [sg])
                    ps2 = PS.next()
                    for k in range(2):
                        K.pe(ps2.a[:mw, :n], wple.a[:, k, co:co + mw], pTb.a[:, k, t0:t0 + n], k == 0, k == 1, [wple, pTb], [ps2])
                    K.tt("dve", sg.a[:mw, :n], ps2.a[:mw, :n], sg.a[:mw, :n], ALU.mult, [ps2, sg], [sg])
                    hb = P["hb"].next()
                    K.ld(hb.a[:mw, :n], hcur.a[co:co + mw, t0:t0 + n], hb, reads=[hcur], writes=[hb])
                    K.tt("dve", sg.a[:mw, :n], sg.a[:mw, :n], hb.a[:mw, :n], ALU.add, [sg, hb], [sg])
                    K.stq(hcur.a[co:co + mw, t0:t0 + n], sg.a[:mw, :n], sg, reads=[sg], mwrites=[hcur])
                proj_fm(P, XTb, tXT, KC, I["w_pleg"].a[l], 0, D, ev_ple, tbl)
        with K.phase():
            P = {"hblk": Pool(K, "hblk", [128, KC, 512], F32, 1), "sq": Pool(K, "sq", [128, KC, 512], BF16, 1),
                 "rs": Pool(K, "rs", [128, 512], F32, 2)}
            yb = K.sb("yb", [128, KC, 512])
            for (t0, n) in TBO:
                norm_stage(hO, gfin.a, gfin, t0, n, yb.a[:, :, :n], yb, P)
                K.stq(O["yT"].a.rearrange("(k p) t -> p k t", p=128)[:, :, t0:t0 + n], yb.a[:, :, :n], yb, reads=[yb], mwrites=[O["yT"]], is_output=True)
    return nc, K


def mixers(K, S, PS, I, O, l, Dm, XTb, tXT, ident, nident, maskT, ones_f, epsb, halfpi, gst, stages):
    with K.phase():
        rowt = [K.sb("row%d" % i, [8, NT]) for i in range(7)]
        A, Bt, Ct, Dt, Et, Fa, Gm = rowt
        onesr = K.sb("onesr", [8, 1]); K.memset("dve", onesr.a[:], 1.0, [onesr])
        TM = K.sb("TM", [64, NCI, 20])
        TMB = K.sb("TMB", [64, NCI, 32])
        GLb = K.sb("GLb", [128, 4 * NCI]); DECb = K.sb("DECb", [128, 8 * NCI])
        sel4 = K.sb("sel4", [4, 4, 128]); K.ld(sel4.a[:], I["sel4"].a, sel4, writes=[sel4])
        sel8 = K.sb("sel8", [8, 8, 128]); K.ld(sel8.a[:], I["sel8"].a, sel8, writes=[sel8])
        small = Pool(K, "small", [8, NCI], F32, 4)

        def to_tm(src, nr, dst, col0):
            for (t0, cl, ci) in CHUNKS:
                ps = PS.next()
                K.tr(ps.a[:cl, :nr], src.a[:nr, t0:t0 + cl], ident.a[:nr, :nr], [src, ident], [ps])
                K.cp("act" if ci % 2 else "dve", dst.a[:cl, ci, col0:col0 + nr], ps.a[:cl, :nr], [ps], [dst])

        def bcast_rows(rows, nr, sel, dst, ncol):
            for h in range(nr):
                ps = PS.next()
                K.pe(ps.a[:, :ncol], sel.a[:nr, h, :], rows.a[:nr, :ncol], True, True, [sel, rows], [ps])
                K.cp("act", dst.a[:, h * ncol:(h + 1) * ncol], ps.a[:, :ncol], [ps], [dst])

        def chunkview(t, nr):
            return t.a[:nr, :L].rearrange("p (c t) -> p c t", t=64)

        def prevlast(Mt, nr, init_s, prev, last):
            K.memset("dve", prev.a[:nr, 0:1], 0.0, [prev])
            K.cp("dve", prev.a[:nr, 1:32], Mt.a[:nr, 63:L - 64:64], [Mt], [prev])
            if init_s is None:
                K.memset("dve", prev.a[:nr, 32:NCI], 0.0, [prev])
            else:
                K.cp("dve", prev.a[:nr, 32:NCI], init_s, [Mt], [prev])
            K.cp("dve", last.a[:nr, 0:32], Mt.a[:nr, 63:L:64], [Mt], [last])
            K.cp("dve", last.a[:nr, 32:NCI], Mt.a[:nr, L:NT], [Mt], [last])

        def sub_chunk(out, x, cvals, nr, sign):
            cb = cvals.a[:nr, 0:32].unsqueeze(2).to_broadcast([nr, 32, 64])
            if sign > 0:
                K.tt("dve", chunkview(out, nr), chunkview(x, nr), cb, ALU.subtract, [x, cvals], [out])
                K.tt("dve", out.a[:nr, L:NT], x.a[:nr, L:NT], cvals.a[:nr, 32:NCI], ALU.subtract, [x, cvals], [out])
            else:
                K.tt("dve", chunkview(out, nr), cb, chunkview(x, nr), ALU.subtract, [x, cvals], [out])
                K.tt("dve", out.a[:nr, L:NT], cvals.a[:nr, 32:NCI], x.a[:nr, L:NT], ALU.subtract, [x, cvals], [out])

        def softplus_neg(x, t1, t2, nr, neg_in):
            K.act(t1.a[:nr, :], x.a[:nr, :], AF.Abs, [x], [t1])
            K.act(t1.a[:nr, :], t1.a[:nr, :], AF.Exp, [t1], [t1], scale=-1.0)
            K.act(t1.a[:nr, :], t1.a[:nr, :], AF.Ln, [t1], [t1], bias=1.0)
            K.ts("dve", t2.a[:nr, :], x.a[:nr, :], 0.0, ALU.min if neg_in else ALU.max, [x], [t2])

        bi = K.sb("bi", [4, 1]); bf = K.sb("bf", [4, 1]); m0s = K.sb("m0s", [4, NS])
        K.ld(bi.a[:], I["b_ig"].a[l], bi, writes=[bi]); K.ld(bf.a[:], I["b_fg"].a[l], bf, writes=[bf])
        K.ld(m0s.a[:], I["smT"].a[l], m0s, writes=[m0s])
        K.ld(A.a[:4, :], Dm["gi_d"].a, A, reads=[Dm["gi_d"]], writes=[A])
        K.ld(Bt.a[:4, :], Dm["gf_d"].a, Bt, reads=[Dm["gf_d"]], writes=[Bt])
        K.ts("dve", A.a[:4, :], A.a[:4, :], bi.a[:, 0:1], ALU.add, [A, bi], [A])
        K.ts("dve", Bt.a[:4, :], Bt.a[:4, :], bf.a[:, 0:1], ALU.add, [Bt, bf], [Bt])
        softplus_neg(Bt, Ct, Dt, 4, True)
        K.tt("dve", Bt.a[:4, :], Dt.a[:4, :], Ct.a[:4, :], ALU.subtract, [Dt, Ct], [Bt])
        K.scan(Et.a[:4, :L], onesr.a[:4, 0:1].to_broadcast([4, L]), Bt.a[:4, :L], 0.0, ALU.mult, ALU.add, [onesr, Bt], [Et])
        K.cp("dve", Et.a[:4, L:NT], Bt.a[:4, L:NT], [Bt], [Et])
        K.tt("dve", Fa.a[:4, :], A.a[:4, :], Et.a[:4, :], ALU.subtract, [A, Et], [Fa])
        K.scan(Gm.a[:4, :L], Fa.a[:4, :L], Fa.a[:4, :L], 0.0, ALU.max, ALU.max, [Fa], [Gm])
        K.tt("dve", Gm.a[:4, L:NT], Fa.a[:4, L:NT], m0s.a[:], ALU.max, [Fa, m0s], [Gm])
        Mprev = small.next(); Mlast = small.next()
        prevlast(Gm, 4, m0s.a[:], Mprev, Mlast)
        glr = small.next()
        K.tt("dve", glr.a[:4, :], Mprev.a[:4, :], Mlast.a[:4, :], ALU.subtract, [Mprev, Mlast], [glr])
        K.act(glr.a[:4, :], glr.a[:4, :], AF.Exp, [glr], [glr])
        bcast_rows(glr, 4, sel4, GLb, NCI)
        sub_chunk(A, Gm, Mprev, 4, -1)
        K.act(A.a[:4, :], A.a[:4, :], AF.Exp, [A], [A])
        sub_chunk(Dt, Fa, Mlast, 4, +1)
        K.act(Dt.a[:4, :], Dt.a[:4, :], AF.Exp, [Dt], [Dt])
        K.tt("dve", Ct.a[:4, :], Et.a[:4, :], Gm.a[:4, :], ALU.add, [Et, Gm], [Ct])
        K.stq(O["m_pT"].a[l], Ct.a[:4, L - 1:L], Ct, reads=[Ct], mwrites=[O["m_pT"]], is_output=True)
        K.stq(O["m_sT"].a[l], Ct.a[:4, L:NT], Ct, reads=[Ct], mwrites=[O["m_sT"]], is_output=True)
        K.act(Ct.a[:4, :], Ct.a[:4, :], AF.Exp, [Ct], [Ct], scale=-1.0)
        for src, c0 in ((Fa, 0), (Gm, 4), (A, 8), (Ct, 12), (Dt, 16)):
            to_tm(src, 4, TM, c0)

        cpool = {"q": Pool(K, "mq", [128, 8, 64], F32, 2), "k": Pool(K, "mk", [128, 8, 64], F32, 2),
                 "kt": Pool(K, "mkt", [64, 1024], F32, 1), "v": Pool(K, "mv", [64, 4, 257], F32, 2),
                 "go": Pool(K, "mgo", [64, 1024], F32, 1), "hm": Pool(K, "mhm", [64, 1024], F32, 1),
                 "w": Pool(K, "mw", [64, 64], F32, 8), "sw": Pool(K, "msw", [64, 64], F32, 8),
                 "nsb": Pool(K, "mnsb", [64, 257], F32, 4), "res": Pool(K, "mres", [64, 257], F32, 4),
                 "kw": Pool(K, "mkw", [64, 256], F32, 4), "sc": Pool(K, "msc", [64, 4, 4], F32, 3),
                 "junk": Pool(K, "mjunk", [64, 256], F32, 2)}
        for b_ in cpool["v"].bufs:
            K.memset("dve", b_.a[:, :, 256:257], 1.0, [b_])
        Cst = [K.sb("Caug%d" % h, [128, 2, 257]) for h in range(4)]

        PSs, PSb = PS, PS

        def mlstm_chunk(t0, cl, ci, Caug):
            nk = dict(allow_slow_non_contiguous=True) if cl == 1 else {}
            q = cpool["q"].next(); kT = cpool["k"].next(); kt = cpool["kt"].next(); v = cpool["v"].next(); go = cpool["go"].next()
            K.ld(q.a[:, :, :cl], Dm["qT_d"].a.rearrange("(j p) t -> p j t", p=128)[:, :, t0:t0 + cl], q, reads=[Dm["qT_d"]], writes=[q], **nk)
            K.ld(kT.a[:, :, :cl], Dm["kT_d"].a.rearrange("(j p) t -> p j t", p=128)[:, :, t0:t0 + cl], kT, reads=[Dm["kT_d"]], writes=[kT], **nk)
            K.ld(kt.a[:cl, :], Dm["k_d"].a[t0:t0 + cl, :], kt, reads=[Dm["k_d"]], writes=[kt])
            K.ld(v.a[:cl, :, 0:256], Dm["v_d"].a[t0:t0 + cl, :].rearrange("t (h d) -> t h d", h=4), v, reads=[Dm["v_d"]], writes=[v])
            K.ld(go.a[:cl, :], Dm["go_d"].a[t0:t0 + cl, :], go, reads=[Dm["go_d"]], writes=[go])
            hm = cpool["hm"].next()
            H = range(4)
            ps_s = {}; ps_d = {}; w = {}; sw = {}; ps_n = {}; ps_i = {}; nsb = {}; res = {}; sc = {}; kw = {}
            for h in H:
                ps_s[h] = PSs.next()
                for kc in range(2):
                    K.pe(ps_s[h].a[:cl, :cl], kT.a[:, h * 2 + kc, :cl], q.a[:, h * 2 + kc, :cl], kc == 0, kc == 1, [kT, q], [ps_s[h]])
            for h in H:
                ps_d[h] = PSs.next()
                K.pe(ps_d[h].a[:cl, :cl], TM.a[:cl, ci, 4 + h:5 + h].to_broadcast([cl, cl]), nident.a[:cl, :cl], True, False, [TM, nident], [ps_d[h]])
                K.pe(ps_d[h].a[:cl, :cl], ident.a[:cl, :cl], maskT.a[:cl, :cl], False, False, [ident, maskT], [ps_d[h]])
                K.pe(ps_d[h].a[:cl, :cl], ident.a[:cl, :cl], TM.a[:cl, ci, h:h + 1].to_broadcast([cl, cl]), False, True, [ident, TM], [ps_d[h]])
            for h in H:
                w[h] = cpool["w"].next()
                K.act(w[h].a[:cl, :cl], ps_d[h].a[:cl, :cl], AF.Exp, [ps_d[h]], [w[h]])
            for h in H:
                kw[h] = cpool["kw"].next()
                K.act(kw[h].a[:cl, :], kt.a[:cl, h * 256:(h + 1) * 256], AF.Copy, [kt, TM], [kw[h]], scale=TM.a[:cl, ci, 16 + h:17 + h])
            for h in H:
                sw[h] = cpool["sw"].next()
                K.tt("dve", sw[h].a[:cl, :cl], ps_s[h].a[:cl, :cl], w[h].a[:cl, :cl], ALU.mult, [ps_s[h], w[h]], [sw[h]])
            for h in H:
                ps_n[h] = PSb.next()
                K.pe(ps_n[h].a[:cl, :257], sw[h].a[:cl, :cl], v.a[:cl, h, :], True, True, [sw[h], v], [ps_n[h]])
            for h in H:
                nsb[h] = cpool["nsb"].next()
                K.cp("act", nsb[h].a[:cl, :], ps_n[h].a[:cl, :257], [ps_n[h]], [nsb[h]])
            for h in H:
                ps_i[h] = PSb.next()
                for kc in range(2):
                    K.pe(ps_i[h].a[:cl, :257], q.a[:, h * 2 + kc, :cl], Caug[h].a[:, kc, :], kc == 0, kc == 1, [q, Caug[h]], [ps_i[h]])
            for h in H:
                res[h] = cpool["res"].next()
                K.stt(res[h].a[:cl, :], ps_i[h].a[:cl, :257], TM.a[:cl, ci, 8 + h:9 + h], nsb[h].a[:cl, :], ALU.mult, ALU.add, [ps_i[h], TM, nsb[h]], [res[h]])
            for hp in range(2):
                pcs = {}
                for h in (2 * hp, 2 * hp + 1):
                    for kc in range(2):
                        pcs[(h, kc)] = PSb.next()
                        K.pe(pcs[(h, kc)].a[:, :257], kw[h].a[:cl, kc * 128:(kc + 1) * 128], v.a[:cl, h, :], True, True, [kw[h], v], [pcs[(h, kc)]])
                for h in (2 * hp, 2 * hp + 1):
                    for kc in range(2):
                        K.stt(Caug[h].a[:, kc, :], Caug[h].a[:, kc, :], GLb.a[:, h * NCI + ci:h * NCI + ci + 1], pcs[(h, kc)].a[:, :257], ALU.mult, ALU.add, [Caug[h], GLb, pcs[(h, kc)]], [Caug[h]])
            sca = cpool["sc"].next()
            for h in H:
                K.act(sca.a[:cl, h, 0:1], res[h].a[:cl, 256:257], AF.Abs, [res[h]], [sca])
            K.tt("dve", sca.a[:cl, :, 0], sca.a[:cl, :, 0], TM.a[:cl, ci, 12:16], ALU.max, [sca, TM], [sca])
            K.recip(sca.a[:cl, :, 0], sca.a[:cl, :, 0], [sca], [sca])
            for h in H:
                junk = cpool["junk"].next()
                K.act(junk.a[:cl, :], res[h].a[:cl, 0:256], AF.Square, [res[h], sca], [junk, sca], scale=sca.a[:cl, h, 0:1], accum=sca.a[:cl, h, 1:2])
            K.act(sca.a[:cl, :, 2], sca.a[:cl, :, 1], AF.Sqrt, [sca, epsb], [sca], scale=1.0 / 256.0, bias=epsb.a[:cl, :])
            K.recip(sca.a[:cl, :, 2], sca.a[:cl, :, 2], [sca], [sca])
            K.tt("dve", sca.a[:cl, :, 3], sca.a[:cl, :, 2], sca.a[:cl, :, 0], ALU.mult, [sca], [sca])
            for h in H:
                K.stt(hm.a[:cl, h * 256:(h + 1) * 256], res[h].a[:cl, 0:256], sca.a[:cl, h, 3:4], go.a[:cl, h * 256:(h + 1) * 256], ALU.mult, ALU.mult, [res[h], sca, go], [hm])
            for j in range(8):
                ps = PSs.next()
                K.tr(ps.a[:, :cl], hm.a[:cl, j * 128:(j + 1) * 128], ident.a[:cl, :cl], [hm, ident], [ps])
                K.cp("act" if j % 2 else "dve", XTb[:, j, t0:t0 + cl], ps.a[:, :cl], [ps], [tXT])

        for h in range(4):
            K.memset("dve", Cst[h].a[:], 0.0, [Cst[h]])
        for (t0, cl, ci) in CHUNKS[:32]:
            mlstm_chunk(t0, cl, ci, Cst)
        for h in range(4):
            K.stq(O["C_p"].a[l, h].rearrange("(kc p) d -> p kc d", p=128), Cst[h].a[:, :, 0:256], Cst[h], reads=[Cst[h]], mwrites=[O["C_p"]], is_output=True)
            K.stq(O["n_p"].a[l, h].rearrange("(kc p) -> p kc", p=128), Cst[h].a[:, :, 256], Cst[h], reads=[Cst[h]], mwrites=[O["n_p"]], is_output=True, allow_slow_non_contiguous=True)
        for (t0, cl, ci) in CHUNKS[32:]:
            j = ci - 32
            Cs = [Cst[h] for h in range(4)]
            for h in range(4):
                K.ld(Cs[h].a[:, :, 0:256], I["sC"].a[l, j, h].rearrange("(kc p) d -> p kc d", p=128), Cs[h], writes=[Cs[h]])
                K.ld(Cs[h].a[:, :, 256], I["sn"].a[l, j, h].rearrange("(kc p) -> p kc", p=128), Cs[h], writes=[Cs[h]], allow_slow_non_contiguous=True)
            mlstm_chunk(t0, cl, ci, Cs)
            for h in range(4):
                K.stq(O["C_s"].a[l, j, h].rearrange("(kc p) d -> p kc d", p=128), Cs[h].a[:, :, 0:256], Cs[h], reads=[Cs[h]], mwrites=[O["C_s"]], is_output=True)
                K.stq(O["n_s"].a[l, j, h].rearrange("(kc p) -> p kc", p=128), Cs[h].a[:, :, 256], Cs[h], reads=[Cs[h]], mwrites=[O["n_s"]], is_output=True, allow_slow_non_contiguous=True)
    if stages >= 2.3:
        ssd_mixer(K, S, PS, I, O, l, Dm, XTb, tXT, ident, nident, maskT, epsb)
    if stages >= 2.6:
        s5_mixer(K, S, PS, I, O, l, Dm, XTb, tXT, ident, ones_f, epsb, halfpi)


def _mk_helpers(K, PS, ident):
    def to_tm(src, nr, dst, col0):
        for (t0, cl, ci) in CHUNKS:
            ps = PS.next()
            K.tr(ps.a[:cl, :nr], src.a[:nr, t0:t0 + cl], ident.a[:nr, :nr], [src, ident], [ps])
            K.cp("act" if ci % 2 else "dve", dst.a[:cl, ci, col0:col0 + nr], ps.a[:cl, :nr], [ps], [dst])

    def bcast_rows(rows, nr, sel, dst, ncol):
        for h in range(nr):
            ps = PS.next()
            K.pe(ps.a[:, :ncol], sel.a[:nr, h, :], rows.a[:nr, :ncol], True, True, [sel, rows], [ps])
            K.cp("act", dst.a[:, h * ncol:(h + 1) * ncol], ps.a[:, :ncol], [ps], [dst])

    def chunkview(t, nr):
        return t.a[:nr, :L].rearrange("p (c t) -> p c t", t=64)

    def prevlast(Mt, nr, init_s, prev, last):
        K.memset("dve", prev.a[:nr, 0:1], 0.0, [prev])
        K.cp("dve", prev.a[:nr, 1:32], Mt.a[:nr, 63:L - 64:64], [Mt], [prev])
        if init_s is None:
            K.memset("dve", prev.a[:nr, 32:NCI], 0.0, [prev])
        else:
            K.cp("dve", prev.a[:nr, 32:NCI], init_s, [Mt], [prev])
        K.cp("dve", last.a[:nr, 0:32], Mt.a[:nr, 63:L:64], [Mt], [last])
        K.cp("dve", last.a[:nr, 32:NCI], Mt.a[:nr, L:NT], [Mt], [last])

    def sub_chunk(out, x, cvals, nr, sign):
        cb = cvals.a[:nr, 0:32].unsqueeze(2).to_broadcast([nr, 32, 64])
        if sign > 0:
            K.tt("dve", chunkview(out, nr), chunkview(x, nr), cb, ALU.subtract, [x, cvals], [out])
            K.tt("dve", out.a[:nr, L:NT], x.a[:nr, L:NT], cvals.a[:nr, 32:NCI], ALU.subtract, [x, cvals], [out])
        else:
            K.tt("dve", chunkview(out, nr), cb, chunkview(x, nr), ALU.subtract, [x, cvals], [out])
            K.tt("dve", out.a[:nr, L:NT], cvals.a[:nr, 32:NCI], x.a[:nr, L:NT], ALU.subtract, [x, cvals], [out])
    return to_tm, bcast_rows, prevlast, sub_chunk


def ssd_mixer(K, S, PS, I, O, l, Dm, XTb, tXT, ident, nident, maskT, epsb):
    xbcT_d, xcT_d = Dm["xbcT_d"], Dm["xcT_d"]
    with K.phase():
        to_tm, bcast_rows, prevlast, sub_chunk = _mk_helpers(K, PS, ident)
        cw = K.sb("cw", [128, 8, 4]); cb = K.sb("cb", [128, 8]); cin = K.sb("cin", [128, 8, 3, NS])
        K.ld(cw.a[:], I["conv_w"].a[l], cw, writes=[cw]); K.ld(cb.a[:], I["conv_b"].a[l], cb, writes=[cb])
        K.ld(cin.a[:], I["convT"].a[l], cin, writes=[cin])
        xp = Pool(K, "xp", [128, 3 + 512], F32, 2); xo = Pool(K, "xo", [128, 512], F32, 2)
        for j in range(8):
            rows = slice(j * 128, (j + 1) * 128)
            for (t0, n) in TB[:4]:
                x = xp.next()
                K.ld(x.a[:, 3:3 + n], xbcT_d.a[rows, t0:t0 + n], x, reads=[xbcT_d], writes=[x])
                if t0 == 0:
                    K.memset("dve", x.a[:, 0:3], 0.0, [x])
                else:
                    K.ld(x.a[:, 0:3], xbcT_d.a[rows, t0 - 3:t0], x, reads=[xbcT_d], writes=[x])
                o = xo.next()
                K.ts("dve", o.a[:, :n], x.a[:, 3:3 + n], cw.a[:, j, 3:4], ALU.mult, [x, cw], [o])
                for tap in (2, 1, 0):
                    K.stt(o.a[:, :n], x.a[:, tap:tap + n], cw.a[:, j, tap:tap + 1], o.a[:, :n], ALU.mult, ALU.add, [x, cw, o], [o])
                K.act(o.a[:, :n], o.a[:, :n], AF.Silu, [o, cb], [o], bias=cb.a[:, j:j + 1])
                K.stq(xcT_d.a[rows, t0:t0 + n], o.a[:, :n], o, reads=[o], mwrites=[xcT_d])
                if t0 == 1536:
                    K.stq(O["conv_pT"].a[l, :, j, :], x.a[:, 512:515], x, reads=[x], mwrites=[O["conv_pT"]], is_output=True)
            x = xp.next()
            K.ld(x.a[:, 0:NS], xbcT_d.a[rows, L:NT], x, reads=[xbcT_d], writes=[x])
            o = xo.next()
            K.ts("dve", o.a[:, :NS], x.a[:, 0:NS], cw.a[:, j, 3:4], ALU.mult, [x, cw], [o])
            for tap in (2, 1, 0):
                K.stt(o.a[:, :NS], cin.a[:, j, tap, :], cw.a[:, j, tap:tap + 1], o.a[:, :NS], ALU.mult, ALU.add, [cin, cw, o], [o])
            K.act(o.a[:, :NS], o.a[:, :NS], AF.Silu, [o, cb], [o], bias=cb.a[:, j:j + 1])
            K.stq(xcT_d.a[rows, L:NT], o.a[:, :NS], o, reads=[o], mwrites=[xcT_d])
            K.stq(O["conv_sT"].a[l, :, j, 0:2, :], cin.a[:, j, 1:3, :], cin, reads=[cin], mwrites=[O["conv_sT"]], is_output=True)
            K.stq(O["conv_sT"].a[l, :, j, 2, :], x.a[:, 0:NS], x, reads=[x], mwrites=[O["conv_sT"]], is_output=True)
        A, Bt, Ct, Dt, Et, Fa, Gm = [K.sb("srow%d" % i, [8, NT]) for i in range(7)]
        onesr = K.sb("onesr", [8, 1]); K.memset("dve", onesr.a[:], 1.0, [onesr])
        TMB = K.sb("TMB", [64, NCI, 32]); DECb = K.sb("DECb", [128, 8 * NCI])
        sel8 = K.sb("sel8", [8, 8, 128]); K.ld(sel8.a[:], I["sel8"].a, sel8, writes=[sel8])
        small = Pool(K, "ssmall", [8, NCI], F32, 4)
        dtb = K.sb("dtb", [8, 1]); alog = K.sb("alog", [8, 1])
        K.ld(dtb.a[:], I["dt_bias"].a[l], dtb, writes=[dtb]); K.ld(alog.a[:], I["a_log"].a[l], alog, writes=[alog])
        K.act(alog.a[:], alog.a[:], AF.Exp, [alog], [alog])
        K.ts("dve", alog.a[:], alog.a[:], -1.0, ALU.mult, [alog], [alog])
        K.ld(A.a[:, :], Dm["gdt_d"].a, A, reads=[Dm["gdt_d"]], writes=[A])
        K.ts("dve", A.a[:, :], A.a[:, :], dtb.a[:, 0:1], ALU.add, [A, dtb], [A])
        K.act(Ct.a[:, :], A.a[:, :], AF.Abs, [A], [Ct])
        K.act(Ct.a[:, :], Ct.a[:, :], AF.Exp, [Ct], [Ct], scale=-1.0)
        K.act(Ct.a[:, :], Ct.a[:, :], AF.Ln, [Ct], [Ct], bias=1.0)
        K.ts("dve", Dt.a[:, :], A.a[:, :], 0.0, ALU.max, [A], [Dt])
        K.tt("dve", A.a[:, :], Dt.a[:, :], Ct.a[:, :], ALU.add, [Dt, Ct], [A])
        K.ts("dve", Bt.a[:, :], A.a[:, :], alog.a[:, 0:1], ALU.mult, [A, alog], [Bt])
        K.scan(Et.a[:, :L], onesr.a[:, 0:1].to_broadcast([8, L]), Bt.a[:, :L], 0.0, ALU.mult, ALU.add, [onesr, Bt], [Et])
        K.cp("dve", Et.a[:, L:NT], Bt.a[:, L:NT], [Bt], [Et])
        Gprev = small.next(); Glast = small.next(); decr = small.next()
        prevlast(Et, 8, None, Gprev, Glast)
        K.tt("dve", decr.a[:, :], Glast.a[:, :], Gprev.a[:, :], ALU.subtract, [Glast, Gprev], [decr])
        K.act(decr.a[:, :], decr.a[:, :], AF.Exp, [decr], [decr])
        bcast_rows(decr, 8, sel8, DECb, NCI)
        sub_chunk(Fa, Et, Gprev, 8, +1)
        K.act(Fa.a[:, :], Fa.a[:, :], AF.Exp, [Fa], [Fa])
        sub_chunk(Gm, Et, Glast, 8, -1)
        K.act(Gm.a[:, :], Gm.a[:, :], AF.Exp, [Gm], [Gm])
        for src, c0 in ((Et, 0), (Fa, 8), (Gm, 16), (A, 24)):
            to_tm(src, 8, TMB, c0)
        dd = K.sb("ssdd", [64, 8]); gs = K.sb("gssd", [64, 512])
        K.ld(dd.a[:], I["ssdd_rep"].a[l], dd, writes=[dd]); K.ld(gs.a[:], I["gssd_rep"].a[l], gs, writes=[gs])
        cp = {"xc": Pool(K, "sxc", [128, 8, 64], F32, 2), "zs": Pool(K, "szs", [64, 512], F32, 2),
              "xtok": Pool(K, "sxt", [64, 512], F32, 2), "btok": Pool(K, "sbt", [64, 256], F32, 2),
              "sc": Pool(K, "ssc", [64, 2, 64], F32, 2), "seg": Pool(K, "sseg", [64, 64], F32, 8),
              "xdt": Pool(K, "sxdt", [64, 64], F32, 8), "xw": Pool(K, "sxw", [64, 64], F32, 8),
              "y1": Pool(K, "sy1", [64, 64], F32, 8), "yss": Pool(K, "syss", [64, 512], F32, 2),
              "s": Pool(K, "ss", [64, 4], F32, 2), "junk": Pool(K, "sjunk", [64, 512], F32, 1)}
        ST = [K.sb("ST%d" % h, [128, 64]) for h in range(8)]

        def chunk(t0, cl, ci):
            nk = dict(allow_slow_non_contiguous=True) if cl == 1 else {}
            xc = cp["xc"].next(); zs = cp["zs"].next()
            K.ld(xc.a[:, :, :cl], xcT_d.a.rearrange("(j p) t -> p j t", p=128)[:, :, t0:t0 + cl], xc, reads=[xcT_d], writes=[xc], **nk)
            K.ld(zs.a[:cl, :], Dm["zs_d"].a[t0:t0 + cl, :], zs, reads=[Dm["zs_d"]], writes=[zs])
            xtok = cp["xtok"].next(); btok = cp["btok"].next()
            for j in range(6):
                ps = PS.next()
                K.tr(ps.a[:cl, :128], xc.a[:, j, :cl], ident.a[:, :], [xc, ident], [ps])
                if j < 4:
                    K.cp("act" if j % 2 else "dve", xtok.a[:cl, j * 128:(j + 1) * 128], ps.a[:cl, :128], [ps], [xtok])
                else:
                    K.cp("act" if j % 2 else "dve", btok.a[:cl, (j - 4) * 128:(j - 3) * 128], ps.a[:cl, :128], [ps], [btok])
            sc = cp["sc"].next()
            for g in range(2):
                ps = PS.next()
                K.pe(ps.a[:cl, :cl], xc.a[:, 4 + g, :cl], xc.a[:, 6 + g, :cl], True, True, [xc], [ps])
                K.cp("act", sc.a[:cl, g, :cl], ps.a[:cl, :cl], [ps], [sc])
            yss = cp["yss"].next()
            for g in range(2):
                HH = range(4 * g, 4 * g + 4)
                ps_d = {}; seg = {}; xdt = {}; ps1 = {}; ps2 = {}; y1 = {}; xw = {}; ps3 = {}
                for h in HH:
                    ps_d[h] = PS.next()
                    K.pe(ps_d[h].a[:cl, :cl], TMB.a[:cl, ci, h:h + 1].to_broadcast([cl, cl]), ident.a[:cl, :cl], True, False, [TMB, ident], [ps_d[h]])
                    K.pe(ps_d[h].a[:cl, :cl], ident.a[:cl, :cl], maskT.a[:cl, :cl], False, False, [ident, maskT], [ps_d[h]])
                    K.pe(ps_d[h].a[:cl, :cl], nident.a[:cl, :cl], TMB.a[:cl, ci, h:h + 1].to_broadcast([cl, cl]), False, True, [nident, TMB], [ps_d[h]])
                for h in HH:
                    seg[h] = cp["seg"].next()
                    K.act(seg[h].a[:cl, :cl], ps_d[h].a[:cl, :cl], AF.Exp, [ps_d[h]], [seg[h]])
                for h in HH:
                    xdt[h] = cp["xdt"].next()
                    K.act(xdt[h].a[:cl, :], xtok.a[:cl, h * 64:(h + 1) * 64], AF.Copy, [xtok, TMB], [xdt[h]], scale=TMB.a[:cl, ci, 24 + h:25 + h])
                for h in HH:
                    K.tt("dve", seg[h].a[:cl, :cl], seg[h].a[:cl, :cl], sc.a[:cl, g, :cl], ALU.mult, [seg[h], sc], [seg[h]])
                for h in HH:
                    xw[h] = cp["xw"].next()
                    K.act(xw[h].a[:cl, :], xdt[h].a[:cl, :], AF.Copy, [xdt[h], TMB], [xw[h]], scale=TMB.a[:cl, ci, 16 + h:17 + h])
                for h in HH:
                    ps1[h] = PS.next()
                    K.pe(ps1[h].a[:cl, :64], seg[h].a[:cl, :cl], xdt[h].a[:cl, :], True, True, [seg[h], xdt[h]], [ps1[h]])
                for h in HH:
                    ps2[h] = PS.next()
                    K.pe(ps2[h].a[:cl, :64], xc.a[:, 6 + g, :cl], ST[h].a[:, :], True, True, [xc, ST[h]], [ps2[h]])
                for h in HH:
                    y1[h] = cp["y1"].next()
                    K.cp("act", y1[h].a[:cl, :], ps1[h].a[:cl, :64], [ps1[h]], [y1[h]])
                for h in HH:
                    K.stt(y1[h].a[:cl, :], ps2[h].a[:cl, :64], TMB.a[:cl, ci, 8 + h:9 + h], y1[h].a[:cl, :], ALU.mult, ALU.add, [ps2[h], TMB, y1[h]], [y1[h]])
                for h in HH:
                    ps3[h] = PS.next()
                    K.pe(ps3[h].a[:, :64], btok.a[:cl, g * 128:(g + 1) * 128], xw[h].a[:cl, :], True, True, [btok, xw[h]], [ps3[h]])
                for h in HH:
                    K.stt(ST[h].a[:, :], ST[h].a[:, :], DECb.a[:, h * NCI + ci:h * NCI + ci + 1], ps3[h].a[:, :64], ALU.mult, ALU.add, [ST[h], DECb, ps3[h]], [ST[h]])
                for h in HH:
                    K.stt(yss.a[:cl, h * 64:(h + 1) * 64], xtok.a[:cl, h * 64:(h + 1) * 64], dd.a[:cl, h:h + 1], y1[h].a[:cl, :], ALU.mult, ALU.add, [xtok, dd, y1[h]], [yss])
            K.tt("dve", yss.a[:cl, :], yss.a[:cl, :], zs.a[:cl, :], ALU.mult, [yss, zs], [yss])
            s_ = cp["s"].next(); junk = cp["junk"].next()
            K.act(junk.a[:cl, :], yss.a[:cl, :], AF.Square, [yss], [junk, s_], accum=s_.a[:cl, 0:1])
            K.act(s_.a[:cl, 1:2], s_.a[:cl, 0:1], AF.Sqrt, [s_, epsb], [s_], scale=1.0 / 512.0, bias=epsb.a[:cl, :])
            K.recip(s_.a[:cl, 1:2], s_.a[:cl, 1:2], [s_], [s_])
            K.stt(yss.a[:cl, :], yss.a[:cl, :], s_.a[:cl, 1:2], gs.a[:cl, :], ALU.mult, ALU.mult, [yss, s_, gs], [yss])
            for j in range(4):
                ps = PS.next()
                K.tr(ps.a[:, :cl], yss.a[:cl, j * 128:(j + 1) * 128], ident.a[:cl, :cl], [yss, ident], [ps])
                K.cp("act" if j % 2 else "dve", XTb[:, 12 + j, t0:t0 + cl], ps.a[:, :cl], [ps], [tXT])

        for h in range(8):
            K.memset("dve", ST[h].a[:], 0.0, [ST[h]])
        for (t0, cl, ci) in CHUNKS[:32]:
            chunk(t0, cl, ci)
        for h in range(8):
            K.stq(O["ssd_pT"].a[l, h], ST[h].a[:, :], ST[h], reads=[ST[h]], mwrites=[O["ssd_pT"]], is_output=True)
        for (t0, cl, ci) in CHUNKS[32:]:
            j = ci - 32
            for h in range(8):
                K.ld(ST[h].a[:, :], I["ssdT"].a[l, j, h], ST[h], writes=[ST[h]])
            chunk(t0, cl, ci)
            for h in range(8):
                K.stq(O["ssd_sT"].a[l, j, h], ST[h].a[:, :], ST[h], reads=[ST[h]], mwrites=[O["ssd_sT"]], is_output=True)


def s5_mixer(K, S, PS, I, O, l, Dm, XTb, tXT, ident, ones_f, epsb, halfpi):
    uT_d = Dm["uT_d"]
    with K.phase():
        def P16(name):
            return K.sb(name, [128, 16])
        lre, lim, dtt, th, r, c, s_, t1, t2, t3, lbr, lbi, nlbi, kr, ki, den, ka, cn, sn = [P16("s5p%d" % i) for i in range(19)]
        K.ld(lre.a[:], I["lam_re"].a[l], lre, writes=[lre]); K.ld(lim.a[:], I["lam_im"].a[l], lim, writes=[lim])
        K.ld(dtt.a[:], I["logdt"].a[l], dtt, writes=[dtt])
        K.act(dtt.a[:], dtt.a[:], AF.Exp, [dtt], [dtt])
        K.tt("dve", th.a[:], lim.a[:], dtt.a[:], ALU.mult, [lim, dtt], [th])
        K.tt("dve", r.a[:], lre.a[:], dtt.a[:], ALU.mult, [lre, dtt], [r])
        K.act(r.a[:], r.a[:], AF.Exp, [r], [r])
        K.act(s_.a[:], th.a[:], AF.Sin, [th], [s_], scale=1.0 / 32.0)
        K.act(c.a[:], th.a[:], AF.Sin, [th, halfpi], [c], scale=1.0 / 32.0, bias=halfpi.a[:, :])

        def cdouble(cc, ss):
            K.tt("dve", t1.a[:], ss.a[:], cc.a[:], ALU.mult, [ss, cc], [t1])
            K.tt("dve", t2.a[:], cc.a[:], cc.a[:], ALU.mult, [cc], [t2])
            K.tt("dve", t3.a[:], ss.a[:], ss.a[:], ALU.mult, [ss], [t3])
            K.ts("dve", ss.a[:], t1.a[:], 2.0, ALU.mult, [t1], [ss])
            K.tt("dve", cc.a[:], t2.a[:], t3.a[:], ALU.subtract, [t2, t3], [cc])
        for _ in range(5):
            cdouble(c, s_)
        K.tt("dve", lbr.a[:], r.a[:], c.a[:], ALU.mult, [r, c], [lbr])
        K.tt("dve", lbi.a[:], r.a[:], s_.a[:], ALU.mult, [r, s_], [lbi])
        K.ts("dve", nlbi.a[:], lbi.a[:], -1.0, ALU.mult, [lbi], [nlbi])
        K.ts("dve", ka.a[:], lbr.a[:], -1.0, ALU.add, [lbr], [ka])
        K.tt("dve", t1.a[:], lre.a[:], lre.a[:], ALU.mult, [lre], [t1])
        K.tt("dve", t2.a[:], lim.a[:], lim.a[:], ALU.mult, [lim], [t2])
        K.tt("dve", den.a[:], t1.a[:], t2.a[:], ALU.add, [t1, t2], [den])
        K.recip(den.a[:], den.a[:], [den], [den])
        K.tt("dve", t1.a[:], ka.a[:], lre.a[:], ALU.mult, [ka, lre], [t1])
        K.tt("dve", t2.a[:], lbi.a[:], lim.a[:], ALU.mult, [lbi, lim], [t2])
        K.tt("dve", kr.a[:], t1.a[:], t2.a[:], ALU.add, [t1, t2], [kr])
        K.tt("dve", kr.a[:], kr.a[:], den.a[:], ALU.mult, [kr, den], [kr])
        K.tt("dve", t1.a[:], lbi.a[:], lre.a[:], ALU.mult, [lbi, lre], [t1])
        K.tt("dve", t2.a[:], ka.a[:], lim.a[:], ALU.mult, [ka, lim], [t2])
        K.tt("dve", ki.a[:], t1.a[:], t2.a[:], ALU.subtract, [t1, t2], [ki])
        K.tt("dve", ki.a[:], ki.a[:], den.a[:], ALU.mult, [ki, den], [ki])
        tabC = K.sb("tabC", [128, 16, 128]); tabS = K.sb("tabS", [128, 16, 128])
        tmpA = K.sb("tmpA", [128, 16, 64]); tmpB = K.sb("tmpB", [128, 16, 64])
        K.cp("dve", tabC.a[:, :, 0], c.a[:], [c], [tabC]); K.cp("dve", tabS.a[:, :, 0], s_.a[:], [s_], [tabS])
        K.cp("dve", cn.a[:], c.a[:], [c], [cn]); K.cp("dve", sn.a[:], s_.a[:], [s_], [sn])
        nn = 1
        while nn < 128:
            cb_ = cn.a[:, :].unsqueeze(2).to_broadcast([128, 16, nn]); sb_ = sn.a[:, :].unsqueeze(2).to_broadcast([128, 16, nn])
            K.tt("dve", tmpA.a[:, :, :nn], tabC.a[:, :, 0:nn], cb_, ALU.mult, [tabC, cn], [tmpA])
            K.tt("dve", tmpB.a[:, :, :nn], tabS.a[:, :, 0:nn], sb_, ALU.mult, [tabS, sn], [tmpB])
            K.tt("dve", tmpA.a[:, :, :nn], tmpA.a[:, :, :nn], tmpB.a[:, :, :nn], ALU.subtract, [tmpA, tmpB], [tmpA])
            K.tt("dve", tmpB.a[:, :, :nn], tabS.a[:, :, 0:nn], cb_, ALU.mult, [tabS, cn], [tmpB])
            K.cp("dve", tabC.a[:, :, nn:2 * nn], tmpA.a[:, :, :nn], [tmpA], [tabC])
            K.tt("dve", tmpA.a[:, :, :nn], tabC.a[:, :, 0:nn], sb_, ALU.mult, [tabC, sn], [tmpA])
            K.tt("dve", tabS.a[:, :, nn:2 * nn], tmpB.a[:, :, :nn], tmpA.a[:, :, :nn], ALU.add, [tmpA, tmpB], [tabS])
            cdouble(cn, sn)
            nn *= 2
        tabKC = K.sb("tabKC", [128, 16, 128]); tabKS = K.sb("tabKS", [128, 16, 128])
        krb = kr.a[:, :].unsqueeze(2).to_broadcast([128, 16, 128]); kib = ki.a[:, :].unsqueeze(2).to_broadcast([128, 16, 128])
        tmpC = K.sb("tmpC", [128, 16, 128])
        K.tt("dve", tabKC.a[:], tabC.a[:], krb, ALU.mult, [tabC, kr], [tabKC])
        K.tt("dve", tmpC.a[:], tabS.a[:], kib, ALU.mult, [tabS, ki], [tmpC])
        K.tt("dve", tabKC.a[:], tabKC.a[:], tmpC.a[:], ALU.add, [tabKC, tmpC], [tabKC])
        K.tt("dve", tabKS.a[:], tabC.a[:], kib, ALU.mult, [tabC, ki], [tabKS])
        K.tt("dve", tmpC.a[:], tabS.a[:], krb, ALU.mult, [tabS, kr], [tmpC])
        K.tt("dve", tabKS.a[:], tabKS.a[:], tmpC.a[:], ALU.subtract, [tabKS, tmpC], [tabKS])
        Bm = {}
        for nm in ("Bre", "Bim", "Cre", "Cim"):
            Bm[nm] = K.sb(nm + "_sb", [128, 16, 128])
            K.ld(Bm[nm].a[:], I[nm].a[l].rearrange("s k m -> k s m"), Bm[nm], writes=[Bm[nm]])
        s5d = K.sb("s5d", [128, 4]); bglu = K.sb("bglu", [128, 4]); gs5 = K.sb("gs5", [128, 4])
        K.ld(s5d.a[:], I["s5d"].a[l], s5d, writes=[s5d]); K.ld(bglu.a[:], I["b_glu"].a[l], bglu, writes=[bglu]); K.ld(gs5.a[:], I["g_s5"].a[l], gs5, writes=[gs5])
        Xr = K.sb("Xr", [128, 16]); Xi = K.sb("Xi", [128, 16])
        K.memset("dve", Xr.a[:], 0.0, [Xr]); K.memset("dve", Xi.a[:], 0.0, [Xi])
        y5g_d = K.dscr("y5g_d%d" % l, [512, NT])
        W8 = lambda nm: K.sb(nm, [128, 8, 128])
        BUr, BUi, Wr, Wi, Zr, Zi, T1, T2, Xr_, Xi_, nXi_ = [W8("s5w%d" % i) for i in range(11)]
        ubp = Pool(K, "ub", [128, 4, 128], F32, 2)
        gp = Pool(K, "s5g", [128, 128], F32, 3)

        def bu_calc(ub, sc, n, our, oui, tA, tB):
            j = sc // 4
            ps = PS.next()
            K.pe(ps.a[:, 0:n], Bm["Bre"].a[:, sc, :], ub.a[:, j, :n], True, True, [Bm["Bre"], ub], [ps])
            K.pe(ps.a[:, 128:128 + n], Bm["Bim"].a[:, sc, :], ub.a[:, j, :n], True, True, [Bm["Bim"], ub], [ps])
            return ps

        def y_out(ub, j, n, t0, xr_of, nxi_of, rd):
            ps_y = PS.next()
            for q in range(4):
                sc = 4 * j + q
                K.pe(ps_y.a[:, :n], Bm["Cre"].a[:, sc, :], xr_of(sc), q == 0, False, [Bm["Cre"]] + rd, [ps_y])
                K.pe(ps_y.a[:, :n], Bm["Cim"].a[:, sc, :], nxi_of(sc), False, q == 3, [Bm["Cim"]] + rd, [ps_y])
            yv = gp.next(); tg = gp.next()
            K.stt(yv.a[:, :n], ub.a[:, j, :n], s5d.a[:, j:j + 1], ps_y.a[:, :n], ALU.mult, ALU.add, [ub, s5d, ps_y], [yv])
            K.tt("dve", tg.a[:, :n], yv.a[:, :n], yv.a[:, :n], ALU.mult, [yv], [tg])
            K.ts("dve", tg.a[:, :n], tg.a[:, :n], 0.044715, ALU.mult, [tg], [tg], s2=1.0, op1=ALU.add)
            K.tt("dve", tg.a[:, :n], tg.a[:, :n], yv.a[:, :n], ALU.mult, [tg, yv], [tg])
            K.act(tg.a[:, :n], tg.a[:, :n], AF.Tanh, [tg], [tg], scale=0.7978845608028654)
            K.ts("dve", tg.a[:, :n], tg.a[:, :n], 1.0, ALU.add, [tg], [tg], s2=0.5, op1=ALU.mult)
            K.tt("dve", tg.a[:, :n], tg.a[:, :n], yv.a[:, :n], ALU.mult, [tg, yv], [tg])
            K.stq(y5g_d.a[j * 128:(j + 1) * 128, t0:t0 + n], tg.a[:, :n], tg, reads=[tg], mwrites=[y5g_d])

        for tc in range(16):
            t0 = tc * 128
            n = 128
            ub = ubp.next()
            K.ld(ub.a[:, :, :n], uT_d.a.rearrange("(j p) t -> p j t", p=128)[:, :, t0:t0 + n], ub, reads=[uT_d], writes=[ub])
            for h2 in range(2):
                for i in range(8):
                    sc = 8 * h2 + i
                    ps = bu_calc(ub, sc, n, None, None, None, None)
                    K.cp("act", BUr.a[:, i, :n], ps.a[:, 0:n], [ps], [BUr])
                    K.cp("act", BUi.a[:, i, :n], ps.a[:, 128:128 + n], [ps], [BUi])
                Cs = tabC.a[:, 8 * h2:8 * h2 + 8, :]; Ss = tabS.a[:, 8 * h2:8 * h2 + 8, :]
                KCs = tabKC.a[:, 8 * h2:8 * h2 + 8, :]; KSs = tabKS.a[:, 8 * h2:8 * h2 + 8, :]
                K.tt("dve", T1.a[:], BUi.a[:], KSs, ALU.mult, [BUi, tabKS], [T1])
                K.tt("pool", Wr.a[:], BUr.a[:], KCs, ALU.mult, [BUr, tabKC], [Wr])
                K.tt("dve", Wr.a[:], Wr.a[:], T1.a[:], ALU.subtract, [Wr, T1], [Wr])
                K.tt("pool", T2.a[:], BUr.a[:], KSs, ALU.mult, [BUr, tabKS], [T2])
                K.tt("dve", Wi.a[:], BUi.a[:], KCs, ALU.mult, [BUi, tabKC], [Wi])
                K.tt("dve", Wi.a[:], Wi.a[:], T2.a[:], ALU.add, [Wi, T2], [Wi])
                for i in range(8):
                    sc = 8 * h2 + i
                    rb = r.a[:, sc:sc + 1].to_broadcast([128, n])
                    K.scan(Zr.a[:, i, :], rb, Wr.a[:, i, :], Xr.a[:, sc:sc + 1], ALU.mult, ALU.add, [r, Wr, Xr], [Zr])
                    K.scan(Zi.a[:, i, :], rb, Wi.a[:, i, :], Xi.a[:, sc:sc + 1], ALU.mult, ALU.add, [r, Wi, Xi], [Zi])
                K.tt("dve", T1.a[:], Zi.a[:], Ss, ALU.mult, [Zi, tabS], [T1])
                K.tt("pool", Xr_.a[:], Zr.a[:], Cs, ALU.mult, [Zr, tabC], [Xr_])
                K.tt("dve", Xr_.a[:], Xr_.a[:], T1.a[:], ALU.subtract, [Xr_, T1], [Xr_])
                K.tt("pool", T2.a[:], Zr.a[:], Ss, ALU.mult, [Zr, tabS], [T2])
                K.tt("dve", T1.a[:], Zi.a[:], Cs, ALU.mult, [Zi, tabC], [T1])
                K.tt("dve", Xi_.a[:], T2.a[:], T1.a[:], ALU.add, [T2, T1], [Xi_])
                K.ts("dve", nXi_.a[:], Xi_.a[:], -1.0, ALU.mult, [Xi_], [nXi_])
                K.cp("dve", Xr.a[:, 8 * h2:8 * h2 + 8], Xr_.a[:, :, n - 1], [Xr_], [Xr])
                K.cp("dve", Xi.a[:, 8 * h2:8 * h2 + 8], Xi_.a[:, :, n - 1], [Xi_], [Xi])
                for jj in range(2):
                    j = 2 * h2 + jj
                    y_out(ub, j, n, t0, lambda sc: Xr_.a[:, sc - 8 * h2, :n], lambda sc: nXi_.a[:, sc - 8 * h2, :n], [Xr_, nXi_])
        K.stq(O["s5re_pT"].a[l], Xr.a[:], Xr, reads=[Xr], mwrites=[O["s5re_pT"]], is_output=True)
        K.stq(O["s5im_pT"].a[l], Xi.a[:], Xi, reads=[Xi], mwrites=[O["s5im_pT"]], is_output=True)
        x0r = K.sb("x0r", [128, 16, NS]); x0i = K.sb("x0i", [128, 16, NS])
        K.ld(x0r.a[:], I["s5reT"].a[l], x0r, writes=[x0r]); K.ld(x0i.a[:], I["s5imT"].a[l], x0i, writes=[x0i])
        SXr = K.sb("SXr", [128, 16, NS]); SXi = K.sb("SXi", [128, 16, NS]); SnXi = K.sb("SnXi", [128, 16, NS])
        SBr = K.sb("SBr", [128, 16, NS]); SBi = K.sb("SBi", [128, 16, NS])
        ub = ubp.next()
        K.ld(ub.a[:, :, :NS], uT_d.a.rearrange("(j p) t -> p j t", p=128)[:, :, L:NT], ub, reads=[uT_d], writes=[ub])
        for sc in range(16):
            ps = bu_calc(ub, sc, NS, None, None, None, None)
            K.ts("dve", SBr.a[:, sc, :], ps.a[:, 128:128 + NS], ki.a[:, sc:sc + 1], ALU.mult, [ps, ki], [SBr])
            K.stt(SBr.a[:, sc, :], ps.a[:, 0:NS], kr.a[:, sc:sc + 1], SBr.a[:, sc, :], ALU.mult, ALU.subtract, [ps, kr, SBr], [SBr])
            K.ts("dve", SBi.a[:, sc, :], ps.a[:, 0:NS], ki.a[:, sc:sc + 1], ALU.mult, [ps, ki], [SBi])
            K.stt(SBi.a[:, sc, :], ps.a[:, 128:128 + NS], kr.a[:, sc:sc + 1], SBi.a[:, sc, :], ALU.mult, ALU.add, [ps, kr, SBi], [SBi])
            K.stt(SXr.a[:, sc, :], x0r.a[:, sc, :], lbr.a[:, sc:sc + 1], SBr.a[:, sc, :], ALU.mult, ALU.add, [x0r, lbr, SBr], [SXr])
            K.stt(SXr.a[:, sc, :], x0i.a[:, sc, :], nlbi.a[:, sc:sc + 1], SXr.a[:, sc, :], ALU.mult, ALU.add, [x0i, nlbi, SXr], [SXr])
            K.stt(SXi.a[:, sc, :], x0r.a[:, sc, :], lbi.a[:, sc:sc + 1], SBi.a[:, sc, :], ALU.mult, ALU.add, [x0r, lbi, SBi], [SXi])
            K.stt(SXi.a[:, sc, :], x0i.a[:, sc, :], lbr.a[:, sc:sc + 1], SXi.a[:, sc, :], ALU.mult, ALU.add, [x0i, lbr, SXi], [SXi])
        K.ts("dve", SnXi.a[:], SXi.a[:], -1.0, ALU.mult, [SXi], [SnXi])
        K.stq(O["s5re_sT"].a[l], SXr.a[:], SXr, reads=[SXr], mwrites=[O["s5re_sT"]], is_output=True)
        K.stq(O["s5im_sT"].a[l], SXi.a[:], SXi, reads=[SXi], mwrites=[O["s5im_sT"]], is_output=True)
        for j in range(4):
            y_out(ub, j, NS, L, lambda sc: SXr.a[:, sc, :], lambda sc: SnXi.a[:, sc, :], [SXr, SnXi])
    with K.phase():
        s5d = K.sb("s5d", [128, 4]); bglu = K.sb("bglu", [128, 4]); gs5 = K.sb("gs5", [128, 4])
        K.ld(bglu.a[:], I["b_glu"].a[l], bglu, writes=[bglu]); K.ld(gs5.a[:], I["g_s5"].a[l], gs5, writes=[gs5])
        ybp = Pool(K, "y5b", [128, 4, 512], F32, 2)
        wglu = K.sb("wglu", [128, 4, 512])
        K.ld(wglu.a[:], I["w_glu"].a[l].rearrange("(k p) c -> p k c", p=128), wglu, writes=[wglu])
        yg = Pool(K, "ygl", [128, 4, 512], F32, 2); sqp = Pool(K, "s5sq", [128, 4, 512], F32, 1); rsp = Pool(K, "s5rs", [128, 512], F32, 2)
        for (t0, n) in TB:
            ygl = yg.next()
            yb = ybp.next()
            K.ld(yb.a[:, :, :n], y5g_d.a.rearrange("(j p) t -> p j t", p=128)[:, :, t0:t0 + n], yb, reads=[y5g_d], writes=[yb])
            for m in range(4):
                ps = PS.next()
                for j in range(4):
                    K.pe(ps.a[:, :n], wglu.a[:, j, m * 128:(m + 1) * 128], yb.a[:, j, :n], j == 0, j == 3, [wglu, yb], [ps])
                K.act(ygl.a[:, m, :n], ps.a[:, :n], AF.Sigmoid, [ps, bglu], [ygl], bias=bglu.a[:, m:m + 1])
                K.tt("dve", ygl.a[:, m, :n], ygl.a[:, m, :n], yb.a[:, m, :n], ALU.mult, [ygl, yb], [ygl])
            sq = sqp.next()
            K.act(sq.a[:, :, :n], ygl.a[:, :, :n], AF.Square, [ygl], [sq])
            ps = PS.next()
            for m in range(4):
                K.pe(ps.a[:, :n], ones_f.a[:], sq.a[:, m, :n], m == 0, m == 3, [ones_f, sq], [ps])
            rs = rsp.next()
            K.act(rs.a[:, :n], ps.a[:, :n], AF.Sqrt, [ps, epsb], [rs], scale=1.0 / 512.0, bias=epsb.a[:, :])
            K.recip(rs.a[:, :n], rs.a[:, :n], [rs], [rs])
            for m in range(4):
                K.stt(XTb[:, 8 + m, t0:t0 + n], ygl.a[:, m, :n], gs5.a[:, m:m + 1], rs.a[:, :n], ALU.mult, ALU.mult, [ygl, gs5, rs], [tXT])


def ffn_phase(K, S, PS, I, l, hT, XTf, tXT, gn, ones_bf, epsb, ident, gst, norm_stage, stages, own):
    moe = (l % 2 == 1)
    if moe:
        experts = [(I["moe_wg"].a[e], I["moe_wu"].a[e], I["moe_wd"].a[e]) for e in range(NE)]
        dff = D_FFE
    else:
        experts = [(I["ffn_wg"].a, I["ffn_wu"].a, I["ffn_wd"].a)]
        dff = D_FF
    B3 = [(0, 347), (347, 347), (694, 346)]
    SBS = [(0, [(0, 512), (512, 512)]), (1024, B3)]
    if own:
        SBS = [(0, B3)]
    for (c0, blocks) in SBS:
        nsb = sum(n for _, n in blocks)
        with K.phase():
            P = {"hblk": Pool(K, "fhblk", [128, KC, 128], F32, 1), "sq": Pool(K, "fsq", [128, KC, 128], BF16, 1),
                 "rs": Pool(K, "frs", [128, 512], F32, 2)}
            cT = K.sb("cT", [128, KC, 1040], BF16)
            for q0 in range(0, nsb, 128):
                n = min(128, nsb - q0)
                norm_stage(hT, gn["g_ffn"].a[:, l, :], gn["g_ffn"], c0 + q0, n, cT.a[:, :, q0:q0 + n], cT, P)
            wgp = Pool(K, "fwg", [128, KC, 256], BF16, 2); wup = Pool(K, "fwu", [128, KC, 256], BF16, 2)
            wdp = Pool(K, "fwd", [128, 2, D], BF16, 3); h1p = Pool(K, "fh1", [128, 2, 1040], BF16, 2)
            sgp = Pool(K, "fsg", [128, 512], F32, 3); hbp = Pool(K, "fhb", [128, 512], F32, 2)
            ntile = (nsb + 127) // 128
            if moe:
                wr = K.sb("wr", [128, KC, NE], BF16)
                K.ld(wr.a[:], I["w_router"].a.rearrange("(k p) c -> p k c", p=128), wr, writes=[wr], q="pool")
                brep = K.sb("brep", [128, NE]); K.ld(brep.a[:], I["b_router_rep"].a, brep, writes=[brep])
                comb = K.sb("comb", [128, 9, NE]); combB = Pool(K, "combB", [128, 1040], F32, 2)
                rt = Pool(K, "rt", [128, 4, NE], F32, 2)
                for i in range(ntile):
                    q0 = i * 128
                    n = min(128, nsb - q0)
                    ps = PS.next()
                    for k in range(KC):
                        K.pe(ps.a[:n, :NE], cT.a[:, k, q0:q0 + n], wr.a[:, k, :], k == 0, k == KC - 1, [cT, wr], [ps])
                    t = rt.next()
                    lg = t.a[:n, 0, :]; mx = t.a[:n, 1, :]; ex = t.a[:n, 2, :]; sc = t.a[:n, 3, :]
                    K.tt("dve", lg, ps.a[:n, :NE], brep.a[:n, :], ALU.add, [ps, brep], [t])
                    K.S.op("dve", lambda e, mx=mx, lg=lg: e.max(out=mx, in_=lg), reads=[t.k], writes=[t.k])
                    K.ts("dve", sc[:, 0:1], mx[:, 0:1], -1.0, ALU.mult, [t], [t])
                    K.act(ex, lg, AF.Exp, [t], [t], bias=sc[:, 0:1])
                    K.act(sc[:, 1:2], mx[:, 1:2], AF.Exp, [t], [t], bias=sc[:, 0:1])
                    K.ts("dve", sc[:, 1:2], sc[:, 1:2], 1.0, ALU.add, [t], [t])
                    K.recip(sc[:, 1:2], sc[:, 1:2], [t], [t])
                    K.ts("dve", lg, lg, mx[:, 1:2], ALU.is_ge, [t], [t])
                    K.stt(comb.a[:n, i, :], ex, sc[:, 1:2], lg, ALU.mult, ALU.mult, [t], [comb])
            panels = [(ei, f0) for ei in range(len(experts)) for f0 in range(0, dff, 256)]
            npan = len(panels)
            cBs = {}
            loaded = {}

            def loads(p):
                ei, f0 = panels[p]
                Wg, Wu, Wd = experts[ei]
                wg = wgp.next(); wu = wup.next(); wd = wdp.next()
                pi = f0 // 256
                for hh in range(2):
                    K.ld(wg.a[:].rearrange("p k c -> p (k c)")[:, hh * 2048:(hh + 1) * 2048], Wg[pi][:, hh * 2048:(hh + 1) * 2048], wg, writes=[wg], q="pool")
                for hh in range(2):
                    K.ld(wu.a[:].rearrange("p k c -> p (k c)")[:, hh * 2048:(hh + 1) * 2048], Wu[pi][:, hh * 2048:(hh + 1) * 2048], wu, writes=[wu], q="pool")
                for hh in range(2):
                    K.ld(wd.a[:].rearrange("p k c -> p (k c)")[:, hh * 2048:(hh + 1) * 2048], Wd[pi][:, hh * 2048:(hh + 1) * 2048], wd, writes=[wd], q="pool")
                loaded[p] = (wg, wu, wd)

            def gate_up(p):
                ei, f0 = panels[p]
                wg, wu, wd = loaded[p]
                if moe and ei not in cBs:
                    cB = combB.next()
                    for i in range(ntile):
                        q0 = i * 128
                        n = min(128, nsb - q0)
                        ps = PS.next()
                        K.pe(ps.a[:, :n], comb.a[:n, i, ei:ei + 1].to_broadcast([n, 128]), ident.a[:n, :n], True, True, [comb, ident], [ps])
                        K.cp("act", cB.a[:, q0:q0 + n], ps.a[:, :n], [ps], [cB])
                    cBs.clear()
                    cBs[ei] = cB
                h1 = h1p.next()
                for m in range(2):
                    for (b0, n) in blocks:
                        pg = PS.next(); pu = PS.next()
                        for k in range(KC):
                            K.pe(pg.a[:, :n], wg.a[:, k, m * 128:(m + 1) * 128], cT.a[:, k, b0:b0 + n], k == 0, k == KC - 1, [wg, cT], [pg])
                        for k in range(KC):
                            K.pe(pu.a[:, :n], wu.a[:, k, m * 128:(m + 1) * 128], cT.a[:, k, b0:b0 + n], k == 0, k == KC - 1, [wu, cT], [pu])
                        sg = sgp.next()
                        K.act(sg.a[:, :n], pg.a[:, :n], AF.Silu, [pg], [sg])
                        if moe:
                            K.tt("pool", sg.a[:, :n], sg.a[:, :n], cBs[ei].a[:, b0:b0 + n], ALU.mult, [sg, cBs[ei]], [sg])
                        K.tt("dve", h1.a[:, m, b0:b0 + n], sg.a[:, :n], pu.a[:, :n], ALU.mult, [sg, pu], [h1])
                return h1

            def down(p, h1):
                wg, wu, wd = loaded.pop(p)
                for mo in range(KC):
                    for (b0, n) in blocks:
                        ps = PS.next()
                        for k in range(2):
                            K.pe(ps.a[:, :n], wd.a[:, k, mo * 128:(mo + 1) * 128], h1.a[:, k, b0:b0 + n], k == 0, k == 1, [wd, h1], [ps])
                        if p == 0:
                            K.cp("act", XTf[:, mo, b0:b0 + n], ps.a[:, :n], [ps], [tXT])
                        else:
                            K.tt("dve", XTf[:, mo, b0:b0 + n], ps.a[:, :n], XTf[:, mo, b0:b0 + n], ALU.add, [ps, tXT], [tXT])

            loads(0)
            prev_h1 = None
            for p in range(npan + 1):
                if p + 1 < npan:
                    loads(p + 1)
                cur_h1 = gate_up(p) if p < npan else None
                if p >= 1:
                    down(p - 1, prev_h1)
                prev_h1 = cur_h1
            for mo in range(KC):
                for (b0, n) in blocks:
                    hb = hbp.next()
                    K.ld(hb.a[:, :n], hT.a[mo * 128:(mo + 1) * 128, c0 + b0:c0 + b0 + n], hb, reads=[hT], writes=[hb])
                    K.tt("dve", hb.a[:, :n], hb.a[:, :n], XTf[:, mo, b0:b0 + n], ALU.add, [hb, tXT], [hb])
                    K.stq(hT.a[mo * 128:(mo + 1) * 128, c0 + b0:c0 + b0 + n], hb.a[:, :n], hb, reads=[hb], mwrites=[hT])


def _consts():
    ident = np.eye(128, dtype=np.float32)
    maskT = np.where(np.arange(64)[:, None] <= np.arange(64)[None, :], 0.0, NEG).astype(np.float32)
    sel4 = np.zeros((4, 4, 128), np.float32)
    for h in range(4):
        sel4[h, h, :] = 1.0
    sel8 = np.zeros((8, 8, 128), np.float32)
    for h in range(8):
        sel8[h, h, :] = 1.0
    return dict(ident=ident, nident=-ident, maskT=maskT, sel4=sel4, sel8=sel8, sel8e=sel8.copy())


def _fm(v):
    v = np.asarray(v, np.float32)
    return np.ascontiguousarray(v.reshape(v.shape[:-1] + (v.shape[-1] // 128, 128)).swapaxes(-1, -2))


def prep_shared(inp):
    f = lambda a: np.ascontiguousarray(np.asarray(a, np.float32))
    sh = {}
    for nm in ("g_mix", "g_ffn", "g_ple"):
        sh[nm] = _fm(inp[nm])
    sh["g_final"] = _fm(inp["g_final"])
    sh["w_in"] = f(inp["w_in"]); sh["w_out"] = f(inp["w_out"])
    sh["b_ig"] = f(inp["b_igate"]).reshape(DEPTH, 4, 1); sh["b_fg"] = f(inp["b_fgate"]).reshape(DEPTH, 4, 1)
    sh["gml_rep"] = np.ascontiguousarray(np.broadcast_to(f(inp["g_ml"]).reshape(DEPTH, 1, 1024), (DEPTH, 64, 1024)))
    sh["lam_re"] = _fm(f(inp["s5_lam_re"]).reshape(DEPTH, 2048)); sh["lam_im"] = _fm(f(inp["s5_lam_im"]).reshape(DEPTH, 2048))
    sh["logdt"] = _fm(np.repeat(f(inp["s5_log_dt"]), 64, axis=1))
    bre = f(inp["s5_b_re"]); bim = f(inp["s5_b_im"]); cre = f(inp["s5_c_re"]); cim = f(inp["s5_c_im"])
    Bre = np.zeros((DEPTH, 16, 128, 128), np.float32); Bim = np.zeros_like(Bre); Cre = np.zeros_like(Bre); Cim = np.zeros_like(Bre)
    for g in range(32):
        sc, g2, gl = g // 2, g % 2, g % 8
        Bre[:, sc, gl * 16:(gl + 1) * 16, g2 * 64:(g2 + 1) * 64] = bre[:, g].transpose(0, 2, 1)
        Bim[:, sc, gl * 16:(gl + 1) * 16, g2 * 64:(g2 + 1) * 64] = bim[:, g].transpose(0, 2, 1)
        Cre[:, sc, g2 * 64:(g2 + 1) * 64, gl * 16:(gl + 1) * 16] = cre[:, g].transpose(0, 2, 1)
        Cim[:, sc, g2 * 64:(g2 + 1) * 64, gl * 16:(gl + 1) * 16] = cim[:, g].transpose(0, 2, 1)
    sh["Bre"], sh["Bim"], sh["Cre"], sh["Cim"] = Bre, Bim, Cre, Cim
    fm4 = lambda v: np.ascontiguousarray(f(v).reshape(DEPTH, 4, 128).swapaxes(1, 2))
    sh["s5d"] = fm4(f(inp["s5_d"]).reshape(DEPTH, 512)); sh["b_glu"] = fm4(inp["s5_b_glu"]); sh["g_s5"] = fm4(inp["g_s5"])
    sh["w_glu"] = f(inp["s5_w_glu"])
    sh["conv_w"] = np.ascontiguousarray(f(inp["ssd_conv_w"]).reshape(DEPTH, 4, 8, 128).transpose(0, 3, 2, 1))
    sh["conv_b"] = np.ascontiguousarray(f(inp["ssd_conv_b"]).reshape(DEPTH, 8, 128).swapaxes(1, 2))
    sh["dt_bias"] = f(inp["ssd_dt_bias"]).reshape(DEPTH, 8, 1); sh["a_log"] = f(inp["ssd_a_log"]).reshape(DEPTH, 8, 1)
    sh["ssdd_rep"] = np.ascontiguousarray(np.broadcast_to(f(inp["ssd_d"]).reshape(DEPTH, 1, 8), (DEPTH, 64, 8)))
    sh["gssd_rep"] = np.ascontiguousarray(np.broadcast_to(f(inp["g_ssd"]).reshape(DEPTH, 1, 512), (DEPTH, 64, 512)))
    def pan_in(W):
        npan = W.shape[1] // 256
        return np.ascontiguousarray(W.reshape(16, 128, npan, 256).transpose(2, 1, 0, 3)).reshape(npan, 128, 4096)

    def pan_dn(W):
        npan = W.shape[0] // 256
        return np.ascontiguousarray(W.reshape(npan, 2, 128, 2048).transpose(0, 2, 1, 3)).reshape(npan, 128, 4096)
    sh["ffn_wg"] = pan_in(f(inp["ffn_w_gate"])[0]); sh["ffn_wu"] = pan_in(f(inp["ffn_w_up"])[0]); sh["ffn_wd"] = pan_dn(f(inp["ffn_w_down"])[0])
    sh["w_router"] = f(inp["w_router"])[0]
    sh["b_router_rep"] = np.ascontiguousarray(np.broadcast_to(f(inp["b_router"])[0].reshape(1, NE), (128, NE)))
    sh["moe_wg"] = np.stack([pan_in(np.asarray(inp["moe_w_gate"][0][e], np.float32)) for e in range(NE)])
    sh["moe_wu"] = np.stack([pan_in(np.asarray(inp["moe_w_up"][0][e], np.float32)) for e in range(NE)])
    sh["moe_wd"] = np.stack([pan_dn(np.asarray(inp["moe_w_down"][0][e], np.float32)) for e in range(NE)])
    sh["w_ple"] = f(inp["w_ple"]); sh["w_pleg"] = f(inp["w_ple_gate"])
    sh.update(_consts())
    return sh


def prep_core(inp, c):
    f = lambda a: np.ascontiguousarray(np.asarray(a, np.float32))
    b = c % 4
    ss = slice(c * NS, (c + 1) * NS)
    m = {}
    m["xT"] = np.ascontiguousarray(np.concatenate([f(inp["x_prompt"])[b], f(inp["x_sample"])[ss, 0]], axis=0).T)
    m["pT"] = np.ascontiguousarray(np.concatenate([f(inp["p_prompt"])[:, b], f(inp["p_sample"])[:, ss, 0]], axis=1).transpose(0, 2, 1))
    m["sC"] = f(inp["state_mlstm_C"])[:, ss]; m["sn"] = f(inp["state_mlstm_n"])[:, ss]
    m["smT"] = np.ascontiguousarray(f(inp["state_mlstm_m"])[:, ss].transpose(0, 2, 1))
    s5 = lambda a: np.ascontiguousarray(f(a)[:, ss].reshape(DEPTH, NS, 16, 128).transpose(0, 3, 2, 1))
    m["s5reT"] = s5(inp["state_s5_re"]); m["s5imT"] = s5(inp["state_s5_im"])
    m["ssdT"] = np.ascontiguousarray(f(inp["state_ssd"])[:, ss].transpose(0, 1, 2, 4, 3))
    m["convT"] = np.ascontiguousarray(f(inp["cache_conv"])[:, ss].reshape(DEPTH, NS, 3, 8, 128).transpose(0, 4, 3, 2, 1))
    m["half"] = np.ascontiguousarray(np.broadcast_to(np.array([[1.0, 0.0]] if c < 4 else [[0.0, 1.0]], np.float32), (128, 2)))
    return m


_NC_CACHE = {}


def run_device(inputs, dbg=None, stages=99, cores=8, trace=False):
    key = (tuple(sorted(dbg or ())), stages)
    if key not in _NC_CACHE:
        _NC_CACHE[key] = build_program(dbg, stages)
    nc, K = _NC_CACHE[key]
    sh = prep_shared(inputs)
    in_maps = []
    for c in range(cores):
        m = dict(sh)
        m.update(prep_core(inputs, c))
        in_maps.append(m)
    if trace:
        res = run_bass_kernel_spmd(nc, in_maps, core_ids=list(range(cores)), trace=True)
        print("EXEC_NS", res.exec_time_ns)
    else:
        res = run_bass_kernel_spmd(nc, in_maps, core_ids=list(range(cores)))
    return res.results


def kernel(**inputs):
    R = run_device(inputs)
    B = 4
    y_p = np.stack([np.concatenate([R[b]["yT"][:, :1024].T, R[b + 4]["yT"][:, :1024].T], axis=0) for b in range(B)])
    y_s = np.concatenate([R[c]["yT"][:, 1024:NO].T for c in range(8)], axis=0)[:, None, :]
    st = lambda nm, idx: np.stack([R[b][nm] for b in range(B)], axis=1)
    C_p = np.stack([R[b]["C_p"] for b in range(B)], axis=1)
    n_p = np.stack([R[b]["n_p"] for b in range(B)], axis=1)
    m_p = np.stack([R[b]["m_pT"][:, :, 0] for b in range(B)], axis=1)
    unfm = lambda a: a.swapaxes(-1, -2).reshape(a.shape[:-2] + (32, 64))
    s5re_p = np.stack([unfm(R[b]["s5re_pT"]) for b in range(B)], axis=1)
    s5im_p = np.stack([unfm(R[b]["s5im_pT"]) for b in range(B)], axis=1)
    ssd_p = np.stack([R[b]["ssd_pT"].transpose(0, 1, 3, 2) for b in range(B)], axis=1)
    conv_p = np.stack([R[b]["conv_pT"].transpose(0, 3, 2, 1).reshape(DEPTH, 3, 1024) for b in range(B)], axis=1)
    C_s = np.concatenate([R[c]["C_s"] for c in range(8)], axis=1)
    n_s = np.concatenate([R[c]["n_s"] for c in range(8)], axis=1)
    m_s = np.concatenate([R[c]["m_sT"].transpose(0, 2, 1) for c in range(8)], axis=1)
    uns = lambda a: a.transpose(0, 3, 2, 1).reshape(DEPTH, NS, 32, 64)
    s5re_s = np.concatenate([uns(R[c]["s5re_sT"]) for c in range(8)], axis=1)
    s5im_s = np.concatenate([uns(R[c]["s5im_sT"]) for c in range(8)], axis=1)
    ssd_s = np.concatenate([R[c]["ssd_sT"].transpose(0, 1, 2, 4, 3) for c in range(8)], axis=1)
    conv_s = np.concatenate([R[c]["conv_sT"].transpose(0, 4, 3, 2, 1).reshape(DEPTH, NS, 3, 1024) for c in range(8)], axis=1)
    outs = (y_p, y_s, C_p, n_p, m_p, s5re_p, s5im_p, ssd_p, conv_p, C_s, n_s, m_s, s5re_s, s5im_s, ssd_s, conv_s)
    return tuple(np.ascontiguousarray(o, dtype=np.float32) for o in outs)
```

```python
import math
import numpy as np
import concourse.bass as bass
import concourse.mybir as mybir
from concourse.bass_utils import run_bass_kernel_spmd
from contextlib import ExitStack

F32 = mybir.dt.float32
BF16 = mybir.dt.bfloat16
AF = mybir.ActivationFunctionType
ALU = mybir.AluOpType
AX = mybir.AxisListType

L = 2048
NS = 16
NT = L + NS
XW = 2080
D = 2048
KC = 16
DEPTH = 2
N_IN = 6160
D_FF = 5632
D_FFE = 7168
NE = 8
EPS = 1e-6
TB = [(0, 512), (512, 512), (1024, 512), (1536, 512), (2048, 16)]
CHUNKS = [(c * 64, 64, c) for c in range(32)] + [(L + j, 1, 32 + j) for j in range(NS)]
NCI = 48
NO = 1040
TBO = [(0, 512), (512, 512), (1024, 16)]
NEG = -30000.0


class Tk:
    __slots__ = ("name", "lw", "rd", "mw", "dsem", "dcnt")

    def __init__(self, name):
        self.name = name
        self.lw = None
        self.rd = []
        self.mw = []
        self.dsem = None
        self.dcnt = 0


class T:
    __slots__ = ("a", "k")

    def __init__(self, a, name):
        self.a = a
        self.k = Tk(name)


class Ins:
    __slots__ = ("eng", "fn", "waits", "needed", "val", "dsem", "dval")

    def __init__(self, eng, fn):
        self.eng = eng
        self.fn = fn
        self.waits = []
        self.needed = False
        self.val = None
        self.dsem = None
        self.dval = None


class Sched:
    ENGS = ("pe", "act", "dve", "pool", "sp")

    def __init__(self, nc, stack):
        self.nc = nc
        self.stack = stack
        self.prog = {e: [] for e in self.ENGS}
        self.esem = {e: stack.enter_context(nc.semaphore("es_" + e)) for e in self.ENGS}
        self.ecnt = {e: 0 for e in self.ENGS}
        self.known = {e: {} for e in self.ENGS}
        self.nsem = 0
        self.out_events = []
        self.toks = []
        self.pend_dma = []
        self.last = {e: None for e in self.ENGS}
        self.semfree = []
        self.semcnt = {}
        self.phase_mark = 0

    def phase_begin(self):
        self.phase_mark = len(self.toks)

    def phase_end(self):
        for k in self.toks[self.phase_mark:]:
            if k.dsem is not None:
                self.semfree.append(k.dsem)
                k.dsem = None
        del self.toks[self.phase_mark:]

    def tok(self, name):
        k = Tk(name)
        self.toks.append(k)
        return k

    def _deps(self, ins, reads, writes, mwrites):
        evs = []
        for r in reads:
            if r.lw is not None:
                evs.append(r.lw)
            evs.extend(r.mw)
        for w in writes:
            if w.lw is not None:
                evs.append(w.lw)
            evs.extend(w.mw)
            evs.extend(w.rd)
        for w in mwrites:
            if w.lw is not None:
                evs.append(w.lw)
            evs.extend(w.rd)
        seen = set()
        for ev in evs:
            if id(ev) in seen or ev is ins:
                continue
            seen.add(id(ev))
            if ev.dsem is None and ev.eng == "pe" and ins.eng == "pe" and ins.dsem is None:
                continue
            ins.waits.append(ev)
            ev.needed = True
        for r in reads:
            r.rd.append(ins)
        for w in writes:
            w.lw = ins
            w.rd = []
            w.mw = []
        for w in mwrites:
            if w.rd:
                w.rd = []
                w.mw = []
                w.lw = None
            w.mw.append(ins)

    def op(self, eng, fn, reads=(), writes=()):
        ins = Ins(eng, fn)
        self._deps(ins, [r.k if isinstance(r, T) else r for r in reads],
                   [w.k if isinstance(w, T) else w for w in writes], [])
        self.prog[eng].append(ins)
        self.last[eng] = ins
        return ins

    def dma(self, q, out_ap, in_ap, tok, reads=(), writes=(), mwrites=(), is_output=False, **kw):
        if isinstance(tok, T):
            tok = tok.k
        if tok.dsem is None:
            if self.semfree:
                tok.dsem = self.semfree.pop()
            else:
                tok.dsem = self.stack.enter_context(self.nc.semaphore("ds_%d" % self.nsem))
                self.nsem += 1
        ins = Ins(q, lambda e: e.dma_start(out=out_ap, in_=in_ap, **kw))
        c = self.semcnt.get(id(tok.dsem), 0) + 16
        self.semcnt[id(tok.dsem)] = c
        ins.dsem = tok.dsem
        ins.dval = c
        self._deps(ins, [r.k if isinstance(r, T) else r for r in reads],
                   [w.k if isinstance(w, T) else w for w in writes],
                   [w.k if isinstance(w, T) else w for w in mwrites])
        self.prog[q].append(ins)
        self.pend_dma.append(ins)
        if is_output:
            self.out_events.append(ins)
        return ins

    def dma_fn(self, q, fn, tok, reads=(), writes=(), mwrites=()):
        if isinstance(tok, T):
            tok = tok.k
        if tok.dsem is None:
            if self.semfree:
                tok.dsem = self.semfree.pop()
            else:
                tok.dsem = self.stack.enter_context(self.nc.semaphore("ds_%d" % self.nsem))
                self.nsem += 1
        ins = Ins(q, fn)
        c = self.semcnt.get(id(tok.dsem), 0) + 16
        self.semcnt[id(tok.dsem)] = c
        ins.dsem = tok.dsem
        ins.dval = c
        self._deps(ins, [r.k if isinstance(r, T) else r for r in reads],
                   [w.k if isinstance(w, T) else w for w in writes],
                   [w.k if isinstance(w, T) else w for w in mwrites])
        self.prog[q].append(ins)
        self.pend_dma.append(ins)
        return ins

    def _emit_engine(self, e, engh):
        known = self.known[e]
        for ins in self.prog[e]:
            need = {}
            for ev in ins.waits:
                if ev.dsem is not None:
                    key, sem, val = ("d", id(ev.dsem)), ev.dsem, ev.dval
                else:
                    key, sem, val = ("e", ev.eng), self.esem[ev.eng], ev.val
                if known.get(key, 0) >= val:
                    continue
                if key not in need or need[key][1] < val:
                    need[key] = (sem, val)
            for key, (sem, val) in need.items():
                engh.wait_ge(sem, val)
                known[key] = val
            if ins.fn is None:
                continue
            bi = ins.fn(engh)
            if ins.dsem is not None:
                bi.then_inc(ins.dsem, 16)
            elif ins.needed:
                bi.then_inc(self.esem[e], 1)

    def flush(self, final=False):
        nc = self.nc
        bar = Ins("sp", lambda e: e.nop())
        seen = set()
        for ev in self.pend_dma:
            if ev.eng != "sp" or True:
                key = (id(ev.dsem))
                bar.waits.append(ev)
        for e in self.ENGS:
            if e != "sp" and self.last[e] is not None:
                self.last[e].needed = True
                bar.waits.append(self.last[e])
        bar.needed = True
        self.prog["sp"].append(bar)
        for e in self.ENGS:
            if e != "sp":
                w = Ins(e, None)
                w.waits.append(bar)
                self.prog[e].append(w)
        for e in self.ENGS:
            c = self.ecnt[e]
            for ins in self.prog[e]:
                if ins.dsem is None and ins.needed and ins.fn is not None:
                    c += 1
                    ins.val = c
            self.ecnt[e] = c
        with nc.Block() as block:
            @block.tensor
            def _(eng):
                self._emit_engine("pe", eng)

            @block.scalar
            def _(eng):
                self._emit_engine("act", eng)

            @block.vector
            def _(eng):
                self._emit_engine("dve", eng)

            @block.gpsimd
            def _(eng):
                self._emit_engine("pool", eng)

            @block.sync
            def _(eng):
                self._emit_engine("sp", eng)
        self.prog = {e: [] for e in self.ENGS}
        self.pend_dma = []
        self.last = {e: None for e in self.ENGS}
        for k in self.toks:
            k.lw = None
            k.rd = []
            k.mw = []


_UID = [0]


class Pool:
    def __init__(self, K, name, shape, dt, n, space="sb"):
        _UID[0] += 1
        name = "%s_%d_" % (name, _UID[0])
        self.bufs = []
        for i in range(n):
            if space == "sb":
                a = K.st.enter_context(K.nc.sbuf_tensor("%s%d" % (name, i), list(shape), dt))
            else:
                a = K.st.enter_context(K.nc.psum_tensor("%s%d" % (name, i), list(shape), dt))
            t = T(a, "%s%d" % (name, i))
            K.S.toks.append(t.k)
            self.bufs.append(t)
        self.i = 0

    def next(self):
        b = self.bufs[self.i % len(self.bufs)]
        self.i += 1
        return b


class _Phase:
    def __init__(self, K):
        self.K = K

    def __enter__(self):
        self.pst = ExitStack()
        self.pst.__enter__()
        self.K.st = self.pst
        self.K.S.phase_begin()
        return self

    def __exit__(self, *a):
        if a[0] is None:
            self.K.S.flush()
            self.K.S.phase_end()
        self.K.st = self.K.gst
        return self.pst.__exit__(*a)


class Builder:
    def __init__(self, nc, st, dbg=None):
        self.nc = nc
        self.gst = st
        self.st = st
        self.S = Sched(nc, st)
        self.dbg = dbg or set()
        self.inputs = {}
        self.outputs = {}

    def phase(self):
        return _Phase(self)

    def din(self, name, shape, dt=F32):
        a = self.nc.dram_tensor(name, list(shape), dt, kind="ExternalInput").ap()
        t = T(a, name)
        self.inputs[name] = t
        return t

    def dout(self, name, shape, dt=F32):
        a = self.nc.dram_tensor(name, list(shape), dt, kind="ExternalOutput").ap()
        t = T(a, name)
        self.S.toks.append(t.k)
        self.outputs[name] = t
        return t

    def dscr(self, name, shape, dt=F32):
        kind = "ExternalOutput" if name in self.dbg else "Internal"
        a = self.nc.dram_tensor(name, list(shape), dt, kind=kind).ap()
        t = T(a, name)
        self.S.toks.append(t.k)
        return t

    def sb(self, name, shape, dt=F32):
        _UID[0] += 1
        name = "%s_%d" % (name, _UID[0])
        a = self.st.enter_context(self.nc.sbuf_tensor(name, list(shape), dt))
        t = T(a, name)
        self.S.toks.append(t.k)
        return t

    def view(self, ap, name):
        t = T(ap, name)
        self.S.toks.append(t.k)
        return t

    def pe(self, out, lhsT, rhs, start, stop, reads, writes):
        self.S.op("pe", lambda e: e.matmul(out, lhsT, rhs, start=start, stop=stop), reads=reads, writes=writes)

    def tr(self, out, in_, ident, reads, writes):
        self.S.op("pe", lambda e: e.transpose(out, in_, ident), reads=reads, writes=writes)

    def act(self, out, in_, func, reads, writes, scale=1.0, bias=None, accum=None):
        def f(e):
            kw = {}
            if bias is not None:
                kw["bias"] = bias
            if accum is not None:
                kw["accum_out"] = accum
            return e.activation(out=out, in_=in_, func=func, scale=scale, **kw)
        self.S.op("act", f, reads=reads, writes=writes)

    def tt(self, eng, out, in0, in1, op, reads, writes):
        self.S.op(eng, lambda e: e.tensor_tensor(out, in0, in1, op), reads=reads, writes=writes)

    def ts(self, eng, out, in0, s1, op0, reads, writes, s2=None, op1=None):
        if op1 is None:
            self.S.op(eng, lambda e: e.tensor_scalar(out, in0, s1, None, op0), reads=reads, writes=writes)
        else:
            self.S.op(eng, lambda e: e.tensor_scalar(out, in0, s1, s2, op0, op1), reads=reads, writes=writes)

    def stt(self, out, in0, scalar, in1, op0, op1, reads, writes):
        self.S.op("dve", lambda e: e.scalar_tensor_tensor(out, in0, scalar, in1, op0, op1), reads=reads, writes=writes)

    def cp(self, eng, out, in_, reads, writes):
        if eng == "act":
            self.S.op("act", lambda e: e.copy(out, in_), reads=reads, writes=writes)
        else:
            self.S.op(eng, lambda e: e.tensor_copy(out, in_), reads=reads, writes=writes)

    def memset(self, eng, ap, val, writes):
        self.S.op(eng, lambda e: e.memset(ap, val), writes=writes)

    def recip(self, out, in_, reads, writes):
        self.S.op("dve", lambda e: e.reciprocal(out, in_), reads=reads, writes=writes)

    def scan(self, out, d0, d1, init, op0, op1, reads, writes):
        self.S.op("dve", lambda e: e.tensor_tensor_scan(out, d0, d1, init, op0, op1), reads=reads, writes=writes)

    def ld(self, dst_ap, src_ap, tok, reads=(), writes=(), q="sp", **kw):
        self.S.dma(q, dst_ap, src_ap, tok, reads=reads, writes=writes, **kw)

    def stq(self, dst_ap, src_ap, tok, reads=(), mwrites=(), writes=(), q="sp", is_output=False, **kw):
        self.S.dma(q, dst_ap, src_ap, tok, reads=reads, mwrites=mwrites, writes=writes, is_output=is_output, **kw)


def build_program(dbg=None, stages=99):
    nc = bass.Bass("TRN2", target_bir_lowering=False)
    with ExitStack() as gst:
        K = Builder(nc, gst, dbg)
        S = K.S
        I = {}
        def din(name, shape):
            I[name] = K.din(name, shape)
            return I[name]
        din("xT", [D, NT])
        din("pT", [DEPTH, 256, NT])
        din("sC", [DEPTH, NS, 4, 256, 256]); din("sn", [DEPTH, NS, 4, 256]); din("smT", [DEPTH, 4, NS])
        din("s5reT", [DEPTH, 128, 16, NS]); din("s5imT", [DEPTH, 128, 16, NS])
        din("ssdT", [DEPTH, NS, 8, 128, 64]); din("convT", [DEPTH, 128, 8, 3, NS])
        for nm in ("g_mix", "g_ffn", "g_ple"):
            din(nm, [DEPTH, 128, 16])
        din("g_final", [128, 16])
        din("w_in", [DEPTH, D, N_IN]); din("w_out", [DEPTH, D, D])
        din("b_ig", [DEPTH, 4, 1]); din("b_fg", [DEPTH, 4, 1]); din("gml_rep", [DEPTH, 64, 1024])
        for nm in ("lam_re", "lam_im", "logdt"):
            din(nm, [DEPTH, 128, 16])
        din("Bre", [DEPTH, 16, 128, 128]); din("Bim", [DEPTH, 16, 128, 128])
        din("Cre", [DEPTH, 16, 128, 128]); din("Cim", [DEPTH, 16, 128, 128])
        din("s5d", [DEPTH, 128, 4]); din("w_glu", [DEPTH, 512, 512]); din("b_glu", [DEPTH, 128, 4]); din("g_s5", [DEPTH, 128, 4])
        din("conv_w", [DEPTH, 128, 8, 4]); din("conv_b", [DEPTH, 128, 8])
        din("dt_bias", [DEPTH, 8, 1]); din("a_log", [DEPTH, 8, 1]); din("ssdd_rep", [DEPTH, 64, 8]); din("gssd_rep", [DEPTH, 64, 512])
        din("ffn_wg", [D_FF // 256, 128, 4096]); din("ffn_wu", [D_FF // 256, 128, 4096]); din("ffn_wd", [D_FF // 256, 128, 4096])
        din("w_router", [D, NE]); din("b_router_rep", [128, NE])
        din("moe_wg", [NE, D_FFE // 256, 128, 4096]); din("moe_wu", [NE, D_FFE // 256, 128, 4096]); din("moe_wd", [NE, D_FFE // 256, 128, 4096])
        din("w_ple", [DEPTH, 256, D]); din("w_pleg", [DEPTH, D, D])
        din("ident", [128, 128]); din("nident", [128, 128]); din("maskT", [64, 64])
        din("half", [128, 2])
        din("sel4", [4, 4, 128]); din("sel8", [8, 8, 128]); din("sel8e", [8, 8, 128])
        O = {}
        def dout(name, shape):
            O[name] = K.dout(name, shape)
            return O[name]
        dout("yT", [D, NO])
        dout("C_p", [DEPTH, 4, 256, 256]); dout("n_p", [DEPTH, 4, 256]); dout("m_pT", [DEPTH, 4, 1])
        dout("s5re_pT", [DEPTH, 128, 16]); dout("s5im_pT", [DEPTH, 128, 16])
        dout("ssd_pT", [DEPTH, 8, 128, 64]); dout("conv_pT", [DEPTH, 128, 8, 3])
        dout("C_s", [DEPTH, NS, 4, 256, 256]); dout("n_s", [DEPTH, NS, 4, 256]); dout("m_sT", [DEPTH, 4, NS])
        dout("s5re_sT", [DEPTH, 128, 16, NS]); dout("s5im_sT", [DEPTH, 128, 16, NS])
        dout("ssd_sT", [DEPTH, NS, 8, 128, 64]); dout("conv_sT", [DEPTH, 128, 8, 3, NS])
        hT = K.dscr("hT", [D, NT])
        qT_d = K.dscr("qT_d", [1024, NT]); kT_d = K.dscr("kT_d", [1024, NT])
        k_d = K.dscr("k_d", [NT, 1024]); v_d = K.dscr("v_d", [NT, 1024]); go_d = K.dscr("go_d", [NT, 1024])
        uT_d = K.dscr("uT_d", [512, NT]); zs_d = K.dscr("zs_d", [NT, 512]); xbcT_d = K.dscr("xbcT_d", [1024, NT])
        xcT_d = K.dscr("xcT_d", [1024, NT])
        mix_d = K.dscr("mix_d", [D, NT])
        hO = K.dscr("hO", [D, NO])
        mixsw = K.dscr("mixsw", [128, KC * XW], BF16)
        halfsb = K.sb("halfsb", [128, 2])
        K.ld(halfsb.a[:], I["half"].a, halfsb, writes=[halfsb])

        def blend(dstA, srcB, toks_r, tok_w):
            K.ts("dve", dstA, dstA, halfsb.a[:, 0:1], ALU.mult, list(toks_r) + [halfsb], [tok_w])
            K.stt(dstA, srcB, halfsb.a[:, 1:2], dstA, ALU.mult, ALU.add, list(toks_r) + [halfsb, tok_w], [tok_w])

        XTraw = K.sb("XT", [128, KC * XW // 2], F32)
        XTb = XTraw.a[:].bitcast(BF16).rearrange("p (k t) -> p k t", k=KC)
        XTf = XTraw.a[:].rearrange("p (k t) -> p k t", k=KC)
        tXT = XTraw
        ident = K.sb("ident_sb", [128, 128]); nident = K.sb("nident_sb", [128, 128]); maskT = K.sb("maskT_sb", [64, 64])
        K.ld(ident.a[:], I["ident"].a, ident, writes=[ident]); K.ld(nident.a[:], I["nident"].a, nident, writes=[nident])
        K.ld(maskT.a[:], I["maskT"].a, maskT, writes=[maskT])
        ones_bf = K.sb("ones_bf", [128, 128], BF16); ones_f = K.sb("ones_f", [128, 128])
        K.memset("dve", ones_bf.a[:], 1.0, [ones_bf]); K.memset("dve", ones_f.a[:], 1.0, [ones_f])
        epsb = K.sb("epsb", [128, 1]); K.memset("dve", epsb.a[:], EPS, [epsb])
        halfpi = K.sb("halfpi", [128, 1]); K.memset("dve", halfpi.a[:], math.pi / 2, [halfpi])
        gn = {}
        for nm in ("g_mix", "g_ffn", "g_ple"):
            gn[nm] = K.sb(nm + "_sb", [128, DEPTH, 16])
            for l in range(DEPTH):
                K.ld(gn[nm].a[:, l, :], I[nm].a[l], gn[nm], writes=[gn[nm]])
        gfin = K.sb("gfin_sb", [128, 16]); K.ld(gfin.a[:], I["g_final"].a, gfin, writes=[gfin])
        PS = Pool(K, "ps", [128, 512], F32, 8, space="ps")

        class ListPool:
            def __init__(self, bufs):
                self.bufs = bufs
                self.i = 0

            def next(self):
                b = self.bufs[self.i % len(self.bufs)]
                self.i += 1
                return b
        PSs = ListPool([K.view(PS.bufs[b].a[:, j * 64:(j + 1) * 64], "pss%d_%d" % (b, j)) for b in range(4) for j in range(8)])
        PSb = ListPool([K.view(PS.bufs[b].a, "psb%d" % b) for b in range(4, 8)])
        K.PSs, K.PSb = PSs, PSb
        S.flush()

        def rstd_from_ps(ps, n, inv_d, rs, parts=128):
            K.act(rs.a[:parts, :n], ps.a[:parts, :n], AF.Sqrt, [ps, epsb], [rs], scale=inv_d, bias=epsb.a[:parts, :])
            K.recip(rs.a[:parts, :n], rs.a[:parts, :n], [rs], [rs])

        def norm_stage(src, gsb_ap, gtile, t0, n, dst_ap, dst_tile, P):
            hb = P["hblk"].next()
            K.ld(hb.a[:, :, :n], src.a.rearrange("(k p) t -> p k t", p=128)[:, :, t0:t0 + n], hb, reads=[src], writes=[hb])
            sq = P["sq"].next()
            K.act(sq.a[:, :, :n], hb.a[:, :, :n], AF.Square, [hb], [sq])
            ps = PS.next()
            for k in range(KC):
                K.pe(ps.a[:, :n], ones_bf.a[:], sq.a[:, k, :n], k == 0, k == KC - 1, [sq, ones_bf], [ps])
            rs = P["rs"].next()
            rstd_from_ps(ps, n, 1.0 / D, rs)
            for k in range(KC):
                K.stt(dst_ap[:, k, :], hb.a[:, k, :n], gsb_ap[:, k:k + 1], rs.a[:, :n], ALU.mult, ALU.mult,
                      [hb, rs, gtile], [dst_tile])
            return hb

        def wload(P, W_ap, kc, pw, name="w"):
            wb = P[name].next()
            K.ld(wb.a[:, :kc, :pw], W_ap.rearrange("(k p) c -> p k c", p=128), wb, writes=[wb], q="pool")
            return wb

        def proj_fm(P, xap, xt, kc, W_ap, c0, ncols, evac, tblocks):
            for p0 in range(0, ncols, 512):
                pw = min(512, ncols - p0)
                wb = wload(P, W_ap[:, c0 + p0:c0 + p0 + pw], kc, pw)
                for m0 in range(0, pw, 128):
                    mw = min(128, pw - m0)
                    for (t0, n) in tblocks:
                        ps = PS.next()
                        for k in range(kc):
                            K.pe(ps.a[:mw, :n], wb.a[:, k, m0:m0 + mw], xap[:, k, t0:t0 + n], k == 0, k == kc - 1, [wb, xt], [ps])
                        evac(ps, p0 + m0, mw, t0, n)

        def proj_tm(P, xap, xt, kc, W_ap, c0, ncols, evac):
            for p0 in range(0, ncols, 512):
                pw = min(512, ncols - p0)
                wb = wload(P, W_ap[:, c0 + p0:c0 + p0 + pw], kc, pw)
                for j in range(17):
                    t0 = j * 128
                    n = 128 if j < 16 else NS
                    ps = PS.next()
                    for k in range(kc):
                        K.pe(ps.a[:n, :pw], xap[:, k, t0:t0 + n], wb.a[:, k, :pw], k == 0, k == kc - 1, [wb, xt], [ps])
                    evac(ps, p0, pw, t0, n)

        evi = [0]
        def evac_copy_to_dram(P, ps, pr, fr, dst_ap, dst, scale=None, func=None, mul=None, multile=None):
            stg = P["stg"].next()
            if func is not None:
                K.act(stg.a[:pr, :fr], ps.a[:pr, :fr], func, [ps], [stg])
                if mul is not None:
                    K.tt("dve", stg.a[:pr, :fr], stg.a[:pr, :fr], mul, ALU.mult, [stg, multile], [stg])
            elif scale is not None:
                K.act(stg.a[:pr, :fr], ps.a[:pr, :fr], AF.Copy, [ps], [stg], scale=scale)
            else:
                evi[0] += 1
                K.cp("act" if evi[0] % 2 else "dve", stg.a[:pr, :fr], ps.a[:pr, :fr], [ps], [stg])
            K.stq(dst_ap, stg.a[:pr, :fr], stg, reads=[stg], mwrites=[dst])

        S.dma("sp", hT.a, I["xT"].a, hT, writes=[hT])
        S.flush()

        for l in range(DEPTH):
            if stages < 1:
                break
            with K.phase():
                P = {"hblk": Pool(K, "hblk", [128, KC, 512], F32, 1), "sq": Pool(K, "sq", [128, KC, 512], BF16, 1),
                     "rs": Pool(K, "rs", [128, 512], F32, 2), "w": Pool(K, "w", [128, KC, 512], BF16, 3),
                     "stg": Pool(K, "stg", [128, 512], F32, 4)}
                gi = K.sb("gi", [4, NT]); gf = K.sb("gf", [4, NT]); gdt = K.sb("gdt", [8, NT])
                gml = K.sb("gml", [128, 1024])
                for r in range(2):
                    K.ld(gml.a[r * 64:(r + 1) * 64, :], I["gml_rep"].a[l], gml, writes=[gml])
                for (t0, n) in TB:
                    norm_stage(hT, gn["g_mix"].a[:, l, :], gn["g_mix"], t0, n, XTb[:, :, t0:t0 + n], tXT, P)
                W = I["w_in"].a[l]
                proj_fm(P, XTb, tXT, KC, W, 0, 1024, lambda ps, co, mw, t0, n: evac_copy_to_dram(P, ps, mw, n, qT_d.a[co:co + mw, t0:t0 + n], qT_d), TB)
                proj_fm(P, XTb, tXT, KC, W, 1024, 1024, lambda ps, co, mw, t0, n: evac_copy_to_dram(P, ps, mw, n, kT_d.a[co:co + mw, t0:t0 + n], kT_d, scale=1.0 / 16.0), TB)
                proj_tm(P, XTb, tXT, KC, W, 1024, 1024, lambda ps, co, pw, t0, n: evac_copy_to_dram(P, ps, n, pw, k_d.a[t0:t0 + n, co:co + pw], k_d, scale=1.0 / 16.0))
                proj_tm(P, XTb, tXT, KC, W, 2048, 1024, lambda ps, co, pw, t0, n: evac_copy_to_dram(P, ps, n, pw, v_d.a[t0:t0 + n, co:co + pw], v_d))
                proj_tm(P, XTb, tXT, KC, W, 3072, 1024, lambda ps, co, pw, t0, n: evac_copy_to_dram(P, ps, n, pw, go_d.a[t0:t0 + n, co:co + pw], go_d, func=AF.Sigmoid, mul=gml.a[:n, co:co + pw], multile=gml))
                gsc = {}
                for nm, c0, nr in (("gi_d", 4096, 4), ("gf_d", 4100, 4), ("gdt_d", 6152, 8)):
                    gsc[nm] = K.dscr(nm + str(l), [nr, NT])
                    proj_fm(P, XTb, tXT, KC, W, c0, nr, lambda ps, co, mw, t0, n, nm=nm: evac_copy_to_dram(P, ps, mw, n, gsc[nm].a[co:co + mw, t0:t0 + n], gsc[nm]), TB)
                proj_fm(P, XTb, tXT, KC, W, 4104, 512, lambda ps, co, mw, t0, n: evac_copy_to_dram(P, ps, mw, n, uT_d.a[co:co + mw, t0:t0 + n], uT_d), TB)
                proj_tm(P, XTb, tXT, KC, W, 4616, 512, lambda ps, co, pw, t0, n: evac_copy_to_dram(P, ps, n, pw, zs_d.a[t0:t0 + n, co:co + pw], zs_d, func=AF.Silu))
                proj_fm(P, XTb, tXT, KC, W, 5128, 1024, lambda ps, co, mw, t0, n: evac_copy_to_dram(P, ps, mw, n, xbcT_d.a[co:co + mw, t0:t0 + n], xbcT_d), TB)
            if stages < 2:
                break
            mixers(K, S, PS, I, O, l, dict(hT=hT, qT_d=qT_d, kT_d=kT_d, k_d=k_d, v_d=v_d, go_d=go_d, uT_d=uT_d, zs_d=zs_d,
                                           xbcT_d=xbcT_d, xcT_d=xcT_d, gi_d=gsc["gi_d"], gf_d=gsc["gf_d"], gdt_d=gsc["gdt_d"]),
                   XTb, tXT, ident, nident, maskT, ones_f, epsb, halfpi, gst, stages)
            if "mix_d" in K.dbg:
                with K.phase():
                    stg = Pool(K, "mstg", [128, 512], F32, 2)
                    for k in range(KC):
                        for (t0, n) in TB:
                            s_ = stg.next()
                            K.cp("dve", s_.a[:, :n], XTb[:, k, t0:t0 + n], [tXT], [s_])
                            K.stq(mix_d.a[k * 128:(k + 1) * 128, t0:t0 + n], s_.a[:, :n], s_, reads=[s_], mwrites=[mix_d])
            if stages < 3:
                break
            own = (l == DEPTH - 1)
            hcur = hO if own else hT
            tbl = TBO if own else TB
            if own:
                with K.phase():
                    for k in range(KC):
                        blend(XTb[:, k, 0:1024], XTb[:, k, 1024:2048], [tXT], tXT)
                    for k in range(KC):
                        K.cp("dve", XTb[:, k, 1024:NO], XTb[:, k, L:NT], [tXT], [tXT])
                    hsw = Pool(K, "hsw", [128, 2, 1024], F32, 2)
                    for k in range(KC):
                        t_ = hsw.next()
                        K.ld(t_.a[:], hT.a[k * 128:(k + 1) * 128, 0:L].rearrange("p (h t) -> p h t", h=2), t_, reads=[hT], writes=[t_])
                        blend(t_.a[:, 0, :], t_.a[:, 1, :], [t_], t_)
                        K.stq(hO.a[k * 128:(k + 1) * 128, 0:1024], t_.a[:, 0, :], t_, reads=[t_], mwrites=[hO])
                    S.dma("sp", hO.a[:, 1024:NO], hT.a[:, L:NT], hO, reads=[hT], mwrites=[hO])
            with K.phase():
                P = {"w": Pool(K, "w", [128, KC, 512], BF16, 3), "stg": Pool(K, "stg", [128, 512], F32, 4),
                     "hb": Pool(K, "hb", [128, 512], F32, 3)}
                def ev_res(ps, co, mw, t0, n):
                    hb = P["hb"].next()
                    K.ld(hb.a[:mw, :n], hcur.a[co:co + mw, t0:t0 + n], hb, reads=[hcur], writes=[hb])
                    stg = P["stg"].next()
                    K.tt("dve", stg.a[:mw, :n], ps.a[:mw, :n], hb.a[:mw, :n], ALU.add, [ps, hb], [stg])
                    K.stq(hcur.a[co:co + mw, t0:t0 + n], stg.a[:mw, :n], stg, reads=[stg], mwrites=[hcur])
                proj_fm(P, XTb, tXT, KC, I["w_out"].a[l], 0, D, ev_res, tbl)
            if stages < 4:
                break
            ffn_phase(K, S, PS, I, l, hcur, XTf, tXT, gn, ones_bf, epsb, ident, gst, norm_stage, stages, own)
            if stages < 5:
                break
            with K.phase():
                P = {"hblk": Pool(K, "hblk", [128, KC, 512], F32, 1), "sq": Pool(K, "sq", [128, KC, 512], BF16, 1),
                     "rs": Pool(K, "rs", [128, 512], F32, 2), "w": Pool(K, "w", [128, KC, 512], BF16, 2),
                     "stg": Pool(K, "stg", [128, 512], F32, 3), "hb": Pool(K, "hb", [128, 512], F32, 2)}
                pTf = K.sb("pTf", [128, 2, NT]); pTb = K.sb("pTb", [128, 2, NT], BF16)
                pTv = I["pT"].a[l].rearrange("(k p) t -> p k t", p=128)
                K.ld(pTf.a[:], pTv, pTf, writes=[pTf])
                if own:
                    for k in range(2):
                        blend(pTf.a[:, k, 0:1024], pTf.a[:, k, 1024:2048], [pTf], pTf)
                    for k in range(2):
                        K.cp("dve", pTf.a[:, k, 1024:NO], pTf.a[:, k, L:NT], [pTf], [pTf])
                K.cp("act", pTb.a[:], pTf.a[:], [pTf], [pTb])
                wple = K.sb("wple", [128, 2, D], BF16)
                K.ld(wple.a[:], I["w_ple"].a[l].rearrange("(k p) c -> p k c", p=128), wple, writes=[wple], q="pool")
                for (t0, n) in tbl:
                    norm_stage(hcur, gn["g_ple"].a[:, l, :], gn["g_ple"], t0, n, XTb[:, :, t0:t0 + n], tXT, P)
                def ev_ple(ps, co, mw, t0, n):
                    sg = P["stg"].next()
                    K.act(sg.a[:mw, :n], ps.a[:mw, :n], AF.Sigmoid, [ps], [sg])
                    ps2 = PS.next()
                    for k in range(2):
                        K.pe(ps2.a[:mw, :n], wple.a[:, k, co:co + mw], pTb.a[:, k, t0:t0 + n], k == 0, k == 1, [wple, pTb], [ps2])
                    K.tt("dve", sg.a[:mw, :n], ps2.a[:mw, :n], sg.a[:mw, :n], ALU.mult, [ps2, sg], [sg])
                    hb = P["hb"].next()
                    K.ld(hb.a[:mw, :n], hcur.a[co:co + mw, t0:t0 + n], hb, reads=[hcur], writes=[hb])
                    K.tt("dve", sg.a[:mw, :n], sg.a[:mw, :n], hb.a[:mw, :n], ALU.add, [sg, hb], [sg])
                    K.stq(hcur.a[co:co + mw, t0:t0 + n], sg.a[:mw, :n], sg, reads=[sg], mwrites=[hcur])
                proj_fm(P, XTb, tXT, KC, I["w_pleg"].a[l], 0, D, ev_ple, tbl)
        with K.phase():
            P = {"hblk": Pool(K, "hblk", [128, KC, 512], F32, 1), "sq": Pool(K, "sq", [128, KC, 512], BF16, 1),
                 "rs": Pool(K, "rs", [128, 512], F32, 2)}
            yb = K.sb("yb", [128, KC, 512])
            for (t0, n) in TBO:
                norm_stage(hO, gfin.a, gfin, t0, n, yb.a[:, :, :n], yb, P)
                K.stq(O["yT"].a.rearrange("(k p) t -> p k t", p=128)[:, :, t0:t0 + n], yb.a[:, :, :n], yb, reads=[yb], mwrites=[O["yT"]], is_output=True)
    return nc, K


def mixers(K, S, PS, I, O, l, Dm, XTb, tXT, ident, nident, maskT, ones_f, epsb, halfpi, gst, stages):
    with K.phase():
        rowt = [K.sb("row%d" % i, [8, NT]) for i in range(7)]
        A, Bt, Ct, Dt, Et, Fa, Gm = rowt
        onesr = K.sb("onesr", [8, 1]); K.memset("dve", onesr.a[:], 1.0, [onesr])
        TM = K.sb("TM", [64, NCI, 20])
        TMB = K.sb("TMB", [64, NCI, 32])
        GLb = K.sb("GLb", [128, 4 * NCI]); DECb = K.sb("DECb", [128, 8 * NCI])
        sel4 = K.sb("sel4", [4, 4, 128]); K.ld(sel4.a[:], I["sel4"].a, sel4, writes=[sel4])
        sel8 = K.sb("sel8", [8, 8, 128]); K.ld(sel8.a[:], I["sel8"].a, sel8, writes=[sel8])
        small = Pool(K, "small", [8, NCI], F32, 4)

        def to_tm(src, nr, dst, col0):
            for (t0, cl, ci) in CHUNKS:
                ps = PS.next()
                K.tr(ps.a[:cl, :nr], src.a[:nr, t0:t0 + cl], ident.a[:nr, :nr], [src, ident], [ps])
                K.cp("act" if ci % 2 else "dve", dst.a[:cl, ci, col0:col0 + nr], ps.a[:cl, :nr], [ps], [dst])

        def bcast_rows(rows, nr, sel, dst, ncol):
            for h in range(nr):
                ps = PS.next()
                K.pe(ps.a[:, :ncol], sel.a[:nr, h, :], rows.a[:nr, :ncol], True, True, [sel, rows], [ps])
                K.cp("act", dst.a[:, h * ncol:(h + 1) * ncol], ps.a[:, :ncol], [ps], [dst])

        def chunkview(t, nr):
            return t.a[:nr, :L].rearrange("p (c t) -> p c t", t=64)

        def prevlast(Mt, nr, init_s, prev, last):
            K.memset("dve", prev.a[:nr, 0:1], 0.0, [prev])
            K.cp("dve", prev.a[:nr, 1:32], Mt.a[:nr, 63:L - 64:64], [Mt], [prev])
            if init_s is None:
                K.memset("dve", prev.a[:nr, 32:NCI], 0.0, [prev])
            else:
                K.cp("dve", prev.a[:nr, 32:NCI], init_s, [Mt], [prev])
            K.cp("dve", last.a[:nr, 0:32], Mt.a[:nr, 63:L:64], [Mt], [last])
            K.cp("dve", last.a[:nr, 32:NCI], Mt.a[:nr, L:NT], [Mt], [last])

        def sub_chunk(out, x, cvals, nr, sign):
            cb = cvals.a[:nr, 0:32].unsqueeze(2).to_broadcast([nr, 32, 64])
            if sign > 0:
                K.tt("dve", chunkview(out, nr), chunkview(x, nr), cb, ALU.subtract, [x, cvals], [out])
                K.tt("dve", out.a[:nr, L:NT], x.a[:nr, L:NT], cvals.a[:nr, 32:NCI], ALU.subtract, [x, cvals], [out])
            else:
                K.tt("dve", chunkview(out, nr), cb, chunkview(x, nr), ALU.subtract, [x, cvals], [out])
                K.tt("dve", out.a[:nr, L:NT], cvals.a[:nr, 32:NCI], x.a[:nr, L:NT], ALU.subtract, [x, cvals], [out])

        def softplus_neg(x, t1, t2, nr, neg_in):
            K.act(t1.a[:nr, :], x.a[:nr, :], AF.Abs, [x], [t1])
            K.act(t1.a[:nr, :], t1.a[:nr, :], AF.Exp, [t1], [t1], scale=-1.0)
            K.act(t1.a[:nr, :], t1.a[:nr, :], AF.Ln, [t1], [t1], bias=1.0)
            K.ts("dve", t2.a[:nr, :], x.a[:nr, :], 0.0, ALU.min if neg_in else ALU.max, [x], [t2])

        bi = K.sb("bi", [4, 1]); bf = K.sb("bf", [4, 1]); m0s = K.sb("m0s", [4, NS])
        K.ld(bi.a[:], I["b_ig"].a[l], bi, writes=[bi]); K.ld(bf.a[:], I["b_fg"].a[l], bf, writes=[bf])
        K.ld(m0s.a[:], I["smT"].a[l], m0s, writes=[m0s])
        K.ld(A.a[:4, :], Dm["gi_d"].a, A, reads=[Dm["gi_d"]], writes=[A])
        K.ld(Bt.a[:4, :], Dm["gf_d"].a, Bt, reads=[Dm["gf_d"]], writes=[Bt])
        K.ts("dve", A.a[:4, :], A.a[:4, :], bi.a[:, 0:1], ALU.add, [A, bi], [A])
        K.ts("dve", Bt.a[:4, :], Bt.a[:4, :], bf.a[:, 0:1], ALU.add, [Bt, bf], [Bt])
        softplus_neg(Bt, Ct, Dt, 4, True)
        K.tt("dve", Bt.a[:4, :], Dt.a[:4, :], Ct.a[:4, :], ALU.subtract, [Dt, Ct], [Bt])
        K.scan(Et.a[:4, :L], onesr.a[:4, 0:1].to_broadcast([4, L]), Bt.a[:4, :L], 0.0, ALU.mult, ALU.add, [onesr, Bt], [Et])
        K.cp("dve", Et.a[:4, L:NT], Bt.a[:4, L:NT], [Bt], [Et])
        K.tt("dve", Fa.a[:4, :], A.a[:4, :], Et.a[:4, :], ALU.subtract, [A, Et], [Fa])
        K.scan(Gm.a[:4, :L], Fa.a[:4, :L], Fa.a[:4, :L], 0.0, ALU.max, ALU.max, [Fa], [Gm])
        K.tt("dve", Gm.a[:4, L:NT], Fa.a[:4, L:NT], m0s.a[:], ALU.max, [Fa, m0s], [Gm])
        Mprev = small.next(); Mlast = small.next()
        prevlast(Gm, 4, m0s.a[:], Mprev, Mlast)
        glr = small.next()
        K.tt("dve", glr.a[:4, :], Mprev.a[:4, :], Mlast.a[:4, :], ALU.subtract, [Mprev, Mlast], [glr])
        K.act(glr.a[:4, :], glr.a[:4, :], AF.Exp, [glr], [glr])
        bcast_rows(glr, 4, sel4, GLb, NCI)
        sub_chunk(A, Gm, Mprev, 4, -1)
        K.act(A.a[:4, :], A.a[:4, :], AF.Exp, [A], [A])
        sub_chunk(Dt, Fa, Mlast, 4, +1)
        K.act(Dt.a[:4, :], Dt.a[:4, :], AF.Exp, [Dt], [Dt])
        K.tt("dve", Ct.a[:4, :], Et.a[:4, :], Gm.a[:4, :], ALU.add, [Et, Gm], [Ct])
        K.stq(O["m_pT"].a[l], Ct.a[:4, L - 1:L], Ct, reads=[Ct], mwrites=[O["m_pT"]], is_output=True)
        K.stq(O["m_sT"].a[l], Ct.a[:4, L:NT], Ct, reads=[Ct], mwrites=[O["m_sT"]], is_output=True)
        K.act(Ct.a[:4, :], Ct.a[:4, :], AF.Exp, [Ct], [Ct], scale=-1.0)
        for src, c0 in ((Fa, 0), (Gm, 4), (A, 8), (Ct, 12), (Dt, 16)):
            to_tm(src, 4, TM, c0)

        cpool = {"q": Pool(K, "mq", [128, 8, 64], F32, 2), "k": Pool(K, "mk", [128, 8, 64], F32, 2),
                 "kt": Pool(K, "mkt", [64, 1024], F32, 1), "v": Pool(K, "mv", [64, 4, 257], F32, 2),
                 "go": Pool(K, "mgo", [64, 1024], F32, 1), "hm": Pool(K, "mhm", [64, 1024], F32, 1),
                 "w": Pool(K, "mw", [64, 64], F32, 8), "sw": Pool(K, "msw", [64, 64], F32, 8),
                 "nsb": Pool(K, "mnsb", [64, 257], F32, 4), "res": Pool(K, "mres", [64, 257], F32, 4),
                 "kw": Pool(K, "mkw", [64, 256], F32, 4), "sc": Pool(K, "msc", [64, 4, 4], F32, 3),
                 "junk": Pool(K, "mjunk", [64, 256], F32, 2)}
        for b_ in cpool["v"].bufs:
            K.memset("dve", b_.a[:, :, 256:257], 1.0, [b_])
        Cst = [K.sb("Caug%d" % h, [128, 2, 257]) for h in range(4)]

        PSs, PSb = PS, PS

        def mlstm_chunk(t0, cl, ci, Caug):
            nk = dict(allow_slow_non_contiguous=True) if cl == 1 else {}
            q = cpool["q"].next(); kT = cpool["k"].next(); kt = cpool["kt"].next(); v = cpool["v"].next(); go = cpool["go"].next()
            K.ld(q.a[:, :, :cl], Dm["qT_d"].a.rearrange("(j p) t -> p j t", p=128)[:, :, t0:t0 + cl], q, reads=[Dm["qT_d"]], writes=[q], **nk)
            K.ld(kT.a[:, :, :cl], Dm["kT_d"].a.rearrange("(j p) t -> p j t", p=128)[:, :, t0:t0 + cl], kT, reads=[Dm["kT_d"]], writes=[kT], **nk)
            K.ld(kt.a[:cl, :], Dm["k_d"].a[t0:t0 + cl, :], kt, reads=[Dm["k_d"]], writes=[kt])
            K.ld(v.a[:cl, :, 0:256], Dm["v_d"].a[t0:t0 + cl, :].rearrange("t (h d) -> t h d", h=4), v, reads=[Dm["v_d"]], writes=[v])
            K.ld(go.a[:cl, :], Dm["go_d"].a[t0:t0 + cl, :], go, reads=[Dm["go_d"]], writes=[go])
            hm = cpool["hm"].next()
            H = range(4)
            ps_s = {}; ps_d = {}; w = {}; sw = {}; ps_n = {}; ps_i = {}; nsb = {}; res = {}; sc = {}; kw = {}
            for h in H:
                ps_s[h] = PSs.next()
                for kc in range(2):
                    K.pe(ps_s[h].a[:cl, :cl], kT.a[:, h * 2 + kc, :cl], q.a[:, h * 2 + kc, :cl], kc == 0, kc == 1, [kT, q], [ps_s[h]])
            for h in H:
                ps_d[h] = PSs.next()
                K.pe(ps_d[h].a[:cl, :cl], TM.a[:cl, ci, 4 + h:5 + h].to_broadcast([cl, cl]), nident.a[:cl, :cl], True, False, [TM, nident], [ps_d[h]])
                K.pe(ps_d[h].a[:cl, :cl], ident.a[:cl, :cl], maskT.a[:cl, :cl], False, False, [ident, maskT], [ps_d[h]])
                K.pe(ps_d[h].a[:cl, :cl], ident.a[:cl, :cl], TM.a[:cl, ci, h:h + 1].to_broadcast([cl, cl]), False, True, [ident, TM], [ps_d[h]])
            for h in H:
                w[h] = cpool["w"].next()
                K.act(w[h].a[:cl, :cl], ps_d[h].a[:cl, :cl], AF.Exp, [ps_d[h]], [w[h]])
            for h in H:
                kw[h] = cpool["kw"].next()
                K.act(kw[h].a[:cl, :], kt.a[:cl, h * 256:(h + 1) * 256], AF.Copy, [kt, TM], [kw[h]], scale=TM.a[:cl, ci, 16 + h:17 + h])
            for h in H:
                sw[h] = cpool["sw"].next()
                K.tt("dve", sw[h].a[:cl, :cl], ps_s[h].a[:cl, :cl], w[h].a[:cl, :cl], ALU.mult, [ps_s[h], w[h]], [sw[h]])
            for h in H:
                ps_n[h] = PSb.next()
                K.pe(ps_n[h].a[:cl, :257], sw[h].a[:cl, :cl], v.a[:cl, h, :], True, True, [sw[h], v], [ps_n[h]])
            for h in H:
                nsb[h] = cpool["nsb"].next()
                K.cp("act", nsb[h].a[:cl, :], ps_n[h].a[:cl, :257], [ps_n[h]], [nsb[h]])
            for h in H:
                ps_i[h] = PSb.next()
                for kc in range(2):
                    K.pe(ps_i[h].a[:cl, :257], q.a[:, h * 2 + kc, :cl], Caug[h].a[:, kc, :], kc == 0, kc == 1, [q, Caug[h]], [ps_i[h]])
            for h in H:
                res[h] = cpool["res"].next()
                K.stt(res[h].a[:cl, :], ps_i[h].a[:cl, :257], TM.a[:cl, ci, 8 + h:9 + h], nsb[h].a[:cl, :], ALU.mult, ALU.add, [ps_i[h], TM, nsb[h]], [res[h]])
            for hp in range(2):
                pcs = {}
                for h in (2 * hp, 2 * hp + 1):
                    for kc in range(2):
                        pcs[(h, kc)] = PSb.next()
                        K.pe(pcs[(h, kc)].a[:, :257], kw[h].a[:cl, kc * 128:(kc + 1) * 128], v.a[:cl, h, :], True, True, [kw[h], v], [pcs[(h, kc)]])
                for h in (2 * hp, 2 * hp + 1):
                    for kc in range(2):
                        K.stt(Caug[h].a[:, kc, :], Caug[h].a[:, kc, :], GLb.a[:, h * NCI + ci:h * NCI + ci + 1], pcs[(h, kc)].a[:, :257], ALU.mult, ALU.add, [Caug[h], GLb, pcs[(h, kc)]], [Caug[h]])
            sca = cpool["sc"].next()
            for h in H:
                K.act(sca.a[:cl, h, 0:1], res[h].a[:cl, 256:257], AF.Abs, [res[h]], [sca])
            K.tt("dve", sca.a[:cl, :, 0], sca.a[:cl, :, 0], TM.a[:cl, ci, 12:16], ALU.max, [sca, TM], [sca])
            K.recip(sca.a[:cl, :, 0], sca.a[:cl, :, 0], [sca], [sca])
            for h in H:
                junk = cpool["junk"].next()
                K.act(junk.a[:cl, :], res[h].a[:cl, 0:256], AF.Square, [res[h], sca], [junk, sca], scale=sca.a[:cl, h, 0:1], accum=sca.a[:cl, h, 1:2])
            K.act(sca.a[:cl, :, 2], sca.a[:cl, :, 1], AF.Sqrt, [sca, epsb], [sca], scale=1.0 / 256.0, bias=epsb.a[:cl, :])
            K.recip(sca.a[:cl, :, 2], sca.a[:cl, :, 2], [sca], [sca])
            K.tt("dve", sca.a[:cl, :, 3], sca.a[:cl, :, 2], sca.a[:cl, :, 0], ALU.mult, [sca], [sca])
            for h in H:
                K.stt(hm.a[:cl, h * 256:(h + 1) * 256], res[h].a[:cl, 0:256], sca.a[:cl, h, 3:4], go.a[:cl, h * 256:(h + 1) * 256], ALU.mult, ALU.mult, [res[h], sca, go], [hm])
            for j in range(8):
                ps = PSs.next()
                K.tr(ps.a[:, :cl], hm.a[:cl, j * 128:(j + 1) * 128], ident.a[:cl, :cl], [hm, ident], [ps])
                K.cp("act" if j % 2 else "dve", XTb[:, j, t0:t0 + cl], ps.a[:, :cl], [ps], [tXT])

        for h in range(4):
            K.memset("dve", Cst[h].a[:], 0.0, [Cst[h]])
        Cs2 = [K.sb("Csmp%d" % h, [128, 2, 257]) for h in range(4)]

        def sample_step(t0, cl, ci):
            j = ci - 32
            Cs = Cs2
            for h in range(4):
                K.ld(Cs[h].a[:, :, 0:256], I["sC"].a[l, j, h].rearrange("(kc p) d -> p kc d", p=128), Cs[h], writes=[Cs[h]])
                K.ld(Cs[h].a[:, :, 256], I["sn"].a[l, j, h].rearrange("(kc p) -> p kc", p=128), Cs[h], writes=[Cs[h]], allow_slow_non_contiguous=True)
            mlstm_chunk(t0, cl, ci, Cs)
            for h in range(4):
                K.stq(O["C_s"].a[l, j, h].rearrange("(kc p) d -> p kc d", p=128), Cs[h].a[:, :, 0:256], Cs[h], reads=[Cs[h]], mwrites=[O["C_s"]], is_output=True)
                K.stq(O["n_s"].a[l, j, h].rearrange("(kc p) -> p kc", p=128), Cs[h].a[:, :, 256], Cs[h], reads=[Cs[h]], mwrites=[O["n_s"]], is_output=True, allow_slow_non_contiguous=True)

        for i, (t0, cl, ci) in enumerate(CHUNKS[:32]):
            mlstm_chunk(t0, cl, ci, Cst)
            if i % 2 == 1:
                sample_step(*CHUNKS[32 + i // 2])
        for h in range(4):
            K.stq(O["C_p"].a[l, h].rearrange("(kc p) d -> p kc d", p=128), Cst[h].a[:, :, 0:256], Cst[h], reads=[Cst[h]], mwrites=[O["C_p"]], is_output=True)
            K.stq(O["n_p"].a[l, h].rearrange("(kc p) -> p kc", p=128), Cst[h].a[:, :, 256], Cst[h], reads=[Cst[h]], mwrites=[O["n_p"]], is_output=True, allow_slow_non_contiguous=True)
    if stages >= 2.3:
        ssd_mixer(K, S, PS, I, O, l, Dm, XTb, tXT, ident, nident, maskT, epsb)
    if stages >= 2.6:
        s5_mixer(K, S, PS, I, O, l, Dm, XTb, tXT, ident, ones_f, epsb, halfpi)


def _mk_helpers(K, PS, ident):
    def to_tm(src, nr, dst, col0):
        for (t0, cl, ci) in CHUNKS:
            ps = PS.next()
            K.tr(ps.a[:cl, :nr], src.a[:nr, t0:t0 + cl], ident.a[:nr, :nr], [src, ident], [ps])
            K.cp("act" if ci % 2 else "dve", dst.a[:cl, ci, col0:col0 + nr], ps.a[:cl, :nr], [ps], [dst])

    def bcast_rows(rows, nr, sel, dst, ncol):
        for h in range(nr):
            ps = PS.next()
            K.pe(ps.a[:, :ncol], sel.a[:nr, h, :], rows.a[:nr, :ncol], True, True, [sel, rows], [ps])
            K.cp("act", dst.a[:, h * ncol:(h + 1) * ncol], ps.a[:, :ncol], [ps], [dst])

    def chunkview(t, nr):
        return t.a[:nr, :L].rearrange("p (c t) -> p c t", t=64)

    def prevlast(Mt, nr, init_s, prev, last):
        K.memset("dve", prev.a[:nr, 0:1], 0.0, [prev])
        K.cp("dve", prev.a[:nr, 1:32], Mt.a[:nr, 63:L - 64:64], [Mt], [prev])
        if init_s is None:
            K.memset("dve", prev.a[:nr, 32:NCI], 0.0, [prev])
        else:
            K.cp("dve", prev.a[:nr, 32:NCI], init_s, [Mt], [prev])
        K.cp("dve", last.a[:nr, 0:32], Mt.a[:nr, 63:L:64], [Mt], [last])
        K.cp("dve", last.a[:nr, 32:NCI], Mt.a[:nr, L:NT], [Mt], [last])

    def sub_chunk(out, x, cvals, nr, sign):
        cb = cvals.a[:nr, 0:32].unsqueeze(2).to_broadcast([nr, 32, 64])
        if sign > 0:
            K.tt("dve", chunkview(out, nr), chunkview(x, nr), cb, ALU.subtract, [x, cvals], [out])
            K.tt("dve", out.a[:nr, L:NT], x.a[:nr, L:NT], cvals.a[:nr, 32:NCI], ALU.subtract, [x, cvals], [out])
        else:
            K.tt("dve", chunkview(out, nr), cb, chunkview(x, nr), ALU.subtract, [x, cvals], [out])
            K.tt("dve", out.a[:nr, L:NT], cvals.a[:nr, 32:NCI], x.a[:nr, L:NT], ALU.subtract, [x, cvals], [out])
    return to_tm, bcast_rows, prevlast, sub_chunk


def ssd_mixer(K, S, PS, I, O, l, Dm, XTb, tXT, ident, nident, maskT, epsb):
    xbcT_d, xcT_d = Dm["xbcT_d"], Dm["xcT_d"]
    with K.phase():
        to_tm, bcast_rows, prevlast, sub_chunk = _mk_helpers(K, PS, ident)
        cw = K.sb("cw", [128, 8, 4]); cb = K.sb("cb", [128, 8]); cin = K.sb("cin", [128, 8, 3, NS])
        K.ld(cw.a[:], I["conv_w"].a[l], cw, writes=[cw]); K.ld(cb.a[:], I["conv_b"].a[l], cb, writes=[cb])
        K.ld(cin.a[:], I["convT"].a[l], cin, writes=[cin])
        xp = Pool(K, "xp", [128, 3 + 512], F32, 2); xo = Pool(K, "xo", [128, 512], F32, 2)
        for j in range(8):
            rows = slice(j * 128, (j + 1) * 128)
            for (t0, n) in TB[:4]:
                x = xp.next()
                K.ld(x.a[:, 3:3 + n], xbcT_d.a[rows, t0:t0 + n], x, reads=[xbcT_d], writes=[x])
                if t0 == 0:
                    K.memset("dve", x.a[:, 0:3], 0.0, [x])
                else:
                    K.ld(x.a[:, 0:3], xbcT_d.a[rows, t0 - 3:t0], x, reads=[xbcT_d], writes=[x])
                o = xo.next()
                K.ts("dve", o.a[:, :n], x.a[:, 3:3 + n], cw.a[:, j, 3:4], ALU.mult, [x, cw], [o])
                for tap in (2, 1, 0):
                    K.stt(o.a[:, :n], x.a[:, tap:tap + n], cw.a[:, j, tap:tap + 1], o.a[:, :n], ALU.mult, ALU.add, [x, cw, o], [o])
                K.act(o.a[:, :n], o.a[:, :n], AF.Silu, [o, cb], [o], bias=cb.a[:, j:j + 1])
                K.stq(xcT_d.a[rows, t0:t0 + n], o.a[:, :n], o, reads=[o], mwrites=[xcT_d])
                if t0 == 1536:
                    K.stq(O["conv_pT"].a[l, :, j, :], x.a[:, 512:515], x, reads=[x], mwrites=[O["conv_pT"]], is_output=True)
            x = xp.next()
            K.ld(x.a[:, 0:NS], xbcT_d.a[rows, L:NT], x, reads=[xbcT_d], writes=[x])
            o = xo.next()
            K.ts("dve", o.a[:, :NS], x.a[:, 0:NS], cw.a[:, j, 3:4], ALU.mult, [x, cw], [o])
            for tap in (2, 1, 0):
                K.stt(o.a[:, :NS], cin.a[:, j, tap, :], cw.a[:, j, tap:tap + 1], o.a[:, :NS], ALU.mult, ALU.add, [cin, cw, o], [o])
            K.act(o.a[:, :NS], o.a[:, :NS], AF.Silu, [o, cb], [o], bias=cb.a[:, j:j + 1])
            K.stq(xcT_d.a[rows, L:NT], o.a[:, :NS], o, reads=[o], mwrites=[xcT_d])
            K.stq(O["conv_sT"].a[l, :, j, 0:2, :], cin.a[:, j, 1:3, :], cin, reads=[cin], mwrites=[O["conv_sT"]], is_output=True)
            K.stq(O["conv_sT"].a[l, :, j, 2, :], x.a[:, 0:NS], x, reads=[x], mwrites=[O["conv_sT"]], is_output=True)
        A, Bt, Ct, Dt, Et, Fa, Gm = [K.sb("srow%d" % i, [8, NT]) for i in range(7)]
        onesr = K.sb("onesr", [8, 1]); K.memset("dve", onesr.a[:], 1.0, [onesr])
        TMB = K.sb("TMB", [64, NCI, 32]); DECb = K.sb("DECb", [128, 8 * NCI])
        sel8 = K.sb("sel8", [8, 8, 128]); K.ld(sel8.a[:], I["sel8"].a, sel8, writes=[sel8])
        small = Pool(K, "ssmall", [8, NCI], F32, 4)
        dtb = K.sb("dtb", [8, 1]); alog = K.sb("alog", [8, 1])
        K.ld(dtb.a[:], I["dt_bias"].a[l], dtb, writes=[dtb]); K.ld(alog.a[:], I["a_log"].a[l], alog, writes=[alog])
        K.act(alog.a[:], alog.a[:], AF.Exp, [alog], [alog])
        K.ts("dve", alog.a[:], alog.a[:], -1.0, ALU.mult, [alog], [alog])
        K.ld(A.a[:, :], Dm["gdt_d"].a, A, reads=[Dm["gdt_d"]], writes=[A])
        K.ts("dve", A.a[:, :], A.a[:, :], dtb.a[:, 0:1], ALU.add, [A, dtb], [A])
        K.act(Ct.a[:, :], A.a[:, :], AF.Abs, [A], [Ct])
        K.act(Ct.a[:, :], Ct.a[:, :], AF.Exp, [Ct], [Ct], scale=-1.0)
        K.act(Ct.a[:, :], Ct.a[:, :], AF.Ln, [Ct], [Ct], bias=1.0)
        K.ts("dve", Dt.a[:, :], A.a[:, :], 0.0, ALU.max, [A], [Dt])
        K.tt("dve", A.a[:, :], Dt.a[:, :], Ct.a[:, :], ALU.add, [Dt, Ct], [A])
        K.ts("dve", Bt.a[:, :], A.a[:, :], alog.a[:, 0:1], ALU.mult, [A, alog], [Bt])
        K.scan(Et.a[:, :L], onesr.a[:, 0:1].to_broadcast([8, L]), Bt.a[:, :L], 0.0, ALU.mult, ALU.add, [onesr, Bt], [Et])
        K.cp("dve", Et.a[:, L:NT], Bt.a[:, L:NT], [Bt], [Et])
        Gprev = small.next(); Glast = small.next(); decr = small.next()
        prevlast(Et, 8, None, Gprev, Glast)
        K.tt("dve", decr.a[:, :], Glast.a[:, :], Gprev.a[:, :], ALU.subtract, [Glast, Gprev], [decr])
        K.act(decr.a[:, :], decr.a[:, :], AF.Exp, [decr], [decr])
        bcast_rows(decr, 8, sel8, DECb, NCI)
        sub_chunk(Fa, Et, Gprev, 8, +1)
        K.act(Fa.a[:, :], Fa.a[:, :], AF.Exp, [Fa], [Fa])
        sub_chunk(Gm, Et, Glast, 8, -1)
        K.act(Gm.a[:, :], Gm.a[:, :], AF.Exp, [Gm], [Gm])
        for src, c0 in ((Et, 0), (Fa, 8), (Gm, 16), (A, 24)):
            to_tm(src, 8, TMB, c0)
        dd = K.sb("ssdd", [64, 8]); gs = K.sb("gssd", [64, 512])
        K.ld(dd.a[:], I["ssdd_rep"].a[l], dd, writes=[dd]); K.ld(gs.a[:], I["gssd_rep"].a[l], gs, writes=[gs])
        cp = {"xc": Pool(K, "sxc", [128, 8, 64], F32, 2), "zs": Pool(K, "szs", [64, 512], F32, 2),
              "xtok": Pool(K, "sxt", [64, 512], F32, 2), "btok": Pool(K, "sbt", [64, 256], F32, 2),
              "sc": Pool(K, "ssc", [64, 2, 64], F32, 2), "seg": Pool(K, "sseg", [64, 64], F32, 8),
              "xdt": Pool(K, "sxdt", [64, 64], F32, 8), "xw": Pool(K, "sxw", [64, 64], F32, 8),
              "y1": Pool(K, "sy1", [64, 64], F32, 8), "yss": Pool(K, "syss", [64, 512], F32, 2),
              "s": Pool(K, "ss", [64, 4], F32, 2), "junk": Pool(K, "sjunk", [64, 512], F32, 1)}
        STp0 = [K.sb("ST%d" % h, [128, 64]) for h in range(8)]
        ST = list(STp0)

        def chunk(t0, cl, ci):
            nk = dict(allow_slow_non_contiguous=True) if cl == 1 else {}
            xc = cp["xc"].next(); zs = cp["zs"].next()
            K.ld(xc.a[:, :, :cl], xcT_d.a.rearrange("(j p) t -> p j t", p=128)[:, :, t0:t0 + cl], xc, reads=[xcT_d], writes=[xc], **nk)
            K.ld(zs.a[:cl, :], Dm["zs_d"].a[t0:t0 + cl, :], zs, reads=[Dm["zs_d"]], writes=[zs])
            xtok = cp["xtok"].next(); btok = cp["btok"].next()
            for j in range(6):
                ps = PS.next()
                K.tr(ps.a[:cl, :128], xc.a[:, j, :cl], ident.a[:, :], [xc, ident], [ps])
                if j < 4:
                    K.cp("act" if j % 2 else "dve", xtok.a[:cl, j * 128:(j + 1) * 128], ps.a[:cl, :128], [ps], [xtok])
                else:
                    K.cp("act" if j % 2 else "dve", btok.a[:cl, (j - 4) * 128:(j - 3) * 128], ps.a[:cl, :128], [ps], [btok])
            sc = cp["sc"].next()
            for g in range(2):
                ps = PS.next()
                K.pe(ps.a[:cl, :cl], xc.a[:, 4 + g, :cl], xc.a[:, 6 + g, :cl], True, True, [xc], [ps])
                K.cp("act", sc.a[:cl, g, :cl], ps.a[:cl, :cl], [ps], [sc])
            yss = cp["yss"].next()
            for g in range(2):
                HH = range(4 * g, 4 * g + 4)
                ps_d = {}; seg = {}; xdt = {}; ps1 = {}; ps2 = {}; y1 = {}; xw = {}; ps3 = {}
                for h in HH:
                    ps_d[h] = PS.next()
                    K.pe(ps_d[h].a[:cl, :cl], TMB.a[:cl, ci, h:h + 1].to_broadcast([cl, cl]), ident.a[:cl, :cl], True, False, [TMB, ident], [ps_d[h]])
                    K.pe(ps_d[h].a[:cl, :cl], ident.a[:cl, :cl], maskT.a[:cl, :cl], False, False, [ident, maskT], [ps_d[h]])
                    K.pe(ps_d[h].a[:cl, :cl], nident.a[:cl, :cl], TMB.a[:cl, ci, h:h + 1].to_broadcast([cl, cl]), False, True, [nident, TMB], [ps_d[h]])
                for h in HH:
                    seg[h] = cp["seg"].next()
                    K.act(seg[h].a[:cl, :cl], ps_d[h].a[:cl, :cl], AF.Exp, [ps_d[h]], [seg[h]])
                for h in HH:
                    xdt[h] = cp["xdt"].next()
                    K.act(xdt[h].a[:cl, :], xtok.a[:cl, h * 64:(h + 1) * 64], AF.Copy, [xtok, TMB], [xdt[h]], scale=TMB.a[:cl, ci, 24 + h:25 + h])
                for h in HH:
                    K.tt("dve", seg[h].a[:cl, :cl], seg[h].a[:cl, :cl], sc.a[:cl, g, :cl], ALU.mult, [seg[h], sc], [seg[h]])
                for h in HH:
                    xw[h] = cp["xw"].next()
                    K.act(xw[h].a[:cl, :], xdt[h].a[:cl, :], AF.Copy, [xdt[h], TMB], [xw[h]], scale=TMB.a[:cl, ci, 16 + h:17 + h])
                for h in HH:
                    ps1[h] = PS.next()
                    K.pe(ps1[h].a[:cl, :64], seg[h].a[:cl, :cl], xdt[h].a[:cl, :], True, True, [seg[h], xdt[h]], [ps1[h]])
                for h in HH:
                    ps2[h] = PS.next()
                    K.pe(ps2[h].a[:cl, :64], xc.a[:, 6 + g, :cl], ST[h].a[:, :], True, True, [xc, ST[h]], [ps2[h]])
                for h in HH:
                    y1[h] = cp["y1"].next()
                    K.cp("act", y1[h].a[:cl, :], ps1[h].a[:cl, :64], [ps1[h]], [y1[h]])
                for h in HH:
                    K.stt(y1[h].a[:cl, :], ps2[h].a[:cl, :64], TMB.a[:cl, ci, 8 + h:9 + h], y1[h].a[:cl, :], ALU.mult, ALU.add, [ps2[h], TMB, y1[h]], [y1[h]])
                for h in HH:
                    ps3[h] = PS.next()
                    K.pe(ps3[h].a[:, :64], btok.a[:cl, g * 128:(g + 1) * 128], xw[h].a[:cl, :], True, True, [btok, xw[h]], [ps3[h]])
                for h in HH:
                    K.stt(ST[h].a[:, :], ST[h].a[:, :], DECb.a[:, h * NCI + ci:h * NCI + ci + 1], ps3[h].a[:, :64], ALU.mult, ALU.add, [ST[h], DECb, ps3[h]], [ST[h]])
                for h in HH:
                    K.stt(yss.a[:cl, h * 64:(h + 1) * 64], xtok.a[:cl, h * 64:(h + 1) * 64], dd.a[:cl, h:h + 1], y1[h].a[:cl, :], ALU.mult, ALU.add, [xtok, dd, y1[h]], [yss])
            K.tt("dve", yss.a[:cl, :], yss.a[:cl, :], zs.a[:cl, :], ALU.mult, [yss, zs], [yss])
            s_ = cp["s"].next(); junk = cp["junk"].next()
            K.act(junk.a[:cl, :], yss.a[:cl, :], AF.Square, [yss], [junk, s_], accum=s_.a[:cl, 0:1])
            K.act(s_.a[:cl, 1:2], s_.a[:cl, 0:1], AF.Sqrt, [s_, epsb], [s_], scale=1.0 / 512.0, bias=epsb.a[:cl, :])
            K.recip(s_.a[:cl, 1:2], s_.a[:cl, 1:2], [s_], [s_])
            K.stt(yss.a[:cl, :], yss.a[:cl, :], s_.a[:cl, 1:2], gs.a[:cl, :], ALU.mult, ALU.mult, [yss, s_, gs], [yss])
            for j in range(4):
                ps = PS.next()
                K.tr(ps.a[:, :cl], yss.a[:cl, j * 128:(j + 1) * 128], ident.a[:cl, :cl], [yss, ident], [ps])
                K.cp("act" if j % 2 else "dve", XTb[:, 12 + j, t0:t0 + cl], ps.a[:, :cl], [ps], [tXT])

        for h in range(8):
            K.memset("dve", STp0[h].a[:], 0.0, [STp0[h]])
        STp = list(STp0)
        STs = [K.sb("STs%d" % h, [128, 64]) for h in range(8)]
        for i, (t0, cl, ci) in enumerate(CHUNKS[:32]):
            ST[:] = STp
            chunk(t0, cl, ci)
            if i % 2 == 1:
                (t0s, cls, cis) = CHUNKS[32 + i // 2]
                j = cis - 32
                ST[:] = STs
                for h in range(8):
                    K.ld(ST[h].a[:, :], I["ssdT"].a[l, j, h], ST[h], writes=[ST[h]])
                chunk(t0s, cls, cis)
                for h in range(8):
                    K.stq(O["ssd_sT"].a[l, j, h], ST[h].a[:, :], ST[h], reads=[ST[h]], mwrites=[O["ssd_sT"]], is_output=True)
        ST[:] = STp
        for h in range(8):
            K.stq(O["ssd_pT"].a[l, h], ST[h].a[:, :], ST[h], reads=[ST[h]], mwrites=[O["ssd_pT"]], is_output=True)


def s5_mixer(K, S, PS, I, O, l, Dm, XTb, tXT, ident, ones_f, epsb, halfpi):
    uT_d = Dm["uT_d"]
    with K.phase():
        def P16(name):
            return K.sb(name, [128, 16])
        lre, lim, dtt, th, r, c, s_, t1, t2, t3, lbr, lbi, nlbi, kr, ki, den, ka, cn, sn = [P16("s5p%d" % i) for i in range(19)]
        K.ld(lre.a[:], I["lam_re"].a[l], lre, writes=[lre]); K.ld(lim.a[:], I["lam_im"].a[l], lim, writes=[lim])
        K.ld(dtt.a[:], I["logdt"].a[l], dtt, writes=[dtt])
        K.act(dtt.a[:], dtt.a[:], AF.Exp, [dtt], [dtt])
        K.tt("dve", th.a[:], lim.a[:], dtt.a[:], ALU.mult, [lim, dtt], [th])
        K.tt("dve", r.a[:], lre.a[:], dtt.a[:], ALU.mult, [lre, dtt], [r])
        K.act(r.a[:], r.a[:], AF.Exp, [r], [r])
        K.act(s_.a[:], th.a[:], AF.Sin, [th], [s_], scale=1.0 / 32.0)
        K.act(c.a[:], th.a[:], AF.Sin, [th, halfpi], [c], scale=1.0 / 32.0, bias=halfpi.a[:, :])

        def cdouble(cc, ss):
            K.tt("dve", t1.a[:], ss.a[:], cc.a[:], ALU.mult, [ss, cc], [t1])
            K.tt("dve", t2.a[:], cc.a[:], cc.a[:], ALU.mult, [cc], [t2])
            K.tt("dve", t3.a[:], ss.a[:], ss.a[:], ALU.mult, [ss], [t3])
            K.ts("dve", ss.a[:], t1.a[:], 2.0, ALU.mult, [t1], [ss])
            K.tt("dve", cc.a[:], t2.a[:], t3.a[:], ALU.subtract, [t2, t3], [cc])
        for _ in range(5):
            cdouble(c, s_)
        K.tt("dve", lbr.a[:], r.a[:], c.a[:], ALU.mult, [r, c], [lbr])
        K.tt("dve", lbi.a[:], r.a[:], s_.a[:], ALU.mult, [r, s_], [lbi])
        K.ts("dve", nlbi.a[:], lbi.a[:], -1.0, ALU.mult, [lbi], [nlbi])
        K.ts("dve", ka.a[:], lbr.a[:], -1.0, ALU.add, [lbr], [ka])
        K.tt("dve", t1.a[:], lre.a[:], lre.a[:], ALU.mult, [lre], [t1])
        K.tt("dve", t2.a[:], lim.a[:], lim.a[:], ALU.mult, [lim], [t2])
        K.tt("dve", den.a[:], t1.a[:], t2.a[:], ALU.add, [t1, t2], [den])
        K.recip(den.a[:], den.a[:], [den], [den])
        K.tt("dve", t1.a[:], ka.a[:], lre.a[:], ALU.mult, [ka, lre], [t1])
        K.tt("dve", t2.a[:], lbi.a[:], lim.a[:], ALU.mult, [lbi, lim], [t2])
        K.tt("dve", kr.a[:], t1.a[:], t2.a[:], ALU.add, [t1, t2], [kr])
        K.tt("dve", kr.a[:], kr.a[:], den.a[:], ALU.mult, [kr, den], [kr])
        K.tt("dve", t1.a[:], lbi.a[:], lre.a[:], ALU.mult, [lbi, lre], [t1])
        K.tt("dve", t2.a[:], ka.a[:], lim.a[:], ALU.mult, [ka, lim], [t2])
        K.tt("dve", ki.a[:], t1.a[:], t2.a[:], ALU.subtract, [t1, t2], [ki])
        K.tt("dve", ki.a[:], ki.a[:], den.a[:], ALU.mult, [ki, den], [ki])
        tabC = K.sb("tabC", [128, 16, 128]); tabS = K.sb("tabS", [128, 16, 128])
        tmpA = K.sb("tmpA", [128, 16, 64]); tmpB = K.sb("tmpB", [128, 16, 64])
        K.cp("dve", tabC.a[:, :, 0], c.a[:], [c], [tabC]); K.cp("dve", tabS.a[:, :, 0], s_.a[:], [s_], [tabS])
        K.cp("dve", cn.a[:], c.a[:], [c], [cn]); K.cp("dve", sn.a[:], s_.a[:], [s_], [sn])
        nn = 1
        while nn < 128:
            cb_ = cn.a[:, :].unsqueeze(2).to_broadcast([128, 16, nn]); sb_ = sn.a[:, :].unsqueeze(2).to_broadcast([128, 16, nn])
            K.tt("dve", tmpA.a[:, :, :nn], tabC.a[:, :, 0:nn], cb_, ALU.mult, [tabC, cn], [tmpA])
            K.tt("dve", tmpB.a[:, :, :nn], tabS.a[:, :, 0:nn], sb_, ALU.mult, [tabS, sn], [tmpB])
            K.tt("dve", tmpA.a[:, :, :nn], tmpA.a[:, :, :nn], tmpB.a[:, :, :nn], ALU.subtract, [tmpA, tmpB], [tmpA])
            K.tt("dve", tmpB.a[:, :, :nn], tabS.a[:, :, 0:nn], cb_, ALU.mult, [tabS, cn], [tmpB])
            K.cp("dve", tabC.a[:, :, nn:2 * nn], tmpA.a[:, :, :nn], [tmpA], [tabC])
            K.tt("dve", tmpA.a[:, :, :nn], tabC.a[:, :, 0:nn], sb_, ALU.mult, [tabC, sn], [tmpA])
            K.tt("dve", tabS.a[:, :, nn:2 * nn], tmpB.a[:, :, :nn], tmpA.a[:, :, :nn], ALU.add, [tmpA, tmpB], [tabS])
            cdouble(cn, sn)
            nn *= 2
        tabKC = K.sb("tabKC", [128, 16, 128]); tabKS = K.sb("tabKS", [128, 16, 128])
        krb = kr.a[:, :].unsqueeze(2).to_broadcast([128, 16, 128]); kib = ki.a[:, :].unsqueeze(2).to_broadcast([128, 16, 128])
        tmpC = K.sb("tmpC", [128, 16, 128])
        K.tt("dve", tabKC.a[:], tabC.a[:], krb, ALU.mult, [tabC, kr], [tabKC])
        K.tt("dve", tmpC.a[:], tabS.a[:], kib, ALU.mult, [tabS, ki], [tmpC])
        K.tt("dve", tabKC.a[:], tabKC.a[:], tmpC.a[:], ALU.add, [tabKC, tmpC], [tabKC])
        K.tt("dve", tabKS.a[:], tabC.a[:], kib, ALU.mult, [tabC, ki], [tabKS])
        K.tt("dve", tmpC.a[:], tabS.a[:], krb, ALU.mult, [tabS, kr], [tmpC])
        K.tt("dve", tabKS.a[:], tabKS.a[:], tmpC.a[:], ALU.subtract, [tabKS, tmpC], [tabKS])
        Bm = {}
        for nm in ("Bre", "Bim", "Cre", "Cim"):
            Bm[nm] = K.sb(nm + "_sb", [128, 16, 128])
            K.ld(Bm[nm].a[:], I[nm].a[l].rearrange("s k m -> k s m"), Bm[nm], writes=[Bm[nm]])
        s5d = K.sb("s5d", [128, 4]); bglu = K.sb("bglu", [128, 4]); gs5 = K.sb("gs5", [128, 4])
        K.ld(s5d.a[:], I["s5d"].a[l], s5d, writes=[s5d]); K.ld(bglu.a[:], I["b_glu"].a[l], bglu, writes=[bglu]); K.ld(gs5.a[:], I["g_s5"].a[l], gs5, writes=[gs5])
        Xr = K.sb("Xr", [128, 16]); Xi = K.sb("Xi", [128, 16])
        K.memset("dve", Xr.a[:], 0.0, [Xr]); K.memset("dve", Xi.a[:], 0.0, [Xi])
        y5g_d = K.dscr("y5g_d%d" % l, [512, NT])
        W8 = lambda nm: K.sb(nm, [128, 8, 128])
        BUr, BUi, Wr, Wi, Zr, Zi, T1, T2, Xr_, Xi_, nXi_ = [W8("s5w%d" % i) for i in range(11)]
        ubp = Pool(K, "ub", [128, 4, 128], F32, 2)
        gp = Pool(K, "s5g", [128, 128], F32, 3)

        def bu_calc(ub, sc, n, our, oui, tA, tB):
            j = sc // 4
            ps = PS.next()
            K.pe(ps.a[:, 0:n], Bm["Bre"].a[:, sc, :], ub.a[:, j, :n], True, True, [Bm["Bre"], ub], [ps])
            K.pe(ps.a[:, 128:128 + n], Bm["Bim"].a[:, sc, :], ub.a[:, j, :n], True, True, [Bm["Bim"], ub], [ps])
            return ps

        def y_out(ub, j, n, t0, xr_of, nxi_of, rd):
            ps_y = PS.next()
            for q in range(4):
                sc = 4 * j + q
                K.pe(ps_y.a[:, :n], Bm["Cre"].a[:, sc, :], xr_of(sc), q == 0, False, [Bm["Cre"]] + rd, [ps_y])
                K.pe(ps_y.a[:, :n], Bm["Cim"].a[:, sc, :], nxi_of(sc), False, q == 3, [Bm["Cim"]] + rd, [ps_y])
            yv = gp.next(); tg = gp.next()
            K.stt(yv.a[:, :n], ub.a[:, j, :n], s5d.a[:, j:j + 1], ps_y.a[:, :n], ALU.mult, ALU.add, [ub, s5d, ps_y], [yv])
            K.tt("dve", tg.a[:, :n], yv.a[:, :n], yv.a[:, :n], ALU.mult, [yv], [tg])
            K.ts("dve", tg.a[:, :n], tg.a[:, :n], 0.044715, ALU.mult, [tg], [tg], s2=1.0, op1=ALU.add)
            K.tt("dve", tg.a[:, :n], tg.a[:, :n], yv.a[:, :n], ALU.mult, [tg, yv], [tg])
            K.act(tg.a[:, :n], tg.a[:, :n], AF.Tanh, [tg], [tg], scale=0.7978845608028654)
            K.ts("dve", tg.a[:, :n], tg.a[:, :n], 1.0, ALU.add, [tg], [tg], s2=0.5, op1=ALU.mult)
            K.tt("dve", tg.a[:, :n], tg.a[:, :n], yv.a[:, :n], ALU.mult, [tg, yv], [tg])
            K.stq(y5g_d.a[j * 128:(j + 1) * 128, t0:t0 + n], tg.a[:, :n], tg, reads=[tg], mwrites=[y5g_d])

        for tc in range(16):
            t0 = tc * 128
            n = 128
            ub = ubp.next()
            K.ld(ub.a[:, :, :n], uT_d.a.rearrange("(j p) t -> p j t", p=128)[:, :, t0:t0 + n], ub, reads=[uT_d], writes=[ub])
            for h2 in range(2):
                for i in range(8):
                    sc = 8 * h2 + i
                    ps = bu_calc(ub, sc, n, None, None, None, None)
                    K.cp("act", BUr.a[:, i, :n], ps.a[:, 0:n], [ps], [BUr])
                    K.cp("act", BUi.a[:, i, :n], ps.a[:, 128:128 + n], [ps], [BUi])
                Cs = tabC.a[:, 8 * h2:8 * h2 + 8, :]; Ss = tabS.a[:, 8 * h2:8 * h2 + 8, :]
                KCs = tabKC.a[:, 8 * h2:8 * h2 + 8, :]; KSs = tabKS.a[:, 8 * h2:8 * h2 + 8, :]
                K.tt("dve", T1.a[:], BUi.a[:], KSs, ALU.mult, [BUi, tabKS], [T1])
                K.tt("pool", Wr.a[:], BUr.a[:], KCs, ALU.mult, [BUr, tabKC], [Wr])
                K.tt("dve", Wr.a[:], Wr.a[:], T1.a[:], ALU.subtract, [Wr, T1], [Wr])
                K.tt("pool", T2.a[:], BUr.a[:], KSs, ALU.mult, [BUr, tabKS], [T2])
                K.tt("dve", Wi.a[:], BUi.a[:], KCs, ALU.mult, [BUi, tabKC], [Wi])
                K.tt("dve", Wi.a[:], Wi.a[:], T2.a[:], ALU.add, [Wi, T2], [Wi])
                for i in range(8):
                    sc = 8 * h2 + i
                    rb = r.a[:, sc:sc + 1].to_broadcast([128, n])
                    K.scan(Zr.a[:, i, :], rb, Wr.a[:, i, :], Xr.a[:, sc:sc + 1], ALU.mult, ALU.add, [r, Wr, Xr], [Zr])
                    K.scan(Zi.a[:, i, :], rb, Wi.a[:, i, :], Xi.a[:, sc:sc + 1], ALU.mult, ALU.add, [r, Wi, Xi], [Zi])
                K.tt("dve", T1.a[:], Zi.a[:], Ss, ALU.mult, [Zi, tabS], [T1])
                K.tt("pool", Xr_.a[:], Zr.a[:], Cs, ALU.mult, [Zr, tabC], [Xr_])
                K.tt("dve", Xr_.a[:], Xr_.a[:], T1.a[:], ALU.subtract, [Xr_, T1], [Xr_])
                K.tt("pool", T2.a[:], Zr.a[:], Ss, ALU.mult, [Zr, tabS], [T2])
                K.tt("dve", T1.a[:], Zi.a[:], Cs, ALU.mult, [Zi, tabC], [T1])
                K.tt("dve", Xi_.a[:], T2.a[:], T1.a[:], ALU.add, [T2, T1], [Xi_])
                K.ts("dve", nXi_.a[:], Xi_.a[:], -1.0, ALU.mult, [Xi_], [nXi_])
                K.cp("dve", Xr.a[:, 8 * h2:8 * h2 + 8], Xr_.a[:, :, n - 1], [Xr_], [Xr])
                K.cp("dve", Xi.a[:, 8 * h2:8 * h2 + 8], Xi_.a[:, :, n - 1], [Xi_], [Xi])
                for jj in range(2):
                    j = 2 * h2 + jj
                    y_out(ub, j, n, t0, lambda sc: Xr_.a[:, sc - 8 * h2, :n], lambda sc: nXi_.a[:, sc - 8 * h2, :n], [Xr_, nXi_])
        K.stq(O["s5re_pT"].a[l], Xr.a[:], Xr, reads=[Xr], mwrites=[O["s5re_pT"]], is_output=True)
        K.stq(O["s5im_pT"].a[l], Xi.a[:], Xi, reads=[Xi], mwrites=[O["s5im_pT"]], is_output=True)
        x0r = K.sb("x0r", [128, 16, NS]); x0i = K.sb("x0i", [128, 16, NS])
        K.ld(x0r.a[:], I["s5reT"].a[l], x0r, writes=[x0r]); K.ld(x0i.a[:], I["s5imT"].a[l], x0i, writes=[x0i])
        SXr = K.sb("SXr", [128, 16, NS]); SXi = K.sb("SXi", [128, 16, NS]); SnXi = K.sb("SnXi", [128, 16, NS])
        SBr = K.sb("SBr", [128, 16, NS]); SBi = K.sb("SBi", [128, 16, NS])
        ub = ubp.next()
        K.ld(ub.a[:, :, :NS], uT_d.a.rearrange("(j p) t -> p j t", p=128)[:, :, L:NT], ub, reads=[uT_d], writes=[ub])
        for sc in range(16):
            ps = bu_calc(ub, sc, NS, None, None, None, None)
            K.ts("dve", SBr.a[:, sc, :], ps.a[:, 128:128 + NS], ki.a[:, sc:sc + 1], ALU.mult, [ps, ki], [SBr])
            K.stt(SBr.a[:, sc, :], ps.a[:, 0:NS], kr.a[:, sc:sc + 1], SBr.a[:, sc, :], ALU.mult, ALU.subtract, [ps, kr, SBr], [SBr])
            K.ts("dve", SBi.a[:, sc, :], ps.a[:, 0:NS], ki.a[:, sc:sc + 1], ALU.mult, [ps, ki], [SBi])
            K.stt(SBi.a[:, sc, :], ps.a[:, 128:128 + NS], kr.a[:, sc:sc + 1], SBi.a[:, sc, :], ALU.mult, ALU.add, [ps, kr, SBi], [SBi])
            K.stt(SXr.a[:, sc, :], x0r.a[:, sc, :], lbr.a[:, sc:sc + 1], SBr.a[:, sc, :], ALU.mult, ALU.add, [x0r, lbr, SBr], [SXr])
            K.stt(SXr.a[:, sc, :], x0i.a[:, sc, :], nlbi.a[:, sc:sc + 1], SXr.a[:, sc, :], ALU.mult, ALU.add, [x0i, nlbi, SXr], [SXr])
            K.stt(SXi.a[:, sc, :], x0r.a[:, sc, :], lbi.a[:, sc:sc + 1], SBi.a[:, sc, :], ALU.mult, ALU.add, [x0r, lbi, SBi], [SXi])
            K.stt(SXi.a[:, sc, :], x0i.a[:, sc, :], lbr.a[:, sc:sc + 1], SXi.a[:, sc, :], ALU.mult, ALU.add, [x0i, lbr, SXi], [SXi])
        K.ts("dve", SnXi.a[:], SXi.a[:], -1.0, ALU.mult, [SXi], [SnXi])
        K.stq(O["s5re_sT"].a[l], SXr.a[:], SXr, reads=[SXr], mwrites=[O["s5re_sT"]], is_output=True)
        K.stq(O["s5im_sT"].a[l], SXi.a[:], SXi, reads=[SXi], mwrites=[O["s5im_sT"]], is_output=True)
        for j in range(4):
            y_out(ub, j, NS, L, lambda sc: SXr.a[:, sc, :], lambda sc: SnXi.a[:, sc, :], [SXr, SnXi])
    with K.phase():
        s5d = K.sb("s5d", [128, 4]); bglu = K.sb("bglu", [128, 4]); gs5 = K.sb("gs5", [128, 4])
        K.ld(bglu.a[:], I["b_glu"].a[l], bglu, writes=[bglu]); K.ld(gs5.a[:], I["g_s5"].a[l], gs5, writes=[gs5])
        ybp = Pool(K, "y5b", [128, 4, 512], F32, 2)
        wglu = K.sb("wglu", [128, 4, 512])
        K.ld(wglu.a[:], I["w_glu"].a[l].rearrange("(k p) c -> p k c", p=128), wglu, writes=[wglu])
        yg = Pool(K, "ygl", [128, 4, 512], F32, 2); sqp = Pool(K, "s5sq", [128, 4, 512], F32, 1); rsp = Pool(K, "s5rs", [128, 512], F32, 2)
        for (t0, n) in TB:
            ygl = yg.next()
            yb = ybp.next()
            K.ld(yb.a[:, :, :n], y5g_d.a.rearrange("(j p) t -> p j t", p=128)[:, :, t0:t0 + n], yb, reads=[y5g_d], writes=[yb])
            for m in range(4):
                ps = PS.next()
                for j in range(4):
                    K.pe(ps.a[:, :n], wglu.a[:, j, m * 128:(m + 1) * 128], yb.a[:, j, :n], j == 0, j == 3, [wglu, yb], [ps])
                K.act(ygl.a[:, m, :n], ps.a[:, :n], AF.Sigmoid, [ps, bglu], [ygl], bias=bglu.a[:, m:m + 1])
                K.tt("dve", ygl.a[:, m, :n], ygl.a[:, m, :n], yb.a[:, m, :n], ALU.mult, [ygl, yb], [ygl])
            sq = sqp.next()
            K.act(sq.a[:, :, :n], ygl.a[:, :, :n], AF.Square, [ygl], [sq])
            ps = PS.next()
            for m in range(4):
                K.pe(ps.a[:, :n], ones_f.a[:], sq.a[:, m, :n], m == 0, m == 3, [ones_f, sq], [ps])
            rs = rsp.next()
            K.act(rs.a[:, :n], ps.a[:, :n], AF.Sqrt, [ps, epsb], [rs], scale=1.0 / 512.0, bias=epsb.a[:, :])
            K.recip(rs.a[:, :n], rs.a[:, :n], [rs], [rs])
            for m in range(4):
                K.stt(XTb[:, 8 + m, t0:t0 + n], ygl.a[:, m, :n], gs5.a[:, m:m + 1], rs.a[:, :n], ALU.mult, ALU.mult, [ygl, gs5, rs], [tXT])


def ffn_phase(K, S, PS, I, l, hT, XTf, tXT, gn, ones_bf, epsb, ident, gst, norm_stage, stages, own):
    moe = (l % 2 == 1)
    if moe:
        experts = [(I["moe_wg"].a[e], I["moe_wu"].a[e], I["moe_wd"].a[e]) for e in range(NE)]
        dff = D_FFE
    else:
        experts = [(I["ffn_wg"].a, I["ffn_wu"].a, I["ffn_wd"].a)]
        dff = D_FF
    B3 = [(0, 347), (347, 347), (694, 346)]
    SBS = [(0, [(0, 512), (512, 512)]), (1024, B3)]
    if own:
        SBS = [(0, B3)]
    for (c0, blocks) in SBS:
        nsb = sum(n for _, n in blocks)
        with K.phase():
            P = {"hblk": Pool(K, "fhblk", [128, KC, 128], F32, 1), "sq": Pool(K, "fsq", [128, KC, 128], BF16, 1),
                 "rs": Pool(K, "frs", [128, 512], F32, 2)}
            cT = K.sb("cT", [128, KC, 1040], BF16)
            for q0 in range(0, nsb, 128):
                n = min(128, nsb - q0)
                norm_stage(hT, gn["g_ffn"].a[:, l, :], gn["g_ffn"], c0 + q0, n, cT.a[:, :, q0:q0 + n], cT, P)
            wgp = Pool(K, "fwg", [128, KC, 256], BF16, 2); wup = Pool(K, "fwu", [128, KC, 256], BF16, 2)
            wdp = Pool(K, "fwd", [128, 2, D], BF16, 3); h1p = Pool(K, "fh1", [128, 2, 1040], BF16, 2)
            sgp = Pool(K, "fsg", [128, 512], F32, 3); hbp = Pool(K, "fhb", [128, 512], F32, 2)
            ntile = (nsb + 127) // 128
            if moe:
                wr = K.sb("wr", [128, KC, NE], BF16)
                K.ld(wr.a[:], I["w_router"].a.rearrange("(k p) c -> p k c", p=128), wr, writes=[wr], q="pool")
                brep = K.sb("brep", [128, NE]); K.ld(brep.a[:], I["b_router_rep"].a, brep, writes=[brep])
                comb = K.sb("comb", [128, 9, NE]); combB = Pool(K, "combB", [128, 1040], F32, 2)
                rt = Pool(K, "rt", [128, 4, NE], F32, 2)
                for i in range(ntile):
                    q0 = i * 128
                    n = min(128, nsb - q0)
                    ps = PS.next()
                    for k in range(KC):
                        K.pe(ps.a[:n, :NE], cT.a[:, k, q0:q0 + n], wr.a[:, k, :], k == 0, k == KC - 1, [cT, wr], [ps])
                    t = rt.next()
                    lg = t.a[:n, 0, :]; mx = t.a[:n, 1, :]; ex = t.a[:n, 2, :]; sc = t.a[:n, 3, :]
                    K.tt("dve", lg, ps.a[:n, :NE], brep.a[:n, :], ALU.add, [ps, brep], [t])
                    K.S.op("dve", lambda e, mx=mx, lg=lg: e.max(out=mx, in_=lg), reads=[t.k], writes=[t.k])
                    K.ts("dve", sc[:, 0:1], mx[:, 0:1], -1.0, ALU.mult, [t], [t])
                    K.act(ex, lg, AF.Exp, [t], [t], bias=sc[:, 0:1])
                    K.act(sc[:, 1:2], mx[:, 1:2], AF.Exp, [t], [t], bias=sc[:, 0:1])
                    K.ts("dve", sc[:, 1:2], sc[:, 1:2], 1.0, ALU.add, [t], [t])
                    K.recip(sc[:, 1:2], sc[:, 1:2], [t], [t])
                    K.ts("dve", lg, lg, mx[:, 1:2], ALU.is_ge, [t], [t])
                    K.stt(comb.a[:n, i, :], ex, sc[:, 1:2], lg, ALU.mult, ALU.mult, [t], [comb])
            panels = [(ei, f0) for ei in range(len(experts)) for f0 in range(0, dff, 256)]
            npan = len(panels)
            cBs = {}
            loaded = {}

            def loads(p):
                ei, f0 = panels[p]
                Wg, Wu, Wd = experts[ei]
                wg = wgp.next(); wu = wup.next(); wd = wdp.next()
                pi = f0 // 256
                for hh in range(2):
                    K.ld(wg.a[:].rearrange("p k c -> p (k c)")[:, hh * 2048:(hh + 1) * 2048], Wg[pi][:, hh * 2048:(hh + 1) * 2048], wg, writes=[wg], q="pool")
                for hh in range(2):
                    K.ld(wu.a[:].rearrange("p k c -> p (k c)")[:, hh * 2048:(hh + 1) * 2048], Wu[pi][:, hh * 2048:(hh + 1) * 2048], wu, writes=[wu], q="pool")
                for hh in range(2):
                    K.ld(wd.a[:].rearrange("p k c -> p (k c)")[:, hh * 2048:(hh + 1) * 2048], Wd[pi][:, hh * 2048:(hh + 1) * 2048], wd, writes=[wd], q="pool")
                loaded[p] = (wg, wu, wd)

            def gate_up(p):
                ei, f0 = panels[p]
                wg, wu, wd = loaded[p]
                if moe and ei not in cBs:
                    cB = combB.next()
                    for i in range(ntile):
                        q0 = i * 128
                        n = min(128, nsb - q0)
                        ps = PS.next()
                        K.pe(ps.a[:, :n], comb.a[:n, i, ei:ei + 1].to_broadcast([n, 128]), ident.a[:n, :n], True, True, [comb, ident], [ps])
                        K.cp("act", cB.a[:, q0:q0 + n], ps.a[:, :n], [ps], [cB])
                    cBs.clear()
                    cBs[ei] = cB
                h1 = h1p.next()
                for m in range(2):
                    for (b0, n) in blocks:
                        pg = PS.next(); pu = PS.next()
                        for k in range(KC):
                            K.pe(pg.a[:, :n], wg.a[:, k, m * 128:(m + 1) * 128], cT.a[:, k, b0:b0 + n], k == 0, k == KC - 1, [wg, cT], [pg])
                        for k in range(KC):
                            K.pe(pu.a[:, :n], wu.a[:, k, m * 128:(m + 1) * 128], cT.a[:, k, b0:b0 + n], k == 0, k == KC - 1, [wu, cT], [pu])
                        sg = sgp.next()
                        K.act(sg.a[:, :n], pg.a[:, :n], AF.Silu, [pg], [sg])
                        if moe:
                            K.tt("pool", sg.a[:, :n], sg.a[:, :n], cBs[ei].a[:, b0:b0 + n], ALU.mult, [sg, cBs[ei]], [sg])
                        K.tt("dve", h1.a[:, m, b0:b0 + n], sg.a[:, :n], pu.a[:, :n], ALU.mult, [sg, pu], [h1])
                return h1

            def down(p, h1):
                wg, wu, wd = loaded.pop(p)
                for mo in range(KC):
                    for (b0, n) in blocks:
                        ps = PS.next()
                        for k in range(2):
                            K.pe(ps.a[:, :n], wd.a[:, k, mo * 128:(mo + 1) * 128], h1.a[:, k, b0:b0 + n], k == 0, k == 1, [wd, h1], [ps])
                        if p == 0:
                            K.cp("act", XTf[:, mo, b0:b0 + n], ps.a[:, :n], [ps], [tXT])
                        else:
                            K.tt("dve", XTf[:, mo, b0:b0 + n], ps.a[:, :n], XTf[:, mo, b0:b0 + n], ALU.add, [ps, tXT], [tXT])

            loads(0)
            prev_h1 = None
            for p in range(npan + 1):
                if p + 1 < npan:
                    loads(p + 1)
                cur_h1 = gate_up(p) if p < npan else None
                if p >= 1:
                    down(p - 1, prev_h1)
                prev_h1 = cur_h1
            for mo in range(KC):
                for (b0, n) in blocks:
                    hb = hbp.next()
                    K.ld(hb.a[:, :n], hT.a[mo * 128:(mo + 1) * 128, c0 + b0:c0 + b0 + n], hb, reads=[hT], writes=[hb])
                    K.tt("dve", hb.a[:, :n], hb.a[:, :n], XTf[:, mo, b0:b0 + n], ALU.add, [hb, tXT], [hb])
                    K.stq(hT.a[mo * 128:(mo + 1) * 128, c0 + b0:c0 + b0 + n], hb.a[:, :n], hb, reads=[hb], mwrites=[hT])


def _consts():
    ident = np.eye(128, dtype=np.float32)
    maskT = np.where(np.arange(64)[:, None] <= np.arange(64)[None, :], 0.0, NEG).astype(np.float32)
    sel4 = np.zeros((4, 4, 128), np.float32)
    for h in range(4):
        sel4[h, h, :] = 1.0
    sel8 = np.zeros((8, 8, 128), np.float32)
    for h in range(8):
        sel8[h, h, :] = 1.0
    return dict(ident=ident, nident=-ident, maskT=maskT, sel4=sel4, sel8=sel8, sel8e=sel8.copy())


def _fm(v):
    v = np.asarray(v, np.float32)
    return np.ascontiguousarray(v.reshape(v.shape[:-1] + (v.shape[-1] // 128, 128)).swapaxes(-1, -2))


def prep_shared(inp):
    f = lambda a: np.ascontiguousarray(np.asarray(a, np.float32))
    sh = {}
    for nm in ("g_mix", "g_ffn", "g_ple"):
        sh[nm] = _fm(inp[nm])
    sh["g_final"] = _fm(inp["g_final"])
    sh["w_in"] = f(inp["w_in"]); sh["w_out"] = f(inp["w_out"])
    sh["b_ig"] = f(inp["b_igate"]).reshape(DEPTH, 4, 1); sh["b_fg"] = f(inp["b_fgate"]).reshape(DEPTH, 4, 1)
    sh["gml_rep"] = np.ascontiguousarray(np.broadcast_to(f(inp["g_ml"]).reshape(DEPTH, 1, 1024), (DEPTH, 64, 1024)))
    sh["lam_re"] = _fm(f(inp["s5_lam_re"]).reshape(DEPTH, 2048)); sh["lam_im"] = _fm(f(inp["s5_lam_im"]).reshape(DEPTH, 2048))
    sh["logdt"] = _fm(np.repeat(f(inp["s5_log_dt"]), 64, axis=1))
    bre = f(inp["s5_b_re"]); bim = f(inp["s5_b_im"]); cre = f(inp["s5_c_re"]); cim = f(inp["s5_c_im"])
    Bre = np.zeros((DEPTH, 16, 128, 128), np.float32); Bim = np.zeros_like(Bre); Cre = np.zeros_like(Bre); Cim = np.zeros_like(Bre)
    for g in range(32):
        sc, g2, gl = g // 2, g % 2, g % 8
        Bre[:, sc, gl * 16:(gl + 1) * 16, g2 * 64:(g2 + 1) * 64] = bre[:, g].transpose(0, 2, 1)
        Bim[:, sc, gl * 16:(gl + 1) * 16, g2 * 64:(g2 + 1) * 64] = bim[:, g].transpose(0, 2, 1)
        Cre[:, sc, g2 * 64:(g2 + 1) * 64, gl * 16:(gl + 1) * 16] = cre[:, g].transpose(0, 2, 1)
        Cim[:, sc, g2 * 64:(g2 + 1) * 64, gl * 16:(gl + 1) * 16] = cim[:, g].transpose(0, 2, 1)
    sh["Bre"], sh["Bim"], sh["Cre"], sh["Cim"] = Bre, Bim, Cre, Cim
    fm4 = lambda v: np.ascontiguousarray(f(v).reshape(DEPTH, 4, 128).swapaxes(1, 2))
    sh["s5d"] = fm4(f(inp["s5_d"]).reshape(DEPTH, 512)); sh["b_glu"] = fm4(inp["s5_b_glu"]); sh["g_s5"] = fm4(inp["g_s5"])
    sh["w_glu"] = f(inp["s5_w_glu"])
    sh["conv_w"] = np.ascontiguousarray(f(inp["ssd_conv_w"]).reshape(DEPTH, 4, 8, 128).transpose(0, 3, 2, 1))
    sh["conv_b"] = np.ascontiguousarray(f(inp["ssd_conv_b"]).reshape(DEPTH, 8, 128).swapaxes(1, 2))
    sh["dt_bias"] = f(inp["ssd_dt_bias"]).reshape(DEPTH, 8, 1); sh["a_log"] = f(inp["ssd_a_log"]).reshape(DEPTH, 8, 1)
    sh["ssdd_rep"] = np.ascontiguousarray(np.broadcast_to(f(inp["ssd_d"]).reshape(DEPTH, 1, 8), (DEPTH, 64, 8)))
    sh["gssd_rep"] = np.ascontiguousarray(np.broadcast_to(f(inp["g_ssd"]).reshape(DEPTH, 1, 512), (DEPTH, 64, 512)))
    def pan_in(W):
        npan = W.shape[1] // 256
        return np.ascontiguousarray(W.reshape(16, 128, npan, 256).transpose(2, 1, 0, 3)).reshape(npan, 128, 4096)

    def pan_dn(W):
        npan = W.shape[0] // 256
        return np.ascontiguousarray(W.reshape(npan, 2, 128, 2048).transpose(0, 2, 1, 3)).reshape(npan, 128, 4096)
    sh["ffn_wg"] = pan_in(f(inp["ffn_w_gate"])[0]); sh["ffn_wu"] = pan_in(f(inp["ffn_w_up"])[0]); sh["ffn_wd"] = pan_dn(f(inp["ffn_w_down"])[0])
    sh["w_router"] = f(inp["w_router"])[0]
    sh["b_router_rep"] = np.ascontiguousarray(np.broadcast_to(f(inp["b_router"])[0].reshape(1, NE), (128, NE)))
    sh["moe_wg"] = np.stack([pan_in(np.asarray(inp["moe_w_gate"][0][e], np.float32)) for e in range(NE)])
    sh["moe_wu"] = np.stack([pan_in(np.asarray(inp["moe_w_up"][0][e], np.float32)) for e in range(NE)])
    sh["moe_wd"] = np.stack([pan_dn(np.asarray(inp["moe_w_down"][0][e], np.float32)) for e in range(NE)])
    sh["w_ple"] = f(inp["w_ple"]); sh["w_pleg"] = f(inp["w_ple_gate"])
    sh.update(_consts())
    return sh


def prep_core(inp, c):
    f = lambda a: np.ascontiguousarray(np.asarray(a, np.float32))
    b = c % 4
    ss = slice(c * NS, (c + 1) * NS)
    m = {}
    m["xT"] = np.ascontiguousarray(np.concatenate([f(inp["x_prompt"])[b], f(inp["x_sample"])[ss, 0]], axis=0).T)
    m["pT"] = np.ascontiguousarray(np.concatenate([f(inp["p_prompt"])[:, b], f(inp["p_sample"])[:, ss, 0]], axis=1).transpose(0, 2, 1))
    m["sC"] = f(inp["state_mlstm_C"])[:, ss]; m["sn"] = f(inp["state_mlstm_n"])[:, ss]
    m["smT"] = np.ascontiguousarray(f(inp["state_mlstm_m"])[:, ss].transpose(0, 2, 1))
    s5 = lambda a: np.ascontiguousarray(f(a)[:, ss].reshape(DEPTH, NS, 16, 128).transpose(0, 3, 2, 1))
    m["s5reT"] = s5(inp["state_s5_re"]); m["s5imT"] = s5(inp["state_s5_im"])
    m["ssdT"] = np.ascontiguousarray(f(inp["state_ssd"])[:, ss].transpose(0, 1, 2, 4, 3))
    m["convT"] = np.ascontiguousarray(f(inp["cache_conv"])[:, ss].reshape(DEPTH, NS, 3, 8, 128).transpose(0, 4, 3, 2, 1))
    m["half"] = np.ascontiguousarray(np.broadcast_to(np.array([[1.0, 0.0]] if c < 4 else [[0.0, 1.0]], np.float32), (128, 2)))
    return m


_NC_CACHE = {}


def run_device(inputs, dbg=None, stages=99, cores=8, trace=False):
    key = (tuple(sorted(dbg or ())), stages)
    if key not in _NC_CACHE:
        _NC_CACHE[key] = build_program(dbg, stages)
    nc, K = _NC_CACHE[key]
    sh = prep_shared(inputs)
    in_maps = []
    for c in range(cores):
        m = dict(sh)
        m.update(prep_core(inputs, c))
        in_maps.append(m)
    if trace:
        res = run_bass_kernel_spmd(nc, in_maps, core_ids=list(range(cores)), trace=True)
        print("EXEC_NS", res.exec_time_ns)
    else:
        res = run_bass_kernel_spmd(nc, in_maps, core_ids=list(range(cores)))
    return res.results


def kernel(**inputs):
    R = run_device(inputs)
    B = 4
    y_p = np.stack([np.concatenate([R[b]["yT"][:, :1024].T, R[b + 4]["yT"][:, :1024].T], axis=0) for b in range(B)])
    y_s = np.concatenate([R[c]["yT"][:, 1024:NO].T for c in range(8)], axis=0)[:, None, :]
    st = lambda nm, idx: np.stack([R[b][nm] for b in range(B)], axis=1)
    C_p = np.stack([R[b]["C_p"] for b in range(B)], axis=1)
    n_p = np.stack([R[b]["n_p"] for b in range(B)], axis=1)
    m_p = np.stack([R[b]["m_pT"][:, :, 0] for b in range(B)], axis=1)
    unfm = lambda a: a.swapaxes(-1, -2).reshape(a.shape[:-2] + (32, 64))
    s5re_p = np.stack([unfm(R[b]["s5re_pT"]) for b in range(B)], axis=1)
    s5im_p = np.stack([unfm(R[b]["s5im_pT"]) for b in range(B)], axis=1)
    ssd_p = np.stack([R[b]["ssd_pT"].transpose(0, 1, 3, 2) for b in range(B)], axis=1)
    conv_p = np.stack([R[b]["conv_pT"].transpose(0, 3, 2, 1).reshape(DEPTH, 3, 1024) for b in range(B)], axis=1)
    C_s = np.concatenate([R[c]["C_s"] for c in range(8)], axis=1)
    n_s = np.concatenate([R[c]["n_s"] for c in range(8)], axis=1)
    m_s = np.concatenate([R[c]["m_sT"].transpose(0, 2, 1) for c in range(8)], axis=1)
    uns = lambda a: a.transpose(0, 3, 2, 1).reshape(DEPTH, NS, 32, 64)
    s5re_s = np.concatenate([uns(R[c]["s5re_sT"]) for c in range(8)], axis=1)
    s5im_s = np.concatenate([uns(R[c]["s5im_sT"]) for c in range(8)], axis=1)
    ssd_s = np.concatenate([R[c]["ssd_sT"].transpose(0, 1, 2, 4, 3) for c in range(8)], axis=1)
    conv_s = np.concatenate([R[c]["conv_sT"].transpose(0, 4, 3, 2, 1).reshape(DEPTH, NS, 3, 1024) for c in range(8)], axis=1)
    outs = (y_p, y_s, C_p, n_p, m_p, s5re_p, s5im_p, ssd_p, conv_p, C_s, n_s, m_s, s5re_s, s5im_s, ssd_s, conv_s)
    return tuple(np.ascontiguousarray(o, dtype=np.float32) for o in outs)
```

```python
import math
import numpy as np
import concourse.bass as bass
import concourse.mybir as mybir
from concourse.bass_utils import run_bass_kernel_spmd
from contextlib import ExitStack

F32 = mybir.dt.float32
BF16 = mybir.dt.bfloat16
AF = mybir.ActivationFunctionType
ALU = mybir.AluOpType
AX = mybir.AxisListType

L = 2048
NS = 16
NT = L + NS
XW = 2080
D = 2048
KC = 16
DEPTH = 2
N_IN = 6160
D_FF = 5632
D_FFE = 7168
NE = 8
EPS = 1e-6
TB = [(0, 512), (512, 512), (1024, 512), (1536, 512), (2048, 16)]
CHUNKS = [(c * 64, 64, c) for c in range(32)] + [(L + j, 1, 32 + j) for j in range(NS)]
NCI = 48
NO = 1040
TBO = [(0, 512), (512, 512), (1024, 16)]
NEG = -30000.0


class Tk:
    __slots__ = ("name", "lw", "rd", "mw", "dsem", "dcnt")

    def __init__(self, name):
        self.name = name
        self.lw = None
        self.rd = []
        self.mw = []
        self.dsem = None
        self.dcnt = 0


class T:
    __slots__ = ("a", "k")

    def __init__(self, a, name):
        self.a = a
        self.k = Tk(name)


class Ins:
    __slots__ = ("eng", "fn", "waits", "needed", "val", "dsem", "dval")

    def __init__(self, eng, fn):
        self.eng = eng
        self.fn = fn
        self.waits = []
        self.needed = False
        self.val = None
        self.dsem = None
        self.dval = None


class Sched:
    ENGS = ("pe", "act", "dve", "pool", "sp")

    def __init__(self, nc, stack):
        self.nc = nc
        self.stack = stack
        self.prog = {e: [] for e in self.ENGS}
        self.esem = {e: stack.enter_context(nc.semaphore("es_" + e)) for e in self.ENGS}
        self.ecnt = {e: 0 for e in self.ENGS}
        self.known = {e: {} for e in self.ENGS}
        self.nsem = 0
        self.out_events = []
        self.toks = []
        self.pend_dma = []
        self.last = {e: None for e in self.ENGS}
        self.semfree = []
        self.semcnt = {}
        self.phase_mark = 0

    def phase_begin(self):
        self.phase_mark = len(self.toks)

    def phase_end(self):
        for k in self.toks[self.phase_mark:]:
            if k.dsem is not None:
                self.semfree.append(k.dsem)
                k.dsem = None
        del self.toks[self.phase_mark:]

    def tok(self, name):
        k = Tk(name)
        self.toks.append(k)
        return k

    def _deps(self, ins, reads, writes, mwrites):
        evs = []
        for r in reads:
            if r.lw is not None:
                evs.append(r.lw)
            evs.extend(r.mw)
        for w in writes:
            if w.lw is not None:
                evs.append(w.lw)
            evs.extend(w.mw)
            evs.extend(w.rd)
        for w in mwrites:
            if w.lw is not None:
                evs.append(w.lw)
            evs.extend(w.rd)
        seen = set()
        for ev in evs:
            if id(ev) in seen or ev is ins:
                continue
            seen.add(id(ev))
            if ev.dsem is None and ev.eng == "pe" and ins.eng == "pe" and ins.dsem is None:
                continue
            ins.waits.append(ev)
            ev.needed = True
        for r in reads:
            r.rd.append(ins)
        for w in writes:
            w.lw = ins
            w.rd = []
            w.mw = []
        for w in mwrites:
            if w.rd:
                w.rd = []
                w.mw = []
                w.lw = None
            w.mw.append(ins)

    def op(self, eng, fn, reads=(), writes=()):
        ins = Ins(eng, fn)
        self._deps(ins, [r.k if isinstance(r, T) else r for r in reads],
                   [w.k if isinstance(w, T) else w for w in writes], [])
        self.prog[eng].append(ins)
        self.last[eng] = ins
        return ins

    def dma(self, q, out_ap, in_ap, tok, reads=(), writes=(), mwrites=(), is_output=False, **kw):
        if isinstance(tok, T):
            tok = tok.k
        if tok.dsem is None:
            if self.semfree:
                tok.dsem = self.semfree.pop()
            else:
                tok.dsem = self.stack.enter_context(self.nc.semaphore("ds_%d" % self.nsem))
                self.nsem += 1
        ins = Ins(q, lambda e: e.dma_start(out=out_ap, in_=in_ap, **kw))
        c = self.semcnt.get(id(tok.dsem), 0) + 16
        self.semcnt[id(tok.dsem)] = c
        ins.dsem = tok.dsem
        ins.dval = c
        self._deps(ins, [r.k if isinstance(r, T) else r for r in reads],
                   [w.k if isinstance(w, T) else w for w in writes],
                   [w.k if isinstance(w, T) else w for w in mwrites])
        self.prog[q].append(ins)
        self.pend_dma.append(ins)
        if is_output:
            self.out_events.append(ins)
        return ins

    def dma_fn(self, q, fn, tok, reads=(), writes=(), mwrites=()):
        if isinstance(tok, T):
            tok = tok.k
        if tok.dsem is None:
            if self.semfree:
                tok.dsem = self.semfree.pop()
            else:
                tok.dsem = self.stack.enter_context(self.nc.semaphore("ds_%d" % self.nsem))
                self.nsem += 1
        ins = Ins(q, fn)
        c = self.semcnt.get(id(tok.dsem), 0) + 16
        self.semcnt[id(tok.dsem)] = c
        ins.dsem = tok.dsem
        ins.dval = c
        self._deps(ins, [r.k if isinstance(r, T) else r for r in reads],
                   [w.k if isinstance(w, T) else w for w in writes],
                   [w.k if isinstance(w, T) else w for w in mwrites])
        self.prog[q].append(ins)
        self.pend_dma.append(ins)
        return ins

    def _emit_engine(self, e, engh):
        known = self.known[e]
        for ins in self.prog[e]:
            need = {}
            for ev in ins.waits:
                if ev.dsem is not None:
                    key, sem, val = ("d", id(ev.dsem)), ev.dsem, ev.dval
                else:
                    key, sem, val = ("e", ev.eng), self.esem[ev.eng], ev.val
                if known.get(key, 0) >= val:
                    continue
                if key not in need or need[key][1] < val:
                    need[key] = (sem, val)
            for key, (sem, val) in need.items():
                engh.wait_ge(sem, val)
                known[key] = val
            if ins.fn is None:
                continue
            bi = ins.fn(engh)
            if ins.dsem is not None:
                bi.then_inc(ins.dsem, 16)
            elif ins.needed:
                bi.then_inc(self.esem[e], 1)

    def flush(self, final=False):
        nc = self.nc
        bar = Ins("sp", lambda e: e.nop())
        seen = set()
        for ev in self.pend_dma:
            if ev.eng != "sp" or True:
                key = (id(ev.dsem))
                bar.waits.append(ev)
        for e in self.ENGS:
            if e != "sp" and self.last[e] is not None:
                self.last[e].needed = True
                bar.waits.append(self.last[e])
        bar.needed = True
        self.prog["sp"].append(bar)
        for e in self.ENGS:
            if e != "sp":
                w = Ins(e, None)
                w.waits.append(bar)
                self.prog[e].append(w)
        for e in self.ENGS:
            c = self.ecnt[e]
            for ins in self.prog[e]:
                if ins.dsem is None and ins.needed and ins.fn is not None:
                    c += 1
                    ins.val = c
            self.ecnt[e] = c
        with nc.Block() as block:
            @block.tensor
            def _(eng):
                self._emit_engine("pe", eng)

            @block.scalar
            def _(eng):
                self._emit_engine("act", eng)

            @block.vector
            def _(eng):
                self._emit_engine("dve", eng)

            @block.gpsimd
            def _(eng):
                self._emit_engine("pool", eng)

            @block.sync
            def _(eng):
                self._emit_engine("sp", eng)
        self.prog = {e: [] for e in self.ENGS}
        self.pend_dma = []
        self.last = {e: None for e in self.ENGS}
        for k in self.toks:
            k.lw = None
            k.rd = []
            k.mw = []


_UID = [0]


class Pool:
    def __init__(self, K, name, shape, dt, n, space="sb"):
        _UID[0] += 1
        name = "%s_%d_" % (name, _UID[0])
        self.bufs = []
        for i in range(n):
            if space == "sb":
                a = K.st.enter_context(K.nc.sbuf_tensor("%s%d" % (name, i), list(shape), dt))
            else:
                a = K.st.enter_context(K.nc.psum_tensor("%s%d" % (name, i), list(shape), dt))
            t = T(a, "%s%d" % (name, i))
            K.S.toks.append(t.k)
            self.bufs.append(t)
        self.i = 0

    def next(self):
        b = self.bufs[self.i % len(self.bufs)]
        self.i += 1
        return b


class _Phase:
    def __init__(self, K):
        self.K = K

    def __enter__(self):
        self.pst = ExitStack()
        self.pst.__enter__()
        self.K.st = self.pst
        self.K.S.phase_begin()
        return self

    def __exit__(self, *a):
        if a[0] is None:
            self.K.S.flush()
            self.K.S.phase_end()
        self.K.st = self.K.gst
        return self.pst.__exit__(*a)


class Builder:
    def __init__(self, nc, st, dbg=None):
        self.nc = nc
        self.gst = st
        self.st = st
        self.S = Sched(nc, st)
        self.dbg = dbg or set()
        self.inputs = {}
        self.outputs = {}

    def phase(self):
        return _Phase(self)

    def din(self, name, shape, dt=F32):
        a = self.nc.dram_tensor(name, list(shape), dt, kind="ExternalInput").ap()
        t = T(a, name)
        self.inputs[name] = t
        return t

    def dout(self, name, shape, dt=F32):
        a = self.nc.dram_tensor(name, list(shape), dt, kind="ExternalOutput").ap()
        t = T(a, name)
        self.S.toks.append(t.k)
        self.outputs[name] = t
        return t

    def dscr(self, name, shape, dt=F32):
        kind = "ExternalOutput" if name in self.dbg else "Internal"
        a = self.nc.dram_tensor(name, list(shape), dt, kind=kind).ap()
        t = T(a, name)
        self.S.toks.append(t.k)
        return t

    def sb(self, name, shape, dt=F32):
        _UID[0] += 1
        name = "%s_%d" % (name, _UID[0])
        a = self.st.enter_context(self.nc.sbuf_tensor(name, list(shape), dt))
        t = T(a, name)
        self.S.toks.append(t.k)
        return t

    def view(self, ap, name):
        t = T(ap, name)
        self.S.toks.append(t.k)
        return t

    def pe(self, out, lhsT, rhs, start, stop, reads, writes):
        self.S.op("pe", lambda e: e.matmul(out, lhsT, rhs, start=start, stop=stop), reads=reads, writes=writes)

    def tr(self, out, in_, ident, reads, writes):
        self.S.op("pe", lambda e: e.transpose(out, in_, ident), reads=reads, writes=writes)

    def act(self, out, in_, func, reads, writes, scale=1.0, bias=None, accum=None):
        def f(e):
            kw = {}
            if bias is not None:
                kw["bias"] = bias
            if accum is not None:
                kw["accum_out"] = accum
            return e.activation(out=out, in_=in_, func=func, scale=scale, **kw)
        self.S.op("act", f, reads=reads, writes=writes)

    def tt(self, eng, out, in0, in1, op, reads, writes):
        self.S.op(eng, lambda e: e.tensor_tensor(out, in0, in1, op), reads=reads, writes=writes)

    def ts(self, eng, out, in0, s1, op0, reads, writes, s2=None, op1=None):
        if op1 is None:
            self.S.op(eng, lambda e: e.tensor_scalar(out, in0, s1, None, op0), reads=reads, writes=writes)
        else:
            self.S.op(eng, lambda e: e.tensor_scalar(out, in0, s1, s2, op0, op1), reads=reads, writes=writes)

    def stt(self, out, in0, scalar, in1, op0, op1, reads, writes):
        self.S.op("dve", lambda e: e.scalar_tensor_tensor(out, in0, scalar, in1, op0, op1), reads=reads, writes=writes)

    def cp(self, eng, out, in_, reads, writes):
        if eng == "act":
            self.S.op("act", lambda e: e.copy(out, in_), reads=reads, writes=writes)
        else:
            self.S.op(eng, lambda e: e.tensor_copy(out, in_), reads=reads, writes=writes)

    def memset(self, eng, ap, val, writes):
        self.S.op(eng, lambda e: e.memset(ap, val), writes=writes)

    def recip(self, out, in_, reads, writes):
        self.S.op("dve", lambda e: e.reciprocal(out, in_), reads=reads, writes=writes)

    def scan(self, out, d0, d1, init, op0, op1, reads, writes):
        self.S.op("dve", lambda e: e.tensor_tensor_scan(out, d0, d1, init, op0, op1), reads=reads, writes=writes)

    def ld(self, dst_ap, src_ap, tok, reads=(), writes=(), q="sp", **kw):
        self.S.dma(q, dst_ap, src_ap, tok, reads=reads, writes=writes, **kw)

    def stq(self, dst_ap, src_ap, tok, reads=(), mwrites=(), writes=(), q="sp", is_output=False, **kw):
        self.S.dma(q, dst_ap, src_ap, tok, reads=reads, mwrites=mwrites, writes=writes, is_output=is_output, **kw)


def build_program(dbg=None, stages=99):
    nc = bass.Bass("TRN2", target_bir_lowering=False)
    with ExitStack() as gst:
        K = Builder(nc, gst, dbg)
        S = K.S
        I = {}
        def din(name, shape):
            I[name] = K.din(name, shape)
            return I[name]
        din("xT", [D, NT])
        din("pT", [DEPTH, 256, NT])
        din("sC", [DEPTH, NS, 4, 256, 256]); din("sn", [DEPTH, NS, 4, 256]); din("smT", [DEPTH, 4, NS])
        din("s5reT", [DEPTH, 128, 16, NS]); din("s5imT", [DEPTH, 128, 16, NS])
        din("ssdT", [DEPTH, NS, 8, 128, 64]); din("convT", [DEPTH, 128, 8, 3, NS])
        for nm in ("g_mix", "g_ffn", "g_ple"):
            din(nm, [DEPTH, 128, 16])
        din("g_final", [128, 16])
        din("w_in", [DEPTH, D, N_IN]); din("w_out", [DEPTH, D, D])
        din("b_ig", [DEPTH, 4, 1]); din("b_fg", [DEPTH, 4, 1]); din("gml_rep", [DEPTH, 64, 1024])
        for nm in ("lam_re", "lam_im", "logdt"):
            din(nm, [DEPTH, 128, 16])
        din("Bre", [DEPTH, 16, 128, 128]); din("Bim", [DEPTH, 16, 128, 128])
        din("Cre", [DEPTH, 16, 128, 128]); din("Cim", [DEPTH, 16, 128, 128])
        din("s5d", [DEPTH, 128, 4]); din("w_glu", [DEPTH, 512, 512]); din("b_glu", [DEPTH, 128, 4]); din("g_s5", [DEPTH, 128, 4])
        din("conv_w", [DEPTH, 128, 8, 4]); din("conv_b", [DEPTH, 128, 8])
        din("dt_bias", [DEPTH, 8, 1]); din("a_log", [DEPTH, 8, 1]); din("ssdd_rep", [DEPTH, 64, 8]); din("gssd_rep", [DEPTH, 64, 512])
        din("ffn_wg", [D_FF // 256, 128, 4096]); din("ffn_wu", [D_FF // 256, 128, 4096]); din("ffn_wd", [D_FF // 256, 128, 4096])
        din("w_router", [D, NE]); din("b_router_rep", [128, NE])
        din("moe_wg", [NE, D_FFE // 256, 128, 4096]); din("moe_wu", [NE, D_FFE // 256, 128, 4096]); din("moe_wd", [NE, D_FFE // 256, 128, 4096])
        din("w_ple", [DEPTH, 256, D]); din("w_pleg", [DEPTH, D, D])
        din("ident", [128, 128]); din("nident", [128, 128]); din("maskT", [64, 64])
        din("half", [128, 2])
        din("sel4", [4, 4, 128]); din("sel8", [8, 8, 128]); din("sel8e", [8, 8, 128])
        O = {}
        def dout(name, shape):
            O[name] = K.dout(name, shape)
            return O[name]
        dout("yT", [D, NO])
        dout("C_p", [DEPTH, 4, 256, 256]); dout("n_p", [DEPTH, 4, 256]); dout("m_pT", [DEPTH, 4, 1])
        dout("s5re_pT", [DEPTH, 128, 16]); dout("s5im_pT", [DEPTH, 128, 16])
        dout("ssd_pT", [DEPTH, 8, 128, 64]); dout("conv_pT", [DEPTH, 128, 8, 3])
        dout("C_s", [DEPTH, NS, 4, 256, 256]); dout("n_s", [DEPTH, NS, 4, 256]); dout("m_sT", [DEPTH, 4, NS])
        dout("s5re_sT", [DEPTH, 128, 16, NS]); dout("s5im_sT", [DEPTH, 128, 16, NS])
        dout("ssd_sT", [DEPTH, NS, 8, 128, 64]); dout("conv_sT", [DEPTH, 128, 8, 3, NS])
        hT = K.dscr("hT", [D, NT])
        qT_d = K.dscr("qT_d", [1024, NT]); kT_d = K.dscr("kT_d", [1024, NT])
        k_d = K.dscr("k_d", [NT, 1024]); v_d = K.dscr("v_d", [NT, 1024]); go_d = K.dscr("go_d", [NT, 1024])
        uT_d = K.dscr("uT_d", [512, NT]); zs_d = K.dscr("zs_d", [NT, 512]); xbcT_d = K.dscr("xbcT_d", [1024, NT])
        xcT_d = K.dscr("xcT_d", [1024, NT])
        mix_d = K.dscr("mix_d", [D, NT])
        hO = K.dscr("hO", [D, NO])
        mixsw = K.dscr("mixsw", [128, KC * XW], BF16)
        halfsb = K.sb("halfsb", [128, 2])
        K.ld(halfsb.a[:], I["half"].a, halfsb, writes=[halfsb])

        def blend(dstA, srcB, toks_r, tok_w):
            K.ts("dve", dstA, dstA, halfsb.a[:, 0:1], ALU.mult, list(toks_r) + [halfsb], [tok_w])
            K.stt(dstA, srcB, halfsb.a[:, 1:2], dstA, ALU.mult, ALU.add, list(toks_r) + [halfsb, tok_w], [tok_w])

        XTraw = K.sb("XT", [128, KC * XW // 2], F32)
        XTb = XTraw.a[:].bitcast(BF16).rearrange("p (k t) -> p k t", k=KC)
        XTf = XTraw.a[:].rearrange("p (k t) -> p k t", k=KC)
        tXT = XTraw
        ident = K.sb("ident_sb", [128, 128]); nident = K.sb("nident_sb", [128, 128]); maskT = K.sb("maskT_sb", [64, 64])
        K.ld(ident.a[:], I["ident"].a, ident, writes=[ident]); K.ld(nident.a[:], I["nident"].a, nident, writes=[nident])
        K.ld(maskT.a[:], I["maskT"].a, maskT, writes=[maskT])
        ones_bf = K.sb("ones_bf", [128, 128], BF16); ones_f = K.sb("ones_f", [128, 128])
        K.memset("dve", ones_bf.a[:], 1.0, [ones_bf]); K.memset("dve", ones_f.a[:], 1.0, [ones_f])
        epsb = K.sb("epsb", [128, 1]); K.memset("dve", epsb.a[:], EPS, [epsb])
        halfpi = K.sb("halfpi", [128, 1]); K.memset("dve", halfpi.a[:], math.pi / 2, [halfpi])
        gn = {}
        for nm in ("g_mix", "g_ffn", "g_ple"):
            gn[nm] = K.sb(nm + "_sb", [128, DEPTH, 16])
            for l in range(DEPTH):
                K.ld(gn[nm].a[:, l, :], I[nm].a[l], gn[nm], writes=[gn[nm]])
        gfin = K.sb("gfin_sb", [128, 16]); K.ld(gfin.a[:], I["g_final"].a, gfin, writes=[gfin])
        PS = Pool(K, "ps", [128, 512], F32, 8, space="ps")

        class ListPool:
            def __init__(self, bufs):
                self.bufs = bufs
                self.i = 0

            def next(self):
                b = self.bufs[self.i % len(self.bufs)]
                self.i += 1
                return b
        PSs = ListPool([K.view(PS.bufs[b].a[:, j * 64:(j + 1) * 64], "pss%d_%d" % (b, j)) for b in range(4) for j in range(8)])
        PSb = ListPool([K.view(PS.bufs[b].a, "psb%d" % b) for b in range(4, 8)])
        K.PSs, K.PSb = PSs, PSb
        S.flush()

        def rstd_from_ps(ps, n, inv_d, rs, parts=128):
            K.act(rs.a[:parts, :n], ps.a[:parts, :n], AF.Sqrt, [ps, epsb], [rs], scale=inv_d, bias=epsb.a[:parts, :])
            K.recip(rs.a[:parts, :n], rs.a[:parts, :n], [rs], [rs])

        def norm_stage(src, gsb_ap, gtile, t0, n, dst_ap, dst_tile, P):
            hb = P["hblk"].next()
            K.ld(hb.a[:, :, :n], src.a.rearrange("(k p) t -> p k t", p=128)[:, :, t0:t0 + n], hb, reads=[src], writes=[hb])
            sq = P["sq"].next()
            K.act(sq.a[:, :, :n], hb.a[:, :, :n], AF.Square, [hb], [sq])
            ps = PS.next()
            for k in range(KC):
                K.pe(ps.a[:, :n], ones_bf.a[:], sq.a[:, k, :n], k == 0, k == KC - 1, [sq, ones_bf], [ps])
            rs = P["rs"].next()
            rstd_from_ps(ps, n, 1.0 / D, rs)
            for k in range(KC):
                K.stt(dst_ap[:, k, :], hb.a[:, k, :n], gsb_ap[:, k:k + 1], rs.a[:, :n], ALU.mult, ALU.mult,
                      [hb, rs, gtile], [dst_tile])
            return hb

        def wload(P, W_ap, kc, pw, name="w"):
            wb = P[name].next()
            K.ld(wb.a[:, :kc, :pw], W_ap.rearrange("(k p) c -> p k c", p=128), wb, writes=[wb], q="pool")
            return wb

        def proj_fm(P, xap, xt, kc, W_ap, c0, ncols, evac, tblocks):
            for p0 in range(0, ncols, 512):
                pw = min(512, ncols - p0)
                wb = wload(P, W_ap[:, c0 + p0:c0 + p0 + pw], kc, pw)
                for m0 in range(0, pw, 128):
                    mw = min(128, pw - m0)
                    for (t0, n) in tblocks:
                        ps = PS.next()
                        for k in range(kc):
                            K.pe(ps.a[:mw, :n], wb.a[:, k, m0:m0 + mw], xap[:, k, t0:t0 + n], k == 0, k == kc - 1, [wb, xt], [ps])
                        evac(ps, p0 + m0, mw, t0, n)

        def proj_tm(P, xap, xt, kc, W_ap, c0, ncols, evac):
            for p0 in range(0, ncols, 512):
                pw = min(512, ncols - p0)
                wb = wload(P, W_ap[:, c0 + p0:c0 + p0 + pw], kc, pw)
                for j in range(17):
                    t0 = j * 128
                    n = 128 if j < 16 else NS
                    ps = PS.next()
                    for k in range(kc):
                        K.pe(ps.a[:n, :pw], xap[:, k, t0:t0 + n], wb.a[:, k, :pw], k == 0, k == kc - 1, [wb, xt], [ps])
                    evac(ps, p0, pw, t0, n)

        evi = [0]
        def evac_copy_to_dram(P, ps, pr, fr, dst_ap, dst, scale=None, func=None, mul=None, multile=None):
            stg = P["stg"].next()
            if func is not None:
                K.act(stg.a[:pr, :fr], ps.a[:pr, :fr], func, [ps], [stg])
                if mul is not None:
                    K.tt("dve", stg.a[:pr, :fr], stg.a[:pr, :fr], mul, ALU.mult, [stg, multile], [stg])
            elif scale is not None:
                K.act(stg.a[:pr, :fr], ps.a[:pr, :fr], AF.Copy, [ps], [stg], scale=scale)
            else:
                evi[0] += 1
                K.cp("act" if evi[0] % 2 else "dve", stg.a[:pr, :fr], ps.a[:pr, :fr], [ps], [stg])
            K.stq(dst_ap, stg.a[:pr, :fr], stg, reads=[stg], mwrites=[dst])

        S.dma("sp", hT.a, I["xT"].a, hT, writes=[hT])
        S.flush()

        for l in range(DEPTH):
            if stages < 1:
                break
            with K.phase():
                P = {"hblk": Pool(K, "hblk", [128, KC, 512], F32, 1), "sq": Pool(K, "sq", [128, KC, 512], BF16, 1),
                     "rs": Pool(K, "rs", [128, 512], F32, 2), "w": Pool(K, "w", [128, KC, 512], BF16, 3),
                     "stg": Pool(K, "stg", [128, 512], F32, 4)}
                gi = K.sb("gi", [4, NT]); gf = K.sb("gf", [4, NT]); gdt = K.sb("gdt", [8, NT])
                gml = K.sb("gml", [128, 1024])
                for r in range(2):
                    K.ld(gml.a[r * 64:(r + 1) * 64, :], I["gml_rep"].a[l], gml, writes=[gml])
                for (t0, n) in TB:
                    norm_stage(hT, gn["g_mix"].a[:, l, :], gn["g_mix"], t0, n, XTb[:, :, t0:t0 + n], tXT, P)
                W = I["w_in"].a[l]
                proj_fm(P, XTb, tXT, KC, W, 0, 1024, lambda ps, co, mw, t0, n: evac_copy_to_dram(P, ps, mw, n, qT_d.a[co:co + mw, t0:t0 + n], qT_d), TB)
                proj_fm(P, XTb, tXT, KC, W, 1024, 1024, lambda ps, co, mw, t0, n: evac_copy_to_dram(P, ps, mw, n, kT_d.a[co:co + mw, t0:t0 + n], kT_d, scale=1.0 / 16.0), TB)
                proj_tm(P, XTb, tXT, KC, W, 1024, 1024, lambda ps, co, pw, t0, n: evac_copy_to_dram(P, ps, n, pw, k_d.a[t0:t0 + n, co:co + pw], k_d, scale=1.0 / 16.0))
                proj_tm(P, XTb, tXT, KC, W, 2048, 1024, lambda ps, co, pw, t0, n: evac_copy_to_dram(P, ps, n, pw, v_d.a[t0:t0 + n, co:co + pw], v_d))
                proj_tm(P, XTb, tXT, KC, W, 3072, 1024, lambda ps, co, pw, t0, n: evac_copy_to_dram(P, ps, n, pw, go_d.a[t0:t0 + n, co:co + pw], go_d, func=AF.Sigmoid, mul=gml.a[:n, co:co + pw], multile=gml))
                gsc = {}
                for nm, c0, nr in (("gi_d", 4096, 4), ("gf_d", 4100, 4), ("gdt_d", 6152, 8)):
                    gsc[nm] = K.dscr(nm + str(l), [nr, NT])
                    proj_fm(P, XTb, tXT, KC, W, c0, nr, lambda ps, co, mw, t0, n, nm=nm: evac_copy_to_dram(P, ps, mw, n, gsc[nm].a[co:co + mw, t0:t0 + n], gsc[nm]), TB)
                proj_fm(P, XTb, tXT, KC, W, 4104, 512, lambda ps, co, mw, t0, n: evac_copy_to_dram(P, ps, mw, n, uT_d.a[co:co + mw, t0:t0 + n], uT_d), TB)
                proj_tm(P, XTb, tXT, KC, W, 4616, 512, lambda ps, co, pw, t0, n: evac_copy_to_dram(P, ps, n, pw, zs_d.a[t0:t0 + n, co:co + pw], zs_d, func=AF.Silu))
                proj_fm(P, XTb, tXT, KC, W, 5128, 1024, lambda ps, co, mw, t0, n: evac_copy_to_dram(P, ps, mw, n, xbcT_d.a[co:co + mw, t0:t0 + n], xbcT_d), TB)
            if stages < 2:
                break
            mixers(K, S, PS, I, O, l, dict(hT=hT, qT_d=qT_d, kT_d=kT_d, k_d=k_d, v_d=v_d, go_d=go_d, uT_d=uT_d, zs_d=zs_d,
                                           xbcT_d=xbcT_d, xcT_d=xcT_d, gi_d=gsc["gi_d"], gf_d=gsc["gf_d"], gdt_d=gsc["gdt_d"]),
                   XTb, tXT, ident, nident, maskT, ones_f, epsb, halfpi, gst, stages)
            if "mix_d" in K.dbg:
                with K.phase():
                    stg = Pool(K, "mstg", [128, 512], F32, 2)
                    for k in range(KC):
                        for (t0, n) in TB:
                            s_ = stg.next()
                            K.cp("dve", s_.a[:, :n], XTb[:, k, t0:t0 + n], [tXT], [s_])
                            K.stq(mix_d.a[k * 128:(k + 1) * 128, t0:t0 + n], s_.a[:, :n], s_, reads=[s_], mwrites=[mix_d])
            if stages < 3:
                break
            own = (l == DEPTH - 1)
            hcur = hO if own else hT
            tbl = TBO if own else TB
            if own:
                with K.phase():
                    for k in range(KC):
                        blend(XTb[:, k, 0:1024], XTb[:, k, 1024:2048], [tXT], tXT)
                    for k in range(KC):
                        K.cp("dve", XTb[:, k, 1024:NO], XTb[:, k, L:NT], [tXT], [tXT])
                    hsw = Pool(K, "hsw", [128, 2, 1024], F32, 2)
                    for k in range(KC):
                        t_ = hsw.next()
                        K.ld(t_.a[:], hT.a[k * 128:(k + 1) * 128, 0:L].rearrange("p (h t) -> p h t", h=2), t_, reads=[hT], writes=[t_])
                        blend(t_.a[:, 0, :], t_.a[:, 1, :], [t_], t_)
                        K.stq(hO.a[k * 128:(k + 1) * 128, 0:1024], t_.a[:, 0, :], t_, reads=[t_], mwrites=[hO])
                    S.dma("sp", hO.a[:, 1024:NO], hT.a[:, L:NT], hO, reads=[hT], mwrites=[hO])
            with K.phase():
                P = {"w": Pool(K, "w", [128, KC, 512], BF16, 3), "stg": Pool(K, "stg", [128, 512], F32, 4),
                     "hb": Pool(K, "hb", [128, 512], F32, 3)}
                def ev_res(ps, co, mw, t0, n):
                    hb = P["hb"].next()
                    K.ld(hb.a[:mw, :n], hcur.a[co:co + mw, t0:t0 + n], hb, reads=[hcur], writes=[hb])
                    stg = P["stg"].next()
                    K.tt("dve", stg.a[:mw, :n], ps.a[:mw, :n], hb.a[:mw, :n], ALU.add, [ps, hb], [stg])
                    K.stq(hcur.a[co:co + mw, t0:t0 + n], stg.a[:mw, :n], stg, reads=[stg], mwrites=[hcur])
                proj_fm(P, XTb, tXT, KC, I["w_out"].a[l], 0, D, ev_res, tbl)
            if stages < 4:
                break
            ffn_phase(K, S, PS, I, l, hcur, XTf, tXT, gn, ones_bf, epsb, ident, gst, norm_stage, stages, own)
            if stages < 5:
                break
            with K.phase():
                P = {"hblk": Pool(K, "hblk", [128, KC, 512], F32, 1), "sq": Pool(K, "sq", [128, KC, 512], BF16, 1),
                     "rs": Pool(K, "rs", [128, 512], F32, 2), "w": Pool(K, "w", [128, KC, 512], BF16, 2),
                     "stg": Pool(K, "stg", [128, 512], F32, 3), "hb": Pool(K, "hb", [128, 512], F32, 2)}
                pTf = K.sb("pTf", [128, 2, NT]); pTb = K.sb("pTb", [128, 2, NT], BF16)
                pTv = I["pT"].a[l].rearrange("(k p) t -> p k t", p=128)
                K.ld(pTf.a[:], pTv, pTf, writes=[pTf])
                if own:
                    for k in range(2):
                        blend(pTf.a[:, k, 0:1024], pTf.a[:, k, 1024:2048], [pTf], pTf)
                    for k in range(2):
                        K.cp("dve", pTf.a[:, k, 1024:NO], pTf.a[:, k, L:NT], [pTf], [pTf])
                K.cp("act", pTb.a[:], pTf.a[:], [pTf], [pTb])
                wple = K.sb("wple", [128, 2, D], BF16)
                K.ld(wple.a[:], I["w_ple"].a[l].rearrange("(k p) c -> p k c", p=128), wple, writes=[wple], q="pool")
                for (t0, n) in tbl:
                    norm_stage(hcur, gn["g_ple"].a[:, l, :], gn["g_ple"], t0, n, XTb[:, :, t0:t0 + n], tXT, P)
                def ev_ple(ps, co, mw, t0, n):
                    sg = P["stg"].next()
                    K.act(sg.a[:mw, :n], ps.a[:mw, :n], AF.Sigmoid, [ps], [sg])
                    ps2 = PS.next()
                    for k in range(2):
                        K.pe(ps2.a[:mw, :n], wple.a[:, k, co:co + mw], pTb.a[:, k, t0:t0 + n], k == 0, k == 1, [wple, pTb], [ps2])
                    K.tt("dve", sg.a[:mw, :n], ps2.a[:mw, :n], sg.a[:mw, :n], ALU.mult, [ps2, sg], [sg])
                    hb = P["hb"].next()
                    K.ld(hb.a[:mw, :n], hcur.a[co:co + mw, t0:t0 + n], hb, reads=[hcur], writes=[hb])
                    K.tt("dve", sg.a[:mw, :n], sg.a[:mw, :n], hb.a[:mw, :n], ALU.add, [sg, hb], [sg])
                    K.stq(hcur.a[co:co + mw, t0:t0 + n], sg.a[:mw, :n], sg, reads=[sg], mwrites=[hcur])
                proj_fm(P, XTb, tXT, KC, I["w_pleg"].a[l], 0, D, ev_ple, tbl)
        with K.phase():
            P = {"hblk": Pool(K, "hblk", [128, KC, 512], F32, 1), "sq": Pool(K, "sq", [128, KC, 512], BF16, 1),
                 "rs": Pool(K, "rs", [128, 512], F32, 2)}
            yb = K.sb("yb", [128, KC, 512])
            for (t0, n) in TBO:
                norm_stage(hO, gfin.a, gfin, t0, n, yb.a[:, :, :n], yb, P)
                K.stq(O["yT"].a.rearrange("(k p) t -> p k t", p=128)[:, :, t0:t0 + n], yb.a[:, :, :n], yb, reads=[yb], mwrites=[O["yT"]], is_output=True)
    return nc, K


def mixers(K, S, PS, I, O, l, Dm, XTb, tXT, ident, nident, maskT, ones_f, epsb, halfpi, gst, stages):
    with K.phase():
        rowt = [K.sb("row%d" % i, [8, NT]) for i in range(7)]
        A, Bt, Ct, Dt, Et, Fa, Gm = rowt
        onesr = K.sb("onesr", [8, 1]); K.memset("dve", onesr.a[:], 1.0, [onesr])
        TM = K.sb("TM", [64, NCI, 20])
        TMB = K.sb("TMB", [64, NCI, 32])
        GLb = K.sb("GLb", [128, 4 * NCI]); DECb = K.sb("DECb", [128, 8 * NCI])
        sel4 = K.sb("sel4", [4, 4, 128]); K.ld(sel4.a[:], I["sel4"].a, sel4, writes=[sel4])
        sel8 = K.sb("sel8", [8, 8, 128]); K.ld(sel8.a[:], I["sel8"].a, sel8, writes=[sel8])
        small = Pool(K, "small", [8, NCI], F32, 4)

        def to_tm(src, nr, dst, col0):
            for (t0, cl, ci) in CHUNKS:
                ps = PS.next()
                K.tr(ps.a[:cl, :nr], src.a[:nr, t0:t0 + cl], ident.a[:nr, :nr], [src, ident], [ps])
                K.cp("act" if ci % 2 else "dve", dst.a[:cl, ci, col0:col0 + nr], ps.a[:cl, :nr], [ps], [dst])

        def bcast_rows(rows, nr, sel, dst, ncol):
            for h in range(nr):
                ps = PS.next()
                K.pe(ps.a[:, :ncol], sel.a[:nr, h, :], rows.a[:nr, :ncol], True, True, [sel, rows], [ps])
                K.cp("act", dst.a[:, h * ncol:(h + 1) * ncol], ps.a[:, :ncol], [ps], [dst])

        def chunkview(t, nr):
            return t.a[:nr, :L].rearrange("p (c t) -> p c t", t=64)

        def prevlast(Mt, nr, init_s, prev, last):
            K.memset("dve", prev.a[:nr, 0:1], 0.0, [prev])
            K.cp("dve", prev.a[:nr, 1:32], Mt.a[:nr, 63:L - 64:64], [Mt], [prev])
            if init_s is None:
                K.memset("dve", prev.a[:nr, 32:NCI], 0.0, [prev])
            else:
                K.cp("dve", prev.a[:nr, 32:NCI], init_s, [Mt], [prev])
            K.cp("dve", last.a[:nr, 0:32], Mt.a[:nr, 63:L:64], [Mt], [last])
            K.cp("dve", last.a[:nr, 32:NCI], Mt.a[:nr, L:NT], [Mt], [last])

        def sub_chunk(out, x, cvals, nr, sign):
            cb = cvals.a[:nr, 0:32].unsqueeze(2).to_broadcast([nr, 32, 64])
            if sign > 0:
                K.tt("dve", chunkview(out, nr), chunkview(x, nr), cb, ALU.subtract, [x, cvals], [out])
                K.tt("dve", out.a[:nr, L:NT], x.a[:nr, L:NT], cvals.a[:nr, 32:NCI], ALU.subtract, [x, cvals], [out])
            else:
                K.tt("dve", chunkview(out, nr), cb, chunkview(x, nr), ALU.subtract, [x, cvals], [out])
                K.tt("dve", out.a[:nr, L:NT], cvals.a[:nr, 32:NCI], x.a[:nr, L:NT], ALU.subtract, [x, cvals], [out])

        def softplus_neg(x, t1, t2, nr, neg_in):
            K.act(t1.a[:nr, :], x.a[:nr, :], AF.Abs, [x], [t1])
            K.act(t1.a[:nr, :], t1.a[:nr, :], AF.Exp, [t1], [t1], scale=-1.0)
            K.act(t1.a[:nr, :], t1.a[:nr, :], AF.Ln, [t1], [t1], bias=1.0)
            K.ts("dve", t2.a[:nr, :], x.a[:nr, :], 0.0, ALU.min if neg_in else ALU.max, [x], [t2])

        bi = K.sb("bi", [4, 1]); bf = K.sb("bf", [4, 1]); m0s = K.sb("m0s", [4, NS])
        K.ld(bi.a[:], I["b_ig"].a[l], bi, writes=[bi]); K.ld(bf.a[:], I["b_fg"].a[l], bf, writes=[bf])
        K.ld(m0s.a[:], I["smT"].a[l], m0s, writes=[m0s])
        K.ld(A.a[:4, :], Dm["gi_d"].a, A, reads=[Dm["gi_d"]], writes=[A])
        K.ld(Bt.a[:4, :], Dm["gf_d"].a, Bt, reads=[Dm["gf_d"]], writes=[Bt])
        K.ts("dve", A.a[:4, :], A.a[:4, :], bi.a[:, 0:1], ALU.add, [A, bi], [A])
        K.ts("dve", Bt.a[:4, :], Bt.a[:4, :], bf.a[:, 0:1], ALU.add, [Bt, bf], [Bt])
        softplus_neg(Bt, Ct, Dt, 4, True)
        K.tt("dve", Bt.a[:4, :], Dt.a[:4, :], Ct.a[:4, :], ALU.subtract, [Dt, Ct], [Bt])
        K.scan(Et.a[:4, :L], onesr.a[:4, 0:1].to_broadcast([4, L]), Bt.a[:4, :L], 0.0, ALU.mult, ALU.add, [onesr, Bt], [Et])
        K.cp("dve", Et.a[:4, L:NT], Bt.a[:4, L:NT], [Bt], [Et])
        K.tt("dve", Fa.a[:4, :], A.a[:4, :], Et.a[:4, :], ALU.subtract, [A, Et], [Fa])
        K.scan(Gm.a[:4, :L], Fa.a[:4, :L], Fa.a[:4, :L], 0.0, ALU.max, ALU.max, [Fa], [Gm])
        K.tt("dve", Gm.a[:4, L:NT], Fa.a[:4, L:NT], m0s.a[:], ALU.max, [Fa, m0s], [Gm])
        Mprev = small.next(); Mlast = small.next()
        prevlast(Gm, 4, m0s.a[:], Mprev, Mlast)
        glr = small.next()
        K.tt("dve", glr.a[:4, :], Mprev.a[:4, :], Mlast.a[:4, :], ALU.subtract, [Mprev, Mlast], [glr])
        K.act(glr.a[:4, :], glr.a[:4, :], AF.Exp, [glr], [glr])
        bcast_rows(glr, 4, sel4, GLb, NCI)
        sub_chunk(A, Gm, Mprev, 4, -1)
        K.act(A.a[:4, :], A.a[:4, :], AF.Exp, [A], [A])
        sub_chunk(Dt, Fa, Mlast, 4, +1)
        K.act(Dt.a[:4, :], Dt.a[:4, :], AF.Exp, [Dt], [Dt])
        K.tt("dve", Ct.a[:4, :], Et.a[:4, :], Gm.a[:4, :], ALU.add, [Et, Gm], [Ct])
        K.stq(O["m_pT"].a[l], Ct.a[:4, L - 1:L], Ct, reads=[Ct], mwrites=[O["m_pT"]], is_output=True)
        K.stq(O["m_sT"].a[l], Ct.a[:4, L:NT], Ct, reads=[Ct], mwrites=[O["m_sT"]], is_output=True)
        K.act(Ct.a[:4, :], Ct.a[:4, :], AF.Exp, [Ct], [Ct], scale=-1.0)
        for src, c0 in ((Fa, 0), (Gm, 4), (A, 8), (Ct, 12), (Dt, 16)):
            to_tm(src, 4, TM, c0)

        cpool = {"q": Pool(K, "mq", [128, 8, 64], F32, 2), "k": Pool(K, "mk", [128, 8, 64], F32, 2),
                 "kt": Pool(K, "mkt", [64, 1024], F32, 1), "v": Pool(K, "mv", [64, 4, 257], F32, 2),
                 "go": Pool(K, "mgo", [64, 1024], F32, 1), "hm": Pool(K, "mhm", [64, 1024], F32, 1),
                 "w": Pool(K, "mw", [64, 64], F32, 8), "sw": Pool(K, "msw", [64, 64], F32, 8),
                 "nsb": Pool(K, "mnsb", [64, 257], F32, 4), "res": Pool(K, "mres", [64, 257], F32, 4),
                 "kw": Pool(K, "mkw", [64, 256], F32, 4), "sc": Pool(K, "msc", [64, 4, 4], F32, 3),
                 "junk": Pool(K, "mjunk", [64, 256], F32, 2)}
        for b_ in cpool["v"].bufs:
            K.memset("dve", b_.a[:, :, 256:257], 1.0, [b_])
        Cst = [K.sb("Caug%d" % h, [128, 2, 257]) for h in range(4)]

        PSs, PSb = PS, PS

        def mlstm_chunk(t0, cl, ci, Caug):
            nk = dict(allow_slow_non_contiguous=True) if cl == 1 else {}
            q = cpool["q"].next(); kT = cpool["k"].next(); kt = cpool["kt"].next(); v = cpool["v"].next(); go = cpool["go"].next()
            K.ld(q.a[:, :, :cl], Dm["qT_d"].a.rearrange("(j p) t -> p j t", p=128)[:, :, t0:t0 + cl], q, reads=[Dm["qT_d"]], writes=[q], **nk)
            K.ld(kT.a[:, :, :cl], Dm["kT_d"].a.rearrange("(j p) t -> p j t", p=128)[:, :, t0:t0 + cl], kT, reads=[Dm["kT_d"]], writes=[kT], **nk)
            K.ld(kt.a[:cl, :], Dm["k_d"].a[t0:t0 + cl, :], kt, reads=[Dm["k_d"]], writes=[kt])
            K.ld(v.a[:cl, :, 0:256], Dm["v_d"].a[t0:t0 + cl, :].rearrange("t (h d) -> t h d", h=4), v, reads=[Dm["v_d"]], writes=[v])
            K.ld(go.a[:cl, :], Dm["go_d"].a[t0:t0 + cl, :], go, reads=[Dm["go_d"]], writes=[go])
            hm = cpool["hm"].next()
            H = range(4)
            ps_s = {}; ps_d = {}; w = {}; sw = {}; ps_n = {}; ps_i = {}; nsb = {}; res = {}; sc = {}; kw = {}
            for h in H:
                ps_s[h] = PSs.next()
                for kc in range(2):
                    K.pe(ps_s[h].a[:cl, :cl], kT.a[:, h * 2 + kc, :cl], q.a[:, h * 2 + kc, :cl], kc == 0, kc == 1, [kT, q], [ps_s[h]])
            for h in H:
                ps_d[h] = PSs.next()
                K.pe(ps_d[h].a[:cl, :cl], TM.a[:cl, ci, 4 + h:5 + h].to_broadcast([cl, cl]), nident.a[:cl, :cl], True, False, [TM, nident], [ps_d[h]])
                K.pe(ps_d[h].a[:cl, :cl], ident.a[:cl, :cl], maskT.a[:cl, :cl], False, False, [ident, maskT], [ps_d[h]])
                K.pe(ps_d[h].a[:cl, :cl], ident.a[:cl, :cl], TM.a[:cl, ci, h:h + 1].to_broadcast([cl, cl]), False, True, [ident, TM], [ps_d[h]])
            for h in H:
                w[h] = cpool["w"].next()
                K.act(w[h].a[:cl, :cl], ps_d[h].a[:cl, :cl], AF.Exp, [ps_d[h]], [w[h]])
            for h in H:
                kw[h] = cpool["kw"].next()
                K.act(kw[h].a[:cl, :], kt.a[:cl, h * 256:(h + 1) * 256], AF.Copy, [kt, TM], [kw[h]], scale=TM.a[:cl, ci, 16 + h:17 + h])
            for h in H:
                sw[h] = cpool["sw"].next()
                K.tt("dve", sw[h].a[:cl, :cl], ps_s[h].a[:cl, :cl], w[h].a[:cl, :cl], ALU.mult, [ps_s[h], w[h]], [sw[h]])
            for h in H:
                ps_n[h] = PSb.next()
                K.pe(ps_n[h].a[:cl, :257], sw[h].a[:cl, :cl], v.a[:cl, h, :], True, True, [sw[h], v], [ps_n[h]])
            for h in H:
                nsb[h] = cpool["nsb"].next()
                K.cp("act", nsb[h].a[:cl, :], ps_n[h].a[:cl, :257], [ps_n[h]], [nsb[h]])
            for h in H:
                ps_i[h] = PSb.next()
                for kc in range(2):
                    K.pe(ps_i[h].a[:cl, :257], q.a[:, h * 2 + kc, :cl], Caug[h].a[:, kc, :], kc == 0, kc == 1, [q, Caug[h]], [ps_i[h]])
            for h in H:
                res[h] = cpool["res"].next()
                K.stt(res[h].a[:cl, :], ps_i[h].a[:cl, :257], TM.a[:cl, ci, 8 + h:9 + h], nsb[h].a[:cl, :], ALU.mult, ALU.add, [ps_i[h], TM, nsb[h]], [res[h]])
            for hp in range(2):
                pcs = {}
                for h in (2 * hp, 2 * hp + 1):
                    for kc in range(2):
                        pcs[(h, kc)] = PSb.next()
                        K.pe(pcs[(h, kc)].a[:, :257], kw[h].a[:cl, kc * 128:(kc + 1) * 128], v.a[:cl, h, :], True, True, [kw[h], v], [pcs[(h, kc)]])
                for h in (2 * hp, 2 * hp + 1):
                    for kc in range(2):
                        K.stt(Caug[h].a[:, kc, :], Caug[h].a[:, kc, :], GLb.a[:, h * NCI + ci:h * NCI + ci + 1], pcs[(h, kc)].a[:, :257], ALU.mult, ALU.add, [Caug[h], GLb, pcs[(h, kc)]], [Caug[h]])
            sca = cpool["sc"].next()
            for h in H:
                K.act(sca.a[:cl, h, 0:1], res[h].a[:cl, 256:257], AF.Abs, [res[h]], [sca])
            K.tt("dve", sca.a[:cl, :, 0], sca.a[:cl, :, 0], TM.a[:cl, ci, 12:16], ALU.max, [sca, TM], [sca])
            K.recip(sca.a[:cl, :, 0], sca.a[:cl, :, 0], [sca], [sca])
            for h in H:
                junk = cpool["junk"].next()
                K.act(junk.a[:cl, :], res[h].a[:cl, 0:256], AF.Square, [res[h], sca], [junk, sca], scale=sca.a[:cl, h, 0:1], accum=sca.a[:cl, h, 1:2])
            K.act(sca.a[:cl, :, 2], sca.a[:cl, :, 1], AF.Sqrt, [sca, epsb], [sca], scale=1.0 / 256.0, bias=epsb.a[:cl, :])
            K.recip(sca.a[:cl, :, 2], sca.a[:cl, :, 2], [sca], [sca])
            K.tt("dve", sca.a[:cl, :, 3], sca.a[:cl, :, 2], sca.a[:cl, :, 0], ALU.mult, [sca], [sca])
            for h in H:
                K.stt(hm.a[:cl, h * 256:(h + 1) * 256], res[h].a[:cl, 0:256], sca.a[:cl, h, 3:4], go.a[:cl, h * 256:(h + 1) * 256], ALU.mult, ALU.mult, [res[h], sca, go], [hm])
            for j in range(8):
                ps = PSs.next()
                K.tr(ps.a[:, :cl], hm.a[:cl, j * 128:(j + 1) * 128], ident.a[:cl, :cl], [hm, ident], [ps])
                K.cp("act" if j % 2 else "dve", XTb[:, j, t0:t0 + cl], ps.a[:, :cl], [ps], [tXT])

        for h in range(4):
            K.memset("dve", Cst[h].a[:], 0.0, [Cst[h]])
        Cs2 = [K.sb("Csmp%d" % h, [128, 2, 257]) for h in range(4)]

        def sample_step(t0, cl, ci):
            j = ci - 32
            Cs = Cs2
            for h in range(4):
                K.ld(Cs[h].a[:, :, 0:256], I["sC"].a[l, j, h].rearrange("(kc p) d -> p kc d", p=128), Cs[h], writes=[Cs[h]])
                K.ld(Cs[h].a[:, :, 256], I["sn"].a[l, j, h].rearrange("(kc p) -> p kc", p=128), Cs[h], writes=[Cs[h]], allow_slow_non_contiguous=True)
            mlstm_chunk(t0, cl, ci, Cs)
            for h in range(4):
                K.stq(O["C_s"].a[l, j, h].rearrange("(kc p) d -> p kc d", p=128), Cs[h].a[:, :, 0:256], Cs[h], reads=[Cs[h]], mwrites=[O["C_s"]], is_output=True)
                K.stq(O["n_s"].a[l, j, h].rearrange("(kc p) -> p kc", p=128), Cs[h].a[:, :, 256], Cs[h], reads=[Cs[h]], mwrites=[O["n_s"]], is_output=True, allow_slow_non_contiguous=True)

        for i, (t0, cl, ci) in enumerate(CHUNKS[:32]):
            mlstm_chunk(t0, cl, ci, Cst)
            if i % 2 == 1:
                sample_step(*CHUNKS[32 + i // 2])
        for h in range(4):
            K.stq(O["C_p"].a[l, h].rearrange("(kc p) d -> p kc d", p=128), Cst[h].a[:, :, 0:256], Cst[h], reads=[Cst[h]], mwrites=[O["C_p"]], is_output=True)
            K.stq(O["n_p"].a[l, h].rearrange("(kc p) -> p kc", p=128), Cst[h].a[:, :, 256], Cst[h], reads=[Cst[h]], mwrites=[O["n_p"]], is_output=True, allow_slow_non_contiguous=True)
    if stages >= 2.3:
        ssd_mixer(K, S, PS, I, O, l, Dm, XTb, tXT, ident, nident, maskT, epsb)
    if stages >= 2.6:
        s5_mixer(K, S, PS, I, O, l, Dm, XTb, tXT, ident, ones_f, epsb, halfpi)


def _mk_helpers(K, PS, ident):
    def to_tm(src, nr, dst, col0):
        for (t0, cl, ci) in CHUNKS:
            ps = PS.next()
            K.tr(ps.a[:cl, :nr], src.a[:nr, t0:t0 + cl], ident.a[:nr, :nr], [src, ident], [ps])
            K.cp("act" if ci % 2 else "dve", dst.a[:cl, ci, col0:col0 + nr], ps.a[:cl, :nr], [ps], [dst])

    def bcast_rows(rows, nr, sel, dst, ncol):
        for h in range(nr):
            ps = PS.next()
            K.pe(ps.a[:, :ncol], sel.a[:nr, h, :], rows.a[:nr, :ncol], True, True, [sel, rows], [ps])
            K.cp("act", dst.a[:, h * ncol:(h + 1) * ncol], ps.a[:, :ncol], [ps], [dst])

    def chunkview(t, nr):
        return t.a[:nr, :L].rearrange("p (c t) -> p c t", t=64)

    def prevlast(Mt, nr, init_s, prev, last):
        K.memset("dve", prev.a[:nr, 0:1], 0.0, [prev])
        K.cp("dve", prev.a[:nr, 1:32], Mt.a[:nr, 63:L - 64:64], [Mt], [prev])
        if init_s is None:
            K.memset("dve", prev.a[:nr, 32:NCI], 0.0, [prev])
        else:
            K.cp("dve", prev.a[:nr, 32:NCI], init_s, [Mt], [prev])
        K.cp("dve", last.a[:nr, 0:32], Mt.a[:nr, 63:L:64], [Mt], [last])
        K.cp("dve", last.a[:nr, 32:NCI], Mt.a[:nr, L:NT], [Mt], [last])

    def sub_chunk(out, x, cvals, nr, sign):
        cb = cvals.a[:nr, 0:32].unsqueeze(2).to_broadcast([nr, 32, 64])
        if sign > 0:
            K.tt("dve", chunkview(out, nr), chunkview(x, nr), cb, ALU.subtract, [x, cvals], [out])
            K.tt("dve", out.a[:nr, L:NT], x.a[:nr, L:NT], cvals.a[:nr, 32:NCI], ALU.subtract, [x, cvals], [out])
        else:
            K.tt("dve", chunkview(out, nr), cb, chunkview(x, nr), ALU.subtract, [x, cvals], [out])
            K.tt("dve", out.a[:nr, L:NT], cvals.a[:nr, 32:NCI], x.a[:nr, L:NT], ALU.subtract, [x, cvals], [out])
    return to_tm, bcast_rows, prevlast, sub_chunk


def ssd_mixer(K, S, PS, I, O, l, Dm, XTb, tXT, ident, nident, maskT, epsb):
    xbcT_d, xcT_d = Dm["xbcT_d"], Dm["xcT_d"]
    with K.phase():
        to_tm, bcast_rows, prevlast, sub_chunk = _mk_helpers(K, PS, ident)
        cw = K.sb("cw", [128, 8, 4]); cb = K.sb("cb", [128, 8]); cin = K.sb("cin", [128, 8, 3, NS])
        K.ld(cw.a[:], I["conv_w"].a[l], cw, writes=[cw]); K.ld(cb.a[:], I["conv_b"].a[l], cb, writes=[cb])
        K.ld(cin.a[:], I["convT"].a[l], cin, writes=[cin])
        xp = Pool(K, "xp", [128, 3 + 512], F32, 2); xo = Pool(K, "xo", [128, 512], F32, 2)
        for j in range(8):
            rows = slice(j * 128, (j + 1) * 128)
            for (t0, n) in TB[:4]:
                x = xp.next()
                K.ld(x.a[:, 3:3 + n], xbcT_d.a[rows, t0:t0 + n], x, reads=[xbcT_d], writes=[x])
                if t0 == 0:
                    K.memset("dve", x.a[:, 0:3], 0.0, [x])
                else:
                    K.ld(x.a[:, 0:3], xbcT_d.a[rows, t0 - 3:t0], x, reads=[xbcT_d], writes=[x])
                o = xo.next()
                K.ts("dve", o.a[:, :n], x.a[:, 3:3 + n], cw.a[:, j, 3:4], ALU.mult, [x, cw], [o])
                for tap in (2, 1, 0):
                    K.stt(o.a[:, :n], x.a[:, tap:tap + n], cw.a[:, j, tap:tap + 1], o.a[:, :n], ALU.mult, ALU.add, [x, cw, o], [o])
                K.act(o.a[:, :n], o.a[:, :n], AF.Silu, [o, cb], [o], bias=cb.a[:, j:j + 1])
                K.stq(xcT_d.a[rows, t0:t0 + n], o.a[:, :n], o, reads=[o], mwrites=[xcT_d])
                if t0 == 1536:
                    K.stq(O["conv_pT"].a[l, :, j, :], x.a[:, 512:515], x, reads=[x], mwrites=[O["conv_pT"]], is_output=True)
            x = xp.next()
            K.ld(x.a[:, 0:NS], xbcT_d.a[rows, L:NT], x, reads=[xbcT_d], writes=[x])
            o = xo.next()
            K.ts("dve", o.a[:, :NS], x.a[:, 0:NS], cw.a[:, j, 3:4], ALU.mult, [x, cw], [o])
            for tap in (2, 1, 0):
                K.stt(o.a[:, :NS], cin.a[:, j, tap, :], cw.a[:, j, tap:tap + 1], o.a[:, :NS], ALU.mult, ALU.add, [cin, cw, o], [o])
            K.act(o.a[:, :NS], o.a[:, :NS], AF.Silu, [o, cb], [o], bias=cb.a[:, j:j + 1])
            K.stq(xcT_d.a[rows, L:NT], o.a[:, :NS], o, reads=[o], mwrites=[xcT_d])
            K.stq(O["conv_sT"].a[l, :, j, 0:2, :], cin.a[:, j, 1:3, :], cin, reads=[cin], mwrites=[O["conv_sT"]], is_output=True)
            K.stq(O["conv_sT"].a[l, :, j, 2, :], x.a[:, 0:NS], x, reads=[x], mwrites=[O["conv_sT"]], is_output=True)
        A, Bt, Ct, Dt, Et, Fa, Gm = [K.sb("srow%d" % i, [8, NT]) for i in range(7)]
        onesr = K.sb("onesr", [8, 1]); K.memset("dve", onesr.a[:], 1.0, [onesr])
        TMB = K.sb("TMB", [64, NCI, 32]); DECb = K.sb("DECb", [128, 8 * NCI])
        sel8 = K.sb("sel8", [8, 8, 128]); K.ld(sel8.a[:], I["sel8"].a, sel8, writes=[sel8])
        small = Pool(K, "ssmall", [8, NCI], F32, 4)
        dtb = K.sb("dtb", [8, 1]); alog = K.sb("alog", [8, 1])
        K.ld(dtb.a[:], I["dt_bias"].a[l], dtb, writes=[dtb]); K.ld(alog.a[:], I["a_log"].a[l], alog, writes=[alog])
        K.act(alog.a[:], alog.a[:], AF.Exp, [alog], [alog])
        K.ts("dve", alog.a[:], alog.a[:], -1.0, ALU.mult, [alog], [alog])
        K.ld(A.a[:, :], Dm["gdt_d"].a, A, reads=[Dm["gdt_d"]], writes=[A])
        K.ts("dve", A.a[:, :], A.a[:, :], dtb.a[:, 0:1], ALU.add, [A, dtb], [A])
        K.act(Ct.a[:, :], A.a[:, :], AF.Abs, [A], [Ct])
        K.act(Ct.a[:, :], Ct.a[:, :], AF.Exp, [Ct], [Ct], scale=-1.0)
        K.act(Ct.a[:, :], Ct.a[:, :], AF.Ln, [Ct], [Ct], bias=1.0)
        K.ts("dve", Dt.a[:, :], A.a[:, :], 0.0, ALU.max, [A], [Dt])
        K.tt("dve", A.a[:, :], Dt.a[:, :], Ct.a[:, :], ALU.add, [Dt, Ct], [A])
        K.ts("dve", Bt.a[:, :], A.a[:, :], alog.a[:, 0:1], ALU.mult, [A, alog], [Bt])
        K.scan(Et.a[:, :L], onesr.a[:, 0:1].to_broadcast([8, L]), Bt.a[:, :L], 0.0, ALU.mult, ALU.add, [onesr, Bt], [Et])
        K.cp("dve", Et.a[:, L:NT], Bt.a[:, L:NT], [Bt], [Et])
        Gprev = small.next(); Glast = small.next(); decr = small.next()
        prevlast(Et, 8, None, Gprev, Glast)
        K.tt("dve", decr.a[:, :], Glast.a[:, :], Gprev.a[:, :], ALU.subtract, [Glast, Gprev], [decr])
        K.act(decr.a[:, :], decr.a[:, :], AF.Exp, [decr], [decr])
        bcast_rows(decr, 8, sel8, DECb, NCI)
        sub_chunk(Fa, Et, Gprev, 8, +1)
        K.act(Fa.a[:, :], Fa.a[:, :], AF.Exp, [Fa], [Fa])
        sub_chunk(Gm, Et, Glast, 8, -1)
        K.act(Gm.a[:, :], Gm.a[:, :], AF.Exp, [Gm], [Gm])
        for src, c0 in ((Et, 0), (Fa, 8), (Gm, 16), (A, 24)):
            to_tm(src, 8, TMB, c0)
        dd = K.sb("ssdd", [64, 8]); gs = K.sb("gssd", [64, 512])
        K.ld(dd.a[:], I["ssdd_rep"].a[l], dd, writes=[dd]); K.ld(gs.a[:], I["gssd_rep"].a[l], gs, writes=[gs])
        cp = {"xc": Pool(K, "sxc", [128, 8, 64], F32, 2), "zs": Pool(K, "szs", [64, 512], F32, 2),
              "xtok": Pool(K, "sxt", [64, 512], F32, 2), "btok": Pool(K, "sbt", [64, 256], F32, 2),
              "sc": Pool(K, "ssc", [64, 2, 64], F32, 2), "seg": Pool(K, "sseg", [64, 64], F32, 8),
              "xdt": Pool(K, "sxdt", [64, 64], F32, 8), "xw": Pool(K, "sxw", [64, 64], F32, 8),
              "y1": Pool(K, "sy1", [64, 64], F32, 8), "yss": Pool(K, "syss", [64, 512], F32, 2),
              "s": Pool(K, "ss", [64, 4], F32, 2), "junk": Pool(K, "sjunk", [64, 512], F32, 1)}
        STp0 = [K.sb("ST%d" % h, [128, 64]) for h in range(8)]
        ST = list(STp0)

        def chunk(t0, cl, ci):
            nk = dict(allow_slow_non_contiguous=True) if cl == 1 else {}
            xc = cp["xc"].next(); zs = cp["zs"].next()
            K.ld(xc.a[:, :, :cl], xcT_d.a.rearrange("(j p) t -> p j t", p=128)[:, :, t0:t0 + cl], xc, reads=[xcT_d], writes=[xc], **nk)
            K.ld(zs.a[:cl, :], Dm["zs_d"].a[t0:t0 + cl, :], zs, reads=[Dm["zs_d"]], writes=[zs])
            xtok = cp["xtok"].next(); btok = cp["btok"].next()
            for j in range(6):
                ps = PS.next()
                K.tr(ps.a[:cl, :128], xc.a[:, j, :cl], ident.a[:, :], [xc, ident], [ps])
                if j < 4:
                    K.cp("act" if j % 2 else "dve", xtok.a[:cl, j * 128:(j + 1) * 128], ps.a[:cl, :128], [ps], [xtok])
                else:
                    K.cp("act" if j % 2 else "dve", btok.a[:cl, (j - 4) * 128:(j - 3) * 128], ps.a[:cl, :128], [ps], [btok])
            sc = cp["sc"].next()
            for g in range(2):
                ps = PS.next()
                K.pe(ps.a[:cl, :cl], xc.a[:, 4 + g, :cl], xc.a[:, 6 + g, :cl], True, True, [xc], [ps])
                K.cp("act", sc.a[:cl, g, :cl], ps.a[:cl, :cl], [ps], [sc])
            yss = cp["yss"].next()
            for g in range(2):
                HH = range(4 * g, 4 * g + 4)
                ps_d = {}; seg = {}; xdt = {}; ps1 = {}; ps2 = {}; y1 = {}; xw = {}; ps3 = {}
                for h in HH:
                    ps_d[h] = PS.next()
                    K.pe(ps_d[h].a[:cl, :cl], TMB.a[:cl, ci, h:h + 1].to_broadcast([cl, cl]), ident.a[:cl, :cl], True, False, [TMB, ident], [ps_d[h]])
                    K.pe(ps_d[h].a[:cl, :cl], ident.a[:cl, :cl], maskT.a[:cl, :cl], False, False, [ident, maskT], [ps_d[h]])
                    K.pe(ps_d[h].a[:cl, :cl], nident.a[:cl, :cl], TMB.a[:cl, ci, h:h + 1].to_broadcast([cl, cl]), False, True, [nident, TMB], [ps_d[h]])
                for h in HH:
                    seg[h] = cp["seg"].next()
                    K.act(seg[h].a[:cl, :cl], ps_d[h].a[:cl, :cl], AF.Exp, [ps_d[h]], [seg[h]])
                for h in HH:
                    xdt[h] = cp["xdt"].next()
                    K.act(xdt[h].a[:cl, :], xtok.a[:cl, h * 64:(h + 1) * 64], AF.Copy, [xtok, TMB], [xdt[h]], scale=TMB.a[:cl, ci, 24 + h:25 + h])
                for h in HH:
                    K.tt("dve", seg[h].a[:cl, :cl], seg[h].a[:cl, :cl], sc.a[:cl, g, :cl], ALU.mult, [seg[h], sc], [seg[h]])
                for h in HH:
                    xw[h] = cp["xw"].next()
                    K.act(xw[h].a[:cl, :], xdt[h].a[:cl, :], AF.Copy, [xdt[h], TMB], [xw[h]], scale=TMB.a[:cl, ci, 16 + h:17 + h])
                for h in HH:
                    ps1[h] = PS.next()
                    K.pe(ps1[h].a[:cl, :64], seg[h].a[:cl, :cl], xdt[h].a[:cl, :], True, True, [seg[h], xdt[h]], [ps1[h]])
                for h in HH:
                    ps2[h] = PS.next()
                    K.pe(ps2[h].a[:cl, :64], xc.a[:, 6 + g, :cl], ST[h].a[:, :], True, True, [xc, ST[h]], [ps2[h]])
                for h in HH:
                    y1[h] = cp["y1"].next()
                    K.cp("act", y1[h].a[:cl, :], ps1[h].a[:cl, :64], [ps1[h]], [y1[h]])
                for h in HH:
                    K.stt(y1[h].a[:cl, :], ps2[h].a[:cl, :64], TMB.a[:cl, ci, 8 + h:9 + h], y1[h].a[:cl, :], ALU.mult, ALU.add, [ps2[h], TMB, y1[h]], [y1[h]])
                for h in HH:
                    ps3[h] = PS.next()
                    K.pe(ps3[h].a[:, :64], btok.a[:cl, g * 128:(g + 1) * 128], xw[h].a[:cl, :], True, True, [btok, xw[h]], [ps3[h]])
                for h in HH:
                    K.stt(ST[h].a[:, :], ST[h].a[:, :], DECb.a[:, h * NCI + ci:h * NCI + ci + 1], ps3[h].a[:, :64], ALU.mult, ALU.add, [ST[h], DECb, ps3[h]], [ST[h]])
                for h in HH:
                    K.stt(yss.a[:cl, h * 64:(h + 1) * 64], xtok.a[:cl, h * 64:(h + 1) * 64], dd.a[:cl, h:h + 1], y1[h].a[:cl, :], ALU.mult, ALU.add, [xtok, dd, y1[h]], [yss])
            K.tt("dve", yss.a[:cl, :], yss.a[:cl, :], zs.a[:cl, :], ALU.mult, [yss, zs], [yss])
            s_ = cp["s"].next(); junk = cp["junk"].next()
            K.act(junk.a[:cl, :], yss.a[:cl, :], AF.Square, [yss], [junk, s_], accum=s_.a[:cl, 0:1])
            K.act(s_.a[:cl, 1:2], s_.a[:cl, 0:1], AF.Sqrt, [s_, epsb], [s_], scale=1.0 / 512.0, bias=epsb.a[:cl, :])
            K.recip(s_.a[:cl, 1:2], s_.a[:cl, 1:2], [s_], [s_])
            K.stt(yss.a[:cl, :], yss.a[:cl, :], s_.a[:cl, 1:2], gs.a[:cl, :], ALU.mult, ALU.mult, [yss, s_, gs], [yss])
            for j in range(4):
                ps = PS.next()
                K.tr(ps.a[:, :cl], yss.a[:cl, j * 128:(j + 1) * 128], ident.a[:cl, :cl], [yss, ident], [ps])
                K.cp("act" if j % 2 else "dve", XTb[:, 12 + j, t0:t0 + cl], ps.a[:, :cl], [ps], [tXT])

        for h in range(8):
            K.memset("dve", STp0[h].a[:], 0.0, [STp0[h]])
        STp = list(STp0)
        STs = [K.sb("STs%d" % h, [128, 64]) for h in range(8)]
        for i, (t0, cl, ci) in enumerate(CHUNKS[:32]):
            ST[:] = STp
            chunk(t0, cl, ci)
            if i % 2 == 1:
                (t0s, cls, cis) = CHUNKS[32 + i // 2]
                j = cis - 32
                ST[:] = STs
                for h in range(8):
                    K.ld(ST[h].a[:, :], I["ssdT"].a[l, j, h], ST[h], writes=[ST[h]])
                chunk(t0s, cls, cis)
                for h in range(8):
                    K.stq(O["ssd_sT"].a[l, j, h], ST[h].a[:, :], ST[h], reads=[ST[h]], mwrites=[O["ssd_sT"]], is_output=True)
        ST[:] = STp
        for h in range(8):
            K.stq(O["ssd_pT"].a[l, h], ST[h].a[:, :], ST[h], reads=[ST[h]], mwrites=[O["ssd_pT"]], is_output=True)


def s5_mixer(K, S, PS, I, O, l, Dm, XTb, tXT, ident, ones_f, epsb, halfpi):
    uT_d = Dm["uT_d"]
    with K.phase():
        def P16(name):
            return K.sb(name, [128, 16])
        lre, lim, dtt, th, r, c, s_, t1, t2, t3, lbr, lbi, nlbi, kr, ki, den, ka, cn, sn = [P16("s5p%d" % i) for i in range(19)]
        K.ld(lre.a[:], I["lam_re"].a[l], lre, writes=[lre]); K.ld(lim.a[:], I["lam_im"].a[l], lim, writes=[lim])
        K.ld(dtt.a[:], I["logdt"].a[l], dtt, writes=[dtt])
        K.act(dtt.a[:], dtt.a[:], AF.Exp, [dtt], [dtt])
        K.tt("dve", th.a[:], lim.a[:], dtt.a[:], ALU.mult, [lim, dtt], [th])
        K.tt("dve", r.a[:], lre.a[:], dtt.a[:], ALU.mult, [lre, dtt], [r])
        K.act(r.a[:], r.a[:], AF.Exp, [r], [r])
        K.act(s_.a[:], th.a[:], AF.Sin, [th], [s_], scale=1.0 / 32.0)
        K.act(c.a[:], th.a[:], AF.Sin, [th, halfpi], [c], scale=1.0 / 32.0, bias=halfpi.a[:, :])

        def cdouble(cc, ss):
            K.tt("dve", t1.a[:], ss.a[:], cc.a[:], ALU.mult, [ss, cc], [t1])
            K.tt("dve", t2.a[:], cc.a[:], cc.a[:], ALU.mult, [cc], [t2])
            K.tt("dve", t3.a[:], ss.a[:], ss.a[:], ALU.mult, [ss], [t3])
            K.ts("dve", ss.a[:], t1.a[:], 2.0, ALU.mult, [t1], [ss])
            K.tt("dve", cc.a[:], t2.a[:], t3.a[:], ALU.subtract, [t2, t3], [cc])
        for _ in range(5):
            cdouble(c, s_)
        K.tt("dve", lbr.a[:], r.a[:], c.a[:], ALU.mult, [r, c], [lbr])
        K.tt("dve", lbi.a[:], r.a[:], s_.a[:], ALU.mult, [r, s_], [lbi])
        K.ts("dve", nlbi.a[:], lbi.a[:], -1.0, ALU.mult, [lbi], [nlbi])
        K.ts("dve", ka.a[:], lbr.a[:], -1.0, ALU.add, [lbr], [ka])
        K.tt("dve", t1.a[:], lre.a[:], lre.a[:], ALU.mult, [lre], [t1])
        K.tt("dve", t2.a[:], lim.a[:], lim.a[:], ALU.mult, [lim], [t2])
        K.tt("dve", den.a[:], t1.a[:], t2.a[:], ALU.add, [t1, t2], [den])
        K.recip(den.a[:], den.a[:], [den], [den])
        K.tt("dve", t1.a[:], ka.a[:], lre.a[:], ALU.mult, [ka, lre], [t1])
        K.tt("dve", t2.a[:], lbi.a[:], lim.a[:], ALU.mult, [lbi, lim], [t2])
        K.tt("dve", kr.a[:], t1.a[:], t2.a[:], ALU.add, [t1, t2], [kr])
        K.tt("dve", kr.a[:], kr.a[:], den.a[:], ALU.mult, [kr, den], [kr])
        K.tt("dve", t1.a[:], lbi.a[:], lre.a[:], ALU.mult, [lbi, lre], [t1])
        K.tt("dve", t2.a[:], ka.a[:], lim.a[:], ALU.mult, [ka, lim], [t2])
        K.tt("dve", ki.a[:], t1.a[:], t2.a[:], ALU.subtract, [t1, t2], [ki])
        K.tt("dve", ki.a[:], ki.a[:], den.a[:], ALU.mult, [ki, den], [ki])
        tabC = K.sb("tabC", [128, 16, 128]); tabS = K.sb("tabS", [128, 16, 128])
        tmpA = K.sb("tmpA", [128, 16, 64]); tmpB = K.sb("tmpB", [128, 16, 64])
        K.cp("dve", tabC.a[:, :, 0], c.a[:], [c], [tabC]); K.cp("dve", tabS.a[:, :, 0], s_.a[:], [s_], [tabS])
        K.cp("dve", cn.a[:], c.a[:], [c], [cn]); K.cp("dve", sn.a[:], s_.a[:], [s_], [sn])
        nn = 1
        while nn < 128:
            cb_ = cn.a[:, :].unsqueeze(2).to_broadcast([128, 16, nn]); sb_ = sn.a[:, :].unsqueeze(2).to_broadcast([128, 16, nn])
            K.tt("dve", tmpA.a[:, :, :nn], tabC.a[:, :, 0:nn], cb_, ALU.mult, [tabC, cn], [tmpA])
            K.tt("dve", tmpB.a[:, :, :nn], tabS.a[:, :, 0:nn], sb_, ALU.mult, [tabS, sn], [tmpB])
            K.tt("dve", tmpA.a[:, :, :nn], tmpA.a[:, :, :nn], tmpB.a[:, :, :nn], ALU.subtract, [tmpA, tmpB], [tmpA])
            K.tt("dve", tmpB.a[:, :, :nn], tabS.a[:, :, 0:nn], cb_, ALU.mult, [tabS, cn], [tmpB])
            K.cp("dve", tabC.a[:, :, nn:2 * nn], tmpA.a[:, :, :nn], [tmpA], [tabC])
            K.tt("dve", tmpA.a[:, :, :nn], tabC.a[:, :, 0:nn], sb_, ALU.mult, [tabC, sn], [tmpA])
            K.tt("dve", tabS.a[:, :, nn:2 * nn], tmpB.a[:, :, :nn], tmpA.a[:, :, :nn], ALU.add, [tmpA, tmpB], [tabS])
            cdouble(cn, sn)
            nn *= 2
        tabKC = K.sb("tabKC", [128, 16, 128]); tabKS = K.sb("tabKS", [128, 16, 128])
        krb = kr.a[:, :].unsqueeze(2).to_broadcast([128, 16, 128]); kib = ki.a[:, :].unsqueeze(2).to_broadcast([128, 16, 128])
        tmpC = K.sb("tmpC", [128, 16, 128])
        K.tt("dve", tabKC.a[:], tabC.a[:], krb, ALU.mult, [tabC, kr], [tabKC])
        K.tt("dve", tmpC.a[:], tabS.a[:], kib, ALU.mult, [tabS, ki], [tmpC])
        K.tt("dve", tabKC.a[:], tabKC.a[:], tmpC.a[:], ALU.add, [tabKC, tmpC], [tabKC])
        K.tt("dve", tabKS.a[:], tabC.a[:], kib, ALU.mult, [tabC, ki], [tabKS])
        K.tt("dve", tmpC.a[:], tabS.a[:], krb, ALU.mult, [tabS, kr], [tmpC])
        K.tt("dve", tabKS.a[:], tabKS.a[:], tmpC.a[:], ALU.subtract, [tabKS, tmpC], [tabKS])
        Bm = {}
        for nm in ("Bre", "Bim", "Cre", "Cim"):
            Bm[nm] = K.sb(nm + "_sb", [128, 16, 128])
            K.ld(Bm[nm].a[:], I[nm].a[l].rearrange("s k m -> k s m"), Bm[nm], writes=[Bm[nm]])
        s5d = K.sb("s5d", [128, 4]); bglu = K.sb("bglu", [128, 4]); gs5 = K.sb("gs5", [128, 4])
        K.ld(s5d.a[:], I["s5d"].a[l], s5d, writes=[s5d]); K.ld(bglu.a[:], I["b_glu"].a[l], bglu, writes=[bglu]); K.ld(gs5.a[:], I["g_s5"].a[l], gs5, writes=[gs5])
        Xr = K.sb("Xr", [128, 16]); Xi = K.sb("Xi", [128, 16])
        K.memset("dve", Xr.a[:], 0.0, [Xr]); K.memset("dve", Xi.a[:], 0.0, [Xi])
        y5g_d = K.dscr("y5g_d%d" % l, [512, NT])
        W8 = lambda nm: K.sb(nm, [128, 8, 128])
        BUr, BUi, Wr, Wi, Zr, Zi, T1, T2, Xr_, Xi_, nXi_ = [W8("s5w%d" % i) for i in range(11)]
        ubp = Pool(K, "ub", [128, 4, 128], F32, 2)
        gp = Pool(K, "s5g", [128, 128], F32, 3)

        def bu_calc(ub, sc, n, our, oui, tA, tB):
            j = sc // 4
            ps = PS.next()
            K.pe(ps.a[:, 0:n], Bm["Bre"].a[:, sc, :], ub.a[:, j, :n], True, True, [Bm["Bre"], ub], [ps])
            K.pe(ps.a[:, 128:128 + n], Bm["Bim"].a[:, sc, :], ub.a[:, j, :n], True, True, [Bm["Bim"], ub], [ps])
            return ps

        def y_out(ub, j, n, t0, xr_of, nxi_of, rd):
            ps_y = PS.next()
            for q in range(4):
                sc = 4 * j + q
                K.pe(ps_y.a[:, :n], Bm["Cre"].a[:, sc, :], xr_of(sc), q == 0, False, [Bm["Cre"]] + rd, [ps_y])
                K.pe(ps_y.a[:, :n], Bm["Cim"].a[:, sc, :], nxi_of(sc), False, q == 3, [Bm["Cim"]] + rd, [ps_y])
            yv = gp.next(); tg = gp.next()
            K.stt(yv.a[:, :n], ub.a[:, j, :n], s5d.a[:, j:j + 1], ps_y.a[:, :n], ALU.mult, ALU.add, [ub, s5d, ps_y], [yv])
            K.tt("dve", tg.a[:, :n], yv.a[:, :n], yv.a[:, :n], ALU.mult, [yv], [tg])
            K.ts("dve", tg.a[:, :n], tg.a[:, :n], 0.044715, ALU.mult, [tg], [tg], s2=1.0, op1=ALU.add)
            K.tt("dve", tg.a[:, :n], tg.a[:, :n], yv.a[:, :n], ALU.mult, [tg, yv], [tg])
            K.act(tg.a[:, :n], tg.a[:, :n], AF.Tanh, [tg], [tg], scale=0.7978845608028654)
            K.ts("dve", tg.a[:, :n], tg.a[:, :n], 1.0, ALU.add, [tg], [tg], s2=0.5, op1=ALU.mult)
            K.tt("dve", tg.a[:, :n], tg.a[:, :n], yv.a[:, :n], ALU.mult, [tg, yv], [tg])
            K.stq(y5g_d.a[j * 128:(j + 1) * 128, t0:t0 + n], tg.a[:, :n], tg, reads=[tg], mwrites=[y5g_d])

        for tc in range(16):
            t0 = tc * 128
            n = 128
            ub = ubp.next()
            K.ld(ub.a[:, :, :n], uT_d.a.rearrange("(j p) t -> p j t", p=128)[:, :, t0:t0 + n], ub, reads=[uT_d], writes=[ub])
            for h2 in range(2):
                for i in range(8):
                    sc = 8 * h2 + i
                    ps = bu_calc(ub, sc, n, None, None, None, None)
                    K.cp("act", BUr.a[:, i, :n], ps.a[:, 0:n], [ps], [BUr])
                    K.cp("act", BUi.a[:, i, :n], ps.a[:, 128:128 + n], [ps], [BUi])
                Cs = tabC.a[:, 8 * h2:8 * h2 + 8, :]; Ss = tabS.a[:, 8 * h2:8 * h2 + 8, :]
                KCs = tabKC.a[:, 8 * h2:8 * h2 + 8, :]; KSs = tabKS.a[:, 8 * h2:8 * h2 + 8, :]
                K.tt("dve", T1.a[:], BUi.a[:], KSs, ALU.mult, [BUi, tabKS], [T1])
                K.tt("pool", Wr.a[:], BUr.a[:], KCs, ALU.mult, [BUr, tabKC], [Wr])
                K.tt("dve", Wr.a[:], Wr.a[:], T1.a[:], ALU.subtract, [Wr, T1], [Wr])
                K.tt("pool", T2.a[:], BUr.a[:], KSs, ALU.mult, [BUr, tabKS], [T2])
                K.tt("dve", Wi.a[:], BUi.a[:], KCs, ALU.mult, [BUi, tabKC], [Wi])
                K.tt("dve", Wi.a[:], Wi.a[:], T2.a[:], ALU.add, [Wi, T2], [Wi])
                for i in range(8):
                    sc = 8 * h2 + i
                    rb = r.a[:, sc:sc + 1].to_broadcast([128, n])
                    K.scan(Zr.a[:, i, :], rb, Wr.a[:, i, :], Xr.a[:, sc:sc + 1], ALU.mult, ALU.add, [r, Wr, Xr], [Zr])
                    K.scan(Zi.a[:, i, :], rb, Wi.a[:, i, :], Xi.a[:, sc:sc + 1], ALU.mult, ALU.add, [r, Wi, Xi], [Zi])
                K.tt("dve", T1.a[:], Zi.a[:], Ss, ALU.mult, [Zi, tabS], [T1])
                K.tt("pool", Xr_.a[:], Zr.a[:], Cs, ALU.mult, [Zr, tabC], [Xr_])
                K.tt("dve", Xr_.a[:], Xr_.a[:], T1.a[:], ALU.subtract, [Xr_, T1], [Xr_])
                K.tt("pool", T2.a[:], Zr.a[:], Ss, ALU.mult, [Zr, tabS], [T2])
                K.tt("dve", T1.a[:], Zi.a[:], Cs, ALU.mult, [Zi, tabC], [T1])
                K.tt("dve", Xi_.a[:], T2.a[:], T1.a[:], ALU.add, [T2, T1], [Xi_])
                K.ts("dve", nXi_.a[:], Xi_.a[:], -1.0, ALU.mult, [Xi_], [nXi_])
                K.cp("dve", Xr.a[:, 8 * h2:8 * h2 + 8], Xr_.a[:, :, n - 1], [Xr_], [Xr])
                K.cp("dve", Xi.a[:, 8 * h2:8 * h2 + 8], Xi_.a[:, :, n - 1], [Xi_], [Xi])
                for jj in range(2):
                    j = 2 * h2 + jj
                    y_out(ub, j, n, t0, lambda sc: Xr_.a[:, sc - 8 * h2, :n], lambda sc: nXi_.a[:, sc - 8 * h2, :n], [Xr_, nXi_])
        K.stq(O["s5re_pT"].a[l], Xr.a[:], Xr, reads=[Xr], mwrites=[O["s5re_pT"]], is_output=True)
        K.stq(O["s5im_pT"].a[l], Xi.a[:], Xi, reads=[Xi], mwrites=[O["s5im_pT"]], is_output=True)
        x0r = K.sb("x0r", [128, 16, NS]); x0i = K.sb("x0i", [128, 16, NS])
        K.ld(x0r.a[:], I["s5reT"].a[l], x0r, writes=[x0r]); K.ld(x0i.a[:], I["s5imT"].a[l], x0i, writes=[x0i])
        SXr = K.sb("SXr", [128, 16, NS]); SXi = K.sb("SXi", [128, 16, NS]); SnXi = K.sb("SnXi", [128, 16, NS])
        SBr = K.sb("SBr", [128, 16, NS]); SBi = K.sb("SBi", [128, 16, NS])
        ub = ubp.next()
        K.ld(ub.a[:, :, :NS], uT_d.a.rearrange("(j p) t -> p j t", p=128)[:, :, L:NT], ub, reads=[uT_d], writes=[ub])
        for sc in range(16):
            ps = bu_calc(ub, sc, NS, None, None, None, None)
            K.ts("dve", SBr.a[:, sc, :], ps.a[:, 128:128 + NS], ki.a[:, sc:sc + 1], ALU.mult, [ps, ki], [SBr])
            K.stt(SBr.a[:, sc, :], ps.a[:, 0:NS], kr.a[:, sc:sc + 1], SBr.a[:, sc, :], ALU.mult, ALU.subtract, [ps, kr, SBr], [SBr])
            K.ts("dve", SBi.a[:, sc, :], ps.a[:, 0:NS], ki.a[:, sc:sc + 1], ALU.mult, [ps, ki], [SBi])
            K.stt(SBi.a[:, sc, :], ps.a[:, 128:128 + NS], kr.a[:, sc:sc + 1], SBi.a[:, sc, :], ALU.mult, ALU.add, [ps, kr, SBi], [SBi])
            K.stt(SXr.a[:, sc, :], x0r.a[:, sc, :], lbr.a[:, sc:sc + 1], SBr.a[:, sc, :], ALU.mult, ALU.add, [x0r, lbr, SBr], [SXr])
            K.stt(SXr.a[:, sc, :], x0i.a[:, sc, :], nlbi.a[:, sc:sc + 1], SXr.a[:, sc, :], ALU.mult, ALU.add, [x0i, nlbi, SXr], [SXr])
            K.stt(SXi.a[:, sc, :], x0r.a[:, sc, :], lbi.a[:, sc:sc + 1], SBi.a[:, sc, :], ALU.mult, ALU.add, [x0r, lbi, SBi], [SXi])
            K.stt(SXi.a[:, sc, :], x0i.a[:, sc, :], lbr.a[:, sc:sc + 1], SXi.a[:, sc, :], ALU.mult, ALU.add, [x0i, lbr, SXi], [SXi])
        K.ts("dve", SnXi.a[:], SXi.a[:], -1.0, ALU.mult, [SXi], [SnXi])
        K.stq(O["s5re_sT"].a[l], SXr.a[:], SXr, reads=[SXr], mwrites=[O["s5re_sT"]], is_output=True)
        K.stq(O["s5im_sT"].a[l], SXi.a[:], SXi, reads=[SXi], mwrites=[O["s5im_sT"]], is_output=True)
        for j in range(4):
            y_out(ub, j, NS, L, lambda sc: SXr.a[:, sc, :], lambda sc: SnXi.a[:, sc, :], [SXr, SnXi])
    with K.phase():
        s5d = K.sb("s5d", [128, 4]); bglu = K.sb("bglu", [128, 4]); gs5 = K.sb("gs5", [128, 4])
        K.ld(bglu.a[:], I["b_glu"].a[l], bglu, writes=[bglu]); K.ld(gs5.a[:], I["g_s5"].a[l], gs5, writes=[gs5])
        ybp = Pool(K, "y5b", [128, 4, 512], F32, 2)
        wglu = K.sb("wglu", [128, 4, 512])
        K.ld(wglu.a[:], I["w_glu"].a[l].rearrange("(k p) c -> p k c", p=128), wglu, writes=[wglu])
        yg = Pool(K, "ygl", [128, 4, 512], F32, 2); sqp = Pool(K, "s5sq", [128, 4, 512], F32, 1); rsp = Pool(K, "s5rs", [128, 512], F32, 2)
        for (t0, n) in TB:
            ygl = yg.next()
            yb = ybp.next()
            K.ld(yb.a[:, :, :n], y5g_d.a.rearrange("(j p) t -> p j t", p=128)[:, :, t0:t0 + n], yb, reads=[y5g_d], writes=[yb])
            for m in range(4):
                ps = PS.next()
                for j in range(4):
                    K.pe(ps.a[:, :n], wglu.a[:, j, m * 128:(m + 1) * 128], yb.a[:, j, :n], j == 0, j == 3, [wglu, yb], [ps])
                K.act(ygl.a[:, m, :n], ps.a[:, :n], AF.Sigmoid, [ps, bglu], [ygl], bias=bglu.a[:, m:m + 1])
                K.tt("dve", ygl.a[:, m, :n], ygl.a[:, m, :n], yb.a[:, m, :n], ALU.mult, [ygl, yb], [ygl])
            sq = sqp.next()
            K.act(sq.a[:, :, :n], ygl.a[:, :, :n], AF.Square, [ygl], [sq])
            ps = PS.next()
            for m in range(4):
                K.pe(ps.a[:, :n], ones_f.a[:], sq.a[:, m, :n], m == 0, m == 3, [ones_f, sq], [ps])
            rs = rsp.next()
            K.act(rs.a[:, :n], ps.a[:, :n], AF.Sqrt, [ps, epsb], [rs], scale=1.0 / 512.0, bias=epsb.a[:, :])
            K.recip(rs.a[:, :n], rs.a[:, :n], [rs], [rs])
            for m in range(4):
                K.stt(XTb[:, 8 + m, t0:t0 + n], ygl.a[:, m, :n], gs5.a[:, m:m + 1], rs.a[:, :n], ALU.mult, ALU.mult, [ygl, gs5, rs], [tXT])


def ffn_phase(K, S, PS, I, l, hT, XTf, tXT, gn, ones_bf, epsb, ident, gst, norm_stage, stages, own):
    moe = (l % 2 == 1)
    if moe:
        experts = [(I["moe_wg"].a[e], I["moe_wu"].a[e], I["moe_wd"].a[e]) for e in range(NE)]
        dff = D_FFE
    else:
        experts = [(I["ffn_wg"].a, I["ffn_wu"].a, I["ffn_wd"].a)]
        dff = D_FF
    B3 = [(0, 347), (347, 347), (694, 346)]
    SBS = [(0, [(0, 512), (512, 512)]), (1024, B3)]
    if own:
        SBS = [(0, B3)]
    for (c0, blocks) in SBS:
        nsb = sum(n for _, n in blocks)
        with K.phase():
            P = {"hblk": Pool(K, "fhblk", [128, KC, 128], F32, 1), "sq": Pool(K, "fsq", [128, KC, 128], BF16, 1),
                 "rs": Pool(K, "frs", [128, 512], F32, 2)}
            cT = K.sb("cT", [128, KC, 1040], BF16)
            for q0 in range(0, nsb, 128):
                n = min(128, nsb - q0)
                norm_stage(hT, gn["g_ffn"].a[:, l, :], gn["g_ffn"], c0 + q0, n, cT.a[:, :, q0:q0 + n], cT, P)
            wgp = Pool(K, "fwg", [128, KC, 256], BF16, 2); wup = Pool(K, "fwu", [128, KC, 256], BF16, 2)
            wdp = Pool(K, "fwd", [128, 2, D], BF16, 3); h1p = Pool(K, "fh1", [128, 2, 1040], BF16, 2)
            sgp = Pool(K, "fsg", [128, 512], F32, 3); hbp = Pool(K, "fhb", [128, 512], F32, 2)
            ntile = (nsb + 127) // 128
            if moe:
                wr = K.sb("wr", [128, KC, NE], BF16)
                K.ld(wr.a[:], I["w_router"].a.rearrange("(k p) c -> p k c", p=128), wr, writes=[wr], q="pool")
                brep = K.sb("brep", [128, NE]); K.ld(brep.a[:], I["b_router_rep"].a, brep, writes=[brep])
                comb = K.sb("comb", [128, 9, NE]); combB = Pool(K, "combB", [128, 1040], F32, 2)
                rt = Pool(K, "rt", [128, 4, NE], F32, 2)
                for i in range(ntile):
                    q0 = i * 128
                    n = min(128, nsb - q0)
                    ps = PS.next()
                    for k in range(KC):
                        K.pe(ps.a[:n, :NE], cT.a[:, k, q0:q0 + n], wr.a[:, k, :], k == 0, k == KC - 1, [cT, wr], [ps])
                    t = rt.next()
                    lg = t.a[:n, 0, :]; mx = t.a[:n, 1, :]; ex = t.a[:n, 2, :]; sc = t.a[:n, 3, :]
                    K.tt("dve", lg, ps.a[:n, :NE], brep.a[:n, :], ALU.add, [ps, brep], [t])
                    K.S.op("dve", lambda e, mx=mx, lg=lg: e.max(out=mx, in_=lg), reads=[t.k], writes=[t.k])
                    K.ts("dve", sc[:, 0:1], mx[:, 0:1], -1.0, ALU.mult, [t], [t])
                    K.act(ex, lg, AF.Exp, [t], [t], bias=sc[:, 0:1])
                    K.act(sc[:, 1:2], mx[:, 1:2], AF.Exp, [t], [t], bias=sc[:, 0:1])
                    K.ts("dve", sc[:, 1:2], sc[:, 1:2], 1.0, ALU.add, [t], [t])
                    K.recip(sc[:, 1:2], sc[:, 1:2], [t], [t])
                    K.ts("dve", lg, lg, mx[:, 1:2], ALU.is_ge, [t], [t])
                    K.stt(comb.a[:n, i, :], ex, sc[:, 1:2], lg, ALU.mult, ALU.mult, [t], [comb])
            panels = [(ei, f0) for ei in range(len(experts)) for f0 in range(0, dff, 256)]
            npan = len(panels)
            cBs = {}
            loaded = {}

            def loads(p):
                ei, f0 = panels[p]
                Wg, Wu, Wd = experts[ei]
                wg = wgp.next(); wu = wup.next(); wd = wdp.next()
                pi = f0 // 256
                for hh in range(2):
                    K.ld(wg.a[:].rearrange("p k c -> p (k c)")[:, hh * 2048:(hh + 1) * 2048], Wg[pi][:, hh * 2048:(hh + 1) * 2048], wg, writes=[wg], q="pool")
                for hh in range(2):
                    K.ld(wu.a[:].rearrange("p k c -> p (k c)")[:, hh * 2048:(hh + 1) * 2048], Wu[pi][:, hh * 2048:(hh + 1) * 2048], wu, writes=[wu], q="pool")
                for hh in range(2):
                    K.ld(wd.a[:].rearrange("p k c -> p (k c)")[:, hh * 2048:(hh + 1) * 2048], Wd[pi][:, hh * 2048:(hh + 1) * 2048], wd, writes=[wd], q="pool")
                loaded[p] = (wg, wu, wd)

            def gate_up(p):
                ei, f0 = panels[p]
                wg, wu, wd = loaded[p]
                if moe and ei not in cBs:
                    cB = combB.next()
                    for i in range(ntile):
                        q0 = i * 128
                        n = min(128, nsb - q0)
                        ps = PS.next()
                        K.pe(ps.a[:, :n], comb.a[:n, i, ei:ei + 1].to_broadcast([n, 128]), ident.a[:n, :n], True, True, [comb, ident], [ps])
                        K.cp("act", cB.a[:, q0:q0 + n], ps.a[:, :n], [ps], [cB])
                    cBs.clear()
                    cBs[ei] = cB
                h1 = h1p.next()
                for m in range(2):
                    for (b0, n) in blocks:
                        pg = PS.next(); pu = PS.next()
                        for k in range(KC):
                            K.pe(pg.a[:, :n], wg.a[:, k, m * 128:(m + 1) * 128], cT.a[:, k, b0:b0 + n], k == 0, k == KC - 1, [wg, cT], [pg])
                        for k in range(KC):
                            K.pe(pu.a[:, :n], wu.a[:, k, m * 128:(m + 1) * 128], cT.a[:, k, b0:b0 + n], k == 0, k == KC - 1, [wu, cT], [pu])
                        sg = sgp.next()
                        K.act(sg.a[:, :n], pg.a[:, :n], AF.Silu, [pg], [sg])
                        if moe:
                            K.tt("dve", sg.a[:, :n], sg.a[:, :n], cBs[ei].a[:, b0:b0 + n], ALU.mult, [sg, cBs[ei]], [sg])
                        K.tt("dve", h1.a[:, m, b0:b0 + n], sg.a[:, :n], pu.a[:, :n], ALU.mult, [sg, pu], [h1])
                return h1

            def down(p, h1):
                wg, wu, wd = loaded.pop(p)
                for mo in range(KC):
                    for (b0, n) in blocks:
                        ps = PS.next()
                        for k in range(2):
                            K.pe(ps.a[:, :n], wd.a[:, k, mo * 128:(mo + 1) * 128], h1.a[:, k, b0:b0 + n], k == 0, k == 1, [wd, h1], [ps])
                        if p == 0:
                            K.cp("act", XTf[:, mo, b0:b0 + n], ps.a[:, :n], [ps], [tXT])
                        else:
                            K.tt("dve", XTf[:, mo, b0:b0 + n], ps.a[:, :n], XTf[:, mo, b0:b0 + n], ALU.add, [ps, tXT], [tXT])

            loads(0)
            prev_h1 = None
            for p in range(npan + 1):
                if p + 1 < npan:
                    loads(p + 1)
                cur_h1 = gate_up(p) if p < npan else None
                if p >= 1:
                    down(p - 1, prev_h1)
                prev_h1 = cur_h1
            for mo in range(KC):
                for (b0, n) in blocks:
                    hb = hbp.next()
                    K.ld(hb.a[:, :n], hT.a[mo * 128:(mo + 1) * 128, c0 + b0:c0 + b0 + n], hb, reads=[hT], writes=[hb])
                    K.tt("dve", hb.a[:, :n], hb.a[:, :n], XTf[:, mo, b0:b0 + n], ALU.add, [hb, tXT], [hb])
                    K.stq(hT.a[mo * 128:(mo + 1) * 128, c0 + b0:c0 + b0 + n], hb.a[:, :n], hb, reads=[hb], mwrites=[hT])


def _consts():
    ident = np.eye(128, dtype=np.float32)
    maskT = np.where(np.arange(64)[:, None] <= np.arange(64)[None, :], 0.0, NEG).astype(np.float32)
    sel4 = np.zeros((4, 4, 128), np.float32)
    for h in range(4):
        sel4[h, h, :] = 1.0
    sel8 = np.zeros((8, 8, 128), np.float32)
    for h in range(8):
        sel8[h, h, :] = 1.0
    return dict(ident=ident, nident=-ident, maskT=maskT, sel4=sel4, sel8=sel8, sel8e=sel8.copy())


def _fm(v):
    v = np.asarray(v, np.float32)
    return np.ascontiguousarray(v.reshape(v.shape[:-1] + (v.shape[-1] // 128, 128)).swapaxes(-1, -2))


def prep_shared(inp):
    f = lambda a: np.ascontiguousarray(np.asarray(a, np.float32))
    sh = {}
    for nm in ("g_mix", "g_ffn", "g_ple"):
        sh[nm] = _fm(inp[nm])
    sh["g_final"] = _fm(inp["g_final"])
    sh["w_in"] = f(inp["w_in"]); sh["w_out"] = f(inp["w_out"])
    sh["b_ig"] = f(inp["b_igate"]).reshape(DEPTH, 4, 1); sh["b_fg"] = f(inp["b_fgate"]).reshape(DEPTH, 4, 1)
    sh["gml_rep"] = np.ascontiguousarray(np.broadcast_to(f(inp["g_ml"]).reshape(DEPTH, 1, 1024), (DEPTH, 64, 1024)))
    sh["lam_re"] = _fm(f(inp["s5_lam_re"]).reshape(DEPTH, 2048)); sh["lam_im"] = _fm(f(inp["s5_lam_im"]).reshape(DEPTH, 2048))
    sh["logdt"] = _fm(np.repeat(f(inp["s5_log_dt"]), 64, axis=1))
    bre = f(inp["s5_b_re"]); bim = f(inp["s5_b_im"]); cre = f(inp["s5_c_re"]); cim = f(inp["s5_c_im"])
    Bre = np.zeros((DEPTH, 16, 128, 128), np.float32); Bim = np.zeros_like(Bre); Cre = np.zeros_like(Bre); Cim = np.zeros_like(Bre)
    for g in range(32):
        sc, g2, gl = g // 2, g % 2, g % 8
        Bre[:, sc, gl * 16:(gl + 1) * 16, g2 * 64:(g2 + 1) * 64] = bre[:, g].transpose(0, 2, 1)
        Bim[:, sc, gl * 16:(gl + 1) * 16, g2 * 64:(g2 + 1) * 64] = bim[:, g].transpose(0, 2, 1)
        Cre[:, sc, g2 * 64:(g2 + 1) * 64, gl * 16:(gl + 1) * 16] = cre[:, g].transpose(0, 2, 1)
        Cim[:, sc, g2 * 64:(g2 + 1) * 64, gl * 16:(gl + 1) * 16] = cim[:, g].transpose(0, 2, 1)
    sh["Bre"], sh["Bim"], sh["Cre"], sh["Cim"] = Bre, Bim, Cre, Cim
    fm4 = lambda v: np.ascontiguousarray(f(v).reshape(DEPTH, 4, 128).swapaxes(1, 2))
    sh["s5d"] = fm4(f(inp["s5_d"]).reshape(DEPTH, 512)); sh["b_glu"] = fm4(inp["s5_b_glu"]); sh["g_s5"] = fm4(inp["g_s5"])
    sh["w_glu"] = f(inp["s5_w_glu"])
    sh["conv_w"] = np.ascontiguousarray(f(inp["ssd_conv_w"]).reshape(DEPTH, 4, 8, 128).transpose(0, 3, 2, 1))
    sh["conv_b"] = np.ascontiguousarray(f(inp["ssd_conv_b"]).reshape(DEPTH, 8, 128).swapaxes(1, 2))
    sh["dt_bias"] = f(inp["ssd_dt_bias"]).reshape(DEPTH, 8, 1); sh["a_log"] = f(inp["ssd_a_log"]).reshape(DEPTH, 8, 1)
    sh["ssdd_rep"] = np.ascontiguousarray(np.broadcast_to(f(inp["ssd_d"]).reshape(DEPTH, 1, 8), (DEPTH, 64, 8)))
    sh["gssd_rep"] = np.ascontiguousarray(np.broadcast_to(f(inp["g_ssd"]).reshape(DEPTH, 1, 512), (DEPTH, 64, 512)))
    def pan_in(W):
        npan = W.shape[1] // 256
        return np.ascontiguousarray(W.reshape(16, 128, npan, 256).transpose(2, 1, 0, 3)).reshape(npan, 128, 4096)

    def pan_dn(W):
        npan = W.shape[0] // 256
        return np.ascontiguousarray(W.reshape(npan, 2, 128, 2048).transpose(0, 2, 1, 3)).reshape(npan, 128, 4096)
    sh["ffn_wg"] = pan_in(f(inp["ffn_w_gate"])[0]); sh["ffn_wu"] = pan_in(f(inp["ffn_w_up"])[0]); sh["ffn_wd"] = pan_dn(f(inp["ffn_w_down"])[0])
    sh["w_router"] = f(inp["w_router"])[0]
    sh["b_router_rep"] = np.ascontiguousarray(np.broadcast_to(f(inp["b_router"])[0].reshape(1, NE), (128, NE)))
    sh["moe_wg"] = np.stack([pan_in(np.asarray(inp["moe_w_gate"][0][e], np.float32)) for e in range(NE)])
    sh["moe_wu"] = np.stack([pan_in(np.asarray(inp["moe_w_up"][0][e], np.float32)) for e in range(NE)])
    sh["moe_wd"] = np.stack([pan_dn(np.asarray(inp["moe_w_down"][0][e], np.float32)) for e in range(NE)])
    sh["w_ple"] = f(inp["w_ple"]); sh["w_pleg"] = f(inp["w_ple_gate"])
    sh.update(_consts())
    return sh


def prep_core(inp, c):
    f = lambda a: np.ascontiguousarray(np.asarray(a, np.float32))
    b = c % 4
    ss = slice(c * NS, (c + 1) * NS)
    m = {}
    m["xT"] = np.ascontiguousarray(np.concatenate([f(inp["x_prompt"])[b], f(inp["x_sample"])[ss, 0]], axis=0).T)
    m["pT"] = np.ascontiguousarray(np.concatenate([f(inp["p_prompt"])[:, b], f(inp["p_sample"])[:, ss, 0]], axis=1).transpose(0, 2, 1))
    m["sC"] = f(inp["state_mlstm_C"])[:, ss]; m["sn"] = f(inp["state_mlstm_n"])[:, ss]
    m["smT"] = np.ascontiguousarray(f(inp["state_mlstm_m"])[:, ss].transpose(0, 2, 1))
    s5 = lambda a: np.ascontiguousarray(f(a)[:, ss].reshape(DEPTH, NS, 16, 128).transpose(0, 3, 2, 1))
    m["s5reT"] = s5(inp["state_s5_re"]); m["s5imT"] = s5(inp["state_s5_im"])
    m["ssdT"] = np.ascontiguousarray(f(inp["state_ssd"])[:, ss].transpose(0, 1, 2, 4, 3))
    m["convT"] = np.ascontiguousarray(f(inp["cache_conv"])[:, ss].reshape(DEPTH, NS, 3, 8, 128).transpose(0, 4, 3, 2, 1))
    m["half"] = np.ascontiguousarray(np.broadcast_to(np.array([[1.0, 0.0]] if c < 4 else [[0.0, 1.0]], np.float32), (128, 2)))
    return m


_NC_CACHE = {}


def run_device(inputs, dbg=None, stages=99, cores=8, trace=False):
    key = (tuple(sorted(dbg or ())), stages)
    if key not in _NC_CACHE:
        _NC_CACHE[key] = build_program(dbg, stages)
    nc, K = _NC_CACHE[key]
    sh = prep_shared(inputs)
    in_maps = []
    for c in range(cores):
        m = dict(sh)
        m.update(prep_core(inputs, c))
        in_maps.append(m)
    if trace:
        res = run_bass_kernel_spmd(nc, in_maps, core_ids=list(range(cores)), trace=True)
        print("EXEC_NS", res.exec_time_ns)
    else:
        res = run_bass_kernel_spmd(nc, in_maps, core_ids=list(range(cores)))
    return res.results


def kernel(**inputs):
    R = run_device(inputs)
    B = 4
    y_p = np.stack([np.concatenate([R[b]["yT"][:, :1024].T, R[b + 4]["yT"][:, :1024].T], axis=0) for b in range(B)])
    y_s = np.concatenate([R[c]["yT"][:, 1024:NO].T for c in range(8)], axis=0)[:, None, :]
    st = lambda nm, idx: np.stack([R[b][nm] for b in range(B)], axis=1)
    C_p = np.stack([R[b]["C_p"] for b in range(B)], axis=1)
    n_p = np.stack([R[b]["n_p"] for b in range(B)], axis=1)
    m_p = np.stack([R[b]["m_pT"][:, :, 0] for b in range(B)], axis=1)
    unfm = lambda a: a.swapaxes(-1, -2).reshape(a.shape[:-2] + (32, 64))
    s5re_p = np.stack([unfm(R[b]["s5re_pT"]) for b in range(B)], axis=1)
    s5im_p = np.stack([unfm(R[b]["s5im_pT"]) for b in range(B)], axis=1)
    ssd_p = np.stack([R[b]["ssd_pT"].transpose(0, 1, 3, 2) for b in range(B)], axis=1)
    conv_p = np.stack([R[b]["conv_pT"].transpose(0, 3, 2, 1).reshape(DEPTH, 3, 1024) for b in range(B)], axis=1)
    C_s = np.concatenate([R[c]["C_s"] for c in range(8)], axis=1)
    n_s = np.concatenate([R[c]["n_s"] for c in range(8)], axis=1)
    m_s = np.concatenate([R[c]["m_sT"].transpose(0, 2, 1) for c in range(8)], axis=1)
    uns = lambda a: a.transpose(0, 3, 2, 1).reshape(DEPTH, NS, 32, 64)
    s5re_s = np.concatenate([uns(R[c]["s5re_sT"]) for c in range(8)], axis=1)
    s5im_s = np.concatenate([uns(R[c]["s5im_sT"]) for c in range(8)], axis=1)
    ssd_s = np.concatenate([R[c]["ssd_sT"].transpose(0, 1, 2, 4, 3) for c in range(8)], axis=1)
    conv_s = np.concatenate([R[c]["conv_sT"].transpose(0, 4, 3, 2, 1).reshape(DEPTH, NS, 3, 1024) for c in range(8)], axis=1)
    outs = (y_p, y_s, C_p, n_p, m_p, s5re_p, s5im_p, ssd_p, conv_p, C_s, n_s, m_s, s5re_s, s5im_s, ssd_s, conv_s)
    return tuple(np.ascontiguousarray(o, dtype=np.float32) for o in outs)
```
